# Optimizing a Trainium2 kernel written in Bass

```python
import math
import jax, jax.numpy as jnp
from jax import lax
import numpy as np

D_MODEL = 4096
BATCH = 32
SEQ = 256
DEPTH = 1
DEC_BATCH = 4
DEC_SEQ = 1024
PAST_LEN = 256

GRID_W = 64
MLA_HEADS = 16
NOPE_DIM = 128
ROPE_DIM = 64
V_DIM = 128
Q_RANK = 768
KV_RANK = 512
MLA_WIDTH = MLA_HEADS * V_DIM
CONV_WIDTH = D_MODEL - MLA_WIDTH
CONV_KERNEL = 31
MIX_WIDTH = MLA_WIDTH + CONV_WIDTH
IN_WIDTH = Q_RANK + KV_RANK + ROPE_DIM + 2 * CONV_WIDTH
PEER_HEADS = 8
PEER_KEYS = 128
PEER_EXPERTS = PEER_KEYS * PEER_KEYS
PEER_QDIM = 512
PEER_HALF = PEER_QDIM // 2
PEER_TOPK = 16
ROPE_BASE = 10000.0
Q_BLOCK = 128
TOKEN_BLOCK = 128
ALPHA = (2 * DEPTH) ** 0.25
BETA = (8 * DEPTH) ** -0.25
EPS = 1e-6

kernel_name = "hybrid_mla_conformer_peer_diffusion_step"


def layer_norm(x, g, b):
    xf = x.astype(jnp.float32)
    mu = jnp.mean(xf, -1, keepdims=True)
    var = jnp.mean(jnp.square(xf - mu), -1, keepdims=True)
    return ((xf - mu) * lax.rsqrt(var + EPS)).astype(x.dtype) * g + b


def rms_norm(x, g):
    xf = x.astype(jnp.float32)
    return (xf * lax.rsqrt(jnp.mean(xf * xf, -1, keepdims=True) + EPS)).astype(x.dtype) * g


def adaln(cond, w_ada, b_ada):
    m = jnp.einsum('bd,dn->bn', jax.nn.silu(cond), w_ada) + b_ada
    return [t[:, None, :] for t in jnp.split(m, 6, axis=-1)]


def axial_rope_tables(n_tokens):
    rows = n_tokens // GRID_W
    row = jnp.repeat(jnp.arange(rows, dtype=jnp.float32), GRID_W)
    col = jnp.tile(jnp.arange(GRID_W, dtype=jnp.float32), rows)
    n_freq = ROPE_DIM // 4
    inv = ROPE_BASE ** (-jnp.arange(n_freq, dtype=jnp.float32) / n_freq)
    ang_r = row[:, None] * inv
    ang_c = col[:, None] * inv
    ang = jnp.concatenate([ang_r, ang_r, ang_c, ang_c], -1)
    return jnp.cos(ang), jnp.sin(ang)


def rotate_half_axial(x):
    def rh(y):
        y1, y2 = jnp.split(y, 2, -1)
        return jnp.concatenate([-y2, y1], -1)
    x_r, x_c = jnp.split(x, 2, -1)
    return jnp.concatenate([rh(x_r), rh(x_c)], -1)


def apply_rope(x, cos, sin):
    return x * cos.astype(x.dtype) + rotate_half_axial(x) * sin.astype(x.dtype)


def expand_kv(ckv, w_ukv):
    B, S, _ = ckv.shape
    kv = jnp.einsum('bsr,rn->bsn', ckv, w_ukv).reshape(B, S, MLA_HEADS, NOPE_DIM + V_DIM)
    return kv[..., :NOPE_DIM], kv[..., NOPE_DIM:]


def mla_attend(q_nope, q_pe, k_nope, k_pe, v):
    B, T = q_nope.shape[:2]
    nb = T // Q_BLOCK
    scale = (NOPE_DIM + ROPE_DIM) ** -0.5

    def to_blocks(a):
        return jnp.moveaxis(a.reshape(B, nb, Q_BLOCK, *a.shape[2:]), 1, 0)

    def block(args):
        qn, qp = args
        s = (jnp.einsum('bqhd,bkhd->bhqk', qn, k_nope, preferred_element_type=jnp.float32)
             + jnp.einsum('bqhd,bkd->bhqk', qp, k_pe, preferred_element_type=jnp.float32))
        p = jax.nn.softmax(s * scale, axis=-1).astype(v.dtype)
        return jnp.einsum('bhqk,bkhd->bqhd', p, v)

    out = lax.map(block, (to_blocks(q_nope), to_blocks(q_pe)))
    return jnp.moveaxis(out, 0, 1).reshape(B, T, MLA_HEADS * V_DIM)


def conv_module(u, w_dw, b_dw, g_cn, b_cn):
    a, gate = jnp.split(u, 2, -1)
    y = a * jax.nn.sigmoid(gate)
    y = lax.conv_general_dilated(
        y, w_dw[:, None, :], window_strides=(1,),
        padding=[(CONV_KERNEL // 2, CONV_KERNEL // 2)],
        dimension_numbers=('NWC', 'WIO', 'NWC'),
        feature_group_count=CONV_WIDTH) + b_dw
    return jax.nn.silu(layer_norm(y, g_cn, b_cn))


def mixing_sublayer(h, w_in, g_q, w_uq, g_kv, w_ukv, w_dw, b_dw, g_cn, b_cn, w_out,
                    rope=None, ctx_ckv=None, ctx_kpe=None):
    B, T, _ = h.shape
    proj = jnp.einsum('btd,dn->btn', h, w_in)
    o1 = Q_RANK
    o2 = o1 + KV_RANK
    o3 = o2 + ROPE_DIM
    q_c = proj[..., :o1]
    ckv = rms_norm(proj[..., o1:o2], g_kv)
    kpe = proj[..., o2:o3]
    u = proj[..., o3:]
    q = jnp.einsum('btr,rn->btn', rms_norm(q_c, g_q), w_uq).reshape(
        B, T, MLA_HEADS, NOPE_DIM + ROPE_DIM)
    q_nope, q_pe = q[..., :NOPE_DIM], q[..., NOPE_DIM:]
    k_nope, v = expand_kv(ckv, w_ukv)
    k_pe = kpe
    if rope is not None:
        cos, sin = rope
        q_pe = apply_rope(q_pe, cos[:, None, :], sin[:, None, :])
        k_pe = apply_rope(kpe, cos, sin)
        ck_nope, cv = expand_kv(ctx_ckv, w_ukv)
        k_nope = jnp.concatenate([ck_nope, k_nope], axis=1)
        v = jnp.concatenate([cv, v], axis=1)
        k_pe = jnp.concatenate([ctx_kpe, k_pe], axis=1)
    attn = mla_attend(q_nope, q_pe, k_nope, k_pe, v)
    conv = conv_module(u, w_dw, b_dw, g_cn, b_cn)
    out = jnp.einsum('btm,md->btd', jnp.concatenate([attn, conv], -1), w_out)
    return out, ckv, kpe


def peer_ffn(h, w_pq, sub_keys, peer_u, peer_v):
    B, T, D = h.shape
    xt = h.reshape(-1, TOKEN_BLOCK, D)

    def block(xb):
        q = jnp.einsum('nd,dq->nq', xb, w_pq).reshape(TOKEN_BLOCK, PEER_HEADS, 2, PEER_HALF)
        s = jnp.einsum('nhpd,hpkd->nhpk', q, sub_keys, preferred_element_type=jnp.float32)
        s1, i1 = lax.top_k(s[:, :, 0], PEER_TOPK)
        s2, i2 = lax.top_k(s[:, :, 1], PEER_TOPK)
        cand = (s1[..., :, None] + s2[..., None, :]).reshape(TOKEN_BLOCK, PEER_HEADS, -1)
        cidx = (i1[..., :, None] * PEER_KEYS + i2[..., None, :]).reshape(TOKEN_BLOCK, PEER_HEADS, -1)
        top_s, pos = lax.top_k(cand, PEER_TOPK)
        expert = jnp.take_along_axis(cidx, pos, axis=-1).reshape(TOKEN_BLOCK, -1)
        g = jax.nn.softmax(top_s, axis=-1).astype(xb.dtype).reshape(TOKEN_BLOCK, -1)
        a = jax.nn.gelu(jnp.einsum('nd,ned->ne', xb, peer_u[expert]), approximate=False)
        return jnp.einsum('ne,ned->nd', g * a, peer_v[expert])

    return lax.map(block, xt).reshape(B, T, D)


def setup_inputs(seed: int = 0) -> dict:
    key = jax.random.key(seed)
    ks = jax.random.split(key, 32)
    f32 = jnp.float32

    def nrm(k, shape, scale):
        return jax.random.normal(k, shape, f32) * scale

    def gain(k, shape):
        return 1.0 + 0.02 * jax.random.normal(k, shape, f32)

    L = DEPTH
    return {
        "x_prompt": nrm(ks[0], (BATCH, SEQ, D_MODEL), 1.0),
        "x_sample": nrm(ks[1], (DEC_BATCH, DEC_SEQ, D_MODEL), 1.0),
        "cache_ckv": nrm(ks[2], (DEC_BATCH, DEPTH, PAST_LEN, KV_RANK), 1.0),
        "cache_kpe": nrm(ks[3], (DEC_BATCH, DEPTH, PAST_LEN, ROPE_DIM), 1.0),
        "c": nrm(ks[4], (DEC_BATCH, D_MODEL), 1.0),
        "c_ctx": nrm(ks[5], (D_MODEL,), 1.0),
        "w_ada": nrm(ks[6], (L, D_MODEL, 6 * D_MODEL), D_MODEL ** -0.5),
        "b_ada": nrm(ks[7], (L, 6 * D_MODEL), 0.02),
        "w_in": nrm(ks[8], (L, D_MODEL, IN_WIDTH), D_MODEL ** -0.5),
        "g_q": gain(ks[9], (L, Q_RANK)),
        "w_uq": nrm(ks[10], (L, Q_RANK, MLA_HEADS * (NOPE_DIM + ROPE_DIM)), Q_RANK ** -0.5),
        "g_kv": gain(ks[11], (L, KV_RANK)),
        "w_ukv": nrm(ks[12], (L, KV_RANK, MLA_HEADS * (NOPE_DIM + V_DIM)), KV_RANK ** -0.5),
        "w_dw": nrm(ks[13], (L, CONV_KERNEL, CONV_WIDTH), CONV_KERNEL ** -0.5),
        "b_dw": nrm(ks[14], (L, CONV_WIDTH), 0.02),
        "g_cn": gain(ks[15], (L, CONV_WIDTH)),
        "b_cn": nrm(ks[16], (L, CONV_WIDTH), 0.02),
        "w_out": nrm(ks[17], (L, MIX_WIDTH, D_MODEL), BETA * MIX_WIDTH ** -0.5),
        "ln1_g": gain(ks[18], (L, D_MODEL)),
        "ln1_b": nrm(ks[19], (L, D_MODEL), 0.02),
        "w_pq": nrm(ks[20], (L, D_MODEL, PEER_HEADS * PEER_QDIM), D_MODEL ** -0.5),
        "sub_keys": nrm(ks[21], (L, PEER_HEADS, 2, PEER_KEYS, PEER_HALF), PEER_HALF ** -0.5),
        "peer_u": nrm(ks[22], (L, PEER_EXPERTS, D_MODEL), D_MODEL ** -0.5),
        "peer_v": nrm(ks[23], (L, PEER_EXPERTS, D_MODEL), BETA * PEER_HEADS ** -0.5),
        "ln2_g": gain(ks[24], (L, D_MODEL)),
        "ln2_b": nrm(ks[25], (L, D_MODEL), 0.02),
    }


def reference(x_prompt, x_sample, cache_ckv, cache_kpe, c, c_ctx,
              w_ada, b_ada, w_in, g_q, w_uq, g_kv, w_ukv, w_dw, b_dw, g_cn, b_cn,
              w_out, ln1_g, ln1_b, w_pq, sub_keys, peer_u, peer_v, ln2_g, ln2_b):
    rope = axial_rope_tables(x_sample.shape[1])
    xp = x_prompt
    xs = x_sample
    ckv_layers = []
    kpe_layers = []
    for l in range(DEPTH):
        mix_w = (w_in[l], g_q[l], w_uq[l], g_kv[l], w_ukv[l], w_dw[l], b_dw[l],
                 g_cn[l], b_cn[l], w_out[l])
        sh1, sc1, g1, sh2, sc2, g2 = adaln(c_ctx[None, :], w_ada[l], b_ada[l])
        mix, ckv, kpe = mixing_sublayer(xp * (1 + sc1) + sh1, *mix_w)
        ckv_layers.append(ckv)
        kpe_layers.append(kpe)
        xp = layer_norm(ALPHA * xp + g1 * mix, ln1_g[l], ln1_b[l])
        ff = peer_ffn(xp * (1 + sc2) + sh2, w_pq[l], sub_keys[l], peer_u[l], peer_v[l])
        xp = layer_norm(ALPHA * xp + g2 * ff, ln2_g[l], ln2_b[l])
        sh1, sc1, g1, sh2, sc2, g2 = adaln(c, w_ada[l], b_ada[l])
        mix, _, _ = mixing_sublayer(xs * (1 + sc1) + sh1, *mix_w, rope=rope,
                                    ctx_ckv=cache_ckv[:, l], ctx_kpe=cache_kpe[:, l])
        xs = layer_norm(ALPHA * xs + g1 * mix, ln1_g[l], ln1_b[l])
        ff = peer_ffn(xs * (1 + sc2) + sh2, w_pq[l], sub_keys[l], peer_u[l], peer_v[l])
        xs = layer_norm(ALPHA * xs + g2 * ff, ln2_g[l], ln2_b[l])
    new_ckv = jnp.stack(ckv_layers, axis=1)
    new_kpe = jnp.stack(kpe_layers, axis=1)
    return (xp, xs, new_ckv, new_kpe)
```

```python
import numpy as np
import ml_dtypes
from contextlib import ExitStack
import concourse.bass as bass
import concourse.mybir as mybir
from concourse.bass_utils import run_bass_kernel_spmd

F32 = mybir.dt.float32
BF16 = mybir.dt.bfloat16
AF = mybir.ActivationFunctionType
ALU = mybir.AluOpType
AX = mybir.AxisListType

D = 4096
NP_ = 1024
NS = 512
NOWN = NP_ + NS
NALL = 2048
NKEY = 2304
ALPHA = 2.0 ** 0.25
EPS = 1e-6
SCALE = 192.0 ** -0.5
YW = 4 * 286 + 542


class Buf:
    __slots__ = ("w", "r")

    def __init__(self):
        self.w = None
        self.r = {}


class Q:
    def __init__(self, K, name, eng, is_pe=False):
        self.name = name
        self.eng = eng
        self.is_pe = is_pe
        self.sem = K.newsem("q_" + name)
        self.cnt = 0
        self.waited = {}
        self.dsems = []
        self.dvals = []
        self.dma_i = 0
        self.lazy = False

    def wait(self, tok):
        sem, val, owner = tok
        key = id(sem)
        if self.waited.get(key, 0) >= val:
            return
        self.eng.wait_ge(sem, val)
        self.waited[key] = val


class Kern:
    NDMA = 6

    def __init__(self, nc):
        self.nc = nc
        self.stack = ExitStack()
        self.pe = Q(self, "pe", nc.tensor, is_pe=True)
        self.act = Q(self, "act", nc.scalar)
        self.dve = Q(self, "dve", nc.vector)
        self.pool = Q(self, "pool", nc.gpsimd)
        self.sp = Q(self, "sp", nc.sync)
        self.qs = [self.pe, self.act, self.dve, self.pool, self.sp]
        for q in (self.sp, self.pool):
            for i in range(self.NDMA):
                q.dsems.append(self.newsem("d_%s%d" % (q.name, i)))
                q.dvals.append(0)
        self.final = []
        self.uid = 0

    def newsem(self, name):
        return self.stack.enter_context(self.nc.semaphore(name))

    def _deps(self, q, reads, writes):
        for b in reads:
            if b.w is not None and not (b.w[2] is q and q.is_pe):
                q.wait(b.w)
        for b in writes:
            if b.w is not None and not (b.w[2] is q and q.is_pe):
                q.wait(b.w)
            for owner, t in b.r.items():
                if owner is not q or not q.is_pe:
                    q.wait(t)

    def _mark(self, tok, reads, writes):
        for b in reads:
            b.r[tok[2]] = tok
        for b in writes:
            b.w = tok
            b.r = {}

    def op(self, q, fn, reads=(), writes=(), inc=True):
        self._deps(q, reads, writes)
        ins = fn()
        if inc:
            q.cnt += 1
            ins.then_inc(q.sem, 1)
            q.lazy = False
            tok = (q.sem, q.cnt, q)
        else:
            q.lazy = True
            tok = (q.sem, q.cnt + 1, q)
        self._mark(tok, reads, writes)
        return tok

    def dma(self, q, out, in_, reads=(), writes=(), final=False):
        slot = q.dma_i % self.NDMA
        q.dma_i += 1
        sem = q.dsems[slot]
        if q.dvals[slot] > 0:
            q.wait((sem, q.dvals[slot], sem))
        self._deps(q, reads, writes)
        ins = q.eng.dma_start(out=out, in_=in_)
        q.dvals[slot] += 16
        ins.then_inc(sem, 16)
        tok = (sem, q.dvals[slot], sem)
        self._mark(tok, reads, writes)
        if final:
            self.final.append(tok)
        return tok

    def barrier(self):
        toks = []
        for q in self.qs:
            assert not q.lazy, q.name
            if q.cnt > 0:
                toks.append((q.sem, q.cnt, q))
            for s, v in zip(q.dsems, q.dvals):
                if v > 0:
                    toks.append((s, v, s))
        for q in self.qs:
            for t in toks:
                if t[2] is not q:
                    q.wait(t)

    def name(self, base):
        self.uid += 1
        return "%s_%d" % (base, self.uid)

    def finish(self):
        self.barrier()


class Scope:
    def __init__(self, K):
        self.K = K
        self.stack = ExitStack()

    def sb(self, name, shape, dtype):
        return self.stack.enter_context(self.K.nc.sbuf_tensor(self.K.name(name), list(shape), dtype))

    def close(self):
        self.stack.close()


def mm(K, out, pairs, reads, writes, first=True, last=True):
    nc = K.nc
    n = len(pairs)
    tok = None
    for i, (l, r) in enumerate(pairs):
        st = first and i == 0
        sp = last and i == n - 1
        tok = K.op(K.pe, lambda: nc.tensor.matmul(out, l, r, start=st, stop=sp),
                   reads=reads, writes=writes, inc=(i == n - 1))
    return tok


def build(dbg=False, stop_after=99):
    nc = bass.Bass("TRN2", target_bir_lowering=False)
    K = Kern(nc)
    V, A, P, S = K.dve, K.act, K.pool, K.sp
    ins_used = []

    def din(name, shape, dt=F32):
        ins_used.append(name)
        return nc.dram_tensor(name, list(shape), dt, kind="ExternalInput").ap()

    def dout(name, shape, dt=F32):
        return nc.dram_tensor(name, list(shape), dt, kind="ExternalOutput").ap()

    def dscr(name, shape, dt):
        return nc.dram_tensor(name, list(shape), dt, kind=("ExternalOutput" if dbg else "Internal")).ap()

    top = Scope(K)
    banks = [K.stack.enter_context(nc.psum_tensor("bank%d" % i, [128, 512], F32)) for i in range(8)]
    bB = [Buf() for _ in range(8)]
    ident_f = top.sb("ident_f", [128, 128], F32)
    ident_b = top.sb("ident_b", [128, 128], BF16)
    ones_f = top.sb("ones_f", [128, 128], F32)
    mod = top.sb("mod", [128, 192, 2], F32)
    opsc = top.sb("opsc", [128, 2, 32, 2], F32)
    bconst = Buf()
    bmod = Buf()

    K.op(P, lambda: nc.gpsimd.memset(ident_f[:], 0.0), writes=[bconst])
    K.op(P, lambda: nc.gpsimd.affine_select(out=ident_f[:], in_=ident_f[:], pattern=[[-1, 128]],
                                            compare_op=ALU.not_equal, fill=1.0, base=0, channel_multiplier=1),
         reads=[bconst], writes=[bconst])
    K.op(P, lambda: nc.gpsimd.tensor_copy(out=ident_b[:], in_=ident_f[:]), reads=[bconst], writes=[bconst])
    K.op(P, lambda: nc.gpsimd.memset(ones_f[:], 1.0), writes=[bconst])
    epsc = top.sb("epsc", [128, 1], F32)
    K.op(P, lambda: nc.gpsimd.memset(epsc[:], EPS), writes=[bconst])

    def MOD(s, kc, j):
        return mod[:, s * 32 + kc, j:j + 1]

    def OPSC(w, kc, j):
        return opsc[:, w, kc, j:j + 1]

    condT = din("condT", [128, 32, 2])
    w_ada = din("w_ada", [D, 6 * D])
    b_adaT = din("b_adaT", [128, 192])
    sc0 = Scope(K)
    cnd = sc0.sb("cnd", [128, 32, 2], F32)
    sil = sc0.sb("sil", [128, 32, 2], BF16)
    bad = sc0.sb("bad", [128, 192], F32)
    bc = Buf()
    K.dma(S, cnd[:], condT, writes=[bc])
    K.dma(S, bad[:], b_adaT, writes=[bc])
    K.op(A, lambda: nc.scalar.activation(sil[:], cnd[:], AF.Silu), reads=[bc], writes=[bc])
    wav = w_ada.rearrange("(kc p) n -> p kc n", p=128)
    wr = [sc0.sb("wada%d" % i, [128, 32, 512], BF16) for i in range(3)]
    bwr = [Buf() for _ in range(3)]
    for ct in range(48):
        i = ct % 3
        K.dma(P, wr[i][:], wav[:, :, ct * 512:(ct + 1) * 512], writes=[bwr[i]])
        pb = ct % 2
        for ob in range(4):
            mm(K, banks[pb][:, ob * 2:ob * 2 + 2],
               [(wr[i][:, kc, ob * 128:(ob + 1) * 128], sil[:, kc, :]) for kc in range(32)],
               reads=[bwr[i], bc], writes=[bB[pb]])
        K.op(V, lambda: nc.vector.tensor_tensor(
            out=mod[:, ct * 4:(ct + 1) * 4, :],
            in0=banks[pb][:, 0:8].rearrange("p (a b) -> p a b", b=2),
            in1=bad[:, ct * 4:(ct + 1) * 4].unsqueeze(2).to_broadcast([128, 4, 2]), op=ALU.add),
            reads=[bB[pb], bc], writes=[bmod])
    K.op(V, lambda: nc.vector.tensor_scalar(out=opsc[:, 0, :, :], in0=mod[:, 32:64, :], scalar1=1.0, scalar2=None,
                                            op0=ALU.add), reads=[bmod], writes=[bmod])
    K.op(V, lambda: nc.vector.tensor_scalar(out=opsc[:, 1, :, :], in0=mod[:, 128:160, :], scalar1=1.0, scalar2=None,
                                            op0=ALU.add), reads=[bmod], writes=[bmod])
    if dbg:
        d_mod = dout("d_mod", [128, 192, 2])
        K.dma(S, d_mod, mod[:], reads=[bmod])
    K.barrier()
    sc0.close()
    if stop_after <= 0:
        K.finish()
        return nc, ins_used

    xT = din("xT", [D, NALL])
    w_in = din("w_in", [D, 5440])
    g_qT = din("g_qT", [128, 6])
    g_kvT = din("g_kvT", [128, 4])
    w_dwT = din("w_dwT", [128, 16, 31])
    b_dwT = din("b_dwT", [128, 16])
    halo_mask = din("halo_mask", [128, 2])
    o_ckvT = dout("o_ckvT", [512, NP_])
    o_kpeT = dout("o_kpeT", [64, NP_])
    qcT_d = dscr("qcT_d", [768, NOWN], BF16)
    rstdq_d = dscr("rstdq_d", [128, NOWN], F32)
    ckvT_d = dscr("ckvT_d", [512, NALL], BF16)
    kpe_d = dscr("kpe_d", [64, NALL], F32)
    conv_d = dscr("conv_d", [2048, NOWN], F32)
    cstat_d = dscr("cstat_d", [2, 128, NOWN], F32)
    xv = xT.rearrange("(kc p) n -> p kc n", p=128)
    wiv = w_in.rearrange("(kc p) n -> p kc n", p=128)

    sc1 = Scope(K)
    gq = sc1.sb("gq", [128, 6], F32)
    gkv = sc1.sb("gkv", [128, 4], F32)
    wdw = sc1.sb("wdw", [128, 16, 31], F32)
    bdw = sc1.sb("bdw", [128, 16], F32)
    hmask = sc1.sb("hmask", [128, 2], F32)
    bsm = Buf()
    for t_, s_ in ((gq, g_qT), (gkv, g_kvT), (wdw, w_dwT), (bdw, b_dwT), (hmask, halo_mask)):
        K.dma(S, t_[:], s_, writes=[bsm])
    wt = [sc1.sb("wt%d" % i, [128, 32, 256], BF16) for i in range(2)]
    bwt = [Buf() for _ in range(2)]
    wti = [0]

    def load_w(c0, ncol, c1=None):
        i = wti[0] % 2
        wti[0] += 1
        if c1 is None:
            K.dma(P, wt[i][:, :, 0:ncol], wiv[:, :, c0:c0 + ncol], writes=[bwt[i]])
        else:
            K.dma(P, wt[i][:, :, 0:128], wiv[:, :, c0:c0 + 128], writes=[bwt[i]])
            K.dma(P, wt[i][:, :, 128:256], wiv[:, :, c1:c1 + 128], writes=[bwt[i]])
        return i

    def modulate(hT, bh, ncols, col0, ranges, sc):
        xs = [sc.sb("xs%d" % i, [128, ncols], F32) for i in range(2)]
        bxs = [Buf() for _ in range(2)]
        for kc in range(32):
            i = kc % 2
            K.dma(S, xs[i][:], xv[:, kc, col0:col0 + ncols], writes=[bxs[i]])
            for ri, (lo, hi, j) in enumerate(ranges):
                if (kc + ri) % 2 == 0:
                    K.op(V, lambda: nc.vector.tensor_scalar(out=hT[:, kc, lo:hi], in0=xs[i][:, lo:hi],
                                                            scalar1=OPSC(0, kc, j), scalar2=MOD(0, kc, j),
                                                            op0=ALU.mult, op1=ALU.add),
                         reads=[bxs[i], bmod], writes=[bh])
                else:
                    K.op(A, lambda: nc.scalar.activation(hT[:, kc, lo:hi], xs[i][:, lo:hi], AF.Identity,
                                                         bias=MOD(0, kc, j), scale=OPSC(0, kc, j)),
                         reads=[bxs[i], bmod], writes=[bh])

    pbi = [0]

    def next_bank(lo=0, hi=4):
        b = lo + pbi[0] % (hi - lo)
        pbi[0] += 1
        return b

    def rstd_from_sumsq(sq, bsq, ncols, nfeat, sc):
        for t in range(0, ncols, 512):
            w = min(512, ncols - t)
            mm(K, banks[7][:, 0:w], [(ones_f[:], sq[:, t:t + w])], reads=[bsq, bconst], writes=[bB[7]])
            K.op(A, lambda: nc.scalar.activation(sq[:, t:t + w], banks[7][:, 0:w], AF.Sqrt, bias=epsc[:, 0:1],
                                                 scale=1.0 / nfeat), reads=[bB[7], bconst], writes=[bsq])
        K.op(V, lambda: nc.vector.reciprocal(out=sq[:, 0:ncols], in_=sq[:, 0:ncols]), reads=[bsq], writes=[bsq])

    def proj_kv(hT, bh, ncols, col0, sc, is_own):
        ntile = ncols // 512
        raw = sc.sb("ckvraw", [128, 4, ncols], F32)
        sq = sc.sb("sqkv", [128, ncols], F32)
        sqt = [sc.sb("sqt%d" % i, [128, 512], F32) for i in range(2)]
        braw, bsq = Buf(), Buf()
        bsqt = [Buf(), Buf()]
        K.op(P, lambda: nc.gpsimd.memset(sq[:], 0.0), writes=[bsq])
        k = 0
        for tl in range(2):
            i = load_w(768 + tl * 256, 256)
            for ob in range(2):
                b4 = tl * 2 + ob
                for t in range(ntile):
                    pb = next_bank()
                    mm(K, banks[pb][:], [(wt[i][:, kc, ob * 128:(ob + 1) * 128], hT[:, kc, t * 512:(t + 1) * 512])
                                         for kc in range(32)], reads=[bwt[i], bh], writes=[bB[pb]])
                    K.op(A, lambda: nc.scalar.copy(raw[:, b4, t * 512:(t + 1) * 512], banks[pb][:]),
                         reads=[bB[pb]], writes=[braw])
                    j = k % 2
                    k += 1
                    K.op(A, lambda: nc.scalar.activation(sqt[j][:], banks[pb][:], AF.Square),
                         reads=[bB[pb]], writes=[bsqt[j]])
                    K.op(V, lambda: nc.vector.tensor_tensor(out=sq[:, t * 512:(t + 1) * 512],
                                                            in0=sq[:, t * 512:(t + 1) * 512], in1=sqt[j][:], op=ALU.add),
                         reads=[bsqt[j], bsq], writes=[bsq])
        rstd_from_sumsq(sq, bsq, ncols, 512.0, sc)
        nrm = [sc.sb("nrm%d" % i, [128, 512], F32) for i in range(2)]
        nrb = [sc.sb("nrb%d" % i, [128, 512], BF16) for i in range(2)]
        bn = [Buf(), Buf()]
        bnb = [Buf(), Buf()]
        k = 0
        for b4 in range(4):
            for t in range(ntile):
                j = k % 2
                k += 1
                cs = slice(t * 512, (t + 1) * 512)
                K.op(V, lambda: nc.vector.scalar_tensor_tensor(out=nrm[j][:], in0=raw[:, b4, cs], scalar=gkv[:, b4:b4 + 1],
                                                               in1=sq[:, cs], op0=ALU.mult, op1=ALU.mult),
                     reads=[braw, bsq, bsm], writes=[bn[j]])
                if is_own and t < 2:
                    K.dma(S, o_ckvT[b4 * 128:(b4 + 1) * 128, cs], nrm[j][:], reads=[bn[j]], final=True)
                K.op(A, lambda: nc.scalar.copy(nrb[j][:], nrm[j][:]), reads=[bn[j]], writes=[bnb[j]])
                K.dma(S, ckvT_d[b4 * 128:(b4 + 1) * 128, col0 + t * 512:col0 + (t + 1) * 512], nrb[j][:],
                      reads=[bnb[j]])
        i = load_w(1280, 64)
        kraw = sc.sb("kraw", [64, ncols], F32)
        bk = Buf()
        for t in range(ntile):
            pb = next_bank()
            mm(K, banks[pb][0:64, :], [(wt[i][:, kc, 0:64], hT[:, kc, t * 512:(t + 1) * 512]) for kc in range(32)],
               reads=[bwt[i], bh], writes=[bB[pb]])
            K.op(A, lambda: nc.scalar.copy(kraw[:, t * 512:(t + 1) * 512], banks[pb][0:64, :]),
                 reads=[bB[pb]], writes=[bk])
        K.dma(S, kpe_d[:, col0:col0 + ncols], kraw[:], reads=[bk])
        if is_own:
            K.dma(S, o_kpeT[:, :], kraw[:, 0:NP_], reads=[bk], final=True)

    scx = Scope(K)
    hTo = scx.sb("hTo", [128, 32, 512], BF16)
    bho = Buf()
    modulate(hTo, bho, 512, NOWN, [(0, 512, 1)], scx)
    proj_kv(hTo, bho, 512, NOWN, scx, False)
    K.barrier()
    scx.close()

    NH = NOWN + 16
    hT = sc1.sb("hT", [128, 32, NH], BF16)
    bh = Buf()
    sca = Scope(K)
    modulate(hT, bh, NH, 0, [(0, NP_, 0), (NP_, NH, 1)], sca)
    K.barrier()
    sca.close()
    sca = Scope(K)
    proj_kv(hT, bh, NOWN, 0, sca, True)
    sqq = sca.sb("sqq", [128, NOWN], F32)
    sqt2 = [sca.sb("sqt2%d" % i, [128, 512], F32) for i in range(2)]
    qst = [sca.sb("qst%d" % i, [128, NOWN], BF16) for i in range(2)]
    bsqq = Buf()
    bsqt2 = [Buf(), Buf()]
    bqst = [Buf(), Buf()]
    K.op(P, lambda: nc.gpsimd.memset(sqq[:], 0.0), writes=[bsqq])
    k = 0
    for tl in range(3):
        i = load_w(tl * 256, 256)
        for ob in range(2):
            qb = tl * 2 + ob
            for t in range(3):
                cs = slice(t * 512, (t + 1) * 512)
                pb = next_bank()
                mm(K, banks[pb][:], [(wt[i][:, kc, ob * 128:(ob + 1) * 128], hT[:, kc, cs]) for kc in range(32)],
                   reads=[bwt[i], bh], writes=[bB[pb]])
                K.op(A, lambda: nc.scalar.activation(qst[qb % 2][:, cs], banks[pb][:], AF.Identity,
                                                     scale=gq[:, qb:qb + 1]),
                     reads=[bB[pb], bsm], writes=[bqst[qb % 2]])
                j = k % 2
                k += 1
                K.op(A, lambda: nc.scalar.activation(sqt2[j][:], banks[pb][:], AF.Square),
                     reads=[bB[pb]], writes=[bsqt2[j]])
                K.op(V, lambda: nc.vector.tensor_tensor(out=sqq[:, cs], in0=sqq[:, cs], in1=sqt2[j][:], op=ALU.add),
                     reads=[bsqt2[j], bsqq], writes=[bsqq])
            K.dma(S, qcT_d[qb * 128:(qb + 1) * 128, :], qst[qb % 2][:], reads=[bqst[qb % 2]])
    rstd_from_sumsq(sqq, bsqq, NOWN, 768.0, sca)
    K.dma(S, rstdq_d, sqq[:], reads=[bsqq])
    K.barrier()
    sca.close()
    if stop_after <= 1:
        K.finish()
        return nc, ins_used

    scb = Scope(K)
    ypad = [scb.sb("ypad%d" % i, [128, YW], BF16) for i in range(2)]
    dg = [scb.sb("dg%d" % i, [128, 31, 128], BF16) for i in range(2)]
    sg = [scb.sb("sg%d" % i, [128, 512], F32) for i in range(2)]
    yh = scb.sb("yh", [128, 16], F32)
    cv = [scb.sb("cv%d" % i, [128, NOWN], F32) for i in range(2)]
    cq = scb.sb("cq", [128, NOWN], F32)
    s1c = scb.sb("s1c", [128, NOWN], F32)
    s2c = scb.sb("s2c", [128, NOWN], F32)
    byp = [Buf(), Buf()]
    bdg = [Buf(), Buf()]
    bsg = [Buf(), Buf()]
    byh, bcq, bs1, bs2 = Buf(), Buf(), Buf(), Buf()
    bcv = [Buf(), Buf()]
    for i in range(2):
        K.op(P, lambda: nc.gpsimd.memset(ypad[i][:], 0.0), writes=[byp[i]])
    K.op(P, lambda: nc.gpsimd.memset(s1c[:], 0.0), writes=[bs1])
    K.op(P, lambda: nc.gpsimd.memset(s2c[:], 0.0), writes=[bs2])

    def ywin(yp, t, k):
        if t < 2:
            return yp[:, t * 572:(t + 1) * 572].rearrange("p (s w) -> p s w", w=286)[:, :, k:k + 256]
        return yp[:, 1144 + k:1144 + k + 512]

    def conv_chunk(j):
        yp = ypad[j % 2]
        for t in range(3):
            if t < 2:
                o = banks[4 + t][:].rearrange("p (s w) -> p s w", w=256)
            else:
                o = banks[4 + t][:]
            mm(K, o, [(dg[j % 2][:, k, :], ywin(yp, t, k)) for k in range(31)],
               reads=[bdg[j % 2], byp[j % 2]], writes=[bB[4 + t]])
            cs = slice(t * 512, (t + 1) * 512)
            K.op(A, lambda: nc.scalar.activation(cv[j % 2][:, cs], banks[4 + t][:], AF.Identity,
                                                 bias=bdw[:, j:j + 1]),
                 reads=[bB[4 + t], bsm], writes=[bcv[j % 2]])
        K.dma(S, conv_d[j * 128:(j + 1) * 128, :], cv[j % 2][:], reads=[bcv[j % 2]])
        K.op(V, lambda: nc.vector.tensor_tensor(out=s1c[:], in0=s1c[:], in1=cv[j % 2][:], op=ALU.add),
             reads=[bcv[j % 2], bs1], writes=[bs1])
        K.op(A, lambda: nc.scalar.activation(cq[:], cv[j % 2][:], AF.Square), reads=[bcv[j % 2]], writes=[bcq])
        K.op(V, lambda: nc.vector.tensor_tensor(out=s2c[:], in0=s2c[:], in1=cq[:], op=ALU.add),
             reads=[bcq, bs2], writes=[bs2])

    for jp in range(8):
        ia = load_w(1344 + 256 * jp, 256)
        ig = load_w(3392 + 256 * jp, 256)
        for jj in range(2):
            j = jp * 2 + jj
            yp = ypad[j % 2]
            K.op(V, lambda: nc.vector.tensor_tensor(
                out=dg[j % 2][:], in0=ident_b[:].unsqueeze(1).to_broadcast([128, 31, 128]),
                in1=wdw[:, j, :].unsqueeze(2).to_broadcast([128, 31, 128]), op=ALU.mult),
                reads=[bconst, bsm], writes=[bdg[j % 2]])
            for t in range(4):
                if t < 3:
                    cs = slice(t * 512, (t + 1) * 512)
                    w_ = 512
                else:
                    cs = slice(NOWN, NOWN + 16)
                    w_ = 16
                pa = next_bank()
                pg = next_bank()
                mm(K, banks[pa][:, 0:w_], [(wt[ia][:, kc, jj * 128:(jj + 1) * 128], hT[:, kc, cs]) for kc in range(32)],
                   reads=[bwt[ia], bh], writes=[bB[pa]])
                mm(K, banks[pg][:, 0:w_], [(wt[ig][:, kc, jj * 128:(jj + 1) * 128], hT[:, kc, cs]) for kc in range(32)],
                   reads=[bwt[ig], bh], writes=[bB[pg]])
                s_ = sg[t % 2]
                K.op(A, lambda: nc.scalar.activation(s_[:, 0:w_], banks[pg][:, 0:w_], AF.Sigmoid),
                     reads=[bB[pg]], writes=[bsg[t % 2]])
                if t < 2:
                    o = yp[:, t * 572:(t + 1) * 572].rearrange("p (s w) -> p s w", w=286)[:, :, 15:271]
                    K.op(V, lambda: nc.vector.tensor_tensor(
                        out=o, in0=banks[pa][:].rearrange("p (s w) -> p s w", w=256),
                        in1=s_[:].rearrange("p (s w) -> p s w", w=256), op=ALU.mult),
                        reads=[bB[pa], bsg[t % 2]], writes=[byp[j % 2]])
                elif t == 2:
                    K.op(V, lambda: nc.vector.tensor_tensor(out=yp[:, 1159:1159 + 512], in0=banks[pa][:], in1=s_[:],
                                                            op=ALU.mult),
                         reads=[bB[pa], bsg[t % 2]], writes=[byp[j % 2]])
                else:
                    K.op(V, lambda: nc.vector.tensor_tensor(out=yh[:], in0=banks[pa][:, 0:16], in1=s_[:, 0:16],
                                                            op=ALU.mult),
                         reads=[bB[pa], bsg[t % 2]], writes=[byh])
                    K.op(V, lambda: nc.vector.tensor_scalar(out=yp[:, 1144:1159], in0=yh[:, 0:15],
                                                            scalar1=hmask[:, 0:1], scalar2=None, op0=ALU.mult),
                         reads=[byh, bsm], writes=[byp[j % 2]])
                    K.op(V, lambda: nc.vector.tensor_scalar(out=yp[:, 1671:1686], in0=yh[:, 0:15],
                                                            scalar1=hmask[:, 1:2], scalar2=None, op0=ALU.mult),
                         reads=[byh, bsm], writes=[byp[j % 2]])
            if j > 0:
                conv_chunk(j - 1)
    conv_chunk(15)
    for t in range(3):
        cs = slice(t * 512, (t + 1) * 512)
        mm(K, banks[0][:], [(ones_f[:], s1c[:, cs])], reads=[bs1, bconst], writes=[bB[0]])
        mm(K, banks[1][:], [(ones_f[:], s2c[:, cs])], reads=[bs2, bconst], writes=[bB[1]])
        K.op(V, lambda: nc.vector.tensor_scalar(out=s1c[:, cs], in0=banks[0][:], scalar1=1.0 / 2048, scalar2=None,
                                                op0=ALU.mult), reads=[bB[0]], writes=[bs1])
        K.op(V, lambda: nc.vector.tensor_tensor(out=cq[:, cs], in0=s1c[:, cs], in1=s1c[:, cs], op=ALU.mult),
             reads=[bs1], writes=[bcq])
        K.op(V, lambda: nc.vector.scalar_tensor_tensor(out=s2c[:, cs], in0=banks[1][:], scalar=1.0 / 2048,
                                                       in1=cq[:, cs], op0=ALU.mult, op1=ALU.subtract),
             reads=[bB[1], bcq], writes=[bs2])
    K.op(A, lambda: nc.scalar.activation(s2c[:], s2c[:], AF.Sqrt, bias=epsc[:, 0:1], scale=1.0),
         reads=[bs2, bconst], writes=[bs2])
    K.op(V, lambda: nc.vector.reciprocal(out=s2c[:], in_=s2c[:]), reads=[bs2], writes=[bs2])
    K.dma(S, cstat_d[0], s1c[:], reads=[bs1])
    K.dma(S, cstat_d[1], s2c[:], reads=[bs2])
    K.barrier()
    scb.close()
    sc1.close()
    if stop_after <= 2:
        K.finish()
        return nc, ins_used

    w_uq = din("w_uq", [768, 3072])
    w_ukv = din("w_ukv", [512, 4096])
    cache_ckvT = din("cache_ckvT", [128, 4, 256])
    cache_kpeT = din("cache_kpeT", [64, 256])
    cosT = din("cosT", [64, 1024])
    sinT = din("sinT", [64, 1024])
    rmatT = din("rmatT", [64, 64])
    attnT_d = dscr("attnT_d", [2048, NOWN], BF16)
    wuqv = w_uq.rearrange("(kc p) n -> p kc n", p=128)
    wukvv = w_ukv.rearrange("(kc p) n -> p kc n", p=128)
    sc2 = Scope(K)
    ckv = sc2.sb("ckv", [128, 4, NKEY], BF16)
    kpe = sc2.sb("kpeb", [64, NKEY], BF16)
    qc = sc2.sb("qc", [128, 6, NOWN], BF16)
    rq = sc2.sb("rq", [128, NOWN], F32)
    kraw = sc2.sb("kraw2", [64, NALL], F32)
    cos = sc2.sb("cos", [64, 1024], F32)
    sin = sc2.sb("sin", [64, 1024], F32)
    rm = sc2.sb("rm", [64, 64], F32)
    rt1 = sc2.sb("rt1", [64, 512], F32)
    rt2 = sc2.sb("rt2", [64, 512], F32)
    qpf = sc2.sb("qpf", [64, 512], F32)
    bckv, bkpe, bqc, brq, bkraw, btab, brt1, brt2, bqpf = [Buf() for _ in range(9)]
    K.dma(S, ckv[:, :, 0:NALL], ckvT_d.rearrange("(kc p) n -> p kc n", p=128), writes=[bckv])
    K.dma(P, ckv[:, :, NALL:NKEY], cache_ckvT, writes=[bckv])
    K.dma(S, kraw[:], kpe_d, writes=[bkraw])
    K.dma(P, kpe[:, NALL:NKEY], cache_kpeT, writes=[bkpe])
    K.dma(S, qc[:], qcT_d.rearrange("(kc p) n -> p kc n", p=128), writes=[bqc])
    K.dma(S, rq[:], rstdq_d, writes=[brq])
    K.dma(S, cos[:], cosT, writes=[btab])
    K.dma(S, sin[:], sinT, writes=[btab])
    K.dma(S, rm[:], rmatT, writes=[btab])
    K.op(A, lambda: nc.scalar.copy(kpe[:, 0:NP_], kraw[:, 0:NP_]), reads=[bkraw], writes=[bkpe])

    def rope(dst, bdst, src, bsrc, tab0):
        pb = next_bank(0, 6)
        mm(K, banks[pb][0:64, :], [(rm[:, :], src)], reads=[btab, bsrc], writes=[bB[pb]])
        K.op(V, lambda: nc.vector.tensor_tensor(out=rt1[:], in0=src, in1=cos[:, tab0:tab0 + 512], op=ALU.mult),
             reads=[bsrc, btab], writes=[brt1])
        K.op(V, lambda: nc.vector.tensor_tensor(out=rt2[:], in0=banks[pb][0:64, :], in1=sin[:, tab0:tab0 + 512],
                                                op=ALU.mult), reads=[bB[pb], btab], writes=[brt2])
        K.op(V, lambda: nc.vector.tensor_tensor(out=dst, in0=rt1[:], in1=rt2[:], op=ALU.add),
             reads=[brt1, brt2], writes=[bdst])

    for t in range(2):
        rope(kpe[:, NP_ + t * 512:NP_ + (t + 1) * 512], bkpe, kraw[:, NP_ + t * 512:NP_ + (t + 1) * 512], bkraw, t * 512)

    wq = [sc2.sb("wq%d" % i, [128, 6, 192], BF16) for i in range(2)]
    wkv = [sc2.sb("wkv%d" % i, [128, 4, 256], BF16) for i in range(2)]
    qn = [sc2.sb("qn%d" % i, [128, NOWN], BF16) for i in range(2)]
    qp = [sc2.sb("qp%d" % i, [64, NOWN], BF16) for i in range(2)]
    kn = [sc2.sb("kn%d" % i, [128, NKEY], BF16) for i in range(2)]
    vh = [sc2.sb("vh%d" % i, [128, 18, 128], BF16) for i in range(2)]
    ast = [sc2.sb("ast%d" % i, [128, NOWN], BF16) for i in range(2)]
    p32 = [sc2.sb("p32%d" % i, [128, 1280], F32) for i in range(2)]
    pn = [sc2.sb("pn%d" % i, [128, 1280], BF16) for i in range(2)]
    pt = [sc2.sb("pt%d" % i, [128, 10, 128], BF16) for i in range(2)]
    st = [sc2.sb("st%d" % i, [128, 8], F32) for i in range(2)]
    bwq, bwkv, bqn, bqp, bkn, bvh, bast, bp32, bpn, bpt, bst, bpv = [[Buf(), Buf()] for _ in range(12)]
    tb = [banks[6][:].bitcast(BF16), banks[7][:].bitcast(BF16)]

    qblocks = []
    for s_ in range(4):
        for hh in range(2):
            qblocks.append((s_ * 256 + hh * 128, s_ * 256, 256, 2 * s_))
    for i_ in range(4):
        qblocks.append((NP_ + i_ * 128, NP_, 1280, 8))

    import os
    _NH = int(os.environ.get('P2_HEADS', 16)); _NQ = int(os.environ.get('P2_QB', 12)); _STG = int(os.environ.get('P2_STAGE', 4))
    for h in range(_NH):
        hp = h % 2
        K.dma(P, wq[hp][:], wuqv[:, :, h * 192:(h + 1) * 192], writes=[bwq[hp]])
        K.dma(P, wkv[hp][:], wukvv[:, :, h * 256:(h + 1) * 256], writes=[bwkv[hp]])
        for t in range(3):
            cs = slice(t * 512, (t + 1) * 512)
            pb = next_bank(0, 6)
            mm(K, banks[pb][:], [(wq[hp][:, kc, 0:128], qc[:, kc, cs]) for kc in range(6)],
               reads=[bwq[hp], bqc], writes=[bB[pb]])
            K.op(V, lambda: nc.vector.tensor_tensor(out=qn[hp][:, cs], in0=banks[pb][:], in1=rq[:, cs], op=ALU.mult),
                 reads=[bB[pb], brq], writes=[bqn[hp]])
            pb = next_bank(0, 6)
            mm(K, banks[pb][0:64, :], [(wq[hp][:, kc, 128:192], qc[:, kc, cs]) for kc in range(6)],
               reads=[bwq[hp], bqc], writes=[bB[pb]])
            if t < 2:
                K.op(V, lambda: nc.vector.tensor_tensor(out=qp[hp][:, cs], in0=banks[pb][0:64, :], in1=rq[0:64, cs],
                                                        op=ALU.mult), reads=[bB[pb], brq], writes=[bqp[hp]])
            else:
                K.op(V, lambda: nc.vector.tensor_tensor(out=qpf[:], in0=banks[pb][0:64, :], in1=rq[0:64, cs],
                                                        op=ALU.mult), reads=[bB[pb], brq], writes=[bqpf])
                rope(qp[hp][:, cs], bqp[hp], qpf[:], bqpf, 0)
        for t in range(5):
            w_ = 512 if t < 4 else 256
            cs = slice(t * 512, t * 512 + w_)
            pb = next_bank(0, 6)
            mm(K, banks[pb][:, 0:w_], [(wkv[hp][:, kc, 0:128], ckv[:, kc, cs]) for kc in range(4)],
               reads=[bwkv[hp], bckv], writes=[bB[pb]])
            K.op(A, lambda: nc.scalar.copy(kn[hp][:, cs], banks[pb][:, 0:w_]), reads=[bB[pb]], writes=[bkn[hp]])
        for g in range(5):
            nb_ = 4 if g < 4 else 2
            pb = next_bank(0, 6)
            for kk in range(nb_):
                kb = g * 4 + kk
                mm(K, banks[pb][:, kk * 128:(kk + 1) * 128],
                   [(ckv[:, kc, kb * 128:(kb + 1) * 128], wkv[hp][:, kc, 128:256]) for kc in range(4)],
                   reads=[bwkv[hp], bckv], writes=[bB[pb]])
            K.op(A if g % 2 else V,
                 (lambda: nc.scalar.copy(vh[hp][:, g * 4:g * 4 + nb_, :],
                                         banks[pb][:, 0:nb_ * 128].rearrange("p (a b) -> p a b", b=128))) if g % 2 else
                 (lambda: nc.vector.tensor_copy(out=vh[hp][:, g * 4:g * 4 + nb_, :],
                                                in_=banks[pb][:, 0:nb_ * 128].rearrange("p (a b) -> p a b", b=128))),
                 reads=[bB[pb]], writes=[bvh[hp]])

        def emit_S(qi):
            q0, k0, nk, vb0 = qblocks[qi]
            base = 3 * (qi % 2)
            for kt in range((nk + 511) // 512):
                w_ = min(512, nk - kt * 512)
                ks = slice(k0 + kt * 512, k0 + kt * 512 + w_)
                mm(K, banks[base + kt][:, 0:w_],
                   [(qn[hp][:, q0:q0 + 128], kn[hp][:, ks]), (qp[hp][:, q0:q0 + 128], kpe[:, ks])],
                   reads=[bqn[hp], bqp[hp], bkn[hp], bkpe], writes=[bB[base + kt]])

        def emit_softmax(qi):
            q0, k0, nk, vb0 = qblocks[qi]
            base = 3 * (qi % 2)
            e = qi % 2
            nt = (nk + 511) // 512
            for kt in range(nt):
                w_ = min(512, nk - kt * 512)
                K.op(V, lambda: nc.vector.reduce_max(out=st[e][:, kt:kt + 1], in_=banks[base + kt][:, 0:w_], axis=AX.X),
                     reads=[bB[base + kt]], writes=[bst[e]])
            if nt > 1:
                K.op(V, lambda: nc.vector.reduce_max(out=st[e][:, 3:4], in_=st[e][:, 0:nt], axis=AX.X),
                     reads=[bst[e]], writes=[bst[e]])
                mcol = 3
            else:
                mcol = 0
            K.op(V, lambda: nc.vector.tensor_scalar(out=st[e][:, 4:5], in0=st[e][:, mcol:mcol + 1], scalar1=-SCALE,
                                                    scalar2=None, op0=ALU.mult), reads=[bst[e]], writes=[bst[e]])
            for kt in range(nt):
                w_ = min(512, nk - kt * 512)
                K.op(A, lambda: nc.scalar.activation(p32[e][:, kt * 512:kt * 512 + w_], banks[base + kt][:, 0:w_],
                                                     AF.Exp, bias=st[e][:, 4:5], scale=SCALE),
                     reads=[bB[base + kt], bst[e]], writes=[bp32[e]])
            K.op(V, lambda: nc.vector.reduce_sum(out=st[e][:, 5:6], in_=p32[e][:, 0:nk], axis=AX.X),
                 reads=[bp32[e]], writes=[bst[e]])
            K.op(V, lambda: nc.vector.reciprocal(out=st[e][:, 6:7], in_=st[e][:, 5:6]), reads=[bst[e]], writes=[bst[e]])
            K.op(A, lambda: nc.scalar.activation(pn[e][:, 0:nk], p32[e][:, 0:nk], AF.Identity, scale=st[e][:, 6:7]),
                 reads=[bp32[e], bst[e]], writes=[bpn[e]])

        def emit_PV(qi):
            q0, k0, nk, vb0 = qblocks[qi]
            base = 3 * (qi % 2)
            e = qi % 2
            nkb = nk // 128
            for kb in range(nkb):
                tbi = kb // 8
                K.op(K.pe, lambda: nc.tensor.transpose(tb[tbi][:, (kb % 8) * 128:(kb % 8 + 1) * 128],
                                                       pn[e][:, kb * 128:(kb + 1) * 128], ident_b[:]),
                     reads=[bpn[e], bconst], writes=[bB[6 + tbi]])
            n0 = min(nkb, 8)
            K.op(V, lambda: nc.vector.tensor_copy(out=pt[e][:, 0:n0, :],
                                                  in_=tb[0][:, 0:n0 * 128].rearrange("p (a b) -> p a b", b=128)),
                 reads=[bB[6]], writes=[bpt[e]])
            if nkb > 8:
                K.op(V, lambda: nc.vector.tensor_copy(out=pt[e][:, 8:nkb, :],
                                                      in_=tb[1][:, 0:(nkb - 8) * 128].rearrange("p (a b) -> p a b", b=128)),
                     reads=[bB[7]], writes=[bpt[e]])
            mm(K, banks[base + 2][:, 256:384], [(vh[hp][:, vb0 + kb, :], pt[e][:, kb, :]) for kb in range(nkb)],
               reads=[bvh[hp], bpt[e]], writes=[bB[base + 2]])
            K.op(A, lambda: nc.scalar.copy(ast[hp][:, q0:q0 + 128], banks[base + 2][:, 256:384]),
                 reads=[bB[base + 2]], writes=[bast[hp]])

        if _STG >= 2:
            emit_S(0)
        for qi in range(_NQ):
            if qi + 1 < _NQ and _STG >= 2:
                emit_S(qi + 1)
            if _STG >= 3:
                emit_softmax(qi)
            if _STG >= 4:
                emit_PV(qi)
        K.dma(S, attnT_d[h * 128:(h + 1) * 128, :], ast[hp][:], reads=[bast[hp]])
    K.barrier()
    sc2.close()
    if stop_after <= 3:
        K.finish()
        return nc, ins_used

    def ln_stats(s1, bs1, s2, bs2, tmp, btmp, nfeat, ncols):
        for t in range(0, ncols, 512):
            cs = slice(t, t + 512)
            mm(K, banks[6][:], [(ones_f[:], s1[:, cs])], reads=[bs1, bconst], writes=[bB[6]])
            mm(K, banks[7][:], [(ones_f[:], s2[:, cs])], reads=[bs2, bconst], writes=[bB[7]])
            K.op(V, lambda: nc.vector.tensor_scalar(out=s1[:, cs], in0=banks[6][:], scalar1=1.0 / nfeat, scalar2=None,
                                                    op0=ALU.mult), reads=[bB[6]], writes=[bs1])
            K.op(V, lambda: nc.vector.tensor_tensor(out=tmp[:, cs], in0=s1[:, cs], in1=s1[:, cs], op=ALU.mult),
                 reads=[bs1], writes=[btmp])
            K.op(V, lambda: nc.vector.scalar_tensor_tensor(out=s2[:, cs], in0=banks[7][:], scalar=1.0 / nfeat,
                                                           in1=tmp[:, cs], op0=ALU.mult, op1=ALU.subtract),
                 reads=[bB[7], btmp], writes=[bs2])
        K.op(A, lambda: nc.scalar.activation(s2[:, 0:ncols], s2[:, 0:ncols], AF.Sqrt, bias=epsc[:, 0:1], scale=1.0),
             reads=[bs2, bconst], writes=[bs2])
        K.op(V, lambda: nc.vector.reciprocal(out=s2[:, 0:ncols], in_=s2[:, 0:ncols]), reads=[bs2], writes=[bs2])

    w_out = din("w_out", [D, D])
    g_cnT = din("g_cnT", [128, 16])
    b_cnT = din("b_cnT", [128, 16])
    ln1_gT = din("ln1_gT", [128, 32])
    ln1_bT = din("ln1_bT", [128, 32])
    zT_d = dscr("zT_d", [D, NOWN], F32)
    stat1_d = dscr("stat1_d", [2, 128, NOWN], F32)
    h2T_d = dscr("h2T_d", [D, NOWN], BF16)
    wov = w_out.rearrange("(kc p) n -> p kc n", p=128)
    sc3 = Scope(K)
    mix = sc3.sb("mix", [128, 32, NOWN], BF16)
    gcn = sc3.sb("gcn", [128, 16], F32)
    bcn = sc3.sb("bcn", [128, 16], F32)
    l1g = sc3.sb("l1g", [128, 32], F32)
    l1b = sc3.sb("l1b", [128, 32], F32)
    a1 = sc3.sb("a1", [128, 32, 2], F32)
    b1 = sc3.sb("b1", [128, 32, 2], F32)
    bmix, bsm3, bab = Buf(), Buf(), Buf()
    for t_, s_ in ((gcn, g_cnT), (bcn, b_cnT), (l1g, ln1_gT), (l1b, ln1_bT)):
        K.dma(S, t_[:], s_, writes=[bsm3])
    K.dma(S, mix[:, 0:16, :], attnT_d.rearrange("(kc p) n -> p kc n", p=128), writes=[bmix])
    K.op(V, lambda: nc.vector.tensor_tensor(out=a1[:], in0=opsc[:, 1, :, :],
                                            in1=l1g[:].unsqueeze(2).to_broadcast([128, 32, 2]), op=ALU.mult),
         reads=[bmod, bsm3], writes=[bab])
    K.op(V, lambda: nc.vector.tensor_tensor(out=b1[:], in0=opsc[:, 1, :, :],
                                            in1=l1b[:].unsqueeze(2).to_broadcast([128, 32, 2]), op=ALU.mult),
         reads=[bmod, bsm3], writes=[bab])
    K.op(V, lambda: nc.vector.tensor_tensor(out=b1[:], in0=b1[:], in1=mod[:, 96:128, :], op=ALU.add),
         reads=[bmod, bab], writes=[bab])
    s3a = Scope(K)
    cm = s3a.sb("cm", [128, NOWN], F32)
    cr = s3a.sb("cr", [128, NOWN], F32)
    cvl = [s3a.sb("cvl%d" % i, [128, NOWN], F32) for i in range(2)]
    cvt = [s3a.sb("cvt%d" % i, [128, NOWN], F32) for i in range(2)]
    bcs = Buf()
    bcvl = [Buf(), Buf()]
    bcvt = [Buf(), Buf()]
    K.dma(S, cm[:], cstat_d[0], writes=[bcs])
    K.dma(S, cr[:], cstat_d[1], writes=[bcs])
    for j in range(16):
        i = j % 2
        K.dma(S, cvl[i][:], conv_d[j * 128:(j + 1) * 128, :], writes=[bcvl[i]])
        K.op(V, lambda: nc.vector.tensor_tensor(out=cvt[i][:], in0=cvl[i][:], in1=cm[:], op=ALU.subtract),
             reads=[bcvl[i], bcs], writes=[bcvt[i]])
        K.op(P, lambda: nc.gpsimd.tensor_tensor(out=cvt[i][:], in0=cvt[i][:], in1=cr[:], op=ALU.mult),
             reads=[bcvt[i], bcs], writes=[bcvt[i]])
        K.op(A, lambda: nc.scalar.activation(mix[:, 16 + j, :], cvt[i][:], AF.Silu, bias=bcn[:, j:j + 1],
                                             scale=gcn[:, j:j + 1]), reads=[bcvt[i], bsm3], writes=[bmix])
    K.barrier()
    s3a.close()
    s3b = Scope(K)
    wo = [s3b.sb("wo%d" % i, [128, 32, 256], BF16) for i in range(2)]
    xs3 = [s3b.sb("xs3%d" % i, [128, NOWN], F32) for i in range(2)]
    zt = [s3b.sb("zt%d" % i, [128, NOWN], F32) for i in range(2)]
    zz = [s3b.sb("zz%d" % i, [128, NOWN], F32) for i in range(2)]
    z1 = s3b.sb("z1", [128, NOWN], F32)
    z2 = s3b.sb("z2", [128, NOWN], F32)
    zq = s3b.sb("zq", [128, NOWN], F32)
    bwo = [Buf() for _ in range(2)]
    bxs3, bzt, bzz = [[Buf(), Buf()] for _ in range(3)]
    bz1, bz2, bzq = Buf(), Buf(), Buf()
    K.op(P, lambda: nc.gpsimd.memset(z1[:], 0.0), writes=[bz1])
    K.op(P, lambda: nc.gpsimd.memset(z2[:], 0.0), writes=[bz2])
    for tl in range(16):
        i = tl % 2
        K.dma(P, wo[i][:], wov[:, :, tl * 256:(tl + 1) * 256], writes=[bwo[i]])
        for ob in range(2):
            db = tl * 2 + ob
            e = db % 2
            K.dma(S, xs3[e][:], xv[:, db, 0:NOWN], writes=[bxs3[e]])
            for t in range(3):
                cs = slice(t * 512, (t + 1) * 512)
                j = 0 if t < 2 else 1
                pb = next_bank(0, 6)
                mm(K, banks[pb][:], [(wo[i][:, kc, ob * 128:(ob + 1) * 128], mix[:, kc, cs]) for kc in range(32)],
                   reads=[bwo[i], bmix], writes=[bB[pb]])
                K.op(A, lambda: nc.scalar.activation(zt[e][:, cs], banks[pb][:], AF.Identity, scale=MOD(2, db, j)),
                     reads=[bB[pb], bmod], writes=[bzt[e]])
                K.op(V, lambda: nc.vector.scalar_tensor_tensor(out=zz[e][:, cs], in0=xs3[e][:, cs], scalar=ALPHA,
                                                               in1=zt[e][:, cs], op0=ALU.mult, op1=ALU.add),
                     reads=[bxs3[e], bzt[e]], writes=[bzz[e]])
            K.dma(S, zT_d[db * 128:(db + 1) * 128, :], zz[e][:], reads=[bzz[e]])
            K.op(P, lambda: nc.gpsimd.tensor_tensor(out=z1[:], in0=z1[:], in1=zz[e][:], op=ALU.add),
                 reads=[bzz[e], bz1], writes=[bz1])
            K.op(A, lambda: nc.scalar.activation(zq[:], zz[e][:], AF.Square), reads=[bzz[e]], writes=[bzq])
            K.op(P, lambda: nc.gpsimd.tensor_tensor(out=z2[:], in0=z2[:], in1=zq[:], op=ALU.add),
                 reads=[bzq, bz2], writes=[bz2])
    ln_stats(z1, bz1, z2, bz2, zq, bzq, 4096.0, NOWN)
    K.dma(S, stat1_d[0], z1[:], reads=[bz1])
    K.dma(S, stat1_d[1], z2[:], reads=[bz2])
    h2s = [s3b.sb("h2s%d" % i, [128, NOWN], BF16) for i in range(2)]
    bh2s = [Buf(), Buf()]
    for db in range(32):
        e = db % 2
        K.dma(S, xs3[e][:], zT_d[db * 128:(db + 1) * 128, :], writes=[bxs3[e]])
        K.op(V, lambda: nc.vector.tensor_tensor(out=zt[e][:], in0=xs3[e][:], in1=z1[:], op=ALU.subtract),
             reads=[bxs3[e], bz1], writes=[bzt[e]])
        K.op(P, lambda: nc.gpsimd.tensor_tensor(out=zt[e][:], in0=zt[e][:], in1=z2[:], op=ALU.mult),
             reads=[bzt[e], bz2], writes=[bzt[e]])
        for (lo, hi, j) in ((0, NP_, 0), (NP_, NOWN, 1)):
            K.op(A, lambda: nc.scalar.activation(h2s[e][:, lo:hi], zt[e][:, lo:hi], AF.Identity,
                                                 bias=b1[:, db, j:j + 1], scale=a1[:, db, j:j + 1]),
                 reads=[bzt[e], bab], writes=[bh2s[e]])
        K.dma(S, h2T_d[db * 128:(db + 1) * 128, :], h2s[e][:], reads=[bh2s[e]])
    K.barrier()
    s3b.close()
    sc3.close()
    if stop_after <= 4:
        K.finish()
        return nc, ins_used

    w_pq = din("w_pq", [D, D])
    skT = din("skT", [128, 32, 128])
    s_d = dscr("s_d", [NOWN, 2048], F32)
    G_d = dscr("G_d", [128, 128, NOWN], BF16)
    wpv = w_pq.rearrange("(kc p) n -> p kc n", p=128)
    sc4 = Scope(K)
    h2 = sc4.sb("h2", [128, 32, NOWN], BF16)
    sk = sc4.sb("sk", [128, 32, 128], BF16)
    wp = [sc4.sb("wp%d" % i, [128, 32, 256], BF16) for i in range(3)]
    qhp = [sc4.sb("qhp%d" % i, [128, 2, NOWN], BF16) for i in range(2)]
    sst = [sc4.sb("sst%d" % i, [128, 12, 128], F32) for i in range(2)]
    bh2, bsk = Buf(), Buf()
    bwp = [Buf() for _ in range(3)]
    bqhp, bsst = [[Buf(), Buf()] for _ in range(2)]
    h2v = h2T_d.rearrange("(kc p) n -> p kc n", p=128)
    for q4 in range(4):
        K.dma(S, h2[:, q4 * 8:(q4 + 1) * 8, :], h2v[:, q4 * 8:(q4 + 1) * 8, :], writes=[bh2])
    K.dma(P, sk[:], skT, writes=[bsk])
    s_dv = s_d.rearrange("(nb p) (hp k) -> p nb hp k", p=128, k=128)
    cpy = [0]

    def evac(out, in_, reads, writes):
        cpy[0] += 1
        if cpy[0] % 2:
            return K.op(A, lambda: nc.scalar.copy(out, in_), reads=reads, writes=writes)
        return K.op(V, lambda: nc.vector.tensor_copy(out=out, in_=in_), reads=reads, writes=writes)

    for hp_ in range(16):
        i = hp_ % 3
        e = hp_ % 2
        K.dma(P, wp[i][:], wpv[:, :, hp_ * 256:(hp_ + 1) * 256], writes=[bwp[i]])
        for half in range(2):
            for t in range(3):
                cs = slice(t * 512, (t + 1) * 512)
                pb = next_bank(0, 4)
                mm(K, banks[pb][:], [(wp[i][:, kc, half * 128:(half + 1) * 128], h2[:, kc, cs]) for kc in range(32)],
                   reads=[bwp[i], bh2], writes=[bB[pb]])
                evac(qhp[e][:, half, cs], banks[pb][:], [bB[pb]], [bqhp[e]])
        for g in range(3):
            pb = 4 + (hp_ * 3 + g) % 4
            for kk in range(4):
                nb = g * 4 + kk
                mm(K, banks[pb][:, kk * 128:(kk + 1) * 128],
                   [(qhp[e][:, half, nb * 128:(nb + 1) * 128], sk[:, hp_ * 2 + half, :]) for half in range(2)],
                   reads=[bqhp[e], bsk], writes=[bB[pb]])
            evac(sst[e][:, g * 4:(g + 1) * 4, :], banks[pb][:].rearrange("p (a b) -> p a b", b=128), [bB[pb]], [bsst[e]])
        K.dma(S, s_dv[:, :, hp_, :], sst[e][:], reads=[bsst[e]])
    K.barrier()
    sc4.close()
    if stop_after <= 5:
        K.finish()
        return nc, ins_used

    scr = Scope(K)
    stm = [scr.sb("stm%d" % i, [128, 2048], F32) for i in range(2)]
    wrk = scr.sb("wrk", [128, 256], F32)
    tp = scr.sb("tp", [128, 16, 16], F32)
    cand = scr.sb("cand", [128, 8, 256], F32)
    ctop = scr.sb("ctop", [128, 8, 16], F32)
    ce = scr.sb("ce", [128, 8, 16], F32)
    sm = scr.sb("sm", [128, 64], F32)
    At = scr.sb("At", [128, 3, 128], F32)
    AT = [scr.sb("AT%d" % i, [128, 3, 128], F32) for i in range(2)]
    srep = [scr.sb("srep%d" % i, [128, 16, 256], F32) for i in range(2)]
    zr = [scr.sb("zr%d" % i, [128, 16, 128], F32) for i in range(2)]
    zc = [scr.sb("zc%d" % i, [128, 16, 128], F32) for i in range(2)]
    er = [scr.sb("er%d" % i, [128, 16, 128], BF16) for i in range(2)]
    mk = [scr.sb("mk%d" % i, [128, 16, 128], BF16) for i in range(2)]
    Rb = [scr.sb("Rb%d" % i, [128, 16, 128], BF16) for i in range(2)]
    P1b = [scr.sb("P1b%d" % i, [128, 16, 128], BF16) for i in range(2)]
    gst = [scr.sb("gst%d" % i, [128, 128, 128], BF16) for i in range(2)]
    bstm, bAT, bsrep, bzr, bzc, ber, bmk, bRb, bP1b, bgst = [[Buf(), Buf()] for _ in range(10)]
    bwrk, btp, bcand, bctop, bce, bsmm, bAt = [Buf() for _ in range(7)]
    tpv = tp[:].rearrange("p (h t) a -> p h t a", t=2)
    G_dv = G_d.rearrange("i j n -> j i n")

    def top16(dst, src, width, rd, wr):
        K.op(V, lambda: nc.vector.max(out=dst[:, 0:8], in_=src), reads=rd, writes=wr)
        K.op(V, lambda: nc.vector.match_replace(out=wrk[:, 0:width], in_to_replace=dst[:, 0:8], in_values=src,
                                                imm_value=-1e30), reads=rd + wr, writes=[bwrk])
        K.op(V, lambda: nc.vector.max(out=dst[:, 8:16], in_=wrk[:, 0:width]), reads=[bwrk], writes=wr)

    def bc816(ap):
        return ap.unsqueeze(2).to_broadcast([128, 8, 16])

    def topk_stage(nb):
        e = nb % 2
        K.dma(S, stm[e][:], s_d[nb * 128:(nb + 1) * 128, :], writes=[bstm[e]])
        for hp_ in range(16):
            top16(tp[:, hp_, :], stm[e][:, hp_ * 128:(hp_ + 1) * 128], 128, [bstm[e]], [btp])
            if hp_ % 4 == 3:
                yield
        for h in range(8):
            K.op(V, lambda: nc.vector.tensor_tensor(
                out=cand[:, h, :].rearrange("p (a b) -> p a b", b=16),
                in0=tpv[:, h, 0, :].unsqueeze(2).to_broadcast([128, 16, 16]),
                in1=tpv[:, h, 1, :].unsqueeze(1).to_broadcast([128, 16, 16]), op=ALU.add),
                reads=[btp], writes=[bcand])
        yield
        for h in range(8):
            top16(ctop[:, h, :], cand[:, h, :], 256, [bcand], [bctop])
            if h % 4 == 3:
                yield
        K.op(V, lambda: nc.vector.tensor_reduce(out=sm[:, 0:8], in_=ctop[:], axis=AX.X, op=ALU.max),
             reads=[bctop], writes=[bsmm])
        K.op(V, lambda: nc.vector.tensor_reduce(out=sm[:, 8:16], in_=ctop[:], axis=AX.X, op=ALU.min),
             reads=[bctop], writes=[bsmm])
        K.op(V, lambda: nc.vector.tensor_tensor(out=ce[:], in0=ctop[:], in1=bc816(sm[:, 0:8]), op=ALU.subtract),
             reads=[bctop, bsmm], writes=[bce])
        K.op(A, lambda: nc.scalar.activation(ce[:], ce[:], AF.Exp), reads=[bce], writes=[bce])
        K.op(V, lambda: nc.vector.tensor_reduce(out=sm[:, 16:24], in_=ce[:], axis=AX.X, op=ALU.add),
             reads=[bce], writes=[bsmm])
        K.op(A, lambda: nc.scalar.activation(sm[:, 24:32], sm[:, 16:24], AF.Ln), reads=[bsmm], writes=[bsmm])
        K.op(V, lambda: nc.vector.tensor_tensor(out=sm[:, 32:40], in0=sm[:, 0:8], in1=sm[:, 24:32], op=ALU.add),
             reads=[bsmm], writes=[bsmm])
        K.op(V, lambda: nc.vector.tensor_copy(out=At[:, 0, :].rearrange("p (h a) -> p h a", a=16), in_=bc816(sm[:, 8:16])),
             reads=[bsmm], writes=[bAt])
        K.op(V, lambda: nc.vector.tensor_tensor(out=At[:, 1, :].rearrange("p (h a) -> p h a", a=16), in0=tpv[:, :, 0, :],
                                                in1=bc816(sm[:, 32:40]), op=ALU.subtract),
             reads=[btp, bsmm], writes=[bAt])
        K.op(V, lambda: nc.vector.tensor_copy(out=At[:, 2, :].rearrange("p (h a) -> p h a", a=16), in_=tpv[:, :, 0, :]),
             reads=[btp], writes=[bAt])
        pbt = nb % 2
        for q3 in range(3):
            K.op(K.pe, lambda: nc.tensor.transpose(banks[pbt][:, q3 * 128:(q3 + 1) * 128], At[:, q3, :], ident_f[:]),
                 reads=[bAt, bconst], writes=[bB[pbt]])
        K.op(V, lambda: nc.vector.tensor_copy(out=AT[e][:], in_=banks[pbt][:, 0:384].rearrange("p (a b) -> p a b", b=128)),
             reads=[bB[pbt]], writes=[bAT[e]])
        yield

    def bcn(ap):
        return ap.unsqueeze(2).to_broadcast([128, 16, 128])

    def sub_block(nb, sb):
        e = nb % 2
        f = (nb * 8 + sb) % 2
        ns = slice(sb * 16, sb * 16 + 16)
        row0 = nb * 128 + sb * 16
        for h in range(8):
            src = bass.AP(s_d.tensor, row0 * 2048 + h * 256, [[0, 16], [2048, 16], [1, 256]])
            K.dma(S, srep[f][h * 16:(h + 1) * 16, :, :], src, writes=[bsrep[f]])
        K.op(P, lambda: nc.gpsimd.tensor_tensor(out=zr[f][:], in0=srep[f][:, :, 128:256], in1=bcn(AT[e][:, 2, ns]),
                                                op=ALU.add), reads=[bsrep[f], bAT[e]], writes=[bzr[f]])
        K.op(P, lambda: nc.gpsimd.tensor_tensor(out=zc[f][:], in0=srep[f][:, :, 128:256], in1=bcn(AT[e][:, 1, ns]),
                                                op=ALU.add), reads=[bsrep[f], bAT[e]], writes=[bzc[f]])
        K.op(V, lambda: nc.vector.tensor_tensor(out=mk[f][:], in0=zr[f][:], in1=bcn(AT[e][:, 0, ns]), op=ALU.is_ge),
             reads=[bzr[f], bAT[e]], writes=[bmk[f]])
        K.op(A, lambda: nc.scalar.activation(er[f][:], zc[f][:], AF.Exp), reads=[bzc[f]], writes=[ber[f]])
        K.op(V, lambda: nc.vector.tensor_tensor(out=P1b[f][:], in0=srep[f][:, :, 0:128], in1=bcn(AT[e][:, 2, ns]),
                                                op=ALU.is_equal), reads=[bsrep[f], bAT[e]], writes=[bP1b[f]])
        K.op(V, lambda: nc.vector.tensor_tensor(out=Rb[f][:], in0=mk[f][:], in1=er[f][:], op=ALU.mult),
             reads=[bmk[f], ber[f]], writes=[bRb[f]])
        for q4 in range(4):
            pb = 2 + (sb * 4 + q4) % 6
            for tk in range(4):
                t16 = q4 * 4 + tk
                mm(K, banks[pb][:, tk * 128:(tk + 1) * 128], [(Rb[f][:, t16, :], P1b[f][:, t16, :])],
                   reads=[bRb[f], bP1b[f]], writes=[bB[pb]])
            n0 = sb * 16 + q4 * 4
            K.op(A, lambda: nc.scalar.copy(gst[e][:, :, n0:n0 + 4], banks[pb][:].rearrange("p (n i) -> p i n", n=4)),
                 reads=[bB[pb]], writes=[bgst[e]])

    for _ in topk_stage(0):
        pass
    for nb in range(12):
        nxt = topk_stage(nb + 1) if nb + 1 < 12 else iter(())
        for sb in range(8):
            sub_block(nb, sb)
            next(nxt, None)
        for _ in nxt:
            pass
        K.dma(S, G_dv[:, :, nb * 128:(nb + 1) * 128], gst[nb % 2][:], reads=[bgst[nb % 2]])
    K.barrier()
    scr.close()
    if stop_after <= 6:
        K.finish()
        return nc, ins_used

    peer_uT = din("peer_uT", [128, 128, 4096])
    peer_v = din("peer_v", [16384, D])
    ln2_gT = din("ln2_gT", [128, 32])
    ln2_bT = din("ln2_bT", [128, 32])
    o_yT = dout("o_yT", [D, NOWN])
    pvv = peer_v.rearrange("(g a p) d -> g p a d", a=4, p=128)
    sc5 = Scope(K)
    h2p = sc5.sb("h2p", [128, 32, 512], BF16)
    acc = sc5.sb("acc", [128, 32, 512], F32)
    l1g5 = sc5.sb("l1g5", [128, 32], F32)
    l1b5 = sc5.sb("l1b5", [128, 32], F32)
    l2g = sc5.sb("l2g", [128, 32], F32)
    l2b = sc5.sb("l2b", [128, 32], F32)
    bh2p, bsm5 = Buf(), Buf()
    bacc = [Buf() for _ in range(32)]
    for t_, s_ in ((l1g5, ln1_gT), (l1b5, ln1_bT), (l2g, ln2_gT), (l2b, ln2_bT)):
        K.dma(S, t_[:], s_, writes=[bsm5])
    _P5P = int(os.environ.get('P5_PASSES', 3)); _P5G = int(os.environ.get('P5_GROUPS', 32)); _P5E = int(os.environ.get('P5_EPI', 1))
    for pt_ in range(_P5P):
        c0 = pt_ * 512
        cj = 0 if pt_ < 2 else 1
        pcs = slice(c0, c0 + 512)
        K.dma(S, h2p[:], h2v[:, :, pcs], writes=[bh2p])
        for db in range(32):
            K.op(V, lambda: nc.vector.memset(acc[:, db, :], 0.0), writes=[bacc[db]])
        s5a = Scope(K)
        ut = [s5a.sb("ut%d" % i, [128, 32, 128], BF16) for i in range(3)]
        vt = [s5a.sb("vt%d" % i, [128, 4, D], BF16) for i in range(2)]
        gt = [s5a.sb("gt%d" % i, [128, 512], BF16) for i in range(3)]
        ga32 = [s5a.sb("ga32%d" % i, [128, 512], F32) for i in range(2)]
        gab = [s5a.sb("gab%d" % i, [128, 4, 512], BF16) for i in range(2)]
        but, bvt, bgt, bga32, bgab = [[Buf(), Buf(), Buf()] for _ in range(5)]

        def phaseB(g):
            gi = g % 2
            for db in range(32):
                pb = 2 + db % 6
                mm(K, banks[pb][:], [(vt[gi][:, a, db * 128:(db + 1) * 128], gab[gi][:, a, :]) for a in range(4)],
                   reads=[bvt[gi], bgab[gi]], writes=[bB[pb]])
                K.op(V, lambda: nc.vector.tensor_tensor(out=acc[:, db, :], in0=acc[:, db, :], in1=banks[pb][:], op=ALU.add),
                     reads=[bB[pb], bacc[db]], writes=[bacc[db]])

        for g in range(_P5G):
            gi = g % 2
            for a in range(4):
                eb = g * 4 + a
                ei = eb % 2
                u3 = eb % 3
                K.dma(P, ut[u3][:].rearrange("p a b -> p (a b)"), peer_uT[eb], writes=[but[u3]])
                K.dma(S, gt[u3][:], G_d[eb, :, pcs], writes=[bgt[u3]])
                mm(K, banks[ei][:], [(ut[u3][:, kc, :], h2p[:, kc, :]) for kc in range(32)],
                   reads=[but[u3], bh2p], writes=[bB[ei]])
                K.op(A, lambda: nc.scalar.activation(ga32[ei][:], banks[ei][:], AF.Gelu), reads=[bB[ei]], writes=[bga32[ei]])
                K.op(V, lambda: nc.vector.tensor_tensor(out=gab[gi][:, a, :], in0=ga32[ei][:], in1=gt[u3][:], op=ALU.mult),
                     reads=[bga32[ei], bgt[u3]], writes=[bgab[gi]])
            K.dma(P, vt[gi][:], pvv[g], writes=[bvt[gi]])
            if g > 0:
                phaseB(g - 1)
        phaseB(_P5G - 1)
        K.barrier()
        s5a.close()
        s5b = Scope(K)
        m1 = s5b.sb("m1", [128, 512], F32)
        r1 = s5b.sb("r1", [128, 512], F32)
        y1 = s5b.sb("y1", [128, 512], F32)
        y2 = s5b.sb("y2", [128, 512], F32)
        yq = s5b.sb("yq", [128, 512], F32)
        zl = [s5b.sb("zl%d" % i, [128, 512], F32) for i in range(2)]
        yst = [s5b.sb("yst%d" % i, [128, 512], F32) for i in range(2)]
        bst1, by1, by2, byq = Buf(), Buf(), Buf(), Buf()
        bzl, byst = [[Buf(), Buf()] for _ in range(2)]
        K.dma(S, m1[:], stat1_d[0][:, pcs], writes=[bst1])
        K.dma(S, r1[:], stat1_d[1][:, pcs], writes=[bst1])
        K.op(P, lambda: nc.gpsimd.memset(y1[:], 0.0), writes=[by1])
        K.op(P, lambda: nc.gpsimd.memset(y2[:], 0.0), writes=[by2])
        for db in range(32 if _P5E else 0):
            e = db % 2
            K.dma(S, zl[e][:], zT_d[db * 128:(db + 1) * 128, pcs], writes=[bzl[e]])
            K.op(V, lambda: nc.vector.tensor_tensor(out=zl[e][:], in0=zl[e][:], in1=m1[:], op=ALU.subtract),
                 reads=[bzl[e], bst1], writes=[bzl[e]])
            K.op(P, lambda: nc.gpsimd.tensor_tensor(out=zl[e][:], in0=zl[e][:], in1=r1[:], op=ALU.mult),
                 reads=[bzl[e], bst1], writes=[bzl[e]])
            K.op(A, lambda: nc.scalar.activation(zl[e][:], zl[e][:], AF.Identity, bias=l1b5[:, db:db + 1],
                                                 scale=l1g5[:, db:db + 1]), reads=[bzl[e], bsm5], writes=[bzl[e]])
            K.op(A, lambda: nc.scalar.activation(acc[:, db, :], acc[:, db, :], AF.Identity, scale=MOD(5, db, cj)),
                 reads=[bacc[db], bmod], writes=[bacc[db]])
            K.op(V, lambda: nc.vector.scalar_tensor_tensor(out=acc[:, db, :], in0=zl[e][:], scalar=ALPHA,
                                                           in1=acc[:, db, :], op0=ALU.mult, op1=ALU.add),
                 reads=[bzl[e], bacc[db]], writes=[bacc[db]])
            K.op(P, lambda: nc.gpsimd.tensor_tensor(out=y1[:], in0=y1[:], in1=acc[:, db, :], op=ALU.add),
                 reads=[bacc[db], by1], writes=[by1])
            K.op(A, lambda: nc.scalar.activation(yq[:], acc[:, db, :], AF.Square), reads=[bacc[db]], writes=[byq])
            K.op(P, lambda: nc.gpsimd.tensor_tensor(out=y2[:], in0=y2[:], in1=yq[:], op=ALU.add),
                 reads=[byq, by2], writes=[by2])
        ln_stats(y1, by1, y2, by2, yq, byq, 4096.0, 512)
        for db in range(32):
            e = db % 2
            K.op(V, lambda: nc.vector.tensor_tensor(out=yst[e][:], in0=acc[:, db, :], in1=y1[:], op=ALU.subtract),
                 reads=[bacc[db], by1], writes=[byst[e]])
            K.op(P, lambda: nc.gpsimd.tensor_tensor(out=yst[e][:], in0=yst[e][:], in1=y2[:], op=ALU.mult),
                 reads=[byst[e], by2], writes=[byst[e]])
            K.op(A, lambda: nc.scalar.activation(yst[e][:], yst[e][:], AF.Identity, bias=l2b[:, db:db + 1],
                                                 scale=l2g[:, db:db + 1]), reads=[byst[e], bsm5], writes=[byst[e]])
            K.dma(S, o_yT[db * 128:(db + 1) * 128, pcs], yst[e][:], reads=[byst[e]], final=True)
        K.barrier()
        s5b.close()
    sc5.close()

    K.finish()
    return nc, ins_used


def _fm(v, nchunk):
    return np.ascontiguousarray(np.asarray(v, np.float32).reshape(nchunk, 128).T)


def _rope_tables(pos):
    row = (pos // 64).astype(np.float32)
    col = (pos % 64).astype(np.float32)
    inv = (10000.0 ** (-np.arange(16, dtype=np.float32) / 16)).astype(np.float32)
    ar = row[:, None] * inv
    ac = col[:, None] * inv
    ang = np.concatenate([ar, ar, ac, ac], -1).astype(np.float32)
    return np.ascontiguousarray(np.cos(ang).T.astype(np.float32)), np.ascontiguousarray(np.sin(ang).T.astype(np.float32))


def _rmat():
    R = np.zeros((64, 64), np.float32)
    for base in (0, 32):
        for i in range(16):
            R[base + i, base + 16 + i] = -1.0
            R[base + 16 + i, base + i] = 1.0
    return np.ascontiguousarray(R.T)


def sample_cols(hf):
    own = np.arange(hf * 512, hf * 512 + 512)
    if hf == 0:
        other = np.arange(512, 1024)
    else:
        other = np.concatenate([np.arange(497, 512), np.arange(0, 497)])
    return own, other


def prep_shared(inp):
    f = lambda k: np.asarray(inp[k], np.float32)
    sh = {}
    sh["w_ada"] = np.ascontiguousarray(f("w_ada")[0])
    sh["b_adaT"] = _fm(f("b_ada")[0], 192)
    sh["w_in"] = np.ascontiguousarray(f("w_in")[0])
    sh["g_qT"] = _fm(f("g_q")[0], 6)
    sh["g_kvT"] = _fm(f("g_kv")[0], 4)
    sh["w_uq"] = np.ascontiguousarray(f("w_uq")[0])
    sh["w_ukv"] = np.ascontiguousarray(f("w_ukv")[0])
    sh["w_dwT"] = np.ascontiguousarray(f("w_dw")[0].T.reshape(16, 128, 31).transpose(1, 0, 2))
    sh["b_dwT"] = _fm(f("b_dw")[0], 16)
    sh["g_cnT"] = _fm(f("g_cn")[0], 16)
    sh["b_cnT"] = _fm(f("b_cn")[0], 16)
    sh["w_out"] = np.ascontiguousarray(f("w_out")[0])
    sh["ln1_gT"] = _fm(f("ln1_g")[0], 32)
    sh["ln1_bT"] = _fm(f("ln1_b")[0], 32)
    sh["w_pq"] = np.ascontiguousarray(f("w_pq")[0])
    sk = f("sub_keys")[0]
    skT = sk.reshape(16, 128, 2, 128).transpose(3, 0, 2, 1)
    sh["skT"] = np.ascontiguousarray(skT.reshape(128, 32, 128))
    pu = f("peer_u")[0]
    sh["peer_uT"] = np.ascontiguousarray(pu.reshape(128, 128, 32, 128).transpose(0, 3, 2, 1)).reshape(128, 128, 4096)
    sh["peer_v"] = np.ascontiguousarray(f("peer_v")[0])
    sh["ln2_gT"] = _fm(f("ln2_g")[0], 32)
    sh["ln2_bT"] = _fm(f("ln2_b")[0], 32)
    sh["rmatT"] = _rmat()
    return sh


def prep_core(inp, c):
    f = lambda k: np.asarray(inp[k], np.float32)
    b, hf = c // 2, c % 2
    own, other = sample_cols(hf)
    xp = f("x_prompt")[4 * c:4 * c + 4].reshape(NP_, D)
    xs = f("x_sample")[b]
    xall = np.concatenate([xp, xs[own], xs[other]], 0)
    m = {}
    m["xT"] = np.ascontiguousarray(xall.T)
    cond = np.stack([f("c_ctx"), f("c")[b]], -1)
    m["condT"] = np.ascontiguousarray(cond.reshape(32, 128, 2).transpose(1, 0, 2))
    m["cache_ckvT"] = np.ascontiguousarray(f("cache_ckv")[b, 0].T.reshape(4, 128, 256).transpose(1, 0, 2))
    m["cache_kpeT"] = np.ascontiguousarray(f("cache_kpe")[b, 0].T)
    cosT, sinT = _rope_tables(np.concatenate([own, other]))
    m["cosT"] = cosT
    m["sinT"] = sinT
    hm = np.zeros((128, 2), np.float32)
    hm[:, 0] = 1.0 if hf == 1 else 0.0
    hm[:, 1] = 1.0 if hf == 0 else 0.0
    m["halo_mask"] = hm
    return m


_CACHE = {}


def kernel(**inputs):
    if "nc" not in _CACHE:
        _CACHE["nc"] = build()
    nc, used = _CACHE["nc"]
    sh = prep_shared(inputs)
    in_maps = []
    for c in range(8):
        m = prep_core(inputs, c)
        m.update(sh)
        in_maps.append({k: m[k] for k in used})
    res = run_bass_kernel_spmd(nc, in_maps, core_ids=list(range(8)))
    y_prompt = np.zeros((32, 256, D), np.float32)
    y_sample = np.zeros((4, 1024, D), np.float32)
    new_ckv = np.zeros((32, 1, 256, 512), np.float32)
    new_kpe = np.zeros((32, 1, 256, 64), np.float32)
    for c in range(8):
        r = res.results[c]
        b, hf = c // 2, c % 2
        yT = np.asarray(r["o_yT"], np.float32)
        y_prompt[4 * c:4 * c + 4] = yT[:, :NP_].T.reshape(4, 256, D)
        y_sample[b, hf * 512:(hf + 1) * 512] = yT[:, NP_:].T
        new_ckv[4 * c:4 * c + 4, 0] = np.asarray(r["o_ckvT"], np.float32).T.reshape(4, 256, 512)
        new_kpe[4 * c:4 * c + 4, 0] = np.asarray(r["o_kpeT"], np.float32).T.reshape(4, 256, 64)
    return (y_prompt, y_sample, new_ckv, new_kpe)
```

```python
import numpy as np
import ml_dtypes
from contextlib import ExitStack
import concourse.bass as bass
import concourse.mybir as mybir
from concourse.bass_utils import run_bass_kernel_spmd

F32 = mybir.dt.float32
BF16 = mybir.dt.bfloat16
AF = mybir.ActivationFunctionType
ALU = mybir.AluOpType
AX = mybir.AxisListType

D = 4096
NP_ = 1024
NS = 512
NOWN = NP_ + NS
NALL = 2048
NKEY = 2304
ALPHA = 2.0 ** 0.25
EPS = 1e-6
SCALE = 192.0 ** -0.5
YW = 4 * 286 + 542


class Buf:
    __slots__ = ("w", "r")

    def __init__(self):
        self.w = None
        self.r = {}


class Q:
    def __init__(self, K, name, eng, is_pe=False):
        self.name = name
        self.eng = eng
        self.is_pe = is_pe
        self.sem = K.newsem("q_" + name)
        self.cnt = 0
        self.waited = {}
        self.dsems = []
        self.dvals = []
        self.dma_i = 0
        self.lazy = False

    def wait(self, tok):
        sem, val, owner = tok
        key = id(sem)
        if self.waited.get(key, 0) >= val:
            return
        self.eng.wait_ge(sem, val)
        self.waited[key] = val


class Kern:
    NDMA = 10

    def __init__(self, nc):
        self.nc = nc
        self.stack = ExitStack()
        self.pe = Q(self, "pe", nc.tensor, is_pe=True)
        self.act = Q(self, "act", nc.scalar)
        self.dve = Q(self, "dve", nc.vector)
        self.pool = Q(self, "pool", nc.gpsimd)
        self.sp = Q(self, "sp", nc.sync)
        self.qs = [self.pe, self.act, self.dve, self.pool, self.sp]
        for q in (self.sp, self.pool):
            for i in range(self.NDMA):
                q.dsems.append(self.newsem("d_%s%d" % (q.name, i)))
                q.dvals.append(0)
        self.final = []
        self.uid = 0

    def newsem(self, name):
        return self.stack.enter_context(self.nc.semaphore(name))

    def _deps(self, q, reads, writes):
        for b in reads:
            if b.w is not None and not (b.w[2] is q and q.is_pe):
                q.wait(b.w)
        for b in writes:
            if b.w is not None and not (b.w[2] is q and q.is_pe):
                q.wait(b.w)
            for owner, t in b.r.items():
                if owner is not q or not q.is_pe:
                    q.wait(t)

    def _mark(self, tok, reads, writes):
        for b in reads:
            b.r[tok[2]] = tok
        for b in writes:
            b.w = tok
            b.r = {}

    def op(self, q, fn, reads=(), writes=(), inc=True):
        self._deps(q, reads, writes)
        ins = fn()
        if inc:
            q.cnt += 1
            ins.then_inc(q.sem, 1)
            q.lazy = False
            tok = (q.sem, q.cnt, q)
        else:
            q.lazy = True
            tok = (q.sem, q.cnt + 1, q)
        self._mark(tok, reads, writes)
        return tok

    def dma(self, q, out, in_, reads=(), writes=(), final=False):
        slot = q.dma_i % self.NDMA
        q.dma_i += 1
        sem = q.dsems[slot]
        if q.dvals[slot] > 0:
            q.wait((sem, q.dvals[slot], sem))
        self._deps(q, reads, writes)
        ins = q.eng.dma_start(out=out, in_=in_)
        q.dvals[slot] += 16
        ins.then_inc(sem, 16)
        tok = (sem, q.dvals[slot], sem)
        self._mark(tok, reads, writes)
        if final:
            self.final.append(tok)
        return tok

    def barrier(self):
        toks = []
        for q in self.qs:
            assert not q.lazy, q.name
            if q.cnt > 0:
                toks.append((q.sem, q.cnt, q))
            for s, v in zip(q.dsems, q.dvals):
                if v > 0:
                    toks.append((s, v, s))
        for q in self.qs:
            for t in toks:
                if t[2] is not q:
                    q.wait(t)

    def name(self, base):
        self.uid += 1
        return "%s_%d" % (base, self.uid)

    def finish(self):
        self.barrier()


class Scope:
    def __init__(self, K):
        self.K = K
        self.stack = ExitStack()

    def sb(self, name, shape, dtype):
        return self.stack.enter_context(self.K.nc.sbuf_tensor(self.K.name(name), list(shape), dtype))

    def close(self):
        self.stack.close()


def mm(K, out, pairs, reads, writes, first=True, last=True):
    nc = K.nc
    n = len(pairs)
    tok = None
    for i, (l, r) in enumerate(pairs):
        st = first and i == 0
        sp = last and i == n - 1
        tok = K.op(K.pe, lambda: nc.tensor.matmul(out, l, r, start=st, stop=sp),
                   reads=reads, writes=writes, inc=(i == n - 1))
    return tok


def build(dbg=False, stop_after=99):
    nc = bass.Bass("TRN2", target_bir_lowering=False)
    K = Kern(nc)
    V, A, P, S = K.dve, K.act, K.pool, K.sp
    ins_used = []

    def din(name, shape, dt=F32):
        ins_used.append(name)
        return nc.dram_tensor(name, list(shape), dt, kind="ExternalInput").ap()

    def dout(name, shape, dt=F32):
        return nc.dram_tensor(name, list(shape), dt, kind="ExternalOutput").ap()

    def dscr(name, shape, dt):
        return nc.dram_tensor(name, list(shape), dt, kind=("ExternalOutput" if dbg else "Internal")).ap()

    top = Scope(K)
    banks = [K.stack.enter_context(nc.psum_tensor("bank%d" % i, [128, 512], F32)) for i in range(8)]
    bB = [Buf() for _ in range(8)]
    ident_f = top.sb("ident_f", [128, 128], F32)
    ident_b = top.sb("ident_b", [128, 128], BF16)
    ones_f = top.sb("ones_f", [128, 128], F32)
    mod = top.sb("mod", [128, 192, 2], F32)
    opsc = top.sb("opsc", [128, 2, 32, 2], F32)
    bconst = Buf()
    bmod = Buf()

    K.op(P, lambda: nc.gpsimd.memset(ident_f[:], 0.0), writes=[bconst])
    K.op(P, lambda: nc.gpsimd.affine_select(out=ident_f[:], in_=ident_f[:], pattern=[[-1, 128]],
                                            compare_op=ALU.not_equal, fill=1.0, base=0, channel_multiplier=1),
         reads=[bconst], writes=[bconst])
    K.op(P, lambda: nc.gpsimd.tensor_copy(out=ident_b[:], in_=ident_f[:]), reads=[bconst], writes=[bconst])
    K.op(P, lambda: nc.gpsimd.memset(ones_f[:], 1.0), writes=[bconst])
    epsc = top.sb("epsc", [128, 1], F32)
    K.op(P, lambda: nc.gpsimd.memset(epsc[:], EPS), writes=[bconst])

    def MOD(s, kc, j):
        return mod[:, s * 32 + kc, j:j + 1]

    def OPSC(w, kc, j):
        return opsc[:, w, kc, j:j + 1]

    condT = din("condT", [128, 32, 2])
    w_ada = din("w_ada", [D, 6 * D])
    b_adaT = din("b_adaT", [128, 192])
    sc0 = Scope(K)
    cnd = sc0.sb("cnd", [128, 32, 2], F32)
    sil = sc0.sb("sil", [128, 32, 2], BF16)
    bad = sc0.sb("bad", [128, 192], F32)
    bc = Buf()
    K.dma(S, cnd[:], condT, writes=[bc])
    K.dma(S, bad[:], b_adaT, writes=[bc])
    K.op(A, lambda: nc.scalar.activation(sil[:], cnd[:], AF.Silu), reads=[bc], writes=[bc])
    wav = w_ada.rearrange("(kc p) n -> p kc n", p=128)
    wr = [sc0.sb("wada%d" % i, [128, 32, 512], BF16) for i in range(3)]
    bwr = [Buf() for _ in range(3)]
    for ct in range(48):
        i = ct % 3
        K.dma(P, wr[i][:], wav[:, :, ct * 512:(ct + 1) * 512], writes=[bwr[i]])
        pb = ct % 2
        for ob in range(4):
            mm(K, banks[pb][:, ob * 2:ob * 2 + 2],
               [(wr[i][:, kc, ob * 128:(ob + 1) * 128], sil[:, kc, :]) for kc in range(32)],
               reads=[bwr[i], bc], writes=[bB[pb]])
        K.op(V, lambda: nc.vector.tensor_tensor(
            out=mod[:, ct * 4:(ct + 1) * 4, :],
            in0=banks[pb][:, 0:8].rearrange("p (a b) -> p a b", b=2),
            in1=bad[:, ct * 4:(ct + 1) * 4].unsqueeze(2).to_broadcast([128, 4, 2]), op=ALU.add),
            reads=[bB[pb], bc], writes=[bmod])
    K.op(V, lambda: nc.vector.tensor_scalar(out=opsc[:, 0, :, :], in0=mod[:, 32:64, :], scalar1=1.0, scalar2=None,
                                            op0=ALU.add), reads=[bmod], writes=[bmod])
    K.op(V, lambda: nc.vector.tensor_scalar(out=opsc[:, 1, :, :], in0=mod[:, 128:160, :], scalar1=1.0, scalar2=None,
                                            op0=ALU.add), reads=[bmod], writes=[bmod])
    if dbg:
        d_mod = dout("d_mod", [128, 192, 2])
        K.dma(S, d_mod, mod[:], reads=[bmod])
    K.barrier()
    sc0.close()
    if stop_after <= 0:
        K.finish()
        return nc, ins_used

    xT = din("xT", [D, NALL])
    w_in = din("w_in", [D, 5440])
    g_qT = din("g_qT", [128, 6])
    g_kvT = din("g_kvT", [128, 4])
    w_dwT = din("w_dwT", [128, 16, 31])
    b_dwT = din("b_dwT", [128, 16])
    halo_mask = din("halo_mask", [128, 2])
    o_ckvT = dout("o_ckvT", [512, NP_])
    o_kpeT = dout("o_kpeT", [64, NP_])
    qcT_d = dscr("qcT_d", [768, NOWN], BF16)
    rstdq_d = dscr("rstdq_d", [128, NOWN], F32)
    ckvT_d = dscr("ckvT_d", [512, NALL], BF16)
    kpe_d = dscr("kpe_d", [64, NALL], F32)
    conv_d = dscr("conv_d", [2048, NOWN], F32)
    cstat_d = dscr("cstat_d", [2, 128, NOWN], F32)
    xv = xT.rearrange("(kc p) n -> p kc n", p=128)
    wiv = w_in.rearrange("(kc p) n -> p kc n", p=128)

    sc1 = Scope(K)
    gq = sc1.sb("gq", [128, 6], F32)
    gkv = sc1.sb("gkv", [128, 4], F32)
    wdw = sc1.sb("wdw", [128, 16, 31], F32)
    bdw = sc1.sb("bdw", [128, 16], F32)
    hmask = sc1.sb("hmask", [128, 2], F32)
    bsm = Buf()
    for t_, s_ in ((gq, g_qT), (gkv, g_kvT), (wdw, w_dwT), (bdw, b_dwT), (hmask, halo_mask)):
        K.dma(S, t_[:], s_, writes=[bsm])
    wt = [sc1.sb("wt%d" % i, [128, 32, 256], BF16) for i in range(2)]
    bwt = [Buf() for _ in range(2)]
    wti = [0]

    def load_w(c0, ncol, c1=None):
        i = wti[0] % 2
        wti[0] += 1
        if c1 is None:
            K.dma(P, wt[i][:, :, 0:ncol], wiv[:, :, c0:c0 + ncol], writes=[bwt[i]])
        else:
            K.dma(P, wt[i][:, :, 0:128], wiv[:, :, c0:c0 + 128], writes=[bwt[i]])
            K.dma(P, wt[i][:, :, 128:256], wiv[:, :, c1:c1 + 128], writes=[bwt[i]])
        return i

    def modulate(hT, bh, ncols, col0, ranges, sc):
        xs = [sc.sb("xs%d" % i, [128, ncols], F32) for i in range(2)]
        bxs = [Buf() for _ in range(2)]
        for kc in range(32):
            i = kc % 2
            K.dma(S, xs[i][:], xv[:, kc, col0:col0 + ncols], writes=[bxs[i]])
            for ri, (lo, hi, j) in enumerate(ranges):
                if (kc + ri) % 2 == 0:
                    K.op(V, lambda: nc.vector.tensor_scalar(out=hT[:, kc, lo:hi], in0=xs[i][:, lo:hi],
                                                            scalar1=OPSC(0, kc, j), scalar2=MOD(0, kc, j),
                                                            op0=ALU.mult, op1=ALU.add),
                         reads=[bxs[i], bmod], writes=[bh])
                else:
                    K.op(A, lambda: nc.scalar.activation(hT[:, kc, lo:hi], xs[i][:, lo:hi], AF.Identity,
                                                         bias=MOD(0, kc, j), scale=OPSC(0, kc, j)),
                         reads=[bxs[i], bmod], writes=[bh])

    pbi = [0]

    def next_bank(lo=0, hi=4):
        b = lo + pbi[0] % (hi - lo)
        pbi[0] += 1
        return b

    def rstd_from_sumsq(sq, bsq, ncols, nfeat, sc):
        for t in range(0, ncols, 512):
            w = min(512, ncols - t)
            mm(K, banks[7][:, 0:w], [(ones_f[:], sq[:, t:t + w])], reads=[bsq, bconst], writes=[bB[7]])
            K.op(A, lambda: nc.scalar.activation(sq[:, t:t + w], banks[7][:, 0:w], AF.Sqrt, bias=epsc[:, 0:1],
                                                 scale=1.0 / nfeat), reads=[bB[7], bconst], writes=[bsq])
        K.op(V, lambda: nc.vector.reciprocal(out=sq[:, 0:ncols], in_=sq[:, 0:ncols]), reads=[bsq], writes=[bsq])

    def proj_kv(hT, bh, ncols, col0, sc, is_own):
        ntile = ncols // 512
        raw = sc.sb("ckvraw", [128, 4, ncols], F32)
        sq = sc.sb("sqkv", [128, ncols], F32)
        sqt = [sc.sb("sqt%d" % i, [128, 512], F32) for i in range(2)]
        braw, bsq = Buf(), Buf()
        bsqt = [Buf(), Buf()]
        K.op(P, lambda: nc.gpsimd.memset(sq[:], 0.0), writes=[bsq])
        k = 0
        for tl in range(2):
            i = load_w(768 + tl * 256, 256)
            for ob in range(2):
                b4 = tl * 2 + ob
                for t in range(ntile):
                    pb = next_bank()
                    mm(K, banks[pb][:], [(wt[i][:, kc, ob * 128:(ob + 1) * 128], hT[:, kc, t * 512:(t + 1) * 512])
                                         for kc in range(32)], reads=[bwt[i], bh], writes=[bB[pb]])
                    K.op(A, lambda: nc.scalar.copy(raw[:, b4, t * 512:(t + 1) * 512], banks[pb][:]),
                         reads=[bB[pb]], writes=[braw])
                    j = k % 2
                    k += 1
                    K.op(A, lambda: nc.scalar.activation(sqt[j][:], banks[pb][:], AF.Square),
                         reads=[bB[pb]], writes=[bsqt[j]])
                    K.op(V, lambda: nc.vector.tensor_tensor(out=sq[:, t * 512:(t + 1) * 512],
                                                            in0=sq[:, t * 512:(t + 1) * 512], in1=sqt[j][:], op=ALU.add),
                         reads=[bsqt[j], bsq], writes=[bsq])
        rstd_from_sumsq(sq, bsq, ncols, 512.0, sc)
        nrm = [sc.sb("nrm%d" % i, [128, 512], F32) for i in range(2)]
        nrb = [sc.sb("nrb%d" % i, [128, 512], BF16) for i in range(2)]
        bn = [Buf(), Buf()]
        bnb = [Buf(), Buf()]
        k = 0
        for b4 in range(4):
            for t in range(ntile):
                j = k % 2
                k += 1
                cs = slice(t * 512, (t + 1) * 512)
                K.op(V, lambda: nc.vector.scalar_tensor_tensor(out=nrm[j][:], in0=raw[:, b4, cs], scalar=gkv[:, b4:b4 + 1],
                                                               in1=sq[:, cs], op0=ALU.mult, op1=ALU.mult),
                     reads=[braw, bsq, bsm], writes=[bn[j]])
                if is_own and t < 2:
                    K.dma(S, o_ckvT[b4 * 128:(b4 + 1) * 128, cs], nrm[j][:], reads=[bn[j]], final=True)
                K.op(A, lambda: nc.scalar.copy(nrb[j][:], nrm[j][:]), reads=[bn[j]], writes=[bnb[j]])
                K.dma(S, ckvT_d[b4 * 128:(b4 + 1) * 128, col0 + t * 512:col0 + (t + 1) * 512], nrb[j][:],
                      reads=[bnb[j]])
        i = load_w(1280, 64)
        kraw = sc.sb("kraw", [64, ncols], F32)
        bk = Buf()
        for t in range(ntile):
            pb = next_bank()
            mm(K, banks[pb][0:64, :], [(wt[i][:, kc, 0:64], hT[:, kc, t * 512:(t + 1) * 512]) for kc in range(32)],
               reads=[bwt[i], bh], writes=[bB[pb]])
            K.op(A, lambda: nc.scalar.copy(kraw[:, t * 512:(t + 1) * 512], banks[pb][0:64, :]),
                 reads=[bB[pb]], writes=[bk])
        K.dma(S, kpe_d[:, col0:col0 + ncols], kraw[:], reads=[bk])
        if is_own:
            K.dma(S, o_kpeT[:, :], kraw[:, 0:NP_], reads=[bk], final=True)

    scx = Scope(K)
    hTo = scx.sb("hTo", [128, 32, 512], BF16)
    bho = Buf()
    modulate(hTo, bho, 512, NOWN, [(0, 512, 1)], scx)
    proj_kv(hTo, bho, 512, NOWN, scx, False)
    K.barrier()
    scx.close()

    NH = NOWN + 16
    hT = sc1.sb("hT", [128, 32, NH], BF16)
    bh = Buf()
    sca = Scope(K)
    modulate(hT, bh, NH, 0, [(0, NP_, 0), (NP_, NH, 1)], sca)
    K.barrier()
    sca.close()
    sca = Scope(K)
    proj_kv(hT, bh, NOWN, 0, sca, True)
    sqq = sca.sb("sqq", [128, NOWN], F32)
    sqt2 = [sca.sb("sqt2%d" % i, [128, 512], F32) for i in range(2)]
    qst = [sca.sb("qst%d" % i, [128, NOWN], BF16) for i in range(2)]
    bsqq = Buf()
    bsqt2 = [Buf(), Buf()]
    bqst = [Buf(), Buf()]
    K.op(P, lambda: nc.gpsimd.memset(sqq[:], 0.0), writes=[bsqq])
    k = 0
    for tl in range(3):
        i = load_w(tl * 256, 256)
        for ob in range(2):
            qb = tl * 2 + ob
            for t in range(3):
                cs = slice(t * 512, (t + 1) * 512)
                pb = next_bank()
                mm(K, banks[pb][:], [(wt[i][:, kc, ob * 128:(ob + 1) * 128], hT[:, kc, cs]) for kc in range(32)],
                   reads=[bwt[i], bh], writes=[bB[pb]])
                K.op(A, lambda: nc.scalar.activation(qst[qb % 2][:, cs], banks[pb][:], AF.Identity,
                                                     scale=gq[:, qb:qb + 1]),
                     reads=[bB[pb], bsm], writes=[bqst[qb % 2]])
                j = k % 2
                k += 1
                K.op(A, lambda: nc.scalar.activation(sqt2[j][:], banks[pb][:], AF.Square),
                     reads=[bB[pb]], writes=[bsqt2[j]])
                K.op(V, lambda: nc.vector.tensor_tensor(out=sqq[:, cs], in0=sqq[:, cs], in1=sqt2[j][:], op=ALU.add),
                     reads=[bsqt2[j], bsqq], writes=[bsqq])
            K.dma(S, qcT_d[qb * 128:(qb + 1) * 128, :], qst[qb % 2][:], reads=[bqst[qb % 2]])
    rstd_from_sumsq(sqq, bsqq, NOWN, 768.0, sca)
    K.dma(S, rstdq_d, sqq[:], reads=[bsqq])
    K.barrier()
    sca.close()
    if stop_after <= 1:
        K.finish()
        return nc, ins_used

    scb = Scope(K)
    ypad = [scb.sb("ypad%d" % i, [128, YW], BF16) for i in range(2)]
    dg = [scb.sb("dg%d" % i, [128, 31, 128], BF16) for i in range(2)]
    sg = [scb.sb("sg%d" % i, [128, 512], F32) for i in range(2)]
    yh = scb.sb("yh", [128, 16], F32)
    cv = [scb.sb("cv%d" % i, [128, NOWN], F32) for i in range(2)]
    cq = scb.sb("cq", [128, NOWN], F32)
    s1c = scb.sb("s1c", [128, NOWN], F32)
    s2c = scb.sb("s2c", [128, NOWN], F32)
    byp = [Buf(), Buf()]
    bdg = [Buf(), Buf()]
    bsg = [Buf(), Buf()]
    byh, bcq, bs1, bs2 = Buf(), Buf(), Buf(), Buf()
    bcv = [Buf(), Buf()]
    for i in range(2):
        K.op(P, lambda: nc.gpsimd.memset(ypad[i][:], 0.0), writes=[byp[i]])
    K.op(P, lambda: nc.gpsimd.memset(s1c[:], 0.0), writes=[bs1])
    K.op(P, lambda: nc.gpsimd.memset(s2c[:], 0.0), writes=[bs2])

    def ywin(yp, t, k):
        if t < 2:
            return yp[:, t * 572:(t + 1) * 572].rearrange("p (s w) -> p s w", w=286)[:, :, k:k + 256]
        return yp[:, 1144 + k:1144 + k + 512]

    def conv_chunk(j):
        yp = ypad[j % 2]
        for t in range(3):
            if t < 2:
                o = banks[4 + t][:].rearrange("p (s w) -> p s w", w=256)
            else:
                o = banks[4 + t][:]
            mm(K, o, [(dg[j % 2][:, k, :], ywin(yp, t, k)) for k in range(31)],
               reads=[bdg[j % 2], byp[j % 2]], writes=[bB[4 + t]])
            cs = slice(t * 512, (t + 1) * 512)
            K.op(A, lambda: nc.scalar.activation(cv[j % 2][:, cs], banks[4 + t][:], AF.Identity,
                                                 bias=bdw[:, j:j + 1]),
                 reads=[bB[4 + t], bsm], writes=[bcv[j % 2]])
        K.dma(S, conv_d[j * 128:(j + 1) * 128, :], cv[j % 2][:], reads=[bcv[j % 2]])
        K.op(V, lambda: nc.vector.tensor_tensor(out=s1c[:], in0=s1c[:], in1=cv[j % 2][:], op=ALU.add),
             reads=[bcv[j % 2], bs1], writes=[bs1])
        K.op(A, lambda: nc.scalar.activation(cq[:], cv[j % 2][:], AF.Square), reads=[bcv[j % 2]], writes=[bcq])
        K.op(V, lambda: nc.vector.tensor_tensor(out=s2c[:], in0=s2c[:], in1=cq[:], op=ALU.add),
             reads=[bcq, bs2], writes=[bs2])

    for jp in range(8):
        ia = load_w(1344 + 256 * jp, 256)
        ig = load_w(3392 + 256 * jp, 256)
        for jj in range(2):
            j = jp * 2 + jj
            yp = ypad[j % 2]
            K.op(V, lambda: nc.vector.tensor_tensor(
                out=dg[j % 2][:], in0=ident_b[:].unsqueeze(1).to_broadcast([128, 31, 128]),
                in1=wdw[:, j, :].unsqueeze(2).to_broadcast([128, 31, 128]), op=ALU.mult),
                reads=[bconst, bsm], writes=[bdg[j % 2]])
            for t in range(4):
                if t < 3:
                    cs = slice(t * 512, (t + 1) * 512)
                    w_ = 512
                else:
                    cs = slice(NOWN, NOWN + 16)
                    w_ = 16
                pa = next_bank()
                pg = next_bank()
                mm(K, banks[pa][:, 0:w_], [(wt[ia][:, kc, jj * 128:(jj + 1) * 128], hT[:, kc, cs]) for kc in range(32)],
                   reads=[bwt[ia], bh], writes=[bB[pa]])
                mm(K, banks[pg][:, 0:w_], [(wt[ig][:, kc, jj * 128:(jj + 1) * 128], hT[:, kc, cs]) for kc in range(32)],
                   reads=[bwt[ig], bh], writes=[bB[pg]])
                s_ = sg[t % 2]
                K.op(A, lambda: nc.scalar.activation(s_[:, 0:w_], banks[pg][:, 0:w_], AF.Sigmoid),
                     reads=[bB[pg]], writes=[bsg[t % 2]])
                if t < 2:
                    o = yp[:, t * 572:(t + 1) * 572].rearrange("p (s w) -> p s w", w=286)[:, :, 15:271]
                    K.op(V, lambda: nc.vector.tensor_tensor(
                        out=o, in0=banks[pa][:].rearrange("p (s w) -> p s w", w=256),
                        in1=s_[:].rearrange("p (s w) -> p s w", w=256), op=ALU.mult),
                        reads=[bB[pa], bsg[t % 2]], writes=[byp[j % 2]])
                elif t == 2:
                    K.op(V, lambda: nc.vector.tensor_tensor(out=yp[:, 1159:1159 + 512], in0=banks[pa][:], in1=s_[:],
                                                            op=ALU.mult),
                         reads=[bB[pa], bsg[t % 2]], writes=[byp[j % 2]])
                else:
                    K.op(V, lambda: nc.vector.tensor_tensor(out=yh[:], in0=banks[pa][:, 0:16], in1=s_[:, 0:16],
                                                            op=ALU.mult),
                         reads=[bB[pa], bsg[t % 2]], writes=[byh])
                    K.op(V, lambda: nc.vector.tensor_scalar(out=yp[:, 1144:1159], in0=yh[:, 0:15],
                                                            scalar1=hmask[:, 0:1], scalar2=None, op0=ALU.mult),
                         reads=[byh, bsm], writes=[byp[j % 2]])
                    K.op(V, lambda: nc.vector.tensor_scalar(out=yp[:, 1671:1686], in0=yh[:, 0:15],
                                                            scalar1=hmask[:, 1:2], scalar2=None, op0=ALU.mult),
                         reads=[byh, bsm], writes=[byp[j % 2]])
            if j > 0:
                conv_chunk(j - 1)
    conv_chunk(15)
    for t in range(3):
        cs = slice(t * 512, (t + 1) * 512)
        mm(K, banks[0][:], [(ones_f[:], s1c[:, cs])], reads=[bs1, bconst], writes=[bB[0]])
        mm(K, banks[1][:], [(ones_f[:], s2c[:, cs])], reads=[bs2, bconst], writes=[bB[1]])
        K.op(V, lambda: nc.vector.tensor_scalar(out=s1c[:, cs], in0=banks[0][:], scalar1=1.0 / 2048, scalar2=None,
                                                op0=ALU.mult), reads=[bB[0]], writes=[bs1])
        K.op(V, lambda: nc.vector.tensor_tensor(out=cq[:, cs], in0=s1c[:, cs], in1=s1c[:, cs], op=ALU.mult),
             reads=[bs1], writes=[bcq])
        K.op(V, lambda: nc.vector.scalar_tensor_tensor(out=s2c[:, cs], in0=banks[1][:], scalar=1.0 / 2048,
                                                       in1=cq[:, cs], op0=ALU.mult, op1=ALU.subtract),
             reads=[bB[1], bcq], writes=[bs2])
    K.op(A, lambda: nc.scalar.activation(s2c[:], s2c[:], AF.Sqrt, bias=epsc[:, 0:1], scale=1.0),
         reads=[bs2, bconst], writes=[bs2])
    K.op(V, lambda: nc.vector.reciprocal(out=s2c[:], in_=s2c[:]), reads=[bs2], writes=[bs2])
    K.dma(S, cstat_d[0], s1c[:], reads=[bs1])
    K.dma(S, cstat_d[1], s2c[:], reads=[bs2])
    K.barrier()
    scb.close()
    sc1.close()
    if stop_after <= 2:
        K.finish()
        return nc, ins_used

    w_uq = din("w_uq", [768, 3072])
    w_ukv = din("w_ukv", [512, 4096])
    cache_ckvT = din("cache_ckvT", [128, 4, 256])
    cache_kpeT = din("cache_kpeT", [64, 256])
    cosT = din("cosT", [64, 1024])
    sinT = din("sinT", [64, 1024])
    rmatT = din("rmatT", [64, 64])
    attnT_d = dscr("attnT_d", [2048, NOWN], BF16)
    wuqv = w_uq.rearrange("(kc p) n -> p kc n", p=128)
    wukvv = w_ukv.rearrange("(kc p) n -> p kc n", p=128)
    sc2 = Scope(K)
    ckv = sc2.sb("ckv", [128, 4, NKEY], BF16)
    kpe = sc2.sb("kpeb", [64, NKEY], BF16)
    qc = sc2.sb("qc", [128, 6, NOWN], BF16)
    rq = sc2.sb("rq", [128, NOWN], F32)
    kraw = sc2.sb("kraw2", [64, NALL], F32)
    cos = sc2.sb("cos", [64, 1024], F32)
    sin = sc2.sb("sin", [64, 1024], F32)
    rm = sc2.sb("rm", [64, 64], F32)
    rt1 = sc2.sb("rt1", [64, 512], F32)
    rt2 = sc2.sb("rt2", [64, 512], F32)
    qpf = sc2.sb("qpf", [64, 512], F32)
    bckv, bkpe, bqc, brq, bkraw, btab, brt1, brt2, bqpf = [Buf() for _ in range(9)]
    K.dma(S, ckv[:, :, 0:NALL], ckvT_d.rearrange("(kc p) n -> p kc n", p=128), writes=[bckv])
    K.dma(P, ckv[:, :, NALL:NKEY], cache_ckvT, writes=[bckv])
    K.dma(S, kraw[:], kpe_d, writes=[bkraw])
    K.dma(P, kpe[:, NALL:NKEY], cache_kpeT, writes=[bkpe])
    K.dma(S, qc[:], qcT_d.rearrange("(kc p) n -> p kc n", p=128), writes=[bqc])
    K.dma(S, rq[:], rstdq_d, writes=[brq])
    K.dma(S, cos[:], cosT, writes=[btab])
    K.dma(S, sin[:], sinT, writes=[btab])
    K.dma(S, rm[:], rmatT, writes=[btab])
    K.op(A, lambda: nc.scalar.copy(kpe[:, 0:NP_], kraw[:, 0:NP_]), reads=[bkraw], writes=[bkpe])

    def rope(dst, bdst, src, bsrc, tab0):
        pb = next_bank(0, 6)
        mm(K, banks[pb][0:64, :], [(rm[:, :], src)], reads=[btab, bsrc], writes=[bB[pb]])
        K.op(V, lambda: nc.vector.tensor_tensor(out=rt1[:], in0=src, in1=cos[:, tab0:tab0 + 512], op=ALU.mult),
             reads=[bsrc, btab], writes=[brt1])
        K.op(V, lambda: nc.vector.tensor_tensor(out=rt2[:], in0=banks[pb][0:64, :], in1=sin[:, tab0:tab0 + 512],
                                                op=ALU.mult), reads=[bB[pb], btab], writes=[brt2])
        K.op(V, lambda: nc.vector.tensor_tensor(out=dst, in0=rt1[:], in1=rt2[:], op=ALU.add),
             reads=[brt1, brt2], writes=[bdst])

    for t in range(2):
        rope(kpe[:, NP_ + t * 512:NP_ + (t + 1) * 512], bkpe, kraw[:, NP_ + t * 512:NP_ + (t + 1) * 512], bkraw, t * 512)

    wq = [sc2.sb("wq%d" % i, [128, 6, 192], BF16) for i in range(2)]
    wkv = [sc2.sb("wkv%d" % i, [128, 4, 256], BF16) for i in range(2)]
    qn = [sc2.sb("qn%d" % i, [128, NOWN], BF16) for i in range(2)]
    qp = [sc2.sb("qp%d" % i, [64, NOWN], BF16) for i in range(2)]
    kn = [sc2.sb("kn%d" % i, [128, NKEY], BF16) for i in range(2)]
    vh = [sc2.sb("vh%d" % i, [128, 18, 128], BF16) for i in range(2)]
    ast = [sc2.sb("ast%d" % i, [128, NOWN], BF16) for i in range(2)]
    p32 = [sc2.sb("p32%d" % i, [128, 1280], F32) for i in range(2)]
    pn = [sc2.sb("pn%d" % i, [128, 1280], BF16) for i in range(2)]
    pt = [sc2.sb("pt%d" % i, [128, 10, 128], BF16) for i in range(2)]
    st = [sc2.sb("st%d" % i, [128, 8], F32) for i in range(2)]
    bwq, bwkv, bqn, bqp, bkn, bvh, bast, bp32, bpn, bpt, bst, bpv = [[Buf(), Buf()] for _ in range(12)]
    tb = [banks[6][:].bitcast(BF16), banks[7][:].bitcast(BF16)]

    qblocks = []
    for s_ in range(4):
        for hh in range(2):
            qblocks.append((s_ * 256 + hh * 128, s_ * 256, 256, 2 * s_))
    for i_ in range(4):
        qblocks.append((NP_ + i_ * 128, NP_, 1280, 8))

    import os
    _NH = int(os.environ.get('P2_HEADS', 16)); _NQ = int(os.environ.get('P2_QB', 12)); _STG = int(os.environ.get('P2_STAGE', 4))
    for h in range(_NH):
        hp = h % 2
        K.dma(P, wq[hp][:], wuqv[:, :, h * 192:(h + 1) * 192], writes=[bwq[hp]])
        K.dma(P, wkv[hp][:], wukvv[:, :, h * 256:(h + 1) * 256], writes=[bwkv[hp]])
        for t in range(3):
            cs = slice(t * 512, (t + 1) * 512)
            pb = next_bank(0, 6)
            mm(K, banks[pb][:], [(wq[hp][:, kc, 0:128], qc[:, kc, cs]) for kc in range(6)],
               reads=[bwq[hp], bqc], writes=[bB[pb]])
            K.op(V, lambda: nc.vector.tensor_tensor(out=qn[hp][:, cs], in0=banks[pb][:], in1=rq[:, cs], op=ALU.mult),
                 reads=[bB[pb], brq], writes=[bqn[hp]])
            pb = next_bank(0, 6)
            mm(K, banks[pb][0:64, :], [(wq[hp][:, kc, 128:192], qc[:, kc, cs]) for kc in range(6)],
               reads=[bwq[hp], bqc], writes=[bB[pb]])
            if t < 2:
                K.op(V, lambda: nc.vector.tensor_tensor(out=qp[hp][:, cs], in0=banks[pb][0:64, :], in1=rq[0:64, cs],
                                                        op=ALU.mult), reads=[bB[pb], brq], writes=[bqp[hp]])
            else:
                K.op(V, lambda: nc.vector.tensor_tensor(out=qpf[:], in0=banks[pb][0:64, :], in1=rq[0:64, cs],
                                                        op=ALU.mult), reads=[bB[pb], brq], writes=[bqpf])
                rope(qp[hp][:, cs], bqp[hp], qpf[:], bqpf, 0)
        for t in range(5):
            w_ = 512 if t < 4 else 256
            cs = slice(t * 512, t * 512 + w_)
            pb = next_bank(0, 6)
            mm(K, banks[pb][:, 0:w_], [(wkv[hp][:, kc, 0:128], ckv[:, kc, cs]) for kc in range(4)],
               reads=[bwkv[hp], bckv], writes=[bB[pb]])
            K.op(A, lambda: nc.scalar.copy(kn[hp][:, cs], banks[pb][:, 0:w_]), reads=[bB[pb]], writes=[bkn[hp]])
        for g in range(5):
            nb_ = 4 if g < 4 else 2
            pb = next_bank(0, 6)
            for kk in range(nb_):
                kb = g * 4 + kk
                mm(K, banks[pb][:, kk * 128:(kk + 1) * 128],
                   [(ckv[:, kc, kb * 128:(kb + 1) * 128], wkv[hp][:, kc, 128:256]) for kc in range(4)],
                   reads=[bwkv[hp], bckv], writes=[bB[pb]])
            K.op(A if g % 2 else V,
                 (lambda: nc.scalar.copy(vh[hp][:, g * 4:g * 4 + nb_, :],
                                         banks[pb][:, 0:nb_ * 128].rearrange("p (a b) -> p a b", b=128))) if g % 2 else
                 (lambda: nc.vector.tensor_copy(out=vh[hp][:, g * 4:g * 4 + nb_, :],
                                                in_=banks[pb][:, 0:nb_ * 128].rearrange("p (a b) -> p a b", b=128))),
                 reads=[bB[pb]], writes=[bvh[hp]])

        def emit_S(qi):
            q0, k0, nk, vb0 = qblocks[qi]
            base = 3 * (qi % 2)
            for kt in range((nk + 511) // 512):
                w_ = min(512, nk - kt * 512)
                ks = slice(k0 + kt * 512, k0 + kt * 512 + w_)
                mm(K, banks[base + kt][:, 0:w_],
                   [(qn[hp][:, q0:q0 + 128], kn[hp][:, ks]), (qp[hp][:, q0:q0 + 128], kpe[:, ks])],
                   reads=[bqn[hp], bqp[hp], bkn[hp], bkpe], writes=[bB[base + kt]])

        def emit_softmax(qi):
            q0, k0, nk, vb0 = qblocks[qi]
            base = 3 * (qi % 2)
            e = qi % 2
            nt = (nk + 511) // 512
            for kt in range(nt):
                w_ = min(512, nk - kt * 512)
                K.op(V, lambda: nc.vector.reduce_max(out=st[e][:, kt:kt + 1], in_=banks[base + kt][:, 0:w_], axis=AX.X),
                     reads=[bB[base + kt]], writes=[bst[e]])
            if nt > 1:
                K.op(V, lambda: nc.vector.reduce_max(out=st[e][:, 3:4], in_=st[e][:, 0:nt], axis=AX.X),
                     reads=[bst[e]], writes=[bst[e]])
                mcol = 3
            else:
                mcol = 0
            K.op(V, lambda: nc.vector.tensor_scalar(out=st[e][:, 4:5], in0=st[e][:, mcol:mcol + 1], scalar1=-SCALE,
                                                    scalar2=None, op0=ALU.mult), reads=[bst[e]], writes=[bst[e]])
            for kt in range(nt):
                w_ = min(512, nk - kt * 512)
                K.op(A, lambda: nc.scalar.activation(p32[e][:, kt * 512:kt * 512 + w_], banks[base + kt][:, 0:w_],
                                                     AF.Exp, bias=st[e][:, 4:5], scale=SCALE),
                     reads=[bB[base + kt], bst[e]], writes=[bp32[e]])
            K.op(V, lambda: nc.vector.reduce_sum(out=st[e][:, 5:6], in_=p32[e][:, 0:nk], axis=AX.X),
                 reads=[bp32[e]], writes=[bst[e]])
            K.op(V, lambda: nc.vector.reciprocal(out=st[e][:, 6:7], in_=st[e][:, 5:6]), reads=[bst[e]], writes=[bst[e]])
            K.op(A, lambda: nc.scalar.activation(pn[e][:, 0:nk], p32[e][:, 0:nk], AF.Identity, scale=st[e][:, 6:7]),
                 reads=[bp32[e], bst[e]], writes=[bpn[e]])

        def emit_PV(qi):
            q0, k0, nk, vb0 = qblocks[qi]
            base = 3 * (qi % 2)
            e = qi % 2
            nkb = nk // 128
            for kb in range(nkb):
                tbi = kb // 8
                K.op(K.pe, lambda: nc.tensor.transpose(tb[tbi][:, (kb % 8) * 128:(kb % 8 + 1) * 128],
                                                       pn[e][:, kb * 128:(kb + 1) * 128], ident_b[:]),
                     reads=[bpn[e], bconst], writes=[bB[6 + tbi]])
            n0 = min(nkb, 8)
            K.op(V, lambda: nc.vector.tensor_copy(out=pt[e][:, 0:n0, :],
                                                  in_=tb[0][:, 0:n0 * 128].rearrange("p (a b) -> p a b", b=128)),
                 reads=[bB[6]], writes=[bpt[e]])
            if nkb > 8:
                K.op(V, lambda: nc.vector.tensor_copy(out=pt[e][:, 8:nkb, :],
                                                      in_=tb[1][:, 0:(nkb - 8) * 128].rearrange("p (a b) -> p a b", b=128)),
                     reads=[bB[7]], writes=[bpt[e]])
            mm(K, banks[base + 2][:, 256:384], [(vh[hp][:, vb0 + kb, :], pt[e][:, kb, :]) for kb in range(nkb)],
               reads=[bvh[hp], bpt[e]], writes=[bB[base + 2]])
            K.op(A, lambda: nc.scalar.copy(ast[hp][:, q0:q0 + 128], banks[base + 2][:, 256:384]),
                 reads=[bB[base + 2]], writes=[bast[hp]])

        if _STG >= 2:
            emit_S(0)
        for qi in range(_NQ):
            if qi + 1 < _NQ and _STG >= 2:
                emit_S(qi + 1)
            if _STG >= 3:
                emit_softmax(qi)
            if _STG >= 4:
                emit_PV(qi)
        K.dma(S, attnT_d[h * 128:(h + 1) * 128, :], ast[hp][:], reads=[bast[hp]])
    K.barrier()
    sc2.close()
    if stop_after <= 3:
        K.finish()
        return nc, ins_used

    def ln_stats(s1, bs1, s2, bs2, tmp, btmp, nfeat, ncols):
        for t in range(0, ncols, 512):
            cs = slice(t, t + 512)
            mm(K, banks[6][:], [(ones_f[:], s1[:, cs])], reads=[bs1, bconst], writes=[bB[6]])
            mm(K, banks[7][:], [(ones_f[:], s2[:, cs])], reads=[bs2, bconst], writes=[bB[7]])
            K.op(V, lambda: nc.vector.tensor_scalar(out=s1[:, cs], in0=banks[6][:], scalar1=1.0 / nfeat, scalar2=None,
                                                    op0=ALU.mult), reads=[bB[6]], writes=[bs1])
            K.op(V, lambda: nc.vector.tensor_tensor(out=tmp[:, cs], in0=s1[:, cs], in1=s1[:, cs], op=ALU.mult),
                 reads=[bs1], writes=[btmp])
            K.op(V, lambda: nc.vector.scalar_tensor_tensor(out=s2[:, cs], in0=banks[7][:], scalar=1.0 / nfeat,
                                                           in1=tmp[:, cs], op0=ALU.mult, op1=ALU.subtract),
                 reads=[bB[7], btmp], writes=[bs2])
        K.op(A, lambda: nc.scalar.activation(s2[:, 0:ncols], s2[:, 0:ncols], AF.Sqrt, bias=epsc[:, 0:1], scale=1.0),
             reads=[bs2, bconst], writes=[bs2])
        K.op(V, lambda: nc.vector.reciprocal(out=s2[:, 0:ncols], in_=s2[:, 0:ncols]), reads=[bs2], writes=[bs2])

    w_out = din("w_out", [D, D])
    g_cnT = din("g_cnT", [128, 16])
    b_cnT = din("b_cnT", [128, 16])
    ln1_gT = din("ln1_gT", [128, 32])
    ln1_bT = din("ln1_bT", [128, 32])
    zT_d = dscr("zT_d", [D, NOWN], F32)
    stat1_d = dscr("stat1_d", [2, 128, NOWN], F32)
    h2T_d = dscr("h2T_d", [D, NOWN], BF16)
    wov = w_out.rearrange("(kc p) n -> p kc n", p=128)
    sc3 = Scope(K)
    mix = sc3.sb("mix", [128, 32, NOWN], BF16)
    gcn = sc3.sb("gcn", [128, 16], F32)
    bcn = sc3.sb("bcn", [128, 16], F32)
    l1g = sc3.sb("l1g", [128, 32], F32)
    l1b = sc3.sb("l1b", [128, 32], F32)
    a1 = sc3.sb("a1", [128, 32, 2], F32)
    b1 = sc3.sb("b1", [128, 32, 2], F32)
    bmix, bsm3, bab = Buf(), Buf(), Buf()
    for t_, s_ in ((gcn, g_cnT), (bcn, b_cnT), (l1g, ln1_gT), (l1b, ln1_bT)):
        K.dma(S, t_[:], s_, writes=[bsm3])
    K.dma(S, mix[:, 0:16, :], attnT_d.rearrange("(kc p) n -> p kc n", p=128), writes=[bmix])
    K.op(V, lambda: nc.vector.tensor_tensor(out=a1[:], in0=opsc[:, 1, :, :],
                                            in1=l1g[:].unsqueeze(2).to_broadcast([128, 32, 2]), op=ALU.mult),
         reads=[bmod, bsm3], writes=[bab])
    K.op(V, lambda: nc.vector.tensor_tensor(out=b1[:], in0=opsc[:, 1, :, :],
                                            in1=l1b[:].unsqueeze(2).to_broadcast([128, 32, 2]), op=ALU.mult),
         reads=[bmod, bsm3], writes=[bab])
    K.op(V, lambda: nc.vector.tensor_tensor(out=b1[:], in0=b1[:], in1=mod[:, 96:128, :], op=ALU.add),
         reads=[bmod, bab], writes=[bab])
    s3a = Scope(K)
    cm = s3a.sb("cm", [128, NOWN], F32)
    cr = s3a.sb("cr", [128, NOWN], F32)
    cvl = [s3a.sb("cvl%d" % i, [128, NOWN], F32) for i in range(2)]
    cvt = [s3a.sb("cvt%d" % i, [128, NOWN], F32) for i in range(2)]
    bcs = Buf()
    bcvl = [Buf(), Buf()]
    bcvt = [Buf(), Buf()]
    K.dma(S, cm[:], cstat_d[0], writes=[bcs])
    K.dma(S, cr[:], cstat_d[1], writes=[bcs])
    for j in range(16):
        i = j % 2
        K.dma(S, cvl[i][:], conv_d[j * 128:(j + 1) * 128, :], writes=[bcvl[i]])
        K.op(V, lambda: nc.vector.tensor_tensor(out=cvt[i][:], in0=cvl[i][:], in1=cm[:], op=ALU.subtract),
             reads=[bcvl[i], bcs], writes=[bcvt[i]])
        K.op(P, lambda: nc.gpsimd.tensor_tensor(out=cvt[i][:], in0=cvt[i][:], in1=cr[:], op=ALU.mult),
             reads=[bcvt[i], bcs], writes=[bcvt[i]])
        K.op(A, lambda: nc.scalar.activation(mix[:, 16 + j, :], cvt[i][:], AF.Silu, bias=bcn[:, j:j + 1],
                                             scale=gcn[:, j:j + 1]), reads=[bcvt[i], bsm3], writes=[bmix])
    K.barrier()
    s3a.close()
    s3b = Scope(K)
    wo = [s3b.sb("wo%d" % i, [128, 32, 256], BF16) for i in range(2)]
    xs3 = [s3b.sb("xs3%d" % i, [128, NOWN], F32) for i in range(2)]
    zt = [s3b.sb("zt%d" % i, [128, NOWN], F32) for i in range(2)]
    zz = [s3b.sb("zz%d" % i, [128, NOWN], F32) for i in range(2)]
    z1 = s3b.sb("z1", [128, NOWN], F32)
    z2 = s3b.sb("z2", [128, NOWN], F32)
    zq = s3b.sb("zq", [128, NOWN], F32)
    bwo = [Buf() for _ in range(2)]
    bxs3, bzt, bzz = [[Buf(), Buf()] for _ in range(3)]
    bz1, bz2, bzq = Buf(), Buf(), Buf()
    K.op(P, lambda: nc.gpsimd.memset(z1[:], 0.0), writes=[bz1])
    K.op(P, lambda: nc.gpsimd.memset(z2[:], 0.0), writes=[bz2])
    for tl in range(16):
        i = tl % 2
        K.dma(P, wo[i][:], wov[:, :, tl * 256:(tl + 1) * 256], writes=[bwo[i]])
        for ob in range(2):
            db = tl * 2 + ob
            e = db % 2
            K.dma(S, xs3[e][:], xv[:, db, 0:NOWN], writes=[bxs3[e]])
            for t in range(3):
                cs = slice(t * 512, (t + 1) * 512)
                j = 0 if t < 2 else 1
                pb = next_bank(0, 6)
                mm(K, banks[pb][:], [(wo[i][:, kc, ob * 128:(ob + 1) * 128], mix[:, kc, cs]) for kc in range(32)],
                   reads=[bwo[i], bmix], writes=[bB[pb]])
                K.op(A, lambda: nc.scalar.activation(zt[e][:, cs], banks[pb][:], AF.Identity, scale=MOD(2, db, j)),
                     reads=[bB[pb], bmod], writes=[bzt[e]])
                K.op(V, lambda: nc.vector.scalar_tensor_tensor(out=zz[e][:, cs], in0=xs3[e][:, cs], scalar=ALPHA,
                                                               in1=zt[e][:, cs], op0=ALU.mult, op1=ALU.add),
                     reads=[bxs3[e], bzt[e]], writes=[bzz[e]])
            K.dma(S, zT_d[db * 128:(db + 1) * 128, :], zz[e][:], reads=[bzz[e]])
            K.op(P, lambda: nc.gpsimd.tensor_tensor(out=z1[:], in0=z1[:], in1=zz[e][:], op=ALU.add),
                 reads=[bzz[e], bz1], writes=[bz1])
            K.op(A, lambda: nc.scalar.activation(zq[:], zz[e][:], AF.Square), reads=[bzz[e]], writes=[bzq])
            K.op(P, lambda: nc.gpsimd.tensor_tensor(out=z2[:], in0=z2[:], in1=zq[:], op=ALU.add),
                 reads=[bzq, bz2], writes=[bz2])
    ln_stats(z1, bz1, z2, bz2, zq, bzq, 4096.0, NOWN)
    K.dma(S, stat1_d[0], z1[:], reads=[bz1])
    K.dma(S, stat1_d[1], z2[:], reads=[bz2])
    h2s = [s3b.sb("h2s%d" % i, [128, NOWN], BF16) for i in range(2)]
    bh2s = [Buf(), Buf()]
    for db in range(32):
        e = db % 2
        K.dma(S, xs3[e][:], zT_d[db * 128:(db + 1) * 128, :], writes=[bxs3[e]])
        K.op(V, lambda: nc.vector.tensor_tensor(out=zt[e][:], in0=xs3[e][:], in1=z1[:], op=ALU.subtract),
             reads=[bxs3[e], bz1], writes=[bzt[e]])
        K.op(P, lambda: nc.gpsimd.tensor_tensor(out=zt[e][:], in0=zt[e][:], in1=z2[:], op=ALU.mult),
             reads=[bzt[e], bz2], writes=[bzt[e]])
        for (lo, hi, j) in ((0, NP_, 0), (NP_, NOWN, 1)):
            K.op(A, lambda: nc.scalar.activation(h2s[e][:, lo:hi], zt[e][:, lo:hi], AF.Identity,
                                                 bias=b1[:, db, j:j + 1], scale=a1[:, db, j:j + 1]),
                 reads=[bzt[e], bab], writes=[bh2s[e]])
        K.dma(S, h2T_d[db * 128:(db + 1) * 128, :], h2s[e][:], reads=[bh2s[e]])
    K.barrier()
    s3b.close()
    sc3.close()
    if stop_after <= 4:
        K.finish()
        return nc, ins_used

    w_pq = din("w_pq", [D, D])
    skT = din("skT", [128, 32, 128])
    s_d = dscr("s_d", [8, NOWN, 256], F32)
    G_d = dscr("G_d", [128, 128, NOWN], BF16)
    wpv = w_pq.rearrange("(kc p) n -> p kc n", p=128)
    sc4 = Scope(K)
    h2 = sc4.sb("h2", [128, 32, NOWN], BF16)
    sk = sc4.sb("sk", [128, 32, 128], BF16)
    wp = [sc4.sb("wp%d" % i, [128, 32, 256], BF16) for i in range(3)]
    qhp = [sc4.sb("qhp%d" % i, [128, 2, NOWN], BF16) for i in range(2)]
    sst = [sc4.sb("sst%d" % i, [128, 12, 128], F32) for i in range(2)]
    bh2, bsk = Buf(), Buf()
    bwp = [Buf() for _ in range(3)]
    bqhp, bsst = [[Buf(), Buf()] for _ in range(2)]
    h2v = h2T_d.rearrange("(kc p) n -> p kc n", p=128)
    for q4 in range(4):
        K.dma(S, h2[:, q4 * 8:(q4 + 1) * 8, :], h2v[:, q4 * 8:(q4 + 1) * 8, :], writes=[bh2])
    K.dma(P, sk[:], skT, writes=[bsk])
    s_dv = s_d.rearrange("h (nb p) (t k) -> h t p nb k", p=128, k=128)
    cpy = [0]

    def evac(out, in_, reads, writes):
        cpy[0] += 1
        if cpy[0] % 2:
            return K.op(A, lambda: nc.scalar.copy(out, in_), reads=reads, writes=writes)
        return K.op(V, lambda: nc.vector.tensor_copy(out=out, in_=in_), reads=reads, writes=writes)

    for hp_ in range(16):
        i = hp_ % 3
        e = hp_ % 2
        K.dma(P, wp[i][:], wpv[:, :, hp_ * 256:(hp_ + 1) * 256], writes=[bwp[i]])
        for half in range(2):
            for t in range(3):
                cs = slice(t * 512, (t + 1) * 512)
                pb = next_bank(0, 4)
                mm(K, banks[pb][:], [(wp[i][:, kc, half * 128:(half + 1) * 128], h2[:, kc, cs]) for kc in range(32)],
                   reads=[bwp[i], bh2], writes=[bB[pb]])
                evac(qhp[e][:, half, cs], banks[pb][:], [bB[pb]], [bqhp[e]])
        for g in range(3):
            pb = 4 + (hp_ * 3 + g) % 4
            for kk in range(4):
                nb = g * 4 + kk
                mm(K, banks[pb][:, kk * 128:(kk + 1) * 128],
                   [(qhp[e][:, half, nb * 128:(nb + 1) * 128], sk[:, hp_ * 2 + half, :]) for half in range(2)],
                   reads=[bqhp[e], bsk], writes=[bB[pb]])
            evac(sst[e][:, g * 4:(g + 1) * 4, :], banks[pb][:].rearrange("p (a b) -> p a b", b=128), [bB[pb]], [bsst[e]])
        K.dma(S, s_dv[hp_ // 2, hp_ % 2], sst[e][:], reads=[bsst[e]])
    K.barrier()
    sc4.close()
    if stop_after <= 5:
        K.finish()
        return nc, ins_used

    scr = Scope(K)
    stm = [scr.sb("stm%d" % i, [128, 2048], F32) for i in range(2)]
    wrk = scr.sb("wrk", [128, 2048], F32)
    tp = scr.sb("tp", [128, 16, 16], F32)
    cand = scr.sb("cand", [128, 8, 256], F32)
    ctop = scr.sb("ctop", [128, 8, 16], F32)
    ce = scr.sb("ce", [128, 8, 16], F32)
    sm = scr.sb("sm", [128, 64], F32)
    At = scr.sb("At", [128, 3, 128], F32)
    AT = [scr.sb("AT%d" % i, [128, 3, 128], F32) for i in range(2)]
    srep = [scr.sb("srep%d" % i, [128, 16, 256], F32) for i in range(3)]
    zr = [scr.sb("zr%d" % i, [128, 16, 128], F32) for i in range(2)]
    er = [scr.sb("er%d" % i, [128, 16, 128], BF16) for i in range(2)]
    mk = [scr.sb("mk%d" % i, [128, 16, 128], BF16) for i in range(2)]
    Rb = [scr.sb("Rb%d" % i, [128, 16, 128], BF16) for i in range(2)]
    P1b = [scr.sb("P1b%d" % i, [128, 16, 128], BF16) for i in range(2)]
    gst = [scr.sb("gst%d" % i, [128, 128, 128], BF16) for i in range(2)]
    bstm, bAT, bsrep, bzr, bzc, ber, bmk, bRb, bP1b, bgst = [[Buf(), Buf(), Buf()] for _ in range(10)]
    bwrk, btp, bcand, bctop, bce, bsmm, bAt = [Buf() for _ in range(7)]
    tpv = tp[:].rearrange("p (h t) a -> p h t a", t=2)
    G_dv = G_d.rearrange("i j n -> j i n")

    bc816 = lambda ap: ap.unsqueeze(2).to_broadcast([128, 8, 16])
    btpg = [Buf() for _ in range(16)]
    bwkg = [Buf() for _ in range(16)]
    bctg = [Buf() for _ in range(8)]
    bcdg = [Buf() for _ in range(8)]

    def top16_batch(dsts, srcs, width, rds, wrs, wks, bwk):
        n = len(dsts)
        for i in range(n):
            K.op(V, lambda: nc.vector.max(out=dsts[i][:, 0:8], in_=srcs[i]), reads=rds[i], writes=[wrs[i]])
        for i in range(n):
            K.op(V, lambda: nc.vector.match_replace(out=wks[i], in_to_replace=dsts[i][:, 0:8], in_values=srcs[i],
                                                    imm_value=-1e30), reads=rds[i] + [wrs[i]], writes=[bwk[i]])
        for i in range(n):
            K.op(V, lambda: nc.vector.max(out=dsts[i][:, 8:16], in_=wks[i]), reads=[bwk[i]], writes=[wrs[i]])

    def topk_stage(nb):
        e = nb % 2
        K.dma(S, stm[e][:].rearrange("p (h c) -> p h c", c=256), s_d[:, nb * 128:(nb + 1) * 128, :].rearrange("h n c -> n h c"), writes=[bstm[e]])
        for q4 in range(4):
            hs = range(q4 * 4, q4 * 4 + 4)
            top16_batch([tp[:, i, :] for i in hs], [stm[e][:, i * 128:(i + 1) * 128] for i in hs], 128,
                        [[bstm[e]] for i in hs], [btpg[i] for i in hs],
                        [wrk[:, i * 128:(i + 1) * 128] for i in hs], [bwkg[i] for i in hs])
            yield
        for h in range(8):
            K.op(V, lambda: nc.vector.tensor_tensor(
                out=cand[:, h, :].rearrange("p (a b) -> p a b", b=16),
                in0=tpv[:, h, 0, :].unsqueeze(2).to_broadcast([128, 16, 16]),
                in1=tpv[:, h, 1, :].unsqueeze(1).to_broadcast([128, 16, 16]), op=ALU.add),
                reads=[btpg[2 * h], btpg[2 * h + 1]], writes=[bcdg[h]])
        yield
        for q2 in range(2):
            hs = range(q2 * 4, q2 * 4 + 4)
            top16_batch([ctop[:, i, :] for i in hs], [cand[:, i, :] for i in hs], 256,
                        [[bcdg[i]] for i in hs], [bctg[i] for i in hs],
                        [wrk[:, i * 256:(i + 1) * 256] for i in hs], [bwkg[2 * i] for i in hs])
            yield
        btp = btpg
        bctop = bctg
        K.op(V, lambda: nc.vector.tensor_reduce(out=sm[:, 0:8], in_=ctop[:], axis=AX.X, op=ALU.max),
             reads=bctop, writes=[bsmm])
        K.op(V, lambda: nc.vector.tensor_reduce(out=sm[:, 8:16], in_=ctop[:], axis=AX.X, op=ALU.min),
             reads=bctop, writes=[bsmm])
        K.op(V, lambda: nc.vector.tensor_tensor(out=ce[:], in0=ctop[:], in1=bc816(sm[:, 0:8]), op=ALU.subtract),
             reads=bctop + [bsmm], writes=[bce])
        K.op(A, lambda: nc.scalar.activation(ce[:], ce[:], AF.Exp), reads=[bce], writes=[bce])
        K.op(V, lambda: nc.vector.tensor_reduce(out=sm[:, 16:24], in_=ce[:], axis=AX.X, op=ALU.add),
             reads=[bce], writes=[bsmm])
        K.op(A, lambda: nc.scalar.activation(sm[:, 24:32], sm[:, 16:24], AF.Ln), reads=[bsmm], writes=[bsmm])
        K.op(V, lambda: nc.vector.tensor_tensor(out=sm[:, 32:40], in0=sm[:, 0:8], in1=sm[:, 24:32], op=ALU.add),
             reads=[bsmm], writes=[bsmm])
        K.op(V, lambda: nc.vector.tensor_copy(out=At[:, 0, :].rearrange("p (a h) -> p h a", h=8), in_=bc816(sm[:, 8:16])),
             reads=[bsmm], writes=[bAt])
        K.op(V, lambda: nc.vector.tensor_tensor(out=At[:, 1, :].rearrange("p (a h) -> p h a", h=8), in0=tpv[:, :, 0, :],
                                                in1=bc816(sm[:, 32:40]), op=ALU.subtract),
             reads=btp + [bsmm], writes=[bAt])
        K.op(V, lambda: nc.vector.tensor_copy(out=At[:, 2, :].rearrange("p (a h) -> p h a", h=8), in_=tpv[:, :, 0, :]),
             reads=btp, writes=[bAt])
        pbt = nb % 2
        for q3 in range(3):
            K.op(K.pe, lambda: nc.tensor.transpose(banks[pbt][:, q3 * 128:(q3 + 1) * 128], At[:, q3, :], ident_f[:]),
                 reads=[bAt, bconst], writes=[bB[pbt]])
        K.op(V, lambda: nc.vector.tensor_copy(out=AT[e][:], in_=banks[pbt][:, 0:384].rearrange("p (a b) -> p a b", b=128)),
             reads=[bB[pbt]], writes=[bAT[e]])
        yield

    def bcn(ap):
        return ap.unsqueeze(2).to_broadcast([128, 16, 128])

    def sub_block(nb, sb):
        e = nb % 2
        f = (nb * 8 + sb) % 2
        f3 = (nb * 8 + sb) % 3
        ns = slice(sb * 16, sb * 16 + 16)
        row0 = nb * 128 + sb * 16
        src = bass.AP(s_d.tensor, row0 * 256, [[0, 16], [NOWN * 256, 8], [1, 4096]])
        K.dma(S, srep[f3][:].rearrange("p a b -> p (a b)"), src, writes=[bsrep[f3]])
        K.op(V, lambda: nc.vector.tensor_tensor(out=zr[f][:], in0=srep[f3][:, :, 128:256], in1=bcn(AT[e][:, 2, ns]),
                                                op=ALU.add), reads=[bsrep[f3], bAT[e]], writes=[bzr[f]])
        for tk in range(16):
            n_ = sb * 16 + tk
            K.op(A, lambda: nc.scalar.activation(er[f][:, tk, :], srep[f3][:, tk, 128:256], AF.Exp,
                                                 bias=AT[e][:, 1, n_:n_ + 1]),
                 reads=[bsrep[f3], bAT[e]], writes=[ber[f]])
        K.op(V, lambda: nc.vector.tensor_tensor(out=mk[f][:], in0=zr[f][:], in1=bcn(AT[e][:, 0, ns]), op=ALU.is_ge),
             reads=[bzr[f], bAT[e]], writes=[bmk[f]])
        K.op(V, lambda: nc.vector.tensor_tensor(out=P1b[f][:], in0=srep[f3][:, :, 0:128], in1=bcn(AT[e][:, 2, ns]),
                                                op=ALU.is_equal), reads=[bsrep[f3], bAT[e]], writes=[bP1b[f]])
        K.op(V, lambda: nc.vector.tensor_tensor(out=Rb[f][:], in0=mk[f][:], in1=er[f][:], op=ALU.mult),
             reads=[bmk[f], ber[f]], writes=[bRb[f]])
        for q4 in range(4):
            pb = 2 + (sb * 4 + q4) % 6
            for tk in range(4):
                t16 = q4 * 4 + tk
                mm(K, banks[pb][:, tk * 128:(tk + 1) * 128], [(Rb[f][:, t16, :], P1b[f][:, t16, :])],
                   reads=[bRb[f], bP1b[f]], writes=[bB[pb]])
            n0 = sb * 16 + q4 * 4
            K.op(A, lambda: nc.scalar.copy(gst[e][:, :, n0:n0 + 4], banks[pb][:].rearrange("p (n i) -> p i n", n=4)),
                 reads=[bB[pb]], writes=[bgst[e]])

    for _ in topk_stage(0):
        pass
    for nb in range(12):
        nxt = topk_stage(nb + 1) if nb + 1 < 12 else iter(())
        for sb in range(8):
            sub_block(nb, sb)
            next(nxt, None)
        for _ in nxt:
            pass
        K.dma(S, G_dv[:, :, nb * 128:(nb + 1) * 128], gst[nb % 2][:], reads=[bgst[nb % 2]])
    K.barrier()
    scr.close()
    if stop_after <= 6:
        K.finish()
        return nc, ins_used

    peer_uT = din("peer_uT", [128, 128, 4096])
    peer_v = din("peer_v", [16384, D])
    ln2_gT = din("ln2_gT", [128, 32])
    ln2_bT = din("ln2_bT", [128, 32])
    o_yT = dout("o_yT", [D, NOWN])
    pvv = peer_v.rearrange("(g a p) d -> g p a d", a=4, p=128)
    sc5 = Scope(K)
    h2p = sc5.sb("h2p", [128, 32, 512], BF16)
    acc = sc5.sb("acc", [128, 32, 512], F32)
    l1g5 = sc5.sb("l1g5", [128, 32], F32)
    l1b5 = sc5.sb("l1b5", [128, 32], F32)
    l2g = sc5.sb("l2g", [128, 32], F32)
    l2b = sc5.sb("l2b", [128, 32], F32)
    bh2p, bsm5 = Buf(), Buf()
    bacc = [Buf() for _ in range(32)]
    for t_, s_ in ((l1g5, ln1_gT), (l1b5, ln1_bT), (l2g, ln2_gT), (l2b, ln2_bT)):
        K.dma(S, t_[:], s_, writes=[bsm5])
    _P5P = int(os.environ.get('P5_PASSES', 3)); _P5G = int(os.environ.get('P5_GROUPS', 32)); _P5E = int(os.environ.get('P5_EPI', 1))
    for pt_ in range(_P5P):
        c0 = pt_ * 512
        cj = 0 if pt_ < 2 else 1
        pcs = slice(c0, c0 + 512)
        K.dma(S, h2p[:], h2v[:, :, pcs], writes=[bh2p])
        for db in range(32):
            K.op(V, lambda: nc.vector.memset(acc[:, db, :], 0.0), writes=[bacc[db]])
        s5a = Scope(K)
        ut = [s5a.sb("ut%d" % i, [128, 32, 128], BF16) for i in range(3)]
        vt = [s5a.sb("vt%d" % i, [128, 4, D], BF16) for i in range(2)]
        gt = [s5a.sb("gt%d" % i, [128, 512], BF16) for i in range(3)]
        ga32 = [s5a.sb("ga32%d" % i, [128, 512], F32) for i in range(2)]
        gab = [s5a.sb("gab%d" % i, [128, 4, 512], BF16) for i in range(2)]
        but, bvt, bgt, bga32, bgab = [[Buf(), Buf(), Buf()] for _ in range(5)]

        def phaseB(g):
            gi = g % 2
            for db in range(32):
                pb = 2 + db % 6
                mm(K, banks[pb][:], [(vt[gi][:, a, db * 128:(db + 1) * 128], gab[gi][:, a, :]) for a in range(4)],
                   reads=[bvt[gi], bgab[gi]], writes=[bB[pb]])
                K.op(V, lambda: nc.vector.tensor_tensor(out=acc[:, db, :], in0=acc[:, db, :], in1=banks[pb][:], op=ALU.add),
                     reads=[bB[pb], bacc[db]], writes=[bacc[db]])

        for g in range(_P5G):
            gi = g % 2
            for a in range(4):
                eb = g * 4 + a
                ei = eb % 2
                u3 = eb % 3
                K.dma(P, ut[u3][:].rearrange("p a b -> p (a b)"), peer_uT[eb], writes=[but[u3]])
                K.dma(S, gt[u3][:], G_d[eb, :, pcs], writes=[bgt[u3]])
                mm(K, banks[ei][:], [(ut[u3][:, kc, :], h2p[:, kc, :]) for kc in range(32)],
                   reads=[but[u3], bh2p], writes=[bB[ei]])
                K.op(A, lambda: nc.scalar.activation(ga32[ei][:], banks[ei][:], AF.Gelu), reads=[bB[ei]], writes=[bga32[ei]])
                K.op(V, lambda: nc.vector.tensor_tensor(out=gab[gi][:, a, :], in0=ga32[ei][:], in1=gt[u3][:], op=ALU.mult),
                     reads=[bga32[ei], bgt[u3]], writes=[bgab[gi]])
            K.dma(P, vt[gi][:], pvv[g], writes=[bvt[gi]])
            if g > 0:
                phaseB(g - 1)
        phaseB(_P5G - 1)
        K.barrier()
        s5a.close()
        s5b = Scope(K)
        m1 = s5b.sb("m1", [128, 512], F32)
        r1 = s5b.sb("r1", [128, 512], F32)
        y1 = s5b.sb("y1", [128, 512], F32)
        y2 = s5b.sb("y2", [128, 512], F32)
        yq = s5b.sb("yq", [128, 512], F32)
        zl = [s5b.sb("zl%d" % i, [128, 512], F32) for i in range(2)]
        yst = [s5b.sb("yst%d" % i, [128, 512], F32) for i in range(2)]
        bst1, by1, by2, byq = Buf(), Buf(), Buf(), Buf()
        bzl, byst = [[Buf(), Buf()] for _ in range(2)]
        K.dma(S, m1[:], stat1_d[0][:, pcs], writes=[bst1])
        K.dma(S, r1[:], stat1_d[1][:, pcs], writes=[bst1])
        K.op(P, lambda: nc.gpsimd.memset(y1[:], 0.0), writes=[by1])
        K.op(P, lambda: nc.gpsimd.memset(y2[:], 0.0), writes=[by2])
        for db in range(32 if _P5E else 0):
            e = db % 2
            K.dma(S, zl[e][:], zT_d[db * 128:(db + 1) * 128, pcs], writes=[bzl[e]])
            K.op(V, lambda: nc.vector.tensor_tensor(out=zl[e][:], in0=zl[e][:], in1=m1[:], op=ALU.subtract),
                 reads=[bzl[e], bst1], writes=[bzl[e]])
            K.op(P, lambda: nc.gpsimd.tensor_tensor(out=zl[e][:], in0=zl[e][:], in1=r1[:], op=ALU.mult),
                 reads=[bzl[e], bst1], writes=[bzl[e]])
            K.op(A, lambda: nc.scalar.activation(zl[e][:], zl[e][:], AF.Identity, bias=l1b5[:, db:db + 1],
                                                 scale=l1g5[:, db:db + 1]), reads=[bzl[e], bsm5], writes=[bzl[e]])
            K.op(A, lambda: nc.scalar.activation(acc[:, db, :], acc[:, db, :], AF.Identity, scale=MOD(5, db, cj)),
                 reads=[bacc[db], bmod], writes=[bacc[db]])
            K.op(V, lambda: nc.vector.scalar_tensor_tensor(out=acc[:, db, :], in0=zl[e][:], scalar=ALPHA,
                                                           in1=acc[:, db, :], op0=ALU.mult, op1=ALU.add),
                 reads=[bzl[e], bacc[db]], writes=[bacc[db]])
            K.op(P, lambda: nc.gpsimd.tensor_tensor(out=y1[:], in0=y1[:], in1=acc[:, db, :], op=ALU.add),
                 reads=[bacc[db], by1], writes=[by1])
            K.op(A, lambda: nc.scalar.activation(yq[:], acc[:, db, :], AF.Square), reads=[bacc[db]], writes=[byq])
            K.op(P, lambda: nc.gpsimd.tensor_tensor(out=y2[:], in0=y2[:], in1=yq[:], op=ALU.add),
                 reads=[byq, by2], writes=[by2])
        ln_stats(y1, by1, y2, by2, yq, byq, 4096.0, 512)
        for db in range(32):
            e = db % 2
            K.op(V, lambda: nc.vector.tensor_tensor(out=yst[e][:], in0=acc[:, db, :], in1=y1[:], op=ALU.subtract),
                 reads=[bacc[db], by1], writes=[byst[e]])
            K.op(P, lambda: nc.gpsimd.tensor_tensor(out=yst[e][:], in0=yst[e][:], in1=y2[:], op=ALU.mult),
                 reads=[byst[e], by2], writes=[byst[e]])
            K.op(A, lambda: nc.scalar.activation(yst[e][:], yst[e][:], AF.Identity, bias=l2b[:, db:db + 1],
                                                 scale=l2g[:, db:db + 1]), reads=[byst[e], bsm5], writes=[byst[e]])
            K.dma(S, o_yT[db * 128:(db + 1) * 128, pcs], yst[e][:], reads=[byst[e]], final=True)
        K.barrier()
        s5b.close()
    sc5.close()

    K.finish()
    return nc, ins_used


def _fm(v, nchunk):
    return np.ascontiguousarray(np.asarray(v, np.float32).reshape(nchunk, 128).T)


def _rope_tables(pos):
    row = (pos // 64).astype(np.float32)
    col = (pos % 64).astype(np.float32)
    inv = (10000.0 ** (-np.arange(16, dtype=np.float32) / 16)).astype(np.float32)
    ar = row[:, None] * inv
    ac = col[:, None] * inv
    ang = np.concatenate([ar, ar, ac, ac], -1).astype(np.float32)
    return np.ascontiguousarray(np.cos(ang).T.astype(np.float32)), np.ascontiguousarray(np.sin(ang).T.astype(np.float32))


def _rmat():
    R = np.zeros((64, 64), np.float32)
    for base in (0, 32):
        for i in range(16):
            R[base + i, base + 16 + i] = -1.0
            R[base + 16 + i, base + i] = 1.0
    return np.ascontiguousarray(R.T)


def sample_cols(hf):
    own = np.arange(hf * 512, hf * 512 + 512)
    if hf == 0:
        other = np.arange(512, 1024)
    else:
        other = np.concatenate([np.arange(497, 512), np.arange(0, 497)])
    return own, other


def prep_shared(inp):
    f = lambda k: np.asarray(inp[k], np.float32)
    sh = {}
    sh["w_ada"] = np.ascontiguousarray(f("w_ada")[0])
    sh["b_adaT"] = _fm(f("b_ada")[0], 192)
    sh["w_in"] = np.ascontiguousarray(f("w_in")[0])
    sh["g_qT"] = _fm(f("g_q")[0], 6)
    sh["g_kvT"] = _fm(f("g_kv")[0], 4)
    sh["w_uq"] = np.ascontiguousarray(f("w_uq")[0])
    sh["w_ukv"] = np.ascontiguousarray(f("w_ukv")[0])
    sh["w_dwT"] = np.ascontiguousarray(f("w_dw")[0].T.reshape(16, 128, 31).transpose(1, 0, 2))
    sh["b_dwT"] = _fm(f("b_dw")[0], 16)
    sh["g_cnT"] = _fm(f("g_cn")[0], 16)
    sh["b_cnT"] = _fm(f("b_cn")[0], 16)
    sh["w_out"] = np.ascontiguousarray(f("w_out")[0])
    sh["ln1_gT"] = _fm(f("ln1_g")[0], 32)
    sh["ln1_bT"] = _fm(f("ln1_b")[0], 32)
    sh["w_pq"] = np.ascontiguousarray(f("w_pq")[0])
    sk = f("sub_keys")[0]
    skT = sk.reshape(16, 128, 2, 128).transpose(3, 0, 2, 1)
    sh["skT"] = np.ascontiguousarray(skT.reshape(128, 32, 128))
    pu = f("peer_u")[0]
    sh["peer_uT"] = np.ascontiguousarray(pu.reshape(128, 128, 32, 128).transpose(0, 3, 2, 1)).reshape(128, 128, 4096)
    sh["peer_v"] = np.ascontiguousarray(f("peer_v")[0])
    sh["ln2_gT"] = _fm(f("ln2_g")[0], 32)
    sh["ln2_bT"] = _fm(f("ln2_b")[0], 32)
    sh["rmatT"] = _rmat()
    return sh


def prep_core(inp, c):
    f = lambda k: np.asarray(inp[k], np.float32)
    b, hf = c // 2, c % 2
    own, other = sample_cols(hf)
    xp = f("x_prompt")[4 * c:4 * c + 4].reshape(NP_, D)
    xs = f("x_sample")[b]
    xall = np.concatenate([xp, xs[own], xs[other]], 0)
    m = {}
    m["xT"] = np.ascontiguousarray(xall.T)
    cond = np.stack([f("c_ctx"), f("c")[b]], -1)
    m["condT"] = np.ascontiguousarray(cond.reshape(32, 128, 2).transpose(1, 0, 2))
    m["cache_ckvT"] = np.ascontiguousarray(f("cache_ckv")[b, 0].T.reshape(4, 128, 256).transpose(1, 0, 2))
    m["cache_kpeT"] = np.ascontiguousarray(f("cache_kpe")[b, 0].T)
    cosT, sinT = _rope_tables(np.concatenate([own, other]))
    m["cosT"] = cosT
    m["sinT"] = sinT
    hm = np.zeros((128, 2), np.float32)
    hm[:, 0] = 1.0 if hf == 1 else 0.0
    hm[:, 1] = 1.0 if hf == 0 else 0.0
    m["halo_mask"] = hm
    return m


_CACHE = {}


def kernel(**inputs):
    if "nc" not in _CACHE:
        _CACHE["nc"] = build()
    nc, used = _CACHE["nc"]
    sh = prep_shared(inputs)
    in_maps = []
    for c in range(8):
        m = prep_core(inputs, c)
        m.update(sh)
        in_maps.append({k: m[k] for k in used})
    res = run_bass_kernel_spmd(nc, in_maps, core_ids=list(range(8)))
    y_prompt = np.zeros((32, 256, D), np.float32)
    y_sample = np.zeros((4, 1024, D), np.float32)
    new_ckv = np.zeros((32, 1, 256, 512), np.float32)
    new_kpe = np.zeros((32, 1, 256, 64), np.float32)
    for c in range(8):
        r = res.results[c]
        b, hf = c // 2, c % 2
        yT = np.asarray(r["o_yT"], np.float32)
        y_prompt[4 * c:4 * c + 4] = yT[:, :NP_].T.reshape(4, 256, D)
        y_sample[b, hf * 512:(hf + 1) * 512] = yT[:, NP_:].T
        new_ckv[4 * c:4 * c + 4, 0] = np.asarray(r["o_ckvT"], np.float32).T.reshape(4, 256, 512)
        new_kpe[4 * c:4 * c + 4, 0] = np.asarray(r["o_kpeT"], np.float32).T.reshape(4, 256, 64)
    return (y_prompt, y_sample, new_ckv, new_kpe)
```

```python
import numpy as np
import ml_dtypes
from contextlib import ExitStack
import concourse.bass as bass
import concourse.mybir as mybir
from concourse.bass_utils import run_bass_kernel_spmd

F32 = mybir.dt.float32
BF16 = mybir.dt.bfloat16
AF = mybir.ActivationFunctionType
ALU = mybir.AluOpType
AX = mybir.AxisListType

D = 4096
NP_ = 1024
NS = 512
NOWN = NP_ + NS
NALL = 2048
NKEY = 2304
ALPHA = 2.0 ** 0.25
EPS = 1e-6
SCALE = 192.0 ** -0.5
YW = 4 * 286 + 542


class Buf:
    __slots__ = ("w", "r")

    def __init__(self):
        self.w = None
        self.r = {}


class Q:
    def __init__(self, K, name, eng, is_pe=False):
        self.name = name
        self.eng = eng
        self.is_pe = is_pe
        self.sem = K.newsem("q_" + name)
        self.cnt = 0
        self.waited = {}
        self.dsems = []
        self.dvals = []
        self.dma_i = 0
        self.lazy = False

    def wait(self, tok):
        sem, val, owner = tok
        key = id(sem)
        if self.waited.get(key, 0) >= val:
            return
        self.eng.wait_ge(sem, val)
        self.waited[key] = val


class Kern:
    NDMA = 10

    def __init__(self, nc):
        self.nc = nc
        self.stack = ExitStack()
        self.pe = Q(self, "pe", nc.tensor, is_pe=True)
        self.act = Q(self, "act", nc.scalar)
        self.dve = Q(self, "dve", nc.vector)
        self.pool = Q(self, "pool", nc.gpsimd)
        self.sp = Q(self, "sp", nc.sync)
        self.qs = [self.pe, self.act, self.dve, self.pool, self.sp]
        for q in (self.sp, self.pool):
            for i in range(self.NDMA):
                q.dsems.append(self.newsem("d_%s%d" % (q.name, i)))
                q.dvals.append(0)
        self.final = []
        self.uid = 0

    def newsem(self, name):
        return self.stack.enter_context(self.nc.semaphore(name))

    def _deps(self, q, reads, writes):
        for b in reads:
            if b.w is not None and not (b.w[2] is q and q.is_pe):
                q.wait(b.w)
        for b in writes:
            if b.w is not None and not (b.w[2] is q and q.is_pe):
                q.wait(b.w)
            for owner, t in b.r.items():
                if owner is not q or not q.is_pe:
                    q.wait(t)

    def _mark(self, tok, reads, writes):
        for b in reads:
            b.r[tok[2]] = tok
        for b in writes:
            b.w = tok
            b.r = {}

    def op(self, q, fn, reads=(), writes=(), inc=True):
        self._deps(q, reads, writes)
        ins = fn()
        if inc:
            q.cnt += 1
            ins.then_inc(q.sem, 1)
            q.lazy = False
            tok = (q.sem, q.cnt, q)
        else:
            q.lazy = True
            tok = (q.sem, q.cnt + 1, q)
        self._mark(tok, reads, writes)
        return tok

    def dma(self, q, out, in_, reads=(), writes=(), final=False):
        slot = q.dma_i % self.NDMA
        q.dma_i += 1
        sem = q.dsems[slot]
        if q.dvals[slot] > 0:
            q.wait((sem, q.dvals[slot], sem))
        self._deps(q, reads, writes)
        ins = q.eng.dma_start(out=out, in_=in_)
        q.dvals[slot] += 16
        ins.then_inc(sem, 16)
        tok = (sem, q.dvals[slot], sem)
        self._mark(tok, reads, writes)
        if final:
            self.final.append(tok)
        return tok

    def barrier(self):
        toks = []
        for q in self.qs:
            assert not q.lazy, q.name
            if q.cnt > 0:
                toks.append((q.sem, q.cnt, q))
            for s, v in zip(q.dsems, q.dvals):
                if v > 0:
                    toks.append((s, v, s))
        for q in self.qs:
            for t in toks:
                if t[2] is not q:
                    q.wait(t)

    def name(self, base):
        self.uid += 1
        return "%s_%d" % (base, self.uid)

    def finish(self):
        self.barrier()


class Scope:
    def __init__(self, K):
        self.K = K
        self.stack = ExitStack()

    def sb(self, name, shape, dtype):
        return self.stack.enter_context(self.K.nc.sbuf_tensor(self.K.name(name), list(shape), dtype))

    def close(self):
        self.stack.close()


def mm(K, out, pairs, reads, writes, first=True, last=True):
    nc = K.nc
    n = len(pairs)
    tok = None
    for i, (l, r) in enumerate(pairs):
        st = first and i == 0
        sp = last and i == n - 1
        tok = K.op(K.pe, lambda: nc.tensor.matmul(out, l, r, start=st, stop=sp),
                   reads=reads, writes=writes, inc=(i == n - 1))
    return tok


def build(dbg=False, stop_after=99):
    nc = bass.Bass("TRN2", target_bir_lowering=False)
    K = Kern(nc)
    V, A, P, S = K.dve, K.act, K.pool, K.sp
    ins_used = []

    def din(name, shape, dt=F32):
        ins_used.append(name)
        return nc.dram_tensor(name, list(shape), dt, kind="ExternalInput").ap()

    def dout(name, shape, dt=F32):
        return nc.dram_tensor(name, list(shape), dt, kind="ExternalOutput").ap()

    def dscr(name, shape, dt):
        return nc.dram_tensor(name, list(shape), dt, kind=("ExternalOutput" if dbg else "Internal")).ap()

    top = Scope(K)
    banks = [K.stack.enter_context(nc.psum_tensor("bank%d" % i, [128, 512], F32)) for i in range(8)]
    bB = [Buf() for _ in range(8)]
    ident_f = top.sb("ident_f", [128, 128], F32)
    ident_b = top.sb("ident_b", [128, 128], BF16)
    ones_f = top.sb("ones_f", [128, 128], F32)
    mod = top.sb("mod", [128, 192, 2], F32)
    opsc = top.sb("opsc", [128, 2, 32, 2], F32)
    bconst = Buf()
    bmod = Buf()

    K.op(P, lambda: nc.gpsimd.memset(ident_f[:], 0.0), writes=[bconst])
    K.op(P, lambda: nc.gpsimd.affine_select(out=ident_f[:], in_=ident_f[:], pattern=[[-1, 128]],
                                            compare_op=ALU.not_equal, fill=1.0, base=0, channel_multiplier=1),
         reads=[bconst], writes=[bconst])
    K.op(P, lambda: nc.gpsimd.tensor_copy(out=ident_b[:], in_=ident_f[:]), reads=[bconst], writes=[bconst])
    K.op(P, lambda: nc.gpsimd.memset(ones_f[:], 1.0), writes=[bconst])
    epsc = top.sb("epsc", [128, 1], F32)
    K.op(P, lambda: nc.gpsimd.memset(epsc[:], EPS), writes=[bconst])

    def MOD(s, kc, j):
        return mod[:, s * 32 + kc, j:j + 1]

    def OPSC(w, kc, j):
        return opsc[:, w, kc, j:j + 1]

    condT = din("condT", [128, 32, 2])
    w_ada = din("w_ada", [D, 6 * D])
    b_adaT = din("b_adaT", [128, 192])
    sc0 = Scope(K)
    cnd = sc0.sb("cnd", [128, 32, 2], F32)
    sil = sc0.sb("sil", [128, 32, 2], BF16)
    bad = sc0.sb("bad", [128, 192], F32)
    bc = Buf()
    K.dma(S, cnd[:], condT, writes=[bc])
    K.dma(S, bad[:], b_adaT, writes=[bc])
    K.op(A, lambda: nc.scalar.activation(sil[:], cnd[:], AF.Silu), reads=[bc], writes=[bc])
    wav = w_ada.rearrange("(kc p) n -> p kc n", p=128)
    wr = [sc0.sb("wada%d" % i, [128, 32, 512], BF16) for i in range(3)]
    bwr = [Buf() for _ in range(3)]
    for ct in range(48):
        i = ct % 3
        K.dma(P, wr[i][:], wav[:, :, ct * 512:(ct + 1) * 512], writes=[bwr[i]])
        pb = ct % 2
        for ob in range(4):
            mm(K, banks[pb][:, ob * 2:ob * 2 + 2],
               [(wr[i][:, kc, ob * 128:(ob + 1) * 128], sil[:, kc, :]) for kc in range(32)],
               reads=[bwr[i], bc], writes=[bB[pb]])
        K.op(V, lambda: nc.vector.tensor_tensor(
            out=mod[:, ct * 4:(ct + 1) * 4, :],
            in0=banks[pb][:, 0:8].rearrange("p (a b) -> p a b", b=2),
            in1=bad[:, ct * 4:(ct + 1) * 4].unsqueeze(2).to_broadcast([128, 4, 2]), op=ALU.add),
            reads=[bB[pb], bc], writes=[bmod])
    K.op(V, lambda: nc.vector.tensor_scalar(out=opsc[:, 0, :, :], in0=mod[:, 32:64, :], scalar1=1.0, scalar2=None,
                                            op0=ALU.add), reads=[bmod], writes=[bmod])
    K.op(V, lambda: nc.vector.tensor_scalar(out=opsc[:, 1, :, :], in0=mod[:, 128:160, :], scalar1=1.0, scalar2=None,
                                            op0=ALU.add), reads=[bmod], writes=[bmod])
    if dbg:
        d_mod = dout("d_mod", [128, 192, 2])
        K.dma(S, d_mod, mod[:], reads=[bmod])
    K.barrier()
    sc0.close()
    if stop_after <= 0:
        K.finish()
        return nc, ins_used

    xT = din("xT", [D, NALL])
    w_in = din("w_in", [D, 5440])
    g_qT = din("g_qT", [128, 6])
    g_kvT = din("g_kvT", [128, 4])
    w_dwT = din("w_dwT", [128, 16, 31])
    b_dwT = din("b_dwT", [128, 16])
    halo_mask = din("halo_mask", [128, 2])
    o_ckvT = dout("o_ckvT", [512, NP_])
    o_kpeT = dout("o_kpeT", [64, NP_])
    qcT_d = dscr("qcT_d", [768, NOWN], BF16)
    rstdq_d = dscr("rstdq_d", [128, NOWN], F32)
    ckvT_d = dscr("ckvT_d", [512, NALL], BF16)
    kpe_d = dscr("kpe_d", [64, NALL], F32)
    conv_d = dscr("conv_d", [2048, NOWN], F32)
    cstat_d = dscr("cstat_d", [2, 128, NOWN], F32)
    xv = xT.rearrange("(kc p) n -> p kc n", p=128)
    wiv = w_in.rearrange("(kc p) n -> p kc n", p=128)

    sc1 = Scope(K)
    gq = sc1.sb("gq", [128, 6], F32)
    gkv = sc1.sb("gkv", [128, 4], F32)
    wdw = sc1.sb("wdw", [128, 16, 31], F32)
    bdw = sc1.sb("bdw", [128, 16], F32)
    hmask = sc1.sb("hmask", [128, 2], F32)
    bsm = Buf()
    for t_, s_ in ((gq, g_qT), (gkv, g_kvT), (wdw, w_dwT), (bdw, b_dwT), (hmask, halo_mask)):
        K.dma(S, t_[:], s_, writes=[bsm])
    wt = [sc1.sb("wt%d" % i, [128, 32, 256], BF16) for i in range(2)]
    bwt = [Buf() for _ in range(2)]
    wti = [0]

    def load_w(c0, ncol, c1=None):
        i = wti[0] % 2
        wti[0] += 1
        if c1 is None:
            K.dma(P, wt[i][:, :, 0:ncol], wiv[:, :, c0:c0 + ncol], writes=[bwt[i]])
        else:
            K.dma(P, wt[i][:, :, 0:128], wiv[:, :, c0:c0 + 128], writes=[bwt[i]])
            K.dma(P, wt[i][:, :, 128:256], wiv[:, :, c1:c1 + 128], writes=[bwt[i]])
        return i

    def modulate(hT, bh, ncols, col0, ranges, sc):
        xs = [sc.sb("xs%d" % i, [128, ncols], F32) for i in range(2)]
        bxs = [Buf() for _ in range(2)]
        for kc in range(32):
            i = kc % 2
            K.dma(S, xs[i][:], xv[:, kc, col0:col0 + ncols], writes=[bxs[i]])
            for ri, (lo, hi, j) in enumerate(ranges):
                if (kc + ri) % 2 == 0:
                    K.op(V, lambda: nc.vector.tensor_scalar(out=hT[:, kc, lo:hi], in0=xs[i][:, lo:hi],
                                                            scalar1=OPSC(0, kc, j), scalar2=MOD(0, kc, j),
                                                            op0=ALU.mult, op1=ALU.add),
                         reads=[bxs[i], bmod], writes=[bh])
                else:
                    K.op(A, lambda: nc.scalar.activation(hT[:, kc, lo:hi], xs[i][:, lo:hi], AF.Identity,
                                                         bias=MOD(0, kc, j), scale=OPSC(0, kc, j)),
                         reads=[bxs[i], bmod], writes=[bh])

    pbi = [0]

    def next_bank(lo=0, hi=4):
        b = lo + pbi[0] % (hi - lo)
        pbi[0] += 1
        return b

    def rstd_from_sumsq(sq, bsq, ncols, nfeat, sc):
        for t in range(0, ncols, 512):
            w = min(512, ncols - t)
            mm(K, banks[7][:, 0:w], [(ones_f[:], sq[:, t:t + w])], reads=[bsq, bconst], writes=[bB[7]])
            K.op(A, lambda: nc.scalar.activation(sq[:, t:t + w], banks[7][:, 0:w], AF.Sqrt, bias=epsc[:, 0:1],
                                                 scale=1.0 / nfeat), reads=[bB[7], bconst], writes=[bsq])
        K.op(V, lambda: nc.vector.reciprocal(out=sq[:, 0:ncols], in_=sq[:, 0:ncols]), reads=[bsq], writes=[bsq])

    def proj_kv(hT, bh, ncols, col0, sc, is_own):
        ntile = ncols // 512
        raw = sc.sb("ckvraw", [128, 4, ncols], F32)
        sq = sc.sb("sqkv", [128, ncols], F32)
        sqt = [sc.sb("sqt%d" % i, [128, 512], F32) for i in range(2)]
        braw, bsq = Buf(), Buf()
        bsqt = [Buf(), Buf()]
        K.op(P, lambda: nc.gpsimd.memset(sq[:], 0.0), writes=[bsq])
        k = 0
        for tl in range(2):
            i = load_w(768 + tl * 256, 256)
            for ob in range(2):
                b4 = tl * 2 + ob
                for t in range(ntile):
                    pb = next_bank()
                    mm(K, banks[pb][:], [(wt[i][:, kc, ob * 128:(ob + 1) * 128], hT[:, kc, t * 512:(t + 1) * 512])
                                         for kc in range(32)], reads=[bwt[i], bh], writes=[bB[pb]])
                    K.op(A, lambda: nc.scalar.copy(raw[:, b4, t * 512:(t + 1) * 512], banks[pb][:]),
                         reads=[bB[pb]], writes=[braw])
                    j = k % 2
                    k += 1
                    K.op(A, lambda: nc.scalar.activation(sqt[j][:], banks[pb][:], AF.Square),
                         reads=[bB[pb]], writes=[bsqt[j]])
                    K.op(V, lambda: nc.vector.tensor_tensor(out=sq[:, t * 512:(t + 1) * 512],
                                                            in0=sq[:, t * 512:(t + 1) * 512], in1=sqt[j][:], op=ALU.add),
                         reads=[bsqt[j], bsq], writes=[bsq])
        rstd_from_sumsq(sq, bsq, ncols, 512.0, sc)
        nrm = [sc.sb("nrm%d" % i, [128, 512], F32) for i in range(2)]
        nrb = [sc.sb("nrb%d" % i, [128, 512], BF16) for i in range(2)]
        bn = [Buf(), Buf()]
        bnb = [Buf(), Buf()]
        k = 0
        for b4 in range(4):
            for t in range(ntile):
                j = k % 2
                k += 1
                cs = slice(t * 512, (t + 1) * 512)
                K.op(V, lambda: nc.vector.scalar_tensor_tensor(out=nrm[j][:], in0=raw[:, b4, cs], scalar=gkv[:, b4:b4 + 1],
                                                               in1=sq[:, cs], op0=ALU.mult, op1=ALU.mult),
                     reads=[braw, bsq, bsm], writes=[bn[j]])
                if is_own and t < 2:
                    K.dma(S, o_ckvT[b4 * 128:(b4 + 1) * 128, cs], nrm[j][:], reads=[bn[j]], final=True)
                K.op(A, lambda: nc.scalar.copy(nrb[j][:], nrm[j][:]), reads=[bn[j]], writes=[bnb[j]])
                K.dma(S, ckvT_d[b4 * 128:(b4 + 1) * 128, col0 + t * 512:col0 + (t + 1) * 512], nrb[j][:],
                      reads=[bnb[j]])
        i = load_w(1280, 64)
        kraw = sc.sb("kraw", [64, ncols], F32)
        bk = Buf()
        for t in range(ntile):
            pb = next_bank()
            mm(K, banks[pb][0:64, :], [(wt[i][:, kc, 0:64], hT[:, kc, t * 512:(t + 1) * 512]) for kc in range(32)],
               reads=[bwt[i], bh], writes=[bB[pb]])
            K.op(A, lambda: nc.scalar.copy(kraw[:, t * 512:(t + 1) * 512], banks[pb][0:64, :]),
                 reads=[bB[pb]], writes=[bk])
        K.dma(S, kpe_d[:, col0:col0 + ncols], kraw[:], reads=[bk])
        if is_own:
            K.dma(S, o_kpeT[:, :], kraw[:, 0:NP_], reads=[bk], final=True)

    scx = Scope(K)
    hTo = scx.sb("hTo", [128, 32, 512], BF16)
    bho = Buf()
    modulate(hTo, bho, 512, NOWN, [(0, 512, 1)], scx)
    proj_kv(hTo, bho, 512, NOWN, scx, False)
    K.barrier()
    scx.close()

    NH = NOWN + 16
    hT = sc1.sb("hT", [128, 32, NH], BF16)
    bh = Buf()
    sca = Scope(K)
    modulate(hT, bh, NH, 0, [(0, NP_, 0), (NP_, NH, 1)], sca)
    K.barrier()
    sca.close()
    sca = Scope(K)
    proj_kv(hT, bh, NOWN, 0, sca, True)
    sqq = sca.sb("sqq", [128, NOWN], F32)
    sqt2 = [sca.sb("sqt2%d" % i, [128, 512], F32) for i in range(2)]
    qst = [sca.sb("qst%d" % i, [128, NOWN], BF16) for i in range(2)]
    bsqq = Buf()
    bsqt2 = [Buf(), Buf()]
    bqst = [Buf(), Buf()]
    K.op(P, lambda: nc.gpsimd.memset(sqq[:], 0.0), writes=[bsqq])
    k = 0
    for tl in range(3):
        i = load_w(tl * 256, 256)
        for ob in range(2):
            qb = tl * 2 + ob
            for t in range(3):
                cs = slice(t * 512, (t + 1) * 512)
                pb = next_bank()
                mm(K, banks[pb][:], [(wt[i][:, kc, ob * 128:(ob + 1) * 128], hT[:, kc, cs]) for kc in range(32)],
                   reads=[bwt[i], bh], writes=[bB[pb]])
                K.op(A, lambda: nc.scalar.activation(qst[qb % 2][:, cs], banks[pb][:], AF.Identity,
                                                     scale=gq[:, qb:qb + 1]),
                     reads=[bB[pb], bsm], writes=[bqst[qb % 2]])
                j = k % 2
                k += 1
                K.op(A, lambda: nc.scalar.activation(sqt2[j][:], banks[pb][:], AF.Square),
                     reads=[bB[pb]], writes=[bsqt2[j]])
                K.op(V, lambda: nc.vector.tensor_tensor(out=sqq[:, cs], in0=sqq[:, cs], in1=sqt2[j][:], op=ALU.add),
                     reads=[bsqt2[j], bsqq], writes=[bsqq])
            K.dma(S, qcT_d[qb * 128:(qb + 1) * 128, :], qst[qb % 2][:], reads=[bqst[qb % 2]])
    rstd_from_sumsq(sqq, bsqq, NOWN, 768.0, sca)
    K.dma(S, rstdq_d, sqq[:], reads=[bsqq])
    K.barrier()
    sca.close()
    if stop_after <= 1:
        K.finish()
        return nc, ins_used

    scb = Scope(K)
    ypad = [scb.sb("ypad%d" % i, [128, YW], BF16) for i in range(2)]
    dg = [scb.sb("dg%d" % i, [128, 31, 128], BF16) for i in range(2)]
    sg = [scb.sb("sg%d" % i, [128, 512], F32) for i in range(2)]
    yh = scb.sb("yh", [128, 16], F32)
    cv = [scb.sb("cv%d" % i, [128, NOWN], F32) for i in range(2)]
    cq = scb.sb("cq", [128, NOWN], F32)
    s1c = scb.sb("s1c", [128, NOWN], F32)
    s2c = scb.sb("s2c", [128, NOWN], F32)
    byp = [Buf(), Buf()]
    bdg = [Buf(), Buf()]
    bsg = [Buf(), Buf()]
    byh, bcq, bs1, bs2 = Buf(), Buf(), Buf(), Buf()
    bcv = [Buf(), Buf()]
    for i in range(2):
        K.op(P, lambda: nc.gpsimd.memset(ypad[i][:], 0.0), writes=[byp[i]])
    K.op(P, lambda: nc.gpsimd.memset(s1c[:], 0.0), writes=[bs1])
    K.op(P, lambda: nc.gpsimd.memset(s2c[:], 0.0), writes=[bs2])

    def ywin(yp, t, k):
        if t < 2:
            return yp[:, t * 572:(t + 1) * 572].rearrange("p (s w) -> p s w", w=286)[:, :, k:k + 256]
        return yp[:, 1144 + k:1144 + k + 512]

    def conv_chunk(j):
        yp = ypad[j % 2]
        for t in range(3):
            if t < 2:
                o = banks[4 + t][:].rearrange("p (s w) -> p s w", w=256)
            else:
                o = banks[4 + t][:]
            mm(K, o, [(dg[j % 2][:, k, :], ywin(yp, t, k)) for k in range(31)],
               reads=[bdg[j % 2], byp[j % 2]], writes=[bB[4 + t]])
            cs = slice(t * 512, (t + 1) * 512)
            K.op(A, lambda: nc.scalar.activation(cv[j % 2][:, cs], banks[4 + t][:], AF.Identity,
                                                 bias=bdw[:, j:j + 1]),
                 reads=[bB[4 + t], bsm], writes=[bcv[j % 2]])
        K.dma(S, conv_d[j * 128:(j + 1) * 128, :], cv[j % 2][:], reads=[bcv[j % 2]])
        K.op(V, lambda: nc.vector.tensor_tensor(out=s1c[:], in0=s1c[:], in1=cv[j % 2][:], op=ALU.add),
             reads=[bcv[j % 2], bs1], writes=[bs1])
        K.op(A, lambda: nc.scalar.activation(cq[:], cv[j % 2][:], AF.Square), reads=[bcv[j % 2]], writes=[bcq])
        K.op(V, lambda: nc.vector.tensor_tensor(out=s2c[:], in0=s2c[:], in1=cq[:], op=ALU.add),
             reads=[bcq, bs2], writes=[bs2])

    for jp in range(8):
        ia = load_w(1344 + 256 * jp, 256)
        ig = load_w(3392 + 256 * jp, 256)
        for jj in range(2):
            j = jp * 2 + jj
            yp = ypad[j % 2]
            K.op(V, lambda: nc.vector.tensor_tensor(
                out=dg[j % 2][:], in0=ident_b[:].unsqueeze(1).to_broadcast([128, 31, 128]),
                in1=wdw[:, j, :].unsqueeze(2).to_broadcast([128, 31, 128]), op=ALU.mult),
                reads=[bconst, bsm], writes=[bdg[j % 2]])
            for t in range(4):
                if t < 3:
                    cs = slice(t * 512, (t + 1) * 512)
                    w_ = 512
                else:
                    cs = slice(NOWN, NOWN + 16)
                    w_ = 16
                pa = next_bank()
                pg = next_bank()
                mm(K, banks[pa][:, 0:w_], [(wt[ia][:, kc, jj * 128:(jj + 1) * 128], hT[:, kc, cs]) for kc in range(32)],
                   reads=[bwt[ia], bh], writes=[bB[pa]])
                mm(K, banks[pg][:, 0:w_], [(wt[ig][:, kc, jj * 128:(jj + 1) * 128], hT[:, kc, cs]) for kc in range(32)],
                   reads=[bwt[ig], bh], writes=[bB[pg]])
                s_ = sg[t % 2]
                K.op(A, lambda: nc.scalar.activation(s_[:, 0:w_], banks[pg][:, 0:w_], AF.Sigmoid),
                     reads=[bB[pg]], writes=[bsg[t % 2]])
                if t < 2:
                    o = yp[:, t * 572:(t + 1) * 572].rearrange("p (s w) -> p s w", w=286)[:, :, 15:271]
                    K.op(V, lambda: nc.vector.tensor_tensor(
                        out=o, in0=banks[pa][:].rearrange("p (s w) -> p s w", w=256),
                        in1=s_[:].rearrange("p (s w) -> p s w", w=256), op=ALU.mult),
                        reads=[bB[pa], bsg[t % 2]], writes=[byp[j % 2]])
                elif t == 2:
                    K.op(V, lambda: nc.vector.tensor_tensor(out=yp[:, 1159:1159 + 512], in0=banks[pa][:], in1=s_[:],
                                                            op=ALU.mult),
                         reads=[bB[pa], bsg[t % 2]], writes=[byp[j % 2]])
                else:
                    K.op(V, lambda: nc.vector.tensor_tensor(out=yh[:], in0=banks[pa][:, 0:16], in1=s_[:, 0:16],
                                                            op=ALU.mult),
                         reads=[bB[pa], bsg[t % 2]], writes=[byh])
                    K.op(V, lambda: nc.vector.tensor_scalar(out=yp[:, 1144:1159], in0=yh[:, 0:15],
                                                            scalar1=hmask[:, 0:1], scalar2=None, op0=ALU.mult),
                         reads=[byh, bsm], writes=[byp[j % 2]])
                    K.op(V, lambda: nc.vector.tensor_scalar(out=yp[:, 1671:1686], in0=yh[:, 0:15],
                                                            scalar1=hmask[:, 1:2], scalar2=None, op0=ALU.mult),
                         reads=[byh, bsm], writes=[byp[j % 2]])
            if j > 0:
                conv_chunk(j - 1)
    conv_chunk(15)
    for t in range(3):
        cs = slice(t * 512, (t + 1) * 512)
        mm(K, banks[0][:], [(ones_f[:], s1c[:, cs])], reads=[bs1, bconst], writes=[bB[0]])
        mm(K, banks[1][:], [(ones_f[:], s2c[:, cs])], reads=[bs2, bconst], writes=[bB[1]])
        K.op(V, lambda: nc.vector.tensor_scalar(out=s1c[:, cs], in0=banks[0][:], scalar1=1.0 / 2048, scalar2=None,
                                                op0=ALU.mult), reads=[bB[0]], writes=[bs1])
        K.op(V, lambda: nc.vector.tensor_tensor(out=cq[:, cs], in0=s1c[:, cs], in1=s1c[:, cs], op=ALU.mult),
             reads=[bs1], writes=[bcq])
        K.op(V, lambda: nc.vector.scalar_tensor_tensor(out=s2c[:, cs], in0=banks[1][:], scalar=1.0 / 2048,
                                                       in1=cq[:, cs], op0=ALU.mult, op1=ALU.subtract),
             reads=[bB[1], bcq], writes=[bs2])
    K.op(A, lambda: nc.scalar.activation(s2c[:], s2c[:], AF.Sqrt, bias=epsc[:, 0:1], scale=1.0),
         reads=[bs2, bconst], writes=[bs2])
    K.op(V, lambda: nc.vector.reciprocal(out=s2c[:], in_=s2c[:]), reads=[bs2], writes=[bs2])
    K.dma(S, cstat_d[0], s1c[:], reads=[bs1])
    K.dma(S, cstat_d[1], s2c[:], reads=[bs2])
    K.barrier()
    scb.close()
    sc1.close()
    if stop_after <= 2:
        K.finish()
        return nc, ins_used

    w_uq = din("w_uq", [768, 3072])
    w_ukv = din("w_ukv", [512, 4096])
    cache_ckvT = din("cache_ckvT", [128, 4, 256])
    cache_kpeT = din("cache_kpeT", [64, 256])
    cosT = din("cosT", [64, 1024])
    sinT = din("sinT", [64, 1024])
    rmatT = din("rmatT", [64, 64])
    attnT_d = dscr("attnT_d", [2048, NOWN], BF16)
    wuqv = w_uq.rearrange("(kc p) n -> p kc n", p=128)
    wukvv = w_ukv.rearrange("(kc p) n -> p kc n", p=128)
    sc2 = Scope(K)
    ckv = sc2.sb("ckv", [128, 4, NKEY], BF16)
    kpe = sc2.sb("kpeb", [64, NKEY], BF16)
    qc = sc2.sb("qc", [128, 6, NOWN], BF16)
    rq = sc2.sb("rq", [128, NOWN], F32)
    kraw = sc2.sb("kraw2", [64, NALL], F32)
    cos = sc2.sb("cos", [64, 1024], F32)
    sin = sc2.sb("sin", [64, 1024], F32)
    rm = sc2.sb("rm", [64, 64], F32)
    rt1 = sc2.sb("rt1", [64, 512], F32)
    rt2 = sc2.sb("rt2", [64, 512], F32)
    qpf = sc2.sb("qpf", [64, 512], F32)
    bckv, bkpe, bqc, brq, bkraw, btab, brt1, brt2, bqpf = [Buf() for _ in range(9)]
    K.dma(S, ckv[:, :, 0:NALL], ckvT_d.rearrange("(kc p) n -> p kc n", p=128), writes=[bckv])
    K.dma(P, ckv[:, :, NALL:NKEY], cache_ckvT, writes=[bckv])
    K.dma(S, kraw[:], kpe_d, writes=[bkraw])
    K.dma(P, kpe[:, NALL:NKEY], cache_kpeT, writes=[bkpe])
    K.dma(S, qc[:], qcT_d.rearrange("(kc p) n -> p kc n", p=128), writes=[bqc])
    K.dma(S, rq[:], rstdq_d, writes=[brq])
    K.dma(S, cos[:], cosT, writes=[btab])
    K.dma(S, sin[:], sinT, writes=[btab])
    K.dma(S, rm[:], rmatT, writes=[btab])
    K.op(A, lambda: nc.scalar.copy(kpe[:, 0:NP_], kraw[:, 0:NP_]), reads=[bkraw], writes=[bkpe])

    def rope(dst, bdst, src, bsrc, tab0):
        pb = next_bank(0, 6)
        mm(K, banks[pb][0:64, :], [(rm[:, :], src)], reads=[btab, bsrc], writes=[bB[pb]])
        K.op(V, lambda: nc.vector.tensor_tensor(out=rt1[:], in0=src, in1=cos[:, tab0:tab0 + 512], op=ALU.mult),
             reads=[bsrc, btab], writes=[brt1])
        K.op(V, lambda: nc.vector.tensor_tensor(out=rt2[:], in0=banks[pb][0:64, :], in1=sin[:, tab0:tab0 + 512],
                                                op=ALU.mult), reads=[bB[pb], btab], writes=[brt2])
        K.op(V, lambda: nc.vector.tensor_tensor(out=dst, in0=rt1[:], in1=rt2[:], op=ALU.add),
             reads=[brt1, brt2], writes=[bdst])

    for t in range(2):
        rope(kpe[:, NP_ + t * 512:NP_ + (t + 1) * 512], bkpe, kraw[:, NP_ + t * 512:NP_ + (t + 1) * 512], bkraw, t * 512)

    wq = [sc2.sb("wq%d" % i, [128, 6, 192], BF16) for i in range(2)]
    wkv = [sc2.sb("wkv%d" % i, [128, 4, 256], BF16) for i in range(2)]
    qn = [sc2.sb("qn%d" % i, [128, NOWN], BF16) for i in range(2)]
    qp = [sc2.sb("qp%d" % i, [64, NOWN], BF16) for i in range(2)]
    kn = [sc2.sb("kn%d" % i, [128, NKEY], BF16) for i in range(2)]
    vh = [sc2.sb("vh%d" % i, [128, 18, 128], BF16) for i in range(2)]
    ast = [sc2.sb("ast%d" % i, [128, NOWN], BF16) for i in range(2)]
    p32 = [sc2.sb("p32%d" % i, [128, 1280], F32) for i in range(2)]
    pn = [sc2.sb("pn%d" % i, [128, 1280], BF16) for i in range(2)]
    pt = [sc2.sb("pt%d" % i, [128, 10, 128], BF16) for i in range(2)]
    st = [sc2.sb("st%d" % i, [128, 8], F32) for i in range(2)]
    bwq, bwkv, bqn, bqp, bkn, bvh, bast, bp32, bpn, bpt, bst, bpv = [[Buf(), Buf()] for _ in range(12)]
    tb = [banks[6][:].bitcast(BF16), banks[7][:].bitcast(BF16)]

    qblocks = []
    for s_ in range(4):
        for hh in range(2):
            qblocks.append((s_ * 256 + hh * 128, s_ * 256, 256, 2 * s_))
    for i_ in range(4):
        qblocks.append((NP_ + i_ * 128, NP_, 1280, 8))

    import os
    _NH = int(os.environ.get('P2_HEADS', 16)); _NQ = int(os.environ.get('P2_QB', 12)); _STG = int(os.environ.get('P2_STAGE', 4))
    for h in range(_NH):
        hp = h % 2
        K.dma(P, wq[hp][:], wuqv[:, :, h * 192:(h + 1) * 192], writes=[bwq[hp]])
        K.dma(P, wkv[hp][:], wukvv[:, :, h * 256:(h + 1) * 256], writes=[bwkv[hp]])
        for t in range(3):
            cs = slice(t * 512, (t + 1) * 512)
            pb = next_bank(0, 6)
            mm(K, banks[pb][:], [(wq[hp][:, kc, 0:128], qc[:, kc, cs]) for kc in range(6)],
               reads=[bwq[hp], bqc], writes=[bB[pb]])
            K.op(V, lambda: nc.vector.tensor_tensor(out=qn[hp][:, cs], in0=banks[pb][:], in1=rq[:, cs], op=ALU.mult),
                 reads=[bB[pb], brq], writes=[bqn[hp]])
            pb = next_bank(0, 6)
            mm(K, banks[pb][0:64, :], [(wq[hp][:, kc, 128:192], qc[:, kc, cs]) for kc in range(6)],
               reads=[bwq[hp], bqc], writes=[bB[pb]])
            if t < 2:
                K.op(V, lambda: nc.vector.tensor_tensor(out=qp[hp][:, cs], in0=banks[pb][0:64, :], in1=rq[0:64, cs],
                                                        op=ALU.mult), reads=[bB[pb], brq], writes=[bqp[hp]])
            else:
                K.op(V, lambda: nc.vector.tensor_tensor(out=qpf[:], in0=banks[pb][0:64, :], in1=rq[0:64, cs],
                                                        op=ALU.mult), reads=[bB[pb], brq], writes=[bqpf])
                rope(qp[hp][:, cs], bqp[hp], qpf[:], bqpf, 0)
        for t in range(5):
            w_ = 512 if t < 4 else 256
            cs = slice(t * 512, t * 512 + w_)
            pb = next_bank(0, 6)
            mm(K, banks[pb][:, 0:w_], [(wkv[hp][:, kc, 0:128], ckv[:, kc, cs]) for kc in range(4)],
               reads=[bwkv[hp], bckv], writes=[bB[pb]])
            K.op(A, lambda: nc.scalar.copy(kn[hp][:, cs], banks[pb][:, 0:w_]), reads=[bB[pb]], writes=[bkn[hp]])
        for g in range(5):
            nb_ = 4 if g < 4 else 2
            pb = next_bank(0, 6)
            for kk in range(nb_):
                kb = g * 4 + kk
                mm(K, banks[pb][:, kk * 128:(kk + 1) * 128],
                   [(ckv[:, kc, kb * 128:(kb + 1) * 128], wkv[hp][:, kc, 128:256]) for kc in range(4)],
                   reads=[bwkv[hp], bckv], writes=[bB[pb]])
            K.op(A if g % 2 else V,
                 (lambda: nc.scalar.copy(vh[hp][:, g * 4:g * 4 + nb_, :],
                                         banks[pb][:, 0:nb_ * 128].rearrange("p (a b) -> p a b", b=128))) if g % 2 else
                 (lambda: nc.vector.tensor_copy(out=vh[hp][:, g * 4:g * 4 + nb_, :],
                                                in_=banks[pb][:, 0:nb_ * 128].rearrange("p (a b) -> p a b", b=128))),
                 reads=[bB[pb]], writes=[bvh[hp]])

        def emit_S(qi):
            q0, k0, nk, vb0 = qblocks[qi]
            base = 3 * (qi % 2)
            for kt in range((nk + 511) // 512):
                w_ = min(512, nk - kt * 512)
                ks = slice(k0 + kt * 512, k0 + kt * 512 + w_)
                mm(K, banks[base + kt][:, 0:w_],
                   [(qn[hp][:, q0:q0 + 128], kn[hp][:, ks]), (qp[hp][:, q0:q0 + 128], kpe[:, ks])],
                   reads=[bqn[hp], bqp[hp], bkn[hp], bkpe], writes=[bB[base + kt]])

        def emit_softmax(qi):
            q0, k0, nk, vb0 = qblocks[qi]
            base = 3 * (qi % 2)
            e = qi % 2
            nt = (nk + 511) // 512
            for kt in range(nt):
                w_ = min(512, nk - kt * 512)
                K.op(V, lambda: nc.vector.reduce_max(out=st[e][:, kt:kt + 1], in_=banks[base + kt][:, 0:w_], axis=AX.X),
                     reads=[bB[base + kt]], writes=[bst[e]])
            if nt > 1:
                K.op(V, lambda: nc.vector.reduce_max(out=st[e][:, 3:4], in_=st[e][:, 0:nt], axis=AX.X),
                     reads=[bst[e]], writes=[bst[e]])
                mcol = 3
            else:
                mcol = 0
            K.op(V, lambda: nc.vector.tensor_scalar(out=st[e][:, 4:5], in0=st[e][:, mcol:mcol + 1], scalar1=-SCALE,
                                                    scalar2=None, op0=ALU.mult), reads=[bst[e]], writes=[bst[e]])
            for kt in range(nt):
                w_ = min(512, nk - kt * 512)
                K.op(A, lambda: nc.scalar.activation(p32[e][:, kt * 512:kt * 512 + w_], banks[base + kt][:, 0:w_],
                                                     AF.Exp, bias=st[e][:, 4:5], scale=SCALE),
                     reads=[bB[base + kt], bst[e]], writes=[bp32[e]])
            K.op(V, lambda: nc.vector.reduce_sum(out=st[e][:, 5:6], in_=p32[e][:, 0:nk], axis=AX.X),
                 reads=[bp32[e]], writes=[bst[e]])
            K.op(V, lambda: nc.vector.reciprocal(out=st[e][:, 6:7], in_=st[e][:, 5:6]), reads=[bst[e]], writes=[bst[e]])
            K.op(A, lambda: nc.scalar.activation(pn[e][:, 0:nk], p32[e][:, 0:nk], AF.Identity, scale=st[e][:, 6:7]),
                 reads=[bp32[e], bst[e]], writes=[bpn[e]])

        def emit_PV(qi):
            q0, k0, nk, vb0 = qblocks[qi]
            base = 3 * (qi % 2)
            e = qi % 2
            nkb = nk // 128
            for kb in range(nkb):
                tbi = kb // 8
                K.op(K.pe, lambda: nc.tensor.transpose(tb[tbi][:, (kb % 8) * 128:(kb % 8 + 1) * 128],
                                                       pn[e][:, kb * 128:(kb + 1) * 128], ident_b[:]),
                     reads=[bpn[e], bconst], writes=[bB[6 + tbi]])
            n0 = min(nkb, 8)
            K.op(V, lambda: nc.vector.tensor_copy(out=pt[e][:, 0:n0, :],
                                                  in_=tb[0][:, 0:n0 * 128].rearrange("p (a b) -> p a b", b=128)),
                 reads=[bB[6]], writes=[bpt[e]])
            if nkb > 8:
                K.op(V, lambda: nc.vector.tensor_copy(out=pt[e][:, 8:nkb, :],
                                                      in_=tb[1][:, 0:(nkb - 8) * 128].rearrange("p (a b) -> p a b", b=128)),
                     reads=[bB[7]], writes=[bpt[e]])
            mm(K, banks[base + 2][:, 256:384], [(vh[hp][:, vb0 + kb, :], pt[e][:, kb, :]) for kb in range(nkb)],
               reads=[bvh[hp], bpt[e]], writes=[bB[base + 2]])
            K.op(A, lambda: nc.scalar.copy(ast[hp][:, q0:q0 + 128], banks[base + 2][:, 256:384]),
                 reads=[bB[base + 2]], writes=[bast[hp]])

        if _STG >= 2:
            emit_S(0)
        for qi in range(_NQ):
            if qi + 1 < _NQ and _STG >= 2:
                emit_S(qi + 1)
            if _STG >= 3:
                emit_softmax(qi)
            if _STG >= 4:
                emit_PV(qi)
        K.dma(S, attnT_d[h * 128:(h + 1) * 128, :], ast[hp][:], reads=[bast[hp]])
    K.barrier()
    sc2.close()
    if stop_after <= 3:
        K.finish()
        return nc, ins_used

    def ln_stats(s1, bs1, s2, bs2, tmp, btmp, nfeat, ncols):
        for t in range(0, ncols, 512):
            cs = slice(t, t + 512)
            mm(K, banks[6][:], [(ones_f[:], s1[:, cs])], reads=[bs1, bconst], writes=[bB[6]])
            mm(K, banks[7][:], [(ones_f[:], s2[:, cs])], reads=[bs2, bconst], writes=[bB[7]])
            K.op(V, lambda: nc.vector.tensor_scalar(out=s1[:, cs], in0=banks[6][:], scalar1=1.0 / nfeat, scalar2=None,
                                                    op0=ALU.mult), reads=[bB[6]], writes=[bs1])
            K.op(V, lambda: nc.vector.tensor_tensor(out=tmp[:, cs], in0=s1[:, cs], in1=s1[:, cs], op=ALU.mult),
                 reads=[bs1], writes=[btmp])
            K.op(V, lambda: nc.vector.scalar_tensor_tensor(out=s2[:, cs], in0=banks[7][:], scalar=1.0 / nfeat,
                                                           in1=tmp[:, cs], op0=ALU.mult, op1=ALU.subtract),
                 reads=[bB[7], btmp], writes=[bs2])
        K.op(A, lambda: nc.scalar.activation(s2[:, 0:ncols], s2[:, 0:ncols], AF.Sqrt, bias=epsc[:, 0:1], scale=1.0),
             reads=[bs2, bconst], writes=[bs2])
        K.op(V, lambda: nc.vector.reciprocal(out=s2[:, 0:ncols], in_=s2[:, 0:ncols]), reads=[bs2], writes=[bs2])

    w_out = din("w_out", [D, D])
    g_cnT = din("g_cnT", [128, 16])
    b_cnT = din("b_cnT", [128, 16])
    ln1_gT = din("ln1_gT", [128, 32])
    ln1_bT = din("ln1_bT", [128, 32])
    zT_d = dscr("zT_d", [D, NOWN], F32)
    stat1_d = dscr("stat1_d", [2, 128, NOWN], F32)
    h2T_d = dscr("h2T_d", [D, NOWN], BF16)
    wov = w_out.rearrange("(kc p) n -> p kc n", p=128)
    sc3 = Scope(K)
    mix = sc3.sb("mix", [128, 32, NOWN], BF16)
    gcn = sc3.sb("gcn", [128, 16], F32)
    bcn = sc3.sb("bcn", [128, 16], F32)
    l1g = sc3.sb("l1g", [128, 32], F32)
    l1b = sc3.sb("l1b", [128, 32], F32)
    a1 = sc3.sb("a1", [128, 32, 2], F32)
    b1 = sc3.sb("b1", [128, 32, 2], F32)
    bmix, bsm3, bab = Buf(), Buf(), Buf()
    for t_, s_ in ((gcn, g_cnT), (bcn, b_cnT), (l1g, ln1_gT), (l1b, ln1_bT)):
        K.dma(S, t_[:], s_, writes=[bsm3])
    K.dma(S, mix[:, 0:16, :], attnT_d.rearrange("(kc p) n -> p kc n", p=128), writes=[bmix])
    K.op(V, lambda: nc.vector.tensor_tensor(out=a1[:], in0=opsc[:, 1, :, :],
                                            in1=l1g[:].unsqueeze(2).to_broadcast([128, 32, 2]), op=ALU.mult),
         reads=[bmod, bsm3], writes=[bab])
    K.op(V, lambda: nc.vector.tensor_tensor(out=b1[:], in0=opsc[:, 1, :, :],
                                            in1=l1b[:].unsqueeze(2).to_broadcast([128, 32, 2]), op=ALU.mult),
         reads=[bmod, bsm3], writes=[bab])
    K.op(V, lambda: nc.vector.tensor_tensor(out=b1[:], in0=b1[:], in1=mod[:, 96:128, :], op=ALU.add),
         reads=[bmod, bab], writes=[bab])
    s3a = Scope(K)
    cm = s3a.sb("cm", [128, NOWN], F32)
    cr = s3a.sb("cr", [128, NOWN], F32)
    cvl = [s3a.sb("cvl%d" % i, [128, NOWN], F32) for i in range(2)]
    cvt = [s3a.sb("cvt%d" % i, [128, NOWN], F32) for i in range(2)]
    bcs = Buf()
    bcvl = [Buf(), Buf()]
    bcvt = [Buf(), Buf()]
    K.dma(S, cm[:], cstat_d[0], writes=[bcs])
    K.dma(S, cr[:], cstat_d[1], writes=[bcs])
    for j in range(16):
        i = j % 2
        K.dma(S, cvl[i][:], conv_d[j * 128:(j + 1) * 128, :], writes=[bcvl[i]])
        K.op(V, lambda: nc.vector.tensor_tensor(out=cvt[i][:], in0=cvl[i][:], in1=cm[:], op=ALU.subtract),
             reads=[bcvl[i], bcs], writes=[bcvt[i]])
        K.op(P, lambda: nc.gpsimd.tensor_tensor(out=cvt[i][:], in0=cvt[i][:], in1=cr[:], op=ALU.mult),
             reads=[bcvt[i], bcs], writes=[bcvt[i]])
        K.op(A, lambda: nc.scalar.activation(mix[:, 16 + j, :], cvt[i][:], AF.Silu, bias=bcn[:, j:j + 1],
                                             scale=gcn[:, j:j + 1]), reads=[bcvt[i], bsm3], writes=[bmix])
    K.barrier()
    s3a.close()
    s3b = Scope(K)
    wo = [s3b.sb("wo%d" % i, [128, 32, 256], BF16) for i in range(2)]
    xs3 = [s3b.sb("xs3%d" % i, [128, NOWN], F32) for i in range(2)]
    zt = [s3b.sb("zt%d" % i, [128, NOWN], F32) for i in range(2)]
    zz = [s3b.sb("zz%d" % i, [128, NOWN], F32) for i in range(2)]
    z1 = s3b.sb("z1", [128, NOWN], F32)
    z2 = s3b.sb("z2", [128, NOWN], F32)
    zq = s3b.sb("zq", [128, NOWN], F32)
    bwo = [Buf() for _ in range(2)]
    bxs3, bzt, bzz = [[Buf(), Buf()] for _ in range(3)]
    bz1, bz2, bzq = Buf(), Buf(), Buf()
    K.op(P, lambda: nc.gpsimd.memset(z1[:], 0.0), writes=[bz1])
    K.op(P, lambda: nc.gpsimd.memset(z2[:], 0.0), writes=[bz2])
    for tl in range(16):
        i = tl % 2
        K.dma(P, wo[i][:], wov[:, :, tl * 256:(tl + 1) * 256], writes=[bwo[i]])
        for ob in range(2):
            db = tl * 2 + ob
            e = db % 2
            K.dma(S, xs3[e][:], xv[:, db, 0:NOWN], writes=[bxs3[e]])
            for t in range(3):
                cs = slice(t * 512, (t + 1) * 512)
                j = 0 if t < 2 else 1
                pb = next_bank(0, 6)
                mm(K, banks[pb][:], [(wo[i][:, kc, ob * 128:(ob + 1) * 128], mix[:, kc, cs]) for kc in range(32)],
                   reads=[bwo[i], bmix], writes=[bB[pb]])
                K.op(A, lambda: nc.scalar.activation(zt[e][:, cs], banks[pb][:], AF.Identity, scale=MOD(2, db, j)),
                     reads=[bB[pb], bmod], writes=[bzt[e]])
                K.op(V, lambda: nc.vector.scalar_tensor_tensor(out=zz[e][:, cs], in0=xs3[e][:, cs], scalar=ALPHA,
                                                               in1=zt[e][:, cs], op0=ALU.mult, op1=ALU.add),
                     reads=[bxs3[e], bzt[e]], writes=[bzz[e]])
            K.dma(S, zT_d[db * 128:(db + 1) * 128, :], zz[e][:], reads=[bzz[e]])
            K.op(P, lambda: nc.gpsimd.tensor_tensor(out=z1[:], in0=z1[:], in1=zz[e][:], op=ALU.add),
                 reads=[bzz[e], bz1], writes=[bz1])
            K.op(A, lambda: nc.scalar.activation(zq[:], zz[e][:], AF.Square), reads=[bzz[e]], writes=[bzq])
            K.op(P, lambda: nc.gpsimd.tensor_tensor(out=z2[:], in0=z2[:], in1=zq[:], op=ALU.add),
                 reads=[bzq, bz2], writes=[bz2])
    ln_stats(z1, bz1, z2, bz2, zq, bzq, 4096.0, NOWN)
    K.dma(S, stat1_d[0], z1[:], reads=[bz1])
    K.dma(S, stat1_d[1], z2[:], reads=[bz2])
    h2s = [s3b.sb("h2s%d" % i, [128, NOWN], BF16) for i in range(2)]
    bh2s = [Buf(), Buf()]
    for db in range(32):
        e = db % 2
        K.dma(S, xs3[e][:], zT_d[db * 128:(db + 1) * 128, :], writes=[bxs3[e]])
        K.op(V, lambda: nc.vector.tensor_tensor(out=zt[e][:], in0=xs3[e][:], in1=z1[:], op=ALU.subtract),
             reads=[bxs3[e], bz1], writes=[bzt[e]])
        K.op(P, lambda: nc.gpsimd.tensor_tensor(out=zt[e][:], in0=zt[e][:], in1=z2[:], op=ALU.mult),
             reads=[bzt[e], bz2], writes=[bzt[e]])
        for (lo, hi, j) in ((0, NP_, 0), (NP_, NOWN, 1)):
            K.op(A, lambda: nc.scalar.activation(h2s[e][:, lo:hi], zt[e][:, lo:hi], AF.Identity,
                                                 bias=b1[:, db, j:j + 1], scale=a1[:, db, j:j + 1]),
                 reads=[bzt[e], bab], writes=[bh2s[e]])
        K.dma(S, h2T_d[db * 128:(db + 1) * 128, :], h2s[e][:], reads=[bh2s[e]])
    K.barrier()
    s3b.close()
    sc3.close()
    if stop_after <= 4:
        K.finish()
        return nc, ins_used

    w_pq = din("w_pq", [D, D])
    skT = din("skT", [128, 32, 128])
    s_d = dscr("s_d", [8, NOWN, 256], F32)
    G_d = dscr("G_d", [128, 128, NOWN], BF16)
    wpv = w_pq.rearrange("(kc p) n -> p kc n", p=128)
    sc4 = Scope(K)
    h2 = sc4.sb("h2", [128, 32, NOWN], BF16)
    sk = sc4.sb("sk", [128, 32, 128], BF16)
    wp = [sc4.sb("wp%d" % i, [128, 32, 256], BF16) for i in range(3)]
    qhp = [sc4.sb("qhp%d" % i, [128, 2, NOWN], BF16) for i in range(2)]
    sst = [sc4.sb("sst%d" % i, [128, 12, 128], F32) for i in range(2)]
    bh2, bsk = Buf(), Buf()
    bwp = [Buf() for _ in range(3)]
    bqhp, bsst = [[Buf(), Buf()] for _ in range(2)]
    h2v = h2T_d.rearrange("(kc p) n -> p kc n", p=128)
    for q4 in range(4):
        K.dma(S, h2[:, q4 * 8:(q4 + 1) * 8, :], h2v[:, q4 * 8:(q4 + 1) * 8, :], writes=[bh2])
    K.dma(P, sk[:], skT, writes=[bsk])
    s_dv = s_d.rearrange("h (nb p) (t k) -> h t p nb k", p=128, k=128)
    cpy = [0]

    def evac(out, in_, reads, writes):
        cpy[0] += 1
        if cpy[0] % 2:
            return K.op(A, lambda: nc.scalar.copy(out, in_), reads=reads, writes=writes)
        return K.op(V, lambda: nc.vector.tensor_copy(out=out, in_=in_), reads=reads, writes=writes)

    for hp_ in range(16):
        i = hp_ % 3
        e = hp_ % 2
        K.dma(P, wp[i][:], wpv[:, :, hp_ * 256:(hp_ + 1) * 256], writes=[bwp[i]])
        for half in range(2):
            for t in range(3):
                cs = slice(t * 512, (t + 1) * 512)
                pb = next_bank(0, 4)
                mm(K, banks[pb][:], [(wp[i][:, kc, half * 128:(half + 1) * 128], h2[:, kc, cs]) for kc in range(32)],
                   reads=[bwp[i], bh2], writes=[bB[pb]])
                evac(qhp[e][:, half, cs], banks[pb][:], [bB[pb]], [bqhp[e]])
        for g in range(3):
            pb = 4 + (hp_ * 3 + g) % 4
            for kk in range(4):
                nb = g * 4 + kk
                mm(K, banks[pb][:, kk * 128:(kk + 1) * 128],
                   [(qhp[e][:, half, nb * 128:(nb + 1) * 128], sk[:, hp_ * 2 + half, :]) for half in range(2)],
                   reads=[bqhp[e], bsk], writes=[bB[pb]])
            evac(sst[e][:, g * 4:(g + 1) * 4, :], banks[pb][:].rearrange("p (a b) -> p a b", b=128), [bB[pb]], [bsst[e]])
        K.dma(S, s_dv[hp_ // 2, hp_ % 2], sst[e][:], reads=[bsst[e]])
    K.barrier()
    sc4.close()
    if stop_after <= 5:
        K.finish()
        return nc, ins_used

    scr = Scope(K)
    stm = [scr.sb("stm%d" % i, [128, 2048], F32) for i in range(2)]
    wrk = scr.sb("wrk", [128, 2048], F32)
    tp = scr.sb("tp", [128, 16, 16], F32)
    cand = scr.sb("cand", [128, 8, 256], F32)
    ctop = scr.sb("ctop", [128, 8, 16], F32)
    ce = scr.sb("ce", [128, 8, 16], F32)
    sm = scr.sb("sm", [128, 64], F32)
    At = scr.sb("At", [128, 3, 128], F32)
    AT = [scr.sb("AT%d" % i, [128, 3, 128], F32) for i in range(2)]
    srep = [scr.sb("srep%d" % i, [128, 16, 256], F32) for i in range(3)]
    zr = [scr.sb("zr%d" % i, [128, 16, 128], F32) for i in range(2)]
    er = [scr.sb("er%d" % i, [128, 16, 128], BF16) for i in range(2)]
    mk = [scr.sb("mk%d" % i, [128, 16, 128], BF16) for i in range(2)]
    Rb = [scr.sb("Rb%d" % i, [128, 16, 128], BF16) for i in range(2)]
    P1b = [scr.sb("P1b%d" % i, [128, 16, 128], BF16) for i in range(2)]
    gst = [scr.sb("gst%d" % i, [128, 128, 128], BF16) for i in range(2)]
    bstm, bAT, bsrep, bzr, bzc, ber, bmk, bRb, bP1b, bgst = [[Buf(), Buf(), Buf()] for _ in range(10)]
    bwrk, btp, bcand, bctop, bce, bsmm, bAt = [Buf() for _ in range(7)]
    tpv = tp[:].rearrange("p (h t) a -> p h t a", t=2)
    G_dv = G_d.rearrange("i j n -> j i n")

    bc816 = lambda ap: ap.unsqueeze(2).to_broadcast([128, 8, 16])
    btpg = [Buf() for _ in range(16)]
    bwkg = [Buf() for _ in range(16)]
    bctg = [Buf() for _ in range(8)]
    bcdg = [Buf() for _ in range(8)]

    def top16_batch(dsts, srcs, width, rds, wrs, wks, bwk):
        n = len(dsts)
        for i in range(n):
            K.op(V, lambda: nc.vector.max(out=dsts[i][:, 0:8], in_=srcs[i]), reads=rds[i], writes=[wrs[i]])
        for i in range(n):
            K.op(V, lambda: nc.vector.match_replace(out=wks[i], in_to_replace=dsts[i][:, 0:8], in_values=srcs[i],
                                                    imm_value=-1e30), reads=rds[i] + [wrs[i]], writes=[bwk[i]])
        for i in range(n):
            K.op(V, lambda: nc.vector.max(out=dsts[i][:, 8:16], in_=wks[i]), reads=[bwk[i]], writes=[wrs[i]])

    def topk_stage(nb):
        e = nb % 2
        K.dma(S, stm[e][:].rearrange("p (h c) -> p h c", c=256), s_d[:, nb * 128:(nb + 1) * 128, :].rearrange("h n c -> n h c"), writes=[bstm[e]])
        for q4 in range(4):
            hs = range(q4 * 4, q4 * 4 + 4)
            top16_batch([tp[:, i, :] for i in hs], [stm[e][:, i * 128:(i + 1) * 128] for i in hs], 128,
                        [[bstm[e]] for i in hs], [btpg[i] for i in hs],
                        [wrk[:, i * 128:(i + 1) * 128] for i in hs], [bwkg[i] for i in hs])
            yield
        for h in range(8):
            K.op(V, lambda: nc.vector.tensor_tensor(
                out=cand[:, h, :].rearrange("p (a b) -> p a b", b=16),
                in0=tpv[:, h, 0, :].unsqueeze(2).to_broadcast([128, 16, 16]),
                in1=tpv[:, h, 1, :].unsqueeze(1).to_broadcast([128, 16, 16]), op=ALU.add),
                reads=[btpg[2 * h], btpg[2 * h + 1]], writes=[bcdg[h]])
        yield
        for q2 in range(2):
            hs = range(q2 * 4, q2 * 4 + 4)
            top16_batch([ctop[:, i, :] for i in hs], [cand[:, i, :] for i in hs], 256,
                        [[bcdg[i]] for i in hs], [bctg[i] for i in hs],
                        [wrk[:, i * 256:(i + 1) * 256] for i in hs], [bwkg[2 * i] for i in hs])
            yield
        btp = btpg
        bctop = bctg
        K.op(V, lambda: nc.vector.tensor_reduce(out=sm[:, 0:8], in_=ctop[:], axis=AX.X, op=ALU.max),
             reads=bctop, writes=[bsmm])
        K.op(V, lambda: nc.vector.tensor_reduce(out=sm[:, 8:16], in_=ctop[:], axis=AX.X, op=ALU.min),
             reads=bctop, writes=[bsmm])
        K.op(V, lambda: nc.vector.tensor_tensor(out=ce[:], in0=ctop[:], in1=bc816(sm[:, 0:8]), op=ALU.subtract),
             reads=bctop + [bsmm], writes=[bce])
        K.op(A, lambda: nc.scalar.activation(ce[:], ce[:], AF.Exp), reads=[bce], writes=[bce])
        K.op(V, lambda: nc.vector.tensor_reduce(out=sm[:, 16:24], in_=ce[:], axis=AX.X, op=ALU.add),
             reads=[bce], writes=[bsmm])
        K.op(A, lambda: nc.scalar.activation(sm[:, 24:32], sm[:, 16:24], AF.Ln), reads=[bsmm], writes=[bsmm])
        K.op(V, lambda: nc.vector.tensor_tensor(out=sm[:, 32:40], in0=sm[:, 0:8], in1=sm[:, 24:32], op=ALU.add),
             reads=[bsmm], writes=[bsmm])
        K.op(V, lambda: nc.vector.tensor_copy(out=At[:, 0, :].rearrange("p (a h) -> p h a", h=8), in_=bc816(sm[:, 8:16])),
             reads=[bsmm], writes=[bAt])
        K.op(V, lambda: nc.vector.tensor_tensor(out=At[:, 1, :].rearrange("p (a h) -> p h a", h=8), in0=tpv[:, :, 0, :],
                                                in1=bc816(sm[:, 32:40]), op=ALU.subtract),
             reads=btp + [bsmm], writes=[bAt])
        K.op(V, lambda: nc.vector.tensor_copy(out=At[:, 2, :].rearrange("p (a h) -> p h a", h=8), in_=tpv[:, :, 0, :]),
             reads=btp, writes=[bAt])
        pbt = nb % 2
        for q3 in range(3):
            K.op(K.pe, lambda: nc.tensor.transpose(banks[pbt][:, q3 * 128:(q3 + 1) * 128], At[:, q3, :], ident_f[:]),
                 reads=[bAt, bconst], writes=[bB[pbt]])
        K.op(V, lambda: nc.vector.tensor_copy(out=AT[e][:], in_=banks[pbt][:, 0:384].rearrange("p (a b) -> p a b", b=128)),
             reads=[bB[pbt]], writes=[bAT[e]])
        yield

    def bcn(ap):
        return ap.unsqueeze(2).to_broadcast([128, 16, 128])

    def sub_block(nb, sb):
        e = nb % 2
        f = (nb * 8 + sb) % 2
        f3 = (nb * 8 + sb) % 3
        ns = slice(sb * 16, sb * 16 + 16)
        row0 = nb * 128 + sb * 16
        src = bass.AP(s_d.tensor, row0 * 256, [[0, 16], [NOWN * 256, 8], [1, 4096]])
        K.dma(S, srep[f3][:].rearrange("p a b -> p (a b)"), src, writes=[bsrep[f3]])
        K.op(V, lambda: nc.vector.tensor_tensor(out=zr[f][:], in0=srep[f3][:, :, 128:256], in1=bcn(AT[e][:, 2, ns]),
                                                op=ALU.add), reads=[bsrep[f3], bAT[e]], writes=[bzr[f]])
        for tk in range(16):
            n_ = sb * 16 + tk
            K.op(A, lambda: nc.scalar.activation(er[f][:, tk, :], srep[f3][:, tk, 128:256], AF.Exp,
                                                 bias=AT[e][:, 1, n_:n_ + 1]),
                 reads=[bsrep[f3], bAT[e]], writes=[ber[f]])
        K.op(V, lambda: nc.vector.tensor_tensor(out=mk[f][:], in0=zr[f][:], in1=bcn(AT[e][:, 0, ns]), op=ALU.is_ge),
             reads=[bzr[f], bAT[e]], writes=[bmk[f]])
        K.op(V, lambda: nc.vector.tensor_tensor(out=P1b[f][:], in0=srep[f3][:, :, 0:128], in1=bcn(AT[e][:, 2, ns]),
                                                op=ALU.is_equal), reads=[bsrep[f3], bAT[e]], writes=[bP1b[f]])
        K.op(V, lambda: nc.vector.tensor_tensor(out=Rb[f][:], in0=mk[f][:], in1=er[f][:], op=ALU.mult),
             reads=[bmk[f], ber[f]], writes=[bRb[f]])
        for q4 in range(4):
            pb = 2 + (sb * 4 + q4) % 6
            for tk in range(4):
                t16 = q4 * 4 + tk
                mm(K, banks[pb][:, tk * 128:(tk + 1) * 128], [(Rb[f][:, t16, :], P1b[f][:, t16, :])],
                   reads=[bRb[f], bP1b[f]], writes=[bB[pb]])
            n0 = sb * 16 + q4 * 4
            K.op(A, lambda: nc.scalar.copy(gst[e][:, :, n0:n0 + 4], banks[pb][:].rearrange("p (n i) -> p i n", n=4)),
                 reads=[bB[pb]], writes=[bgst[e]])

    for _ in topk_stage(0):
        pass
    for nb in range(12):
        nxt = topk_stage(nb + 1) if nb + 1 < 12 else iter(())
        for sb in range(8):
            sub_block(nb, sb)
            next(nxt, None)
        for _ in nxt:
            pass
        K.dma(S, G_dv[:, :, nb * 128:(nb + 1) * 128], gst[nb % 2][:], reads=[bgst[nb % 2]])
    K.barrier()
    scr.close()
    if stop_after <= 6:
        K.finish()
        return nc, ins_used

    peer_uT = din("peer_uT", [128, 128, 4096])
    peer_v = din("peer_v", [16384, D])
    ln2_gT = din("ln2_gT", [128, 32])
    ln2_bT = din("ln2_bT", [128, 32])
    o_yT = dout("o_yT", [D, NOWN])
    pvv = peer_v.rearrange("(g a p) d -> g p a d", a=4, p=128)
    sc5 = Scope(K)
    h2p = sc5.sb("h2p", [128, 32, 512], BF16)
    acc = sc5.sb("acc", [128, 32, 512], F32)
    l1g5 = sc5.sb("l1g5", [128, 32], F32)
    l1b5 = sc5.sb("l1b5", [128, 32], F32)
    l2g = sc5.sb("l2g", [128, 32], F32)
    l2b = sc5.sb("l2b", [128, 32], F32)
    bh2p, bsm5 = Buf(), Buf()
    bacc = [Buf() for _ in range(32)]
    for t_, s_ in ((l1g5, ln1_gT), (l1b5, ln1_bT), (l2g, ln2_gT), (l2b, ln2_bT)):
        K.dma(S, t_[:], s_, writes=[bsm5])
    _P5P = int(os.environ.get('P5_PASSES', 3)); _P5G = int(os.environ.get('P5_GROUPS', 32)); _P5E = int(os.environ.get('P5_EPI', 1))
    for pt_ in range(_P5P):
        c0 = pt_ * 512
        cj = 0 if pt_ < 2 else 1
        pcs = slice(c0, c0 + 512)
        K.dma(S, h2p[:], h2v[:, :, pcs], writes=[bh2p])
        for db in range(32):
            K.op(V, lambda: nc.vector.memset(acc[:, db, :], 0.0), writes=[bacc[db]])
        s5a = Scope(K)
        ut = [s5a.sb("ut%d" % i, [128, 32, 128], BF16) for i in range(3)]
        vt = [s5a.sb("vt%d" % i, [128, 4, D], BF16) for i in range(2)]
        gt = [s5a.sb("gt%d" % i, [128, 512], BF16) for i in range(3)]
        ga32 = [s5a.sb("ga32%d" % i, [128, 512], F32) for i in range(2)]
        gab = [s5a.sb("gab%d" % i, [128, 4, 512], BF16) for i in range(2)]
        but, bvt, bgt, bga32, bgab = [[Buf(), Buf(), Buf()] for _ in range(5)]

        def phaseB(g):
            gi = g % 2
            for db in range(32):
                pb = 2 + db % 6
                mm(K, banks[pb][:], [(vt[gi][:, a, db * 128:(db + 1) * 128], gab[gi][:, a, :]) for a in range(4)],
                   reads=[bvt[gi], bgab[gi]], writes=[bB[pb]])
                K.op(V, lambda: nc.vector.tensor_tensor(out=acc[:, db, :], in0=acc[:, db, :], in1=banks[pb][:], op=ALU.add),
                     reads=[bB[pb], bacc[db]], writes=[bacc[db]])

        for g in range(_P5G):
            gi = g % 2
            for a in range(4):
                eb = g * 4 + a
                ei = eb % 2
                u3 = eb % 3
                K.dma(P, ut[u3][:].rearrange("p a b -> p (a b)"), peer_uT[eb], writes=[but[u3]])
                K.dma(S, gt[u3][:], G_d[eb, :, pcs], writes=[bgt[u3]])
                mm(K, banks[ei][:], [(ut[u3][:, kc, :], h2p[:, kc, :]) for kc in range(32)],
                   reads=[but[u3], bh2p], writes=[bB[ei]])
                K.op(A, lambda: nc.scalar.activation(ga32[ei][:], banks[ei][:], AF.Gelu), reads=[bB[ei]], writes=[bga32[ei]])
                K.op(V, lambda: nc.vector.tensor_tensor(out=gab[gi][:, a, :], in0=ga32[ei][:], in1=gt[u3][:], op=ALU.mult),
                     reads=[bga32[ei], bgt[u3]], writes=[bgab[gi]])
            K.dma(P, vt[gi][:], pvv[g], writes=[bvt[gi]])
            if g > 0:
                phaseB(g - 1)
        phaseB(_P5G - 1)
        K.barrier()
        s5a.close()
        s5b = Scope(K)
        m1 = s5b.sb("m1", [128, 512], F32)
        r1 = s5b.sb("r1", [128, 512], F32)
        y1 = s5b.sb("y1", [128, 512], F32)
        y2 = s5b.sb("y2", [128, 512], F32)
        yq = s5b.sb("yq", [128, 512], F32)
        y1w = s5b.sb("y1w", [128, 4, 512], F32)
        y2w = s5b.sb("y2w", [128, 4, 512], F32)
        yq4 = s5b.sb("yq4", [128, 4, 512], F32)
        zl = [s5b.sb("zl%d" % i, [128, 4, 512], F32) for i in range(2)]
        yst = [s5b.sb("yst%d" % i, [128, 4, 512], F32) for i in range(2)]
        bst1, by1, by2, byq, by1w, by2w, byq4 = [Buf() for _ in range(7)]
        bzl, byst = [[Buf(), Buf()] for _ in range(2)]
        zTv = zT_d.rearrange("(kc p) n -> p kc n", p=128)
        oyv = o_yT.rearrange("(kc p) n -> p kc n", p=128)
        b4 = lambda ap: ap.unsqueeze(1).to_broadcast([128, 4, 512])
        K.dma(S, m1[:], stat1_d[0][:, pcs], writes=[bst1])
        K.dma(S, r1[:], stat1_d[1][:, pcs], writes=[bst1])
        K.op(P, lambda: nc.gpsimd.memset(y1w[:], 0.0), writes=[by1w])
        K.op(P, lambda: nc.gpsimd.memset(y2w[:], 0.0), writes=[by2w])
        for c4 in range(8 if _P5E else 0):
            e = c4 % 2
            dbs = range(c4 * 4, c4 * 4 + 4)
            ba4 = [bacc[db] for db in dbs]
            a4 = acc[:, c4 * 4:c4 * 4 + 4, :]
            K.dma(S, zl[e][:], zTv[:, c4 * 4:c4 * 4 + 4, pcs], writes=[bzl[e]])
            K.op(V, lambda: nc.vector.tensor_tensor(out=zl[e][:], in0=zl[e][:], in1=b4(m1[:]), op=ALU.subtract),
                 reads=[bzl[e], bst1], writes=[bzl[e]])
            K.op(P, lambda: nc.gpsimd.tensor_tensor(out=zl[e][:], in0=zl[e][:], in1=b4(r1[:]), op=ALU.mult),
                 reads=[bzl[e], bst1], writes=[bzl[e]])
            for q, db in enumerate(dbs):
                K.op(A, lambda: nc.scalar.activation(zl[e][:, q, :], zl[e][:, q, :], AF.Identity, bias=l1b5[:, db:db + 1],
                                                     scale=l1g5[:, db:db + 1]), reads=[bzl[e], bsm5], writes=[bzl[e]])
                K.op(A, lambda: nc.scalar.activation(acc[:, db, :], acc[:, db, :], AF.Identity, scale=MOD(5, db, cj)),
                     reads=[bacc[db], bmod], writes=[bacc[db]])
            K.op(V, lambda: nc.vector.scalar_tensor_tensor(out=a4, in0=zl[e][:], scalar=ALPHA, in1=a4,
                                                           op0=ALU.mult, op1=ALU.add),
                 reads=[bzl[e]] + ba4, writes=ba4)
            K.op(P, lambda: nc.gpsimd.tensor_tensor(out=y1w[:], in0=y1w[:], in1=a4, op=ALU.add),
                 reads=ba4 + [by1w], writes=[by1w])
            K.op(A, lambda: nc.scalar.activation(yq4[:], a4, AF.Square), reads=ba4, writes=[byq4])
            K.op(P, lambda: nc.gpsimd.tensor_tensor(out=y2w[:], in0=y2w[:], in1=yq4[:], op=ALU.add),
                 reads=[byq4, by2w], writes=[by2w])
        for (yw, byw, y_, by_) in ((y1w, by1w, y1, by1), (y2w, by2w, y2, by2)):
            K.op(V, lambda: nc.vector.tensor_tensor(out=y_[:], in0=yw[:, 0, :], in1=yw[:, 1, :], op=ALU.add),
                 reads=[byw], writes=[by_])
            K.op(V, lambda: nc.vector.tensor_tensor(out=y_[:], in0=y_[:], in1=yw[:, 2, :], op=ALU.add),
                 reads=[byw, by_], writes=[by_])
            K.op(V, lambda: nc.vector.tensor_tensor(out=y_[:], in0=y_[:], in1=yw[:, 3, :], op=ALU.add),
                 reads=[byw, by_], writes=[by_])
        ln_stats(y1, by1, y2, by2, yq, byq, 4096.0, 512)
        for c4 in range(8):
            e = c4 % 2
            dbs = range(c4 * 4, c4 * 4 + 4)
            ba4 = [bacc[db] for db in dbs]
            a4 = acc[:, c4 * 4:c4 * 4 + 4, :]
            K.op(V, lambda: nc.vector.tensor_tensor(out=yst[e][:], in0=a4, in1=b4(y1[:]), op=ALU.subtract),
                 reads=ba4 + [by1], writes=[byst[e]])
            K.op(P, lambda: nc.gpsimd.tensor_tensor(out=yst[e][:], in0=yst[e][:], in1=b4(y2[:]), op=ALU.mult),
                 reads=[byst[e], by2], writes=[byst[e]])
            for q, db in enumerate(dbs):
                K.op(A, lambda: nc.scalar.activation(yst[e][:, q, :], yst[e][:, q, :], AF.Identity, bias=l2b[:, db:db + 1],
                                                     scale=l2g[:, db:db + 1]), reads=[byst[e], bsm5], writes=[byst[e]])
            K.dma(S, oyv[:, c4 * 4:c4 * 4 + 4, pcs], yst[e][:], reads=[byst[e]], final=True)
        K.barrier()
        s5b.close()
    sc5.close()

    K.finish()
    return nc, ins_used


def _fm(v, nchunk):
    return np.ascontiguousarray(np.asarray(v, np.float32).reshape(nchunk, 128).T)


def _rope_tables(pos):
    row = (pos // 64).astype(np.float32)
    col = (pos % 64).astype(np.float32)
    inv = (10000.0 ** (-np.arange(16, dtype=np.float32) / 16)).astype(np.float32)
    ar = row[:, None] * inv
    ac = col[:, None] * inv
    ang = np.concatenate([ar, ar, ac, ac], -1).astype(np.float32)
    return np.ascontiguousarray(np.cos(ang).T.astype(np.float32)), np.ascontiguousarray(np.sin(ang).T.astype(np.float32))


def _rmat():
    R = np.zeros((64, 64), np.float32)
    for base in (0, 32):
        for i in range(16):
            R[base + i, base + 16 + i] = -1.0
            R[base + 16 + i, base + i] = 1.0
    return np.ascontiguousarray(R.T)


def sample_cols(hf):
    own = np.arange(hf * 512, hf * 512 + 512)
    if hf == 0:
        other = np.arange(512, 1024)
    else:
        other = np.concatenate([np.arange(497, 512), np.arange(0, 497)])
    return own, other


def prep_shared(inp):
    f = lambda k: np.asarray(inp[k], np.float32)
    sh = {}
    sh["w_ada"] = np.ascontiguousarray(f("w_ada")[0])
    sh["b_adaT"] = _fm(f("b_ada")[0], 192)
    sh["w_in"] = np.ascontiguousarray(f("w_in")[0])
    sh["g_qT"] = _fm(f("g_q")[0], 6)
    sh["g_kvT"] = _fm(f("g_kv")[0], 4)
    sh["w_uq"] = np.ascontiguousarray(f("w_uq")[0])
    sh["w_ukv"] = np.ascontiguousarray(f("w_ukv")[0])
    sh["w_dwT"] = np.ascontiguousarray(f("w_dw")[0].T.reshape(16, 128, 31).transpose(1, 0, 2))
    sh["b_dwT"] = _fm(f("b_dw")[0], 16)
    sh["g_cnT"] = _fm(f("g_cn")[0], 16)
    sh["b_cnT"] = _fm(f("b_cn")[0], 16)
    sh["w_out"] = np.ascontiguousarray(f("w_out")[0])
    sh["ln1_gT"] = _fm(f("ln1_g")[0], 32)
    sh["ln1_bT"] = _fm(f("ln1_b")[0], 32)
    sh["w_pq"] = np.ascontiguousarray(f("w_pq")[0])
    sk = f("sub_keys")[0]
    skT = sk.reshape(16, 128, 2, 128).transpose(3, 0, 2, 1)
    sh["skT"] = np.ascontiguousarray(skT.reshape(128, 32, 128))
    pu = f("peer_u")[0]
    sh["peer_uT"] = np.ascontiguousarray(pu.reshape(128, 128, 32, 128).transpose(0, 3, 2, 1)).reshape(128, 128, 4096)
    sh["peer_v"] = np.ascontiguousarray(f("peer_v")[0])
    sh["ln2_gT"] = _fm(f("ln2_g")[0], 32)
    sh["ln2_bT"] = _fm(f("ln2_b")[0], 32)
    sh["rmatT"] = _rmat()
    return sh


def prep_core(inp, c):
    f = lambda k: np.asarray(inp[k], np.float32)
    b, hf = c // 2, c % 2
    own, other = sample_cols(hf)
    xp = f("x_prompt")[4 * c:4 * c + 4].reshape(NP_, D)
    xs = f("x_sample")[b]
    xall = np.concatenate([xp, xs[own], xs[other]], 0)
    m = {}
    m["xT"] = np.ascontiguousarray(xall.T)
    cond = np.stack([f("c_ctx"), f("c")[b]], -1)
    m["condT"] = np.ascontiguousarray(cond.reshape(32, 128, 2).transpose(1, 0, 2))
    m["cache_ckvT"] = np.ascontiguousarray(f("cache_ckv")[b, 0].T.reshape(4, 128, 256).transpose(1, 0, 2))
    m["cache_kpeT"] = np.ascontiguousarray(f("cache_kpe")[b, 0].T)
    cosT, sinT = _rope_tables(np.concatenate([own, other]))
    m["cosT"] = cosT
    m["sinT"] = sinT
    hm = np.zeros((128, 2), np.float32)
    hm[:, 0] = 1.0 if hf == 1 else 0.0
    hm[:, 1] = 1.0 if hf == 0 else 0.0
    m["halo_mask"] = hm
    return m


_CACHE = {}


def kernel(**inputs):
    if "nc" not in _CACHE:
        _CACHE["nc"] = build()
    nc, used = _CACHE["nc"]
    sh = prep_shared(inputs)
    in_maps = []
    for c in range(8):
        m = prep_core(inputs, c)
        m.update(sh)
        in_maps.append({k: m[k] for k in used})
    res = run_bass_kernel_spmd(nc, in_maps, core_ids=list(range(8)))
    y_prompt = np.zeros((32, 256, D), np.float32)
    y_sample = np.zeros((4, 1024, D), np.float32)
    new_ckv = np.zeros((32, 1, 256, 512), np.float32)
    new_kpe = np.zeros((32, 1, 256, 64), np.float32)
    for c in range(8):
        r = res.results[c]
        b, hf = c // 2, c % 2
        yT = np.asarray(r["o_yT"], np.float32)
        y_prompt[4 * c:4 * c + 4] = yT[:, :NP_].T.reshape(4, 256, D)
        y_sample[b, hf * 512:(hf + 1) * 512] = yT[:, NP_:].T
        new_ckv[4 * c:4 * c + 4, 0] = np.asarray(r["o_ckvT"], np.float32).T.reshape(4, 256, 512)
        new_kpe[4 * c:4 * c + 4, 0] = np.asarray(r["o_kpeT"], np.float32).T.reshape(4, 256, 64)
    return (y_prompt, y_sample, new_ckv, new_kpe)
```

```python
import os
import numpy as np
import ml_dtypes
from contextlib import ExitStack
import concourse.bass as bass
import concourse.mybir as mybir
from concourse.bass_utils import run_bass_kernel_spmd

F32 = mybir.dt.float32
BF16 = mybir.dt.bfloat16
AF = mybir.ActivationFunctionType
ALU = mybir.AluOpType
AX = mybir.AxisListType

D = 4096
NP_ = 1024
NS = 512
NOWN = NP_ + NS
NALL = 2048
NKEY = 2304
ALPHA = 2.0 ** 0.25
EPS = 1e-6
SCALE = 192.0 ** -0.5
YW = 4 * 286 + 542


class Buf:
    __slots__ = ("w", "r")

    def __init__(self):
        self.w = None
        self.r = {}


class Q:
    def __init__(self, K, name, eng, is_pe=False):
        self.name = name
        self.eng = eng
        self.is_pe = is_pe
        self.sem = K.newsem("q_" + name)
        self.cnt = 0
        self.waited = {}
        self.dsems = []
        self.dvals = []
        self.dma_i = 0
        self.lazy = False

    def wait(self, tok):
        sem, val, owner = tok
        key = id(sem)
        if self.waited.get(key, 0) >= val:
            return
        self.eng.wait_ge(sem, val)
        self.waited[key] = val


class Kern:
    NDMA = 10

    def __init__(self, nc):
        self.nc = nc
        self.stack = ExitStack()
        self.pe = Q(self, "pe", nc.tensor, is_pe=True)
        self.act = Q(self, "act", nc.scalar)
        self.dve = Q(self, "dve", nc.vector)
        self.pool = Q(self, "pool", nc.gpsimd)
        self.sp = Q(self, "sp", nc.sync)
        self.qs = [self.pe, self.act, self.dve, self.pool, self.sp]
        for q in (self.sp, self.pool):
            for i in range(self.NDMA):
                q.dsems.append(self.newsem("d_%s%d" % (q.name, i)))
                q.dvals.append(0)
        self.final = []
        self.uid = 0

    def newsem(self, name):
        return self.stack.enter_context(self.nc.semaphore(name))

    def _deps(self, q, reads, writes):
        for b in reads:
            if b.w is not None and not (b.w[2] is q and q.is_pe):
                q.wait(b.w)
        for b in writes:
            if b.w is not None and not (b.w[2] is q and q.is_pe):
                q.wait(b.w)
            for owner, t in b.r.items():
                if owner is not q or not q.is_pe:
                    q.wait(t)

    def _mark(self, tok, reads, writes):
        for b in reads:
            b.r[tok[2]] = tok
        for b in writes:
            b.w = tok
            b.r = {}

    def op(self, q, fn, reads=(), writes=(), inc=True):
        self._deps(q, reads, writes)
        ins = fn()
        if inc:
            q.cnt += 1
            ins.then_inc(q.sem, 1)
            q.lazy = False
            tok = (q.sem, q.cnt, q)
        else:
            q.lazy = True
            tok = (q.sem, q.cnt + 1, q)
        self._mark(tok, reads, writes)
        return tok

    def dma(self, q, out, in_, reads=(), writes=(), final=False):
        slot = q.dma_i % self.NDMA
        q.dma_i += 1
        sem = q.dsems[slot]
        if q.dvals[slot] > 0:
            q.wait((sem, q.dvals[slot], sem))
        self._deps(q, reads, writes)
        ins = q.eng.dma_start(out=out, in_=in_)
        q.dvals[slot] += 16
        ins.then_inc(sem, 16)
        tok = (sem, q.dvals[slot], sem)
        self._mark(tok, reads, writes)
        if final:
            self.final.append(tok)
        return tok

    def barrier(self):
        toks = []
        for q in self.qs:
            assert not q.lazy, q.name
            if q.cnt > 0:
                toks.append((q.sem, q.cnt, q))
            for s, v in zip(q.dsems, q.dvals):
                if v > 0:
                    toks.append((s, v, s))
        for q in self.qs:
            for t in toks:
                if t[2] is not q:
                    q.wait(t)

    def name(self, base):
        self.uid += 1
        return "%s_%d" % (base, self.uid)

    def finish(self):
        self.barrier()


class Scope:
    def __init__(self, K):
        self.K = K
        self.stack = ExitStack()

    def sb(self, name, shape, dtype):
        return self.stack.enter_context(self.K.nc.sbuf_tensor(self.K.name(name), list(shape), dtype))

    def close(self):
        self.stack.close()


def mm(K, out, pairs, reads, writes, first=True, last=True):
    nc = K.nc
    n = len(pairs)
    tok = None
    for i, (l, r) in enumerate(pairs):
        st = first and i == 0
        sp = last and i == n - 1
        tok = K.op(K.pe, lambda: nc.tensor.matmul(out, l, r, start=st, stop=sp),
                   reads=reads, writes=writes, inc=(i == n - 1))
    return tok


def build(dbg=False, stop_after=99):
    nc = bass.Bass("TRN2", target_bir_lowering=False)
    K = Kern(nc)
    V, A, P, S = K.dve, K.act, K.pool, K.sp
    ins_used = []

    def din(name, shape, dt=F32):
        ins_used.append(name)
        return nc.dram_tensor(name, list(shape), dt, kind="ExternalInput").ap()

    def dout(name, shape, dt=F32):
        return nc.dram_tensor(name, list(shape), dt, kind="ExternalOutput").ap()

    def dscr(name, shape, dt):
        return nc.dram_tensor(name, list(shape), dt, kind=("ExternalOutput" if dbg else "Internal")).ap()

    top = Scope(K)
    banks = [K.stack.enter_context(nc.psum_tensor("bank%d" % i, [128, 512], F32)) for i in range(8)]
    bB = [Buf() for _ in range(8)]
    ident_f = top.sb("ident_f", [128, 128], F32)
    ident_b = top.sb("ident_b", [128, 128], BF16)
    ones_f = top.sb("ones_f", [128, 128], F32)
    mod = top.sb("mod", [128, 192, 2], F32)
    opsc = top.sb("opsc", [128, 2, 32, 2], F32)
    bconst = Buf()
    bmod = Buf()

    K.op(P, lambda: nc.gpsimd.memset(ident_f[:], 0.0), writes=[bconst])
    K.op(P, lambda: nc.gpsimd.affine_select(out=ident_f[:], in_=ident_f[:], pattern=[[-1, 128]],
                                            compare_op=ALU.not_equal, fill=1.0, base=0, channel_multiplier=1),
         reads=[bconst], writes=[bconst])
    K.op(P, lambda: nc.gpsimd.tensor_copy(out=ident_b[:], in_=ident_f[:]), reads=[bconst], writes=[bconst])
    K.op(P, lambda: nc.gpsimd.memset(ones_f[:], 1.0), writes=[bconst])
    epsc = top.sb("epsc", [128, 1], F32)
    K.op(P, lambda: nc.gpsimd.memset(epsc[:], EPS), writes=[bconst])

    def MOD(s, kc, j):
        return mod[:, s * 32 + kc, j:j + 1]

    def OPSC(w, kc, j):
        return opsc[:, w, kc, j:j + 1]

    condT = din("condT", [128, 32, 2])
    w_ada = din("w_ada", [D, 6 * D])
    b_adaT = din("b_adaT", [128, 192])
    sc0 = Scope(K)
    cnd = top.sb("cnd", [128, 32, 2], F32)
    sil = top.sb("sil", [128, 32, 2], BF16)
    bad = top.sb("bad", [128, 192], F32)
    bc = Buf()
    K.dma(S, cnd[:], condT, writes=[bc])
    K.dma(S, bad[:], b_adaT, writes=[bc])
    K.op(A, lambda: nc.scalar.activation(sil[:], cnd[:], AF.Silu), reads=[bc], writes=[bc])
    wav = w_ada.rearrange("(kc p) n -> p kc n", p=128)
    wr = [sc0.sb("wada%d" % i, [128, 32, 512], BF16) for i in range(3)]
    bwr = [Buf() for _ in range(3)]
    for ct in range(16):
        i = ct % 3
        K.dma(P, wr[i][:], wav[:, :, ct * 512:(ct + 1) * 512], writes=[bwr[i]])
        pb = ct % 2
        for ob in range(4):
            mm(K, banks[pb][:, ob * 2:ob * 2 + 2],
               [(wr[i][:, kc, ob * 128:(ob + 1) * 128], sil[:, kc, :]) for kc in range(32)],
               reads=[bwr[i], bc], writes=[bB[pb]])
        K.op(V, lambda: nc.vector.tensor_tensor(
            out=mod[:, ct * 4:(ct + 1) * 4, :],
            in0=banks[pb][:, 0:8].rearrange("p (a b) -> p a b", b=2),
            in1=bad[:, ct * 4:(ct + 1) * 4].unsqueeze(2).to_broadcast([128, 4, 2]), op=ALU.add),
            reads=[bB[pb], bc], writes=[bmod])
    K.op(V, lambda: nc.vector.tensor_scalar(out=opsc[:, 0, :, :], in0=mod[:, 32:64, :], scalar1=1.0, scalar2=None,
                                            op0=ALU.add), reads=[bmod], writes=[bmod])
    K.barrier()
    sc0.close()
    if stop_after <= 0:
        K.finish()
        return nc, ins_used

    xT = din("xT", [D, NALL])
    w_in = din("w_in", [D, 5440])
    g_qT = din("g_qT", [128, 6])
    g_kvT = din("g_kvT", [128, 4])
    w_dwT = din("w_dwT", [128, 16, 31])
    b_dwT = din("b_dwT", [128, 16])
    halo_mask = din("halo_mask", [128, 2])
    o_ckvT = dout("o_ckvT", [512, NP_])
    o_kpeT = dout("o_kpeT", [64, NP_])
    qcT_d = dscr("qcT_d", [768, NOWN], BF16)
    rstdq_d = dscr("rstdq_d", [128, NOWN], F32)
    ckvT_d = dscr("ckvT_d", [512, NALL], BF16)
    kpe_d = dscr("kpe_d", [64, NALL], F32)
    conv_d = dscr("conv_d", [2048, NOWN], F32)
    cstat_d = dscr("cstat_d", [2, 128, NOWN], F32)
    xv = xT.rearrange("(kc p) n -> p kc n", p=128)
    wiv = w_in.rearrange("(kc p) n -> p kc n", p=128)

    sc1 = Scope(K)
    gq = sc1.sb("gq", [128, 6], F32)
    gkv = sc1.sb("gkv", [128, 4], F32)
    wdw = sc1.sb("wdw", [128, 16, 31], F32)
    bdw = sc1.sb("bdw", [128, 16], F32)
    hmask = sc1.sb("hmask", [128, 2], F32)
    bsm = Buf()
    for t_, s_ in ((gq, g_qT), (gkv, g_kvT), (wdw, w_dwT), (bdw, b_dwT), (hmask, halo_mask)):
        K.dma(S, t_[:], s_, writes=[bsm])
    wt = [sc1.sb("wt%d" % i, [128, 32, 256], BF16) for i in range(2)]
    bwt = [Buf() for _ in range(2)]
    wti = [0]

    def load_w(c0, ncol, c1=None):
        i = wti[0] % 2
        wti[0] += 1
        if c1 is None:
            K.dma(P, wt[i][:, :, 0:ncol], wiv[:, :, c0:c0 + ncol], writes=[bwt[i]])
        else:
            K.dma(P, wt[i][:, :, 0:128], wiv[:, :, c0:c0 + 128], writes=[bwt[i]])
            K.dma(P, wt[i][:, :, 128:256], wiv[:, :, c1:c1 + 128], writes=[bwt[i]])
        return i

    def modulate(hT, bh, ncols, col0, ranges, sc):
        xs = [sc.sb("xs%d" % i, [128, ncols], F32) for i in range(2)]
        bxs = [Buf() for _ in range(2)]
        for kc in range(32):
            i = kc % 2
            K.dma(S, xs[i][:], xv[:, kc, col0:col0 + ncols], writes=[bxs[i]])
            for ri, (lo, hi, j) in enumerate(ranges):
                if (kc + ri) % 2 == 0:
                    K.op(V, lambda: nc.vector.tensor_scalar(out=hT[:, kc, lo:hi], in0=xs[i][:, lo:hi],
                                                            scalar1=OPSC(0, kc, j), scalar2=MOD(0, kc, j),
                                                            op0=ALU.mult, op1=ALU.add),
                         reads=[bxs[i], bmod], writes=[bh])
                else:
                    K.op(A, lambda: nc.scalar.activation(hT[:, kc, lo:hi], xs[i][:, lo:hi], AF.Identity,
                                                         bias=MOD(0, kc, j), scale=OPSC(0, kc, j)),
                         reads=[bxs[i], bmod], writes=[bh])

    pbi = [0]

    def next_bank(lo=0, hi=4):
        b = lo + pbi[0] % (hi - lo)
        pbi[0] += 1
        return b

    def rstd_from_sumsq(sq, bsq, ncols, nfeat, sc):
        for t in range(0, ncols, 512):
            w = min(512, ncols - t)
            mm(K, banks[7][:, 0:w], [(ones_f[:], sq[:, t:t + w])], reads=[bsq, bconst], writes=[bB[7]])
            K.op(A, lambda: nc.scalar.activation(sq[:, t:t + w], banks[7][:, 0:w], AF.Sqrt, bias=epsc[:, 0:1],
                                                 scale=1.0 / nfeat), reads=[bB[7], bconst], writes=[bsq])
        K.op(V, lambda: nc.vector.reciprocal(out=sq[:, 0:ncols], in_=sq[:, 0:ncols]), reads=[bsq], writes=[bsq])

    def proj_kv(hT, bh, ncols, col0, sc, is_own):
        ntile = ncols // 512
        raw = sc.sb("ckvraw", [128, 4, ncols], F32)
        sq = sc.sb("sqkv", [128, ncols], F32)
        sqt = [sc.sb("sqt%d" % i, [128, 512], F32) for i in range(2)]
        braw, bsq = Buf(), Buf()
        bsqt = [Buf(), Buf()]
        K.op(P, lambda: nc.gpsimd.memset(sq[:], 0.0), writes=[bsq])
        k = 0
        for tl in range(2):
            i = load_w(768 + tl * 256, 256)
            for ob in range(2):
                b4 = tl * 2 + ob
                for t in range(ntile):
                    pb = next_bank()
                    mm(K, banks[pb][:], [(wt[i][:, kc, ob * 128:(ob + 1) * 128], hT[:, kc, t * 512:(t + 1) * 512])
                                         for kc in range(32)], reads=[bwt[i], bh], writes=[bB[pb]])
                    K.op(A, lambda: nc.scalar.copy(raw[:, b4, t * 512:(t + 1) * 512], banks[pb][:]),
                         reads=[bB[pb]], writes=[braw])
                    j = k % 2
                    k += 1
                    K.op(A, lambda: nc.scalar.activation(sqt[j][:], banks[pb][:], AF.Square),
                         reads=[bB[pb]], writes=[bsqt[j]])
                    K.op(V, lambda: nc.vector.tensor_tensor(out=sq[:, t * 512:(t + 1) * 512],
                                                            in0=sq[:, t * 512:(t + 1) * 512], in1=sqt[j][:], op=ALU.add),
                         reads=[bsqt[j], bsq], writes=[bsq])
        rstd_from_sumsq(sq, bsq, ncols, 512.0, sc)
        nrm = [sc.sb("nrm%d" % i, [128, 512], F32) for i in range(2)]
        nrb = [sc.sb("nrb%d" % i, [128, 512], BF16) for i in range(2)]
        bn = [Buf(), Buf()]
        bnb = [Buf(), Buf()]
        k = 0
        for b4 in range(4):
            for t in range(ntile):
                j = k % 2
                k += 1
                cs = slice(t * 512, (t + 1) * 512)
                K.op(V, lambda: nc.vector.scalar_tensor_tensor(out=nrm[j][:], in0=raw[:, b4, cs], scalar=gkv[:, b4:b4 + 1],
                                                               in1=sq[:, cs], op0=ALU.mult, op1=ALU.mult),
                     reads=[braw, bsq, bsm], writes=[bn[j]])
                if is_own and t < 2:
                    K.dma(S, o_ckvT[b4 * 128:(b4 + 1) * 128, cs], nrm[j][:], reads=[bn[j]], final=True)
                K.op(A, lambda: nc.scalar.copy(nrb[j][:], nrm[j][:]), reads=[bn[j]], writes=[bnb[j]])
                K.dma(S, ckvT_d[b4 * 128:(b4 + 1) * 128, col0 + t * 512:col0 + (t + 1) * 512], nrb[j][:],
                      reads=[bnb[j]])
        i = load_w(1280, 64)
        kraw = sc.sb("kraw", [64, ncols], F32)
        bk = Buf()
        for t in range(ntile):
            pb = next_bank()
            mm(K, banks[pb][0:64, :], [(wt[i][:, kc, 0:64], hT[:, kc, t * 512:(t + 1) * 512]) for kc in range(32)],
               reads=[bwt[i], bh], writes=[bB[pb]])
            K.op(A, lambda: nc.scalar.copy(kraw[:, t * 512:(t + 1) * 512], banks[pb][0:64, :]),
                 reads=[bB[pb]], writes=[bk])
        K.dma(S, kpe_d[:, col0:col0 + ncols], kraw[:], reads=[bk])
        if is_own:
            K.dma(S, o_kpeT[:, :], kraw[:, 0:NP_], reads=[bk], final=True)

    scx = Scope(K)
    hTo = scx.sb("hTo", [128, 32, 512], BF16)
    bho = Buf()
    modulate(hTo, bho, 512, NOWN, [(0, 512, 1)], scx)
    proj_kv(hTo, bho, 512, NOWN, scx, False)
    K.barrier()
    scx.close()

    NH = NOWN + 16
    hT = sc1.sb("hT", [128, 32, NH], BF16)
    bh = Buf()
    sca = Scope(K)
    modulate(hT, bh, NH, 0, [(0, NP_, 0), (NP_, NH, 1)], sca)
    K.barrier()
    sca.close()
    sca = Scope(K)
    proj_kv(hT, bh, NOWN, 0, sca, True)
    sqq = sca.sb("sqq", [128, NOWN], F32)
    sqt2 = [sca.sb("sqt2%d" % i, [128, 512], F32) for i in range(2)]
    qst = [sca.sb("qst%d" % i, [128, NOWN], BF16) for i in range(2)]
    bsqq = Buf()
    bsqt2 = [Buf(), Buf()]
    bqst = [Buf(), Buf()]
    K.op(P, lambda: nc.gpsimd.memset(sqq[:], 0.0), writes=[bsqq])
    k = 0
    for tl in range(3):
        i = load_w(tl * 256, 256)
        for ob in range(2):
            qb = tl * 2 + ob
            for t in range(3):
                cs = slice(t * 512, (t + 1) * 512)
                pb = next_bank()
                mm(K, banks[pb][:], [(wt[i][:, kc, ob * 128:(ob + 1) * 128], hT[:, kc, cs]) for kc in range(32)],
                   reads=[bwt[i], bh], writes=[bB[pb]])
                K.op(A, lambda: nc.scalar.activation(qst[qb % 2][:, cs], banks[pb][:], AF.Identity,
                                                     scale=gq[:, qb:qb + 1]),
                     reads=[bB[pb], bsm], writes=[bqst[qb % 2]])
                j = k % 2
                k += 1
                K.op(A, lambda: nc.scalar.activation(sqt2[j][:], banks[pb][:], AF.Square),
                     reads=[bB[pb]], writes=[bsqt2[j]])
                K.op(V, lambda: nc.vector.tensor_tensor(out=sqq[:, cs], in0=sqq[:, cs], in1=sqt2[j][:], op=ALU.add),
                     reads=[bsqt2[j], bsqq], writes=[bsqq])
            K.dma(S, qcT_d[qb * 128:(qb + 1) * 128, :], qst[qb % 2][:], reads=[bqst[qb % 2]])
    rstd_from_sumsq(sqq, bsqq, NOWN, 768.0, sca)
    K.dma(S, rstdq_d, sqq[:], reads=[bsqq])
    K.barrier()
    sca.close()
    if stop_after <= 1:
        K.finish()
        return nc, ins_used

    scb = Scope(K)
    ypad = [scb.sb("ypad%d" % i, [128, YW], BF16) for i in range(2)]
    dg = [scb.sb("dg%d" % i, [128, 31, 128], BF16) for i in range(2)]
    sg = [scb.sb("sg%d" % i, [128, 512], F32) for i in range(2)]
    yh = scb.sb("yh", [128, 16], F32)
    cv = [scb.sb("cv%d" % i, [128, NOWN], F32) for i in range(2)]
    cq = scb.sb("cq", [128, NOWN], F32)
    s1c = scb.sb("s1c", [128, NOWN], F32)
    s2c = scb.sb("s2c", [128, NOWN], F32)
    byp = [Buf(), Buf()]
    bdg = [Buf(), Buf()]
    bsg = [Buf(), Buf()]
    byh, bcq, bs1, bs2 = Buf(), Buf(), Buf(), Buf()
    bcv = [Buf(), Buf()]
    wa = [scb.sb("wa%d" % i, [128, 32, 128], BF16) for i in range(2)]
    bwa = [Buf(), Buf()]
    bada = [Buf() for _ in range(8)]
    bmod2 = Buf()

    def emit_ada(blk):
        for q in range(2):
            K.dma(P, wa[q][:], wav[:, :, (blk + q) * 128:(blk + q + 1) * 128], writes=[bwa[q]])
            mm(K, banks[7][:, q * 2:q * 2 + 2], [(wa[q][:, kc, :], sil[:, kc, :]) for kc in range(32)],
               reads=[bwa[q], bc], writes=[bB[7]])
        K.op(V, lambda: nc.vector.tensor_tensor(out=mod[:, blk:blk + 2, :],
                                                in0=banks[7][:, 0:4].rearrange("p (a b) -> p a b", b=2),
                                                in1=bad[:, blk:blk + 2].unsqueeze(2).to_broadcast([128, 2, 2]), op=ALU.add),
             reads=[bB[7], bc], writes=[bmod2])

    for i in range(2):
        K.op(P, lambda: nc.gpsimd.memset(ypad[i][:], 0.0), writes=[byp[i]])
    K.op(P, lambda: nc.gpsimd.memset(s1c[:], 0.0), writes=[bs1])
    K.op(P, lambda: nc.gpsimd.memset(s2c[:], 0.0), writes=[bs2])

    def ywin(yp, t, k):
        if t < 2:
            return yp[:, t * 572:(t + 1) * 572].rearrange("p (s w) -> p s w", w=286)[:, :, k:k + 256]
        return yp[:, 1144 + k:1144 + k + 512]

    def conv_chunk(j):
        yp = ypad[j % 2]
        for t in range(3):
            if t < 2:
                o = banks[4 + t][:].rearrange("p (s w) -> p s w", w=256)
            else:
                o = banks[4 + t][:]
            mm(K, o, [(dg[j % 2][:, k, :], ywin(yp, t, k)) for k in range(31)],
               reads=[bdg[j % 2], byp[j % 2]], writes=[bB[4 + t]])
            cs = slice(t * 512, (t + 1) * 512)
            K.op(A, lambda: nc.scalar.activation(cv[j % 2][:, cs], banks[4 + t][:], AF.Identity,
                                                 bias=bdw[:, j:j + 1]),
                 reads=[bB[4 + t], bsm], writes=[bcv[j % 2]])
        K.dma(S, conv_d[j * 128:(j + 1) * 128, :], cv[j % 2][:], reads=[bcv[j % 2]])
        K.op(V, lambda: nc.vector.tensor_tensor(out=s1c[:], in0=s1c[:], in1=cv[j % 2][:], op=ALU.add),
             reads=[bcv[j % 2], bs1], writes=[bs1])
        K.op(A, lambda: nc.scalar.activation(cq[:], cv[j % 2][:], AF.Square), reads=[bcv[j % 2]], writes=[bcq])
        K.op(V, lambda: nc.vector.tensor_tensor(out=s2c[:], in0=s2c[:], in1=cq[:], op=ALU.add),
             reads=[bcq, bs2], writes=[bs2])

    for jp in range(8):
        ia = load_w(1344 + 256 * jp, 256)
        ig = load_w(3392 + 256 * jp, 256)
        for jj in range(2):
            j = jp * 2 + jj
            yp = ypad[j % 2]
            K.op(V, lambda: nc.vector.tensor_tensor(
                out=dg[j % 2][:], in0=ident_b[:].unsqueeze(1).to_broadcast([128, 31, 128]),
                in1=wdw[:, j, :].unsqueeze(2).to_broadcast([128, 31, 128]), op=ALU.mult),
                reads=[bconst, bsm], writes=[bdg[j % 2]])
            for t in range(4):
                if t < 3:
                    cs = slice(t * 512, (t + 1) * 512)
                    w_ = 512
                else:
                    cs = slice(NOWN, NOWN + 16)
                    w_ = 16
                pa = next_bank()
                pg = next_bank()
                mm(K, banks[pa][:, 0:w_], [(wt[ia][:, kc, jj * 128:(jj + 1) * 128], hT[:, kc, cs]) for kc in range(32)],
                   reads=[bwt[ia], bh], writes=[bB[pa]])
                mm(K, banks[pg][:, 0:w_], [(wt[ig][:, kc, jj * 128:(jj + 1) * 128], hT[:, kc, cs]) for kc in range(32)],
                   reads=[bwt[ig], bh], writes=[bB[pg]])
                s_ = sg[t % 2]
                K.op(A, lambda: nc.scalar.activation(s_[:, 0:w_], banks[pg][:, 0:w_], AF.Sigmoid),
                     reads=[bB[pg]], writes=[bsg[t % 2]])
                if t < 2:
                    o = yp[:, t * 572:(t + 1) * 572].rearrange("p (s w) -> p s w", w=286)[:, :, 15:271]
                    K.op(V, lambda: nc.vector.tensor_tensor(
                        out=o, in0=banks[pa][:].rearrange("p (s w) -> p s w", w=256),
                        in1=s_[:].rearrange("p (s w) -> p s w", w=256), op=ALU.mult),
                        reads=[bB[pa], bsg[t % 2]], writes=[byp[j % 2]])
                elif t == 2:
                    K.op(V, lambda: nc.vector.tensor_tensor(out=yp[:, 1159:1159 + 512], in0=banks[pa][:], in1=s_[:],
                                                            op=ALU.mult),
                         reads=[bB[pa], bsg[t % 2]], writes=[byp[j % 2]])
                else:
                    K.op(V, lambda: nc.vector.tensor_tensor(out=yh[:], in0=banks[pa][:, 0:16], in1=s_[:, 0:16],
                                                            op=ALU.mult),
                         reads=[bB[pa], bsg[t % 2]], writes=[byh])
                    K.op(V, lambda: nc.vector.tensor_scalar(out=yp[:, 1144:1159], in0=yh[:, 0:15],
                                                            scalar1=hmask[:, 0:1], scalar2=None, op0=ALU.mult),
                         reads=[byh, bsm], writes=[byp[j % 2]])
                    K.op(V, lambda: nc.vector.tensor_scalar(out=yp[:, 1671:1686], in0=yh[:, 0:15],
                                                            scalar1=hmask[:, 1:2], scalar2=None, op0=ALU.mult),
                         reads=[byh, bsm], writes=[byp[j % 2]])
                emit_ada(64 + j * 8 + t * 2)
            if j > 0:
                conv_chunk(j - 1)
    conv_chunk(15)
    for t in range(3):
        cs = slice(t * 512, (t + 1) * 512)
        mm(K, banks[0][:], [(ones_f[:], s1c[:, cs])], reads=[bs1, bconst], writes=[bB[0]])
        mm(K, banks[1][:], [(ones_f[:], s2c[:, cs])], reads=[bs2, bconst], writes=[bB[1]])
        K.op(V, lambda: nc.vector.tensor_scalar(out=s1c[:, cs], in0=banks[0][:], scalar1=1.0 / 2048, scalar2=None,
                                                op0=ALU.mult), reads=[bB[0]], writes=[bs1])
        K.op(V, lambda: nc.vector.tensor_tensor(out=cq[:, cs], in0=s1c[:, cs], in1=s1c[:, cs], op=ALU.mult),
             reads=[bs1], writes=[bcq])
        K.op(V, lambda: nc.vector.scalar_tensor_tensor(out=s2c[:, cs], in0=banks[1][:], scalar=1.0 / 2048,
                                                       in1=cq[:, cs], op0=ALU.mult, op1=ALU.subtract),
             reads=[bB[1], bcq], writes=[bs2])
    K.op(A, lambda: nc.scalar.activation(s2c[:], s2c[:], AF.Sqrt, bias=epsc[:, 0:1], scale=1.0),
         reads=[bs2, bconst], writes=[bs2])
    K.op(V, lambda: nc.vector.reciprocal(out=s2c[:], in_=s2c[:]), reads=[bs2], writes=[bs2])
    K.dma(S, cstat_d[0], s1c[:], reads=[bs1])
    K.dma(S, cstat_d[1], s2c[:], reads=[bs2])
    K.op(V, lambda: nc.vector.tensor_scalar(out=opsc[:, 1, :, :], in0=mod[:, 128:160, :], scalar1=1.0, scalar2=None,
                                            op0=ALU.add), reads=[bmod2], writes=[bmod])
    if dbg:
        d_mod = dout("d_mod", [128, 192, 2])
        K.dma(S, d_mod, mod[:], reads=[bmod, bmod2])
    K.barrier()
    scb.close()
    sc1.close()
    if stop_after <= 2:
        K.finish()
        return nc, ins_used

    w_uq = din("w_uq", [768, 3072])
    w_ukv = din("w_ukv", [512, 4096])
    cache_ckvT = din("cache_ckvT", [128, 4, 256])
    cache_kpeT = din("cache_kpeT", [64, 256])
    cosT = din("cosT", [64, 1024])
    sinT = din("sinT", [64, 1024])
    rmatT = din("rmatT", [64, 64])
    attnT_d = dscr("attnT_d", [2048, NOWN], BF16)
    wuqv = w_uq.rearrange("(kc p) n -> p kc n", p=128)
    wukvv = w_ukv.rearrange("(kc p) n -> p kc n", p=128)
    sc2 = Scope(K)
    ckv = sc2.sb("ckv", [128, 4, NKEY], BF16)
    kpe = sc2.sb("kpeb", [64, NKEY], BF16)
    qc = sc2.sb("qc", [128, 6, NOWN], BF16)
    rq = sc2.sb("rq", [128, NOWN], F32)
    kraw = sc2.sb("kraw2", [64, NALL], F32)
    cos = sc2.sb("cos", [64, 1024], F32)
    sin = sc2.sb("sin", [64, 1024], F32)
    rm = sc2.sb("rm", [64, 64], F32)
    rt1 = sc2.sb("rt1", [64, 512], F32)
    rt2 = sc2.sb("rt2", [64, 512], F32)
    qpf = sc2.sb("qpf", [64, 512], F32)
    bckv, bkpe, bqc, brq, bkraw, btab, brt1, brt2, bqpf = [Buf() for _ in range(9)]
    K.dma(S, ckv[:, :, 0:NALL], ckvT_d.rearrange("(kc p) n -> p kc n", p=128), writes=[bckv])
    K.dma(P, ckv[:, :, NALL:NKEY], cache_ckvT, writes=[bckv])
    K.dma(S, kraw[:], kpe_d, writes=[bkraw])
    K.dma(P, kpe[:, NALL:NKEY], cache_kpeT, writes=[bkpe])
    K.dma(S, qc[:], qcT_d.rearrange("(kc p) n -> p kc n", p=128), writes=[bqc])
    K.dma(S, rq[:], rstdq_d, writes=[brq])
    K.dma(S, cos[:], cosT, writes=[btab])
    K.dma(S, sin[:], sinT, writes=[btab])
    K.dma(S, rm[:], rmatT, writes=[btab])
    K.op(A, lambda: nc.scalar.copy(kpe[:, 0:NP_], kraw[:, 0:NP_]), reads=[bkraw], writes=[bkpe])

    def rope(dst, bdst, src, bsrc, tab0):
        pb = next_bank(0, 6)
        mm(K, banks[pb][0:64, :], [(rm[:, :], src)], reads=[btab, bsrc], writes=[bB[pb]])
        K.op(V, lambda: nc.vector.tensor_tensor(out=rt1[:], in0=src, in1=cos[:, tab0:tab0 + 512], op=ALU.mult),
             reads=[bsrc, btab], writes=[brt1])
        K.op(V, lambda: nc.vector.tensor_tensor(out=rt2[:], in0=banks[pb][0:64, :], in1=sin[:, tab0:tab0 + 512],
                                                op=ALU.mult), reads=[bB[pb], btab], writes=[brt2])
        K.op(V, lambda: nc.vector.tensor_tensor(out=dst, in0=rt1[:], in1=rt2[:], op=ALU.add),
             reads=[brt1, brt2], writes=[bdst])

    for t in range(2):
        rope(kpe[:, NP_ + t * 512:NP_ + (t + 1) * 512], bkpe, kraw[:, NP_ + t * 512:NP_ + (t + 1) * 512], bkraw, t * 512)

    wq = [sc2.sb("wq%d" % i, [128, 6, 192], BF16) for i in range(2)]
    wkv = [sc2.sb("wkv%d" % i, [128, 4, 256], BF16) for i in range(2)]
    qn = [sc2.sb("qn%d" % i, [128, NOWN], BF16) for i in range(2)]
    qp = [sc2.sb("qp%d" % i, [64, NOWN], BF16) for i in range(2)]
    kn = [sc2.sb("kn%d" % i, [128, NKEY], BF16) for i in range(2)]
    vh = [sc2.sb("vh%d" % i, [128, 18, 128], BF16) for i in range(2)]
    ast = [sc2.sb("ast%d" % i, [128, NOWN], BF16) for i in range(2)]
    p32 = [sc2.sb("p32%d" % i, [128, 1280], F32) for i in range(2)]
    pn = [sc2.sb("pn%d" % i, [128, 1280], BF16) for i in range(2)]
    pt = [sc2.sb("pt%d" % i, [128, 10, 128], BF16) for i in range(2)]
    st = [sc2.sb("st%d" % i, [128, 8], F32) for i in range(2)]
    bwq, bwkv, bqn, bqp, bkn, bvh, bast, bp32, bpn, bpt, bst, bpv = [[Buf(), Buf()] for _ in range(12)]
    tb = [banks[6][:].bitcast(BF16), banks[7][:].bitcast(BF16)]

    qblocks = []
    for s_ in range(4):
        for hh in range(2):
            qblocks.append((s_ * 256 + hh * 128, s_ * 256, 256, 2 * s_))
    for i_ in range(4):
        qblocks.append((NP_ + i_ * 128, NP_, 1280, 8))

    _NH = int(os.environ.get('P2_HEADS', 16)); _NQ = int(os.environ.get('P2_QB', 12)); _STG = int(os.environ.get('P2_STAGE', 4))
    for h in range(_NH):
        hp = h % 2
        K.dma(P, wq[hp][:], wuqv[:, :, h * 192:(h + 1) * 192], writes=[bwq[hp]])
        K.dma(P, wkv[hp][:], wukvv[:, :, h * 256:(h + 1) * 256], writes=[bwkv[hp]])
        for t in range(3):
            cs = slice(t * 512, (t + 1) * 512)
            pb = next_bank(0, 6)
            mm(K, banks[pb][:], [(wq[hp][:, kc, 0:128], qc[:, kc, cs]) for kc in range(6)],
               reads=[bwq[hp], bqc], writes=[bB[pb]])
            K.op(V, lambda: nc.vector.tensor_tensor(out=qn[hp][:, cs], in0=banks[pb][:], in1=rq[:, cs], op=ALU.mult),
                 reads=[bB[pb], brq], writes=[bqn[hp]])
            pb = next_bank(0, 6)
            mm(K, banks[pb][0:64, :], [(wq[hp][:, kc, 128:192], qc[:, kc, cs]) for kc in range(6)],
               reads=[bwq[hp], bqc], writes=[bB[pb]])
            if t < 2:
                K.op(V, lambda: nc.vector.tensor_tensor(out=qp[hp][:, cs], in0=banks[pb][0:64, :], in1=rq[0:64, cs],
                                                        op=ALU.mult), reads=[bB[pb], brq], writes=[bqp[hp]])
            else:
                K.op(V, lambda: nc.vector.tensor_tensor(out=qpf[:], in0=banks[pb][0:64, :], in1=rq[0:64, cs],
                                                        op=ALU.mult), reads=[bB[pb], brq], writes=[bqpf])
                rope(qp[hp][:, cs], bqp[hp], qpf[:], bqpf, 0)
        for t in range(5):
            w_ = 512 if t < 4 else 256
            cs = slice(t * 512, t * 512 + w_)
            pb = next_bank(0, 6)
            mm(K, banks[pb][:, 0:w_], [(wkv[hp][:, kc, 0:128], ckv[:, kc, cs]) for kc in range(4)],
               reads=[bwkv[hp], bckv], writes=[bB[pb]])
            K.op(A, lambda: nc.scalar.copy(kn[hp][:, cs], banks[pb][:, 0:w_]), reads=[bB[pb]], writes=[bkn[hp]])
        for g in range(5):
            nb_ = 4 if g < 4 else 2
            pb = next_bank(0, 6)
            for kk in range(nb_):
                kb = g * 4 + kk
                mm(K, banks[pb][:, kk * 128:(kk + 1) * 128],
                   [(ckv[:, kc, kb * 128:(kb + 1) * 128], wkv[hp][:, kc, 128:256]) for kc in range(4)],
                   reads=[bwkv[hp], bckv], writes=[bB[pb]])
            K.op(A if g % 2 else V,
                 (lambda: nc.scalar.copy(vh[hp][:, g * 4:g * 4 + nb_, :],
                                         banks[pb][:, 0:nb_ * 128].rearrange("p (a b) -> p a b", b=128))) if g % 2 else
                 (lambda: nc.vector.tensor_copy(out=vh[hp][:, g * 4:g * 4 + nb_, :],
                                                in_=banks[pb][:, 0:nb_ * 128].rearrange("p (a b) -> p a b", b=128))),
                 reads=[bB[pb]], writes=[bvh[hp]])

        def emit_S(qi):
            q0, k0, nk, vb0 = qblocks[qi]
            base = 3 * (qi % 2)
            for kt in range((nk + 511) // 512):
                w_ = min(512, nk - kt * 512)
                ks = slice(k0 + kt * 512, k0 + kt * 512 + w_)
                mm(K, banks[base + kt][:, 0:w_],
                   [(qn[hp][:, q0:q0 + 128], kn[hp][:, ks]), (qp[hp][:, q0:q0 + 128], kpe[:, ks])],
                   reads=[bqn[hp], bqp[hp], bkn[hp], bkpe], writes=[bB[base + kt]])

        def emit_softmax(qi):
            q0, k0, nk, vb0 = qblocks[qi]
            base = 3 * (qi % 2)
            e = qi % 2
            nt = (nk + 511) // 512
            for kt in range(nt):
                w_ = min(512, nk - kt * 512)
                K.op(V, lambda: nc.vector.reduce_max(out=st[e][:, kt:kt + 1], in_=banks[base + kt][:, 0:w_], axis=AX.X),
                     reads=[bB[base + kt]], writes=[bst[e]])
            if nt > 1:
                K.op(V, lambda: nc.vector.reduce_max(out=st[e][:, 3:4], in_=st[e][:, 0:nt], axis=AX.X),
                     reads=[bst[e]], writes=[bst[e]])
                mcol = 3
            else:
                mcol = 0
            K.op(V, lambda: nc.vector.tensor_scalar(out=st[e][:, 4:5], in0=st[e][:, mcol:mcol + 1], scalar1=-SCALE,
                                                    scalar2=None, op0=ALU.mult), reads=[bst[e]], writes=[bst[e]])
            for kt in range(nt):
                w_ = min(512, nk - kt * 512)
                K.op(A, lambda: nc.scalar.activation(p32[e][:, kt * 512:kt * 512 + w_], banks[base + kt][:, 0:w_],
                                                     AF.Exp, bias=st[e][:, 4:5], scale=SCALE),
                     reads=[bB[base + kt], bst[e]], writes=[bp32[e]])
            K.op(V, lambda: nc.vector.reduce_sum(out=st[e][:, 5:6], in_=p32[e][:, 0:nk], axis=AX.X),
                 reads=[bp32[e]], writes=[bst[e]])
            K.op(V, lambda: nc.vector.reciprocal(out=st[e][:, 6:7], in_=st[e][:, 5:6]), reads=[bst[e]], writes=[bst[e]])
            K.op(A, lambda: nc.scalar.activation(pn[e][:, 0:nk], p32[e][:, 0:nk], AF.Identity, scale=st[e][:, 6:7]),
                 reads=[bp32[e], bst[e]], writes=[bpn[e]])

        def emit_PV(qi):
            q0, k0, nk, vb0 = qblocks[qi]
            base = 3 * (qi % 2)
            e = qi % 2
            nkb = nk // 128
            for kb in range(nkb):
                tbi = kb // 8
                K.op(K.pe, lambda: nc.tensor.transpose(tb[tbi][:, (kb % 8) * 128:(kb % 8 + 1) * 128],
                                                       pn[e][:, kb * 128:(kb + 1) * 128], ident_b[:]),
                     reads=[bpn[e], bconst], writes=[bB[6 + tbi]])
            n0 = min(nkb, 8)
            K.op(V, lambda: nc.vector.tensor_copy(out=pt[e][:, 0:n0, :],
                                                  in_=tb[0][:, 0:n0 * 128].rearrange("p (a b) -> p a b", b=128)),
                 reads=[bB[6]], writes=[bpt[e]])
            if nkb > 8:
                K.op(V, lambda: nc.vector.tensor_copy(out=pt[e][:, 8:nkb, :],
                                                      in_=tb[1][:, 0:(nkb - 8) * 128].rearrange("p (a b) -> p a b", b=128)),
                     reads=[bB[7]], writes=[bpt[e]])
            mm(K, banks[base + 2][:, 256:384], [(vh[hp][:, vb0 + kb, :], pt[e][:, kb, :]) for kb in range(nkb)],
               reads=[bvh[hp], bpt[e]], writes=[bB[base + 2]])
            K.op(A, lambda: nc.scalar.copy(ast[hp][:, q0:q0 + 128], banks[base + 2][:, 256:384]),
                 reads=[bB[base + 2]], writes=[bast[hp]])

        if _STG >= 2:
            emit_S(0)
        for qi in range(_NQ):
            if qi + 1 < _NQ and _STG >= 2:
                emit_S(qi + 1)
            if _STG >= 3:
                emit_softmax(qi)
            if _STG >= 4:
                emit_PV(qi)
        K.dma(S, attnT_d[h * 128:(h + 1) * 128, :], ast[hp][:], reads=[bast[hp]])
    K.barrier()
    sc2.close()
    if stop_after <= 3:
        K.finish()
        return nc, ins_used

    def ln_stats(s1, bs1, s2, bs2, tmp, btmp, nfeat, ncols):
        for t in range(0, ncols, 512):
            cs = slice(t, t + 512)
            mm(K, banks[6][:], [(ones_f[:], s1[:, cs])], reads=[bs1, bconst], writes=[bB[6]])
            mm(K, banks[7][:], [(ones_f[:], s2[:, cs])], reads=[bs2, bconst], writes=[bB[7]])
            K.op(V, lambda: nc.vector.tensor_scalar(out=s1[:, cs], in0=banks[6][:], scalar1=1.0 / nfeat, scalar2=None,
                                                    op0=ALU.mult), reads=[bB[6]], writes=[bs1])
            K.op(V, lambda: nc.vector.tensor_tensor(out=tmp[:, cs], in0=s1[:, cs], in1=s1[:, cs], op=ALU.mult),
                 reads=[bs1], writes=[btmp])
            K.op(V, lambda: nc.vector.scalar_tensor_tensor(out=s2[:, cs], in0=banks[7][:], scalar=1.0 / nfeat,
                                                           in1=tmp[:, cs], op0=ALU.mult, op1=ALU.subtract),
                 reads=[bB[7], btmp], writes=[bs2])
        K.op(A, lambda: nc.scalar.activation(s2[:, 0:ncols], s2[:, 0:ncols], AF.Sqrt, bias=epsc[:, 0:1], scale=1.0),
             reads=[bs2, bconst], writes=[bs2])
        K.op(V, lambda: nc.vector.reciprocal(out=s2[:, 0:ncols], in_=s2[:, 0:ncols]), reads=[bs2], writes=[bs2])

    w_out = din("w_out", [D, D])
    g_cnT = din("g_cnT", [128, 16])
    b_cnT = din("b_cnT", [128, 16])
    ln1_gT = din("ln1_gT", [128, 32])
    ln1_bT = din("ln1_bT", [128, 32])
    zT_d = dscr("zT_d", [D, NOWN], F32)
    stat1_d = dscr("stat1_d", [2, 128, NOWN], F32)
    h2T_d = dscr("h2T_d", [D, NOWN], BF16)
    wov = w_out.rearrange("(kc p) n -> p kc n", p=128)
    sc3 = Scope(K)
    mix = sc3.sb("mix", [128, 32, NOWN], BF16)
    gcn = sc3.sb("gcn", [128, 16], F32)
    bcn = sc3.sb("bcn", [128, 16], F32)
    l1g = sc3.sb("l1g", [128, 32], F32)
    l1b = sc3.sb("l1b", [128, 32], F32)
    a1 = sc3.sb("a1", [128, 32, 2], F32)
    b1 = sc3.sb("b1", [128, 32, 2], F32)
    bmix, bsm3, bab = Buf(), Buf(), Buf()
    for t_, s_ in ((gcn, g_cnT), (bcn, b_cnT), (l1g, ln1_gT), (l1b, ln1_bT)):
        K.dma(S, t_[:], s_, writes=[bsm3])
    K.dma(S, mix[:, 0:16, :], attnT_d.rearrange("(kc p) n -> p kc n", p=128), writes=[bmix])
    K.op(V, lambda: nc.vector.tensor_tensor(out=a1[:], in0=opsc[:, 1, :, :],
                                            in1=l1g[:].unsqueeze(2).to_broadcast([128, 32, 2]), op=ALU.mult),
         reads=[bmod, bsm3], writes=[bab])
    K.op(V, lambda: nc.vector.tensor_tensor(out=b1[:], in0=opsc[:, 1, :, :],
                                            in1=l1b[:].unsqueeze(2).to_broadcast([128, 32, 2]), op=ALU.mult),
         reads=[bmod, bsm3], writes=[bab])
    K.op(V, lambda: nc.vector.tensor_tensor(out=b1[:], in0=b1[:], in1=mod[:, 96:128, :], op=ALU.add),
         reads=[bmod, bab], writes=[bab])
    s3a = Scope(K)
    cm = s3a.sb("cm", [128, NOWN], F32)
    cr = s3a.sb("cr", [128, NOWN], F32)
    cvl = [s3a.sb("cvl%d" % i, [128, NOWN], F32) for i in range(2)]
    cvt = [s3a.sb("cvt%d" % i, [128, NOWN], F32) for i in range(2)]
    bcs = Buf()
    bcvl = [Buf(), Buf()]
    bcvt = [Buf(), Buf()]
    K.dma(S, cm[:], cstat_d[0], writes=[bcs])
    K.dma(S, cr[:], cstat_d[1], writes=[bcs])
    for j in range(16):
        i = j % 2
        K.dma(S, cvl[i][:], conv_d[j * 128:(j + 1) * 128, :], writes=[bcvl[i]])
        K.op(V, lambda: nc.vector.tensor_tensor(out=cvt[i][:], in0=cvl[i][:], in1=cm[:], op=ALU.subtract),
             reads=[bcvl[i], bcs], writes=[bcvt[i]])
        K.op(P, lambda: nc.gpsimd.tensor_tensor(out=cvt[i][:], in0=cvt[i][:], in1=cr[:], op=ALU.mult),
             reads=[bcvt[i], bcs], writes=[bcvt[i]])
        K.op(A, lambda: nc.scalar.activation(mix[:, 16 + j, :], cvt[i][:], AF.Silu, bias=bcn[:, j:j + 1],
                                             scale=gcn[:, j:j + 1]), reads=[bcvt[i], bsm3], writes=[bmix])
    K.barrier()
    s3a.close()
    s3b = Scope(K)
    wo = [s3b.sb("wo%d" % i, [128, 32, 256], BF16) for i in range(2)]
    xs3 = [s3b.sb("xs3%d" % i, [128, NOWN], F32) for i in range(2)]
    zt = [s3b.sb("zt%d" % i, [128, NOWN], F32) for i in range(2)]
    zz = [s3b.sb("zz%d" % i, [128, NOWN], F32) for i in range(2)]
    z1 = s3b.sb("z1", [128, NOWN], F32)
    z2 = s3b.sb("z2", [128, NOWN], F32)
    zq = s3b.sb("zq", [128, NOWN], F32)
    bwo = [Buf() for _ in range(2)]
    bxs3, bzt, bzz = [[Buf(), Buf()] for _ in range(3)]
    bz1, bz2, bzq = Buf(), Buf(), Buf()
    K.op(P, lambda: nc.gpsimd.memset(z1[:], 0.0), writes=[bz1])
    K.op(P, lambda: nc.gpsimd.memset(z2[:], 0.0), writes=[bz2])
    for tl in range(16):
        i = tl % 2
        K.dma(P, wo[i][:], wov[:, :, tl * 256:(tl + 1) * 256], writes=[bwo[i]])
        for ob in range(2):
            db = tl * 2 + ob
            e = db % 2
            K.dma(S, xs3[e][:], xv[:, db, 0:NOWN], writes=[bxs3[e]])
            for t in range(3):
                cs = slice(t * 512, (t + 1) * 512)
                j = 0 if t < 2 else 1
                pb = next_bank(0, 6)
                mm(K, banks[pb][:], [(wo[i][:, kc, ob * 128:(ob + 1) * 128], mix[:, kc, cs]) for kc in range(32)],
                   reads=[bwo[i], bmix], writes=[bB[pb]])
                K.op(A, lambda: nc.scalar.activation(zt[e][:, cs], banks[pb][:], AF.Identity, scale=MOD(2, db, j)),
                     reads=[bB[pb], bmod], writes=[bzt[e]])
                K.op(V, lambda: nc.vector.scalar_tensor_tensor(out=zz[e][:, cs], in0=xs3[e][:, cs], scalar=ALPHA,
                                                               in1=zt[e][:, cs], op0=ALU.mult, op1=ALU.add),
                     reads=[bxs3[e], bzt[e]], writes=[bzz[e]])
            K.dma(S, zT_d[db * 128:(db + 1) * 128, :], zz[e][:], reads=[bzz[e]])
            K.op(P, lambda: nc.gpsimd.tensor_tensor(out=z1[:], in0=z1[:], in1=zz[e][:], op=ALU.add),
                 reads=[bzz[e], bz1], writes=[bz1])
            K.op(A, lambda: nc.scalar.activation(zq[:], zz[e][:], AF.Square), reads=[bzz[e]], writes=[bzq])
            K.op(P, lambda: nc.gpsimd.tensor_tensor(out=z2[:], in0=z2[:], in1=zq[:], op=ALU.add),
                 reads=[bzq, bz2], writes=[bz2])
    ln_stats(z1, bz1, z2, bz2, zq, bzq, 4096.0, NOWN)
    K.dma(S, stat1_d[0], z1[:], reads=[bz1])
    K.dma(S, stat1_d[1], z2[:], reads=[bz2])
    h2s = [s3b.sb("h2s%d" % i, [128, NOWN], BF16) for i in range(2)]
    bh2s = [Buf(), Buf()]
    for db in range(32):
        e = db % 2
        K.dma(S, xs3[e][:], zT_d[db * 128:(db + 1) * 128, :], writes=[bxs3[e]])
        K.op(V, lambda: nc.vector.tensor_tensor(out=zt[e][:], in0=xs3[e][:], in1=z1[:], op=ALU.subtract),
             reads=[bxs3[e], bz1], writes=[bzt[e]])
        K.op(P, lambda: nc.gpsimd.tensor_tensor(out=zt[e][:], in0=zt[e][:], in1=z2[:], op=ALU.mult),
             reads=[bzt[e], bz2], writes=[bzt[e]])
        for (lo, hi, j) in ((0, NP_, 0), (NP_, NOWN, 1)):
            K.op(A, lambda: nc.scalar.activation(h2s[e][:, lo:hi], zt[e][:, lo:hi], AF.Identity,
                                                 bias=b1[:, db, j:j + 1], scale=a1[:, db, j:j + 1]),
                 reads=[bzt[e], bab], writes=[bh2s[e]])
        K.dma(S, h2T_d[db * 128:(db + 1) * 128, :], h2s[e][:], reads=[bh2s[e]])
    K.barrier()
    s3b.close()
    sc3.close()
    if stop_after <= 4:
        K.finish()
        return nc, ins_used

    w_pq = din("w_pq", [D, D])
    skT = din("skT", [128, 32, 128])
    s_d = dscr("s_d", [8, NOWN, 256], F32)
    G_d = dscr("G_d", [128, 128, NOWN], BF16)
    wpv = w_pq.rearrange("(kc p) n -> p kc n", p=128)
    sc4 = Scope(K)
    h2 = sc4.sb("h2", [128, 32, NOWN], BF16)
    sk = sc4.sb("sk", [128, 32, 128], BF16)
    wp = [sc4.sb("wp%d" % i, [128, 32, 256], BF16) for i in range(3)]
    qhp = [sc4.sb("qhp%d" % i, [128, 2, NOWN], BF16) for i in range(2)]
    sst = [sc4.sb("sst%d" % i, [128, 12, 128], F32) for i in range(2)]
    bh2, bsk = Buf(), Buf()
    bwp = [Buf() for _ in range(3)]
    bqhp, bsst = [[Buf(), Buf()] for _ in range(2)]
    h2v = h2T_d.rearrange("(kc p) n -> p kc n", p=128)
    for q4 in range(4):
        K.dma(S, h2[:, q4 * 8:(q4 + 1) * 8, :], h2v[:, q4 * 8:(q4 + 1) * 8, :], writes=[bh2])
    K.dma(P, sk[:], skT, writes=[bsk])
    s_dv = s_d.rearrange("h (nb p) (t k) -> h t p nb k", p=128, k=128)
    cpy = [0]

    def evac(out, in_, reads, writes):
        cpy[0] += 1
        if cpy[0] % 2:
            return K.op(A, lambda: nc.scalar.copy(out, in_), reads=reads, writes=writes)
        return K.op(V, lambda: nc.vector.tensor_copy(out=out, in_=in_), reads=reads, writes=writes)

    for hp_ in range(16):
        i = hp_ % 3
        e = hp_ % 2
        K.dma(P, wp[i][:], wpv[:, :, hp_ * 256:(hp_ + 1) * 256], writes=[bwp[i]])
        for half in range(2):
            for t in range(3):
                cs = slice(t * 512, (t + 1) * 512)
                pb = next_bank(0, 4)
                mm(K, banks[pb][:], [(wp[i][:, kc, half * 128:(half + 1) * 128], h2[:, kc, cs]) for kc in range(32)],
                   reads=[bwp[i], bh2], writes=[bB[pb]])
                evac(qhp[e][:, half, cs], banks[pb][:], [bB[pb]], [bqhp[e]])
        for g in range(3):
            pb = 4 + (hp_ * 3 + g) % 4
            for kk in range(4):
                nb = g * 4 + kk
                mm(K, banks[pb][:, kk * 128:(kk + 1) * 128],
                   [(qhp[e][:, half, nb * 128:(nb + 1) * 128], sk[:, hp_ * 2 + half, :]) for half in range(2)],
                   reads=[bqhp[e], bsk], writes=[bB[pb]])
            evac(sst[e][:, g * 4:(g + 1) * 4, :], banks[pb][:].rearrange("p (a b) -> p a b", b=128), [bB[pb]], [bsst[e]])
        K.dma(S, s_dv[hp_ // 2, hp_ % 2], sst[e][:], reads=[bsst[e]])
    K.barrier()
    sc4.close()
    if stop_after <= 5:
        K.finish()
        return nc, ins_used

    scr = Scope(K)
    stm = [scr.sb("stm%d" % i, [128, 2048], F32) for i in range(2)]
    wrk = scr.sb("wrk", [128, 2048], F32)
    tp = scr.sb("tp", [128, 16, 16], F32)
    cand = scr.sb("cand", [128, 8, 256], F32)
    ctop = scr.sb("ctop", [128, 8, 16], F32)
    ce = scr.sb("ce", [128, 8, 16], F32)
    sm = scr.sb("sm", [128, 64], F32)
    At = scr.sb("At", [128, 3, 128], F32)
    AT = [scr.sb("AT%d" % i, [128, 3, 128], F32) for i in range(2)]
    srep = [scr.sb("srep%d" % i, [128, 16, 256], F32) for i in range(3)]
    zr = [scr.sb("zr%d" % i, [128, 16, 128], F32) for i in range(2)]
    er = [scr.sb("er%d" % i, [128, 16, 128], BF16) for i in range(2)]
    mk = [scr.sb("mk%d" % i, [128, 16, 128], BF16) for i in range(2)]
    Rb = [scr.sb("Rb%d" % i, [128, 16, 128], BF16) for i in range(2)]
    P1b = [scr.sb("P1b%d" % i, [128, 16, 128], BF16) for i in range(2)]
    gst = [scr.sb("gst%d" % i, [128, 128, 128], BF16) for i in range(2)]
    bstm, bAT, bsrep, bzr, bzc, ber, bmk, bRb, bP1b, bgst = [[Buf(), Buf(), Buf()] for _ in range(10)]
    bwrk, btp, bcand, bctop, bce, bsmm, bAt = [Buf() for _ in range(7)]
    tpv = tp[:].rearrange("p (h t) a -> p h t a", t=2)
    G_dv = G_d.rearrange("i j n -> j i n")

    bc816 = lambda ap: ap.unsqueeze(2).to_broadcast([128, 8, 16])
    btpg = [Buf() for _ in range(16)]
    bwkg = [Buf() for _ in range(16)]
    bctg = [Buf() for _ in range(8)]
    bcdg = [Buf() for _ in range(8)]

    def top16_batch(dsts, srcs, width, rds, wrs, wks, bwk):
        n = len(dsts)
        for i in range(n):
            K.op(V, lambda: nc.vector.max(out=dsts[i][:, 0:8], in_=srcs[i]), reads=rds[i], writes=[wrs[i]])
        for i in range(n):
            K.op(V, lambda: nc.vector.match_replace(out=wks[i], in_to_replace=dsts[i][:, 0:8], in_values=srcs[i],
                                                    imm_value=-1e30), reads=rds[i] + [wrs[i]], writes=[bwk[i]])
        for i in range(n):
            K.op(V, lambda: nc.vector.max(out=dsts[i][:, 8:16], in_=wks[i]), reads=[bwk[i]], writes=[wrs[i]])

    def topk_stage(nb):
        e = nb % 2
        K.dma(S, stm[e][:].rearrange("p (h c) -> p h c", c=256), s_d[:, nb * 128:(nb + 1) * 128, :].rearrange("h n c -> n h c"), writes=[bstm[e]])
        for q4 in range(4):
            hs = range(q4 * 4, q4 * 4 + 4)
            top16_batch([tp[:, i, :] for i in hs], [stm[e][:, i * 128:(i + 1) * 128] for i in hs], 128,
                        [[bstm[e]] for i in hs], [btpg[i] for i in hs],
                        [wrk[:, i * 128:(i + 1) * 128] for i in hs], [bwkg[i] for i in hs])
            yield
        for h in range(8):
            K.op(V, lambda: nc.vector.tensor_tensor(
                out=cand[:, h, :].rearrange("p (a b) -> p a b", b=16),
                in0=tpv[:, h, 0, :].unsqueeze(2).to_broadcast([128, 16, 16]),
                in1=tpv[:, h, 1, :].unsqueeze(1).to_broadcast([128, 16, 16]), op=ALU.add),
                reads=[btpg[2 * h], btpg[2 * h + 1]], writes=[bcdg[h]])
        yield
        for q2 in range(2):
            hs = range(q2 * 4, q2 * 4 + 4)
            top16_batch([ctop[:, i, :] for i in hs], [cand[:, i, :] for i in hs], 256,
                        [[bcdg[i]] for i in hs], [bctg[i] for i in hs],
                        [wrk[:, i * 256:(i + 1) * 256] for i in hs], [bwkg[2 * i] for i in hs])
            yield
        btp = btpg
        bctop = bctg
        K.op(V, lambda: nc.vector.tensor_reduce(out=sm[:, 0:8], in_=ctop[:], axis=AX.X, op=ALU.max),
             reads=bctop, writes=[bsmm])
        K.op(V, lambda: nc.vector.tensor_reduce(out=sm[:, 8:16], in_=ctop[:], axis=AX.X, op=ALU.min),
             reads=bctop, writes=[bsmm])
        K.op(V, lambda: nc.vector.tensor_tensor(out=ce[:], in0=ctop[:], in1=bc816(sm[:, 0:8]), op=ALU.subtract),
             reads=bctop + [bsmm], writes=[bce])
        K.op(A, lambda: nc.scalar.activation(ce[:], ce[:], AF.Exp), reads=[bce], writes=[bce])
        K.op(V, lambda: nc.vector.tensor_reduce(out=sm[:, 16:24], in_=ce[:], axis=AX.X, op=ALU.add),
             reads=[bce], writes=[bsmm])
        K.op(A, lambda: nc.scalar.activation(sm[:, 24:32], sm[:, 16:24], AF.Ln), reads=[bsmm], writes=[bsmm])
        K.op(V, lambda: nc.vector.tensor_tensor(out=sm[:, 32:40], in0=sm[:, 0:8], in1=sm[:, 24:32], op=ALU.add),
             reads=[bsmm], writes=[bsmm])
        K.op(V, lambda: nc.vector.tensor_copy(out=At[:, 0, :].rearrange("p (a h) -> p h a", h=8), in_=bc816(sm[:, 8:16])),
             reads=[bsmm], writes=[bAt])
        K.op(V, lambda: nc.vector.tensor_tensor(out=At[:, 1, :].rearrange("p (a h) -> p h a", h=8), in0=tpv[:, :, 0, :],
                                                in1=bc816(sm[:, 32:40]), op=ALU.subtract),
             reads=btp + [bsmm], writes=[bAt])
        K.op(V, lambda: nc.vector.tensor_copy(out=At[:, 2, :].rearrange("p (a h) -> p h a", h=8), in_=tpv[:, :, 0, :]),
             reads=btp, writes=[bAt])
        pbt = nb % 2
        for q3 in range(3):
            K.op(K.pe, lambda: nc.tensor.transpose(banks[pbt][:, q3 * 128:(q3 + 1) * 128], At[:, q3, :], ident_f[:]),
                 reads=[bAt, bconst], writes=[bB[pbt]])
        K.op(V, lambda: nc.vector.tensor_copy(out=AT[e][:], in_=banks[pbt][:, 0:384].rearrange("p (a b) -> p a b", b=128)),
             reads=[bB[pbt]], writes=[bAT[e]])
        yield

    def bcn(ap):
        return ap.unsqueeze(2).to_broadcast([128, 16, 128])

    def sub_block(nb, sb):
        e = nb % 2
        f = (nb * 8 + sb) % 2
        f3 = (nb * 8 + sb) % 3
        ns = slice(sb * 16, sb * 16 + 16)
        row0 = nb * 128 + sb * 16
        src = bass.AP(s_d.tensor, row0 * 256, [[0, 16], [NOWN * 256, 8], [1, 4096]])
        K.dma(S, srep[f3][:].rearrange("p a b -> p (a b)"), src, writes=[bsrep[f3]])
        K.op(V, lambda: nc.vector.tensor_tensor(out=zr[f][:], in0=srep[f3][:, :, 128:256], in1=bcn(AT[e][:, 2, ns]),
                                                op=ALU.add), reads=[bsrep[f3], bAT[e]], writes=[bzr[f]])
        for tk in range(16):
            n_ = sb * 16 + tk
            K.op(A, lambda: nc.scalar.activation(er[f][:, tk, :], srep[f3][:, tk, 128:256], AF.Exp,
                                                 bias=AT[e][:, 1, n_:n_ + 1]),
                 reads=[bsrep[f3], bAT[e]], writes=[ber[f]])
        K.op(V, lambda: nc.vector.tensor_tensor(out=mk[f][:], in0=zr[f][:], in1=bcn(AT[e][:, 0, ns]), op=ALU.is_ge),
             reads=[bzr[f], bAT[e]], writes=[bmk[f]])
        K.op(V, lambda: nc.vector.tensor_tensor(out=P1b[f][:], in0=srep[f3][:, :, 0:128], in1=bcn(AT[e][:, 2, ns]),
                                                op=ALU.is_equal), reads=[bsrep[f3], bAT[e]], writes=[bP1b[f]])
        K.op(V, lambda: nc.vector.tensor_tensor(out=Rb[f][:], in0=mk[f][:], in1=er[f][:], op=ALU.mult),
             reads=[bmk[f], ber[f]], writes=[bRb[f]])
        for q4 in range(4):
            pb = 2 + (sb * 4 + q4) % 6
            for tk in range(4):
                t16 = q4 * 4 + tk
                mm(K, banks[pb][:, tk * 128:(tk + 1) * 128], [(Rb[f][:, t16, :], P1b[f][:, t16, :])],
                   reads=[bRb[f], bP1b[f]], writes=[bB[pb]])
            n0 = sb * 16 + q4 * 4
            K.op(A, lambda: nc.scalar.copy(gst[e][:, :, n0:n0 + 4], banks[pb][:].rearrange("p (n i) -> p i n", n=4)),
                 reads=[bB[pb]], writes=[bgst[e]])

    for _ in topk_stage(0):
        pass
    for nb in range(12):
        nxt = topk_stage(nb + 1) if nb + 1 < 12 else iter(())
        for sb in range(8):
            sub_block(nb, sb)
            next(nxt, None)
        for _ in nxt:
            pass
        K.dma(S, G_dv[:, :, nb * 128:(nb + 1) * 128], gst[nb % 2][:], reads=[bgst[nb % 2]])
    K.barrier()
    scr.close()
    if stop_after <= 6:
        K.finish()
        return nc, ins_used

    peer_uT = din("peer_uT", [128, 128, 4096])
    peer_v = din("peer_v", [16384, D])
    ln2_gT = din("ln2_gT", [128, 32])
    ln2_bT = din("ln2_bT", [128, 32])
    o_yT = dout("o_yT", [D, NOWN])
    pvv = peer_v.rearrange("(g a p) d -> g p a d", a=4, p=128)
    sc5 = Scope(K)
    h2p = sc5.sb("h2p", [128, 32, 512], BF16)
    acc = sc5.sb("acc", [128, 32, 512], F32)
    l1g5 = sc5.sb("l1g5", [128, 32], F32)
    l1b5 = sc5.sb("l1b5", [128, 32], F32)
    l2g = sc5.sb("l2g", [128, 32], F32)
    l2b = sc5.sb("l2b", [128, 32], F32)
    bh2p, bsm5 = Buf(), Buf()
    bacc = [Buf() for _ in range(32)]
    for t_, s_ in ((l1g5, ln1_gT), (l1b5, ln1_bT), (l2g, ln2_gT), (l2b, ln2_bT)):
        K.dma(S, t_[:], s_, writes=[bsm5])
    _P5P = int(os.environ.get('P5_PASSES', 3)); _P5G = int(os.environ.get('P5_GROUPS', 32)); _P5E = int(os.environ.get('P5_EPI', 1))
    for pt_ in range(_P5P):
        c0 = pt_ * 512
        cj = 0 if pt_ < 2 else 1
        pcs = slice(c0, c0 + 512)
        K.dma(S, h2p[:], h2v[:, :, pcs], writes=[bh2p])
        for db in range(32):
            K.op(V, lambda: nc.vector.memset(acc[:, db, :], 0.0), writes=[bacc[db]])
        s5a = Scope(K)
        ut = [s5a.sb("ut%d" % i, [128, 32, 128], BF16) for i in range(3)]
        vt = [s5a.sb("vt%d" % i, [128, 4, D], BF16) for i in range(2)]
        gt = [s5a.sb("gt%d" % i, [128, 512], BF16) for i in range(3)]
        ga32 = [s5a.sb("ga32%d" % i, [128, 512], F32) for i in range(2)]
        gab = [s5a.sb("gab%d" % i, [128, 4, 512], BF16) for i in range(2)]
        but, bvt, bgt, bga32, bgab = [[Buf(), Buf(), Buf()] for _ in range(5)]

        def phaseB(g):
            gi = g % 2
            for db in range(32):
                pb = 2 + db % 6
                mm(K, banks[pb][:], [(vt[gi][:, a, db * 128:(db + 1) * 128], gab[gi][:, a, :]) for a in range(4)],
                   reads=[bvt[gi], bgab[gi]], writes=[bB[pb]])
                K.op(V, lambda: nc.vector.tensor_tensor(out=acc[:, db, :], in0=acc[:, db, :], in1=banks[pb][:], op=ALU.add),
                     reads=[bB[pb], bacc[db]], writes=[bacc[db]])

        for g in range(_P5G):
            gi = g % 2
            for a in range(4):
                eb = g * 4 + a
                ei = eb % 2
                u3 = eb % 3
                K.dma(P, ut[u3][:].rearrange("p a b -> p (a b)"), peer_uT[eb], writes=[but[u3]])
                K.dma(S, gt[u3][:], G_d[eb, :, pcs], writes=[bgt[u3]])
                mm(K, banks[ei][:], [(ut[u3][:, kc, :], h2p[:, kc, :]) for kc in range(32)],
                   reads=[but[u3], bh2p], writes=[bB[ei]])
                K.op(A, lambda: nc.scalar.activation(ga32[ei][:], banks[ei][:], AF.Gelu), reads=[bB[ei]], writes=[bga32[ei]])
                K.op(V, lambda: nc.vector.tensor_tensor(out=gab[gi][:, a, :], in0=ga32[ei][:], in1=gt[u3][:], op=ALU.mult),
                     reads=[bga32[ei], bgt[u3]], writes=[bgab[gi]])
            K.dma(P, vt[gi][:], pvv[g], writes=[bvt[gi]])
            if g > 0:
                phaseB(g - 1)
        phaseB(_P5G - 1)
        K.barrier()
        s5a.close()
        s5b = Scope(K)
        m1 = s5b.sb("m1", [128, 512], F32)
        r1 = s5b.sb("r1", [128, 512], F32)
        y1 = s5b.sb("y1", [128, 512], F32)
        y2 = s5b.sb("y2", [128, 512], F32)
        yq = s5b.sb("yq", [128, 512], F32)
        y1w = s5b.sb("y1w", [128, 4, 512], F32)
        y2w = s5b.sb("y2w", [128, 4, 512], F32)
        yq4 = s5b.sb("yq4", [128, 4, 512], F32)
        zl = [s5b.sb("zl%d" % i, [128, 4, 512], F32) for i in range(2)]
        yst = [s5b.sb("yst%d" % i, [128, 4, 512], F32) for i in range(2)]
        bst1, by1, by2, byq, by1w, by2w, byq4 = [Buf() for _ in range(7)]
        bzl, byst = [[Buf(), Buf()] for _ in range(2)]
        zTv = zT_d.rearrange("(kc p) n -> p kc n", p=128)
        oyv = o_yT.rearrange("(kc p) n -> p kc n", p=128)
        b4 = lambda ap: ap.unsqueeze(1).to_broadcast([128, 4, 512])
        K.dma(S, m1[:], stat1_d[0][:, pcs], writes=[bst1])
        K.dma(S, r1[:], stat1_d[1][:, pcs], writes=[bst1])
        K.op(P, lambda: nc.gpsimd.memset(y1w[:], 0.0), writes=[by1w])
        K.op(P, lambda: nc.gpsimd.memset(y2w[:], 0.0), writes=[by2w])
        for c4 in range(8 if _P5E else 0):
            e = c4 % 2
            dbs = range(c4 * 4, c4 * 4 + 4)
            ba4 = [bacc[db] for db in dbs]
            a4 = acc[:, c4 * 4:c4 * 4 + 4, :]
            K.dma(S, zl[e][:], zTv[:, c4 * 4:c4 * 4 + 4, pcs], writes=[bzl[e]])
            K.op(V, lambda: nc.vector.tensor_tensor(out=zl[e][:], in0=zl[e][:], in1=b4(m1[:]), op=ALU.subtract),
                 reads=[bzl[e], bst1], writes=[bzl[e]])
            K.op(P, lambda: nc.gpsimd.tensor_tensor(out=zl[e][:], in0=zl[e][:], in1=b4(r1[:]), op=ALU.mult),
                 reads=[bzl[e], bst1], writes=[bzl[e]])
            for q, db in enumerate(dbs):
                K.op(A, lambda: nc.scalar.activation(zl[e][:, q, :], zl[e][:, q, :], AF.Identity, bias=l1b5[:, db:db + 1],
                                                     scale=l1g5[:, db:db + 1]), reads=[bzl[e], bsm5], writes=[bzl[e]])
                K.op(A, lambda: nc.scalar.activation(acc[:, db, :], acc[:, db, :], AF.Identity, scale=MOD(5, db, cj)),
                     reads=[bacc[db], bmod], writes=[bacc[db]])
            K.op(V, lambda: nc.vector.scalar_tensor_tensor(out=a4, in0=zl[e][:], scalar=ALPHA, in1=a4,
                                                           op0=ALU.mult, op1=ALU.add),
                 reads=[bzl[e]] + ba4, writes=ba4)
            K.op(P, lambda: nc.gpsimd.tensor_tensor(out=y1w[:], in0=y1w[:], in1=a4, op=ALU.add),
                 reads=ba4 + [by1w], writes=[by1w])
            K.op(A, lambda: nc.scalar.activation(yq4[:], a4, AF.Square), reads=ba4, writes=[byq4])
            K.op(P, lambda: nc.gpsimd.tensor_tensor(out=y2w[:], in0=y2w[:], in1=yq4[:], op=ALU.add),
                 reads=[byq4, by2w], writes=[by2w])
        for (yw, byw, y_, by_) in ((y1w, by1w, y1, by1), (y2w, by2w, y2, by2)):
            K.op(V, lambda: nc.vector.tensor_tensor(out=y_[:], in0=yw[:, 0, :], in1=yw[:, 1, :], op=ALU.add),
                 reads=[byw], writes=[by_])
            K.op(V, lambda: nc.vector.tensor_tensor(out=y_[:], in0=y_[:], in1=yw[:, 2, :], op=ALU.add),
                 reads=[byw, by_], writes=[by_])
            K.op(V, lambda: nc.vector.tensor_tensor(out=y_[:], in0=y_[:], in1=yw[:, 3, :], op=ALU.add),
                 reads=[byw, by_], writes=[by_])
        ln_stats(y1, by1, y2, by2, yq, byq, 4096.0, 512)
        for c4 in range(8):
            e = c4 % 2
            dbs = range(c4 * 4, c4 * 4 + 4)
            ba4 = [bacc[db] for db in dbs]
            a4 = acc[:, c4 * 4:c4 * 4 + 4, :]
            K.op(V, lambda: nc.vector.tensor_tensor(out=yst[e][:], in0=a4, in1=b4(y1[:]), op=ALU.subtract),
                 reads=ba4 + [by1], writes=[byst[e]])
            K.op(P, lambda: nc.gpsimd.tensor_tensor(out=yst[e][:], in0=yst[e][:], in1=b4(y2[:]), op=ALU.mult),
                 reads=[byst[e], by2], writes=[byst[e]])
            for q, db in enumerate(dbs):
                K.op(A, lambda: nc.scalar.activation(yst[e][:, q, :], yst[e][:, q, :], AF.Identity, bias=l2b[:, db:db + 1],
                                                     scale=l2g[:, db:db + 1]), reads=[byst[e], bsm5], writes=[byst[e]])
            K.dma(S, oyv[:, c4 * 4:c4 * 4 + 4, pcs], yst[e][:], reads=[byst[e]], final=True)
        K.barrier()
        s5b.close()
    sc5.close()

    K.finish()
    return nc, ins_used


def _fm(v, nchunk):
    return np.ascontiguousarray(np.asarray(v, np.float32).reshape(nchunk, 128).T)


def _rope_tables(pos):
    row = (pos // 64).astype(np.float32)
    col = (pos % 64).astype(np.float32)
    inv = (10000.0 ** (-np.arange(16, dtype=np.float32) / 16)).astype(np.float32)
    ar = row[:, None] * inv
    ac = col[:, None] * inv
    ang = np.concatenate([ar, ar, ac, ac], -1).astype(np.float32)
    return np.ascontiguousarray(np.cos(ang).T.astype(np.float32)), np.ascontiguousarray(np.sin(ang).T.astype(np.float32))


def _rmat():
    R = np.zeros((64, 64), np.float32)
    for base in (0, 32):
        for i in range(16):
            R[base + i, base + 16 + i] = -1.0
            R[base + 16 + i, base + i] = 1.0
    return np.ascontiguousarray(R.T)


def sample_cols(hf):
    own = np.arange(hf * 512, hf * 512 + 512)
    if hf == 0:
        other = np.arange(512, 1024)
    else:
        other = np.concatenate([np.arange(497, 512), np.arange(0, 497)])
    return own, other


def prep_shared(inp):
    f = lambda k: np.asarray(inp[k], np.float32)
    sh = {}
    sh["w_ada"] = np.ascontiguousarray(f("w_ada")[0])
    sh["b_adaT"] = _fm(f("b_ada")[0], 192)
    sh["w_in"] = np.ascontiguousarray(f("w_in")[0])
    sh["g_qT"] = _fm(f("g_q")[0], 6)
    sh["g_kvT"] = _fm(f("g_kv")[0], 4)
    sh["w_uq"] = np.ascontiguousarray(f("w_uq")[0])
    sh["w_ukv"] = np.ascontiguousarray(f("w_ukv")[0])
    sh["w_dwT"] = np.ascontiguousarray(f("w_dw")[0].T.reshape(16, 128, 31).transpose(1, 0, 2))
    sh["b_dwT"] = _fm(f("b_dw")[0], 16)
    sh["g_cnT"] = _fm(f("g_cn")[0], 16)
    sh["b_cnT"] = _fm(f("b_cn")[0], 16)
    sh["w_out"] = np.ascontiguousarray(f("w_out")[0])
    sh["ln1_gT"] = _fm(f("ln1_g")[0], 32)
    sh["ln1_bT"] = _fm(f("ln1_b")[0], 32)
    sh["w_pq"] = np.ascontiguousarray(f("w_pq")[0])
    sk = f("sub_keys")[0]
    skT = sk.reshape(16, 128, 2, 128).transpose(3, 0, 2, 1)
    sh["skT"] = np.ascontiguousarray(skT.reshape(128, 32, 128))
    pu = f("peer_u")[0]
    sh["peer_uT"] = np.ascontiguousarray(pu.reshape(128, 128, 32, 128).transpose(0, 3, 2, 1)).reshape(128, 128, 4096)
    sh["peer_v"] = np.ascontiguousarray(f("peer_v")[0])
    sh["ln2_gT"] = _fm(f("ln2_g")[0], 32)
    sh["ln2_bT"] = _fm(f("ln2_b")[0], 32)
    sh["rmatT"] = _rmat()
    return sh


def prep_core(inp, c):
    f = lambda k: np.asarray(inp[k], np.float32)
    b, hf = c // 2, c % 2
    own, other = sample_cols(hf)
    xp = f("x_prompt")[4 * c:4 * c + 4].reshape(NP_, D)
    xs = f("x_sample")[b]
    xall = np.concatenate([xp, xs[own], xs[other]], 0)
    m = {}
    m["xT"] = np.ascontiguousarray(xall.T)
    cond = np.stack([f("c_ctx"), f("c")[b]], -1)
    m["condT"] = np.ascontiguousarray(cond.reshape(32, 128, 2).transpose(1, 0, 2))
    m["cache_ckvT"] = np.ascontiguousarray(f("cache_ckv")[b, 0].T.reshape(4, 128, 256).transpose(1, 0, 2))
    m["cache_kpeT"] = np.ascontiguousarray(f("cache_kpe")[b, 0].T)
    cosT, sinT = _rope_tables(np.concatenate([own, other]))
    m["cosT"] = cosT
    m["sinT"] = sinT
    hm = np.zeros((128, 2), np.float32)
    hm[:, 0] = 1.0 if hf == 1 else 0.0
    hm[:, 1] = 1.0 if hf == 0 else 0.0
    m["halo_mask"] = hm
    return m


_CACHE = {}


def kernel(**inputs):
    if "nc" not in _CACHE:
        _CACHE["nc"] = build()
    nc, used = _CACHE["nc"]
    sh = prep_shared(inputs)
    in_maps = []
    for c in range(8):
        m = prep_core(inputs, c)
        m.update(sh)
        in_maps.append({k: m[k] for k in used})
    res = run_bass_kernel_spmd(nc, in_maps, core_ids=list(range(8)))
    y_prompt = np.zeros((32, 256, D), np.float32)
    y_sample = np.zeros((4, 1024, D), np.float32)
    new_ckv = np.zeros((32, 1, 256, 512), np.float32)
    new_kpe = np.zeros((32, 1, 256, 64), np.float32)
    for c in range(8):
        r = res.results[c]
        b, hf = c // 2, c % 2
        yT = np.asarray(r["o_yT"], np.float32)
        y_prompt[4 * c:4 * c + 4] = yT[:, :NP_].T.reshape(4, 256, D)
        y_sample[b, hf * 512:(hf + 1) * 512] = yT[:, NP_:].T
        new_ckv[4 * c:4 * c + 4, 0] = np.asarray(r["o_ckvT"], np.float32).T.reshape(4, 256, 512)
        new_kpe[4 * c:4 * c + 4, 0] = np.asarray(r["o_kpeT"], np.float32).T.reshape(4, 256, 64)
    return (y_prompt, y_sample, new_ckv, new_kpe)
```

```python
import os
import numpy as np
import ml_dtypes
from contextlib import ExitStack
import concourse.bass as bass
import concourse.mybir as mybir
from concourse.bass_utils import run_bass_kernel_spmd

F32 = mybir.dt.float32
BF16 = mybir.dt.bfloat16
AF = mybir.ActivationFunctionType
ALU = mybir.AluOpType
AX = mybir.AxisListType

D = 4096
NP_ = 1024
NS = 512
NOWN = NP_ + NS
NALL = 2048
NKEY = 2304
ALPHA = 2.0 ** 0.25
EPS = 1e-6
SCALE = 192.0 ** -0.5
YW = 4 * 286 + 542


class Buf:
    __slots__ = ("w", "r")

    def __init__(self):
        self.w = None
        self.r = {}


class Q:
    def __init__(self, K, name, eng, is_pe=False):
        self.name = name
        self.eng = eng
        self.is_pe = is_pe
        self.sem = K.newsem("q_" + name)
        self.cnt = 0
        self.waited = {}
        self.dsems = []
        self.dvals = []
        self.dma_i = 0
        self.lazy = False

    def wait(self, tok):
        sem, val, owner = tok
        key = id(sem)
        if self.waited.get(key, 0) >= val:
            return
        self.eng.wait_ge(sem, val)
        self.waited[key] = val


class Kern:
    NDMA = 10

    def __init__(self, nc):
        self.nc = nc
        self.stack = ExitStack()
        self.pe = Q(self, "pe", nc.tensor, is_pe=True)
        self.act = Q(self, "act", nc.scalar)
        self.dve = Q(self, "dve", nc.vector)
        self.pool = Q(self, "pool", nc.gpsimd)
        self.sp = Q(self, "sp", nc.sync)
        self.qs = [self.pe, self.act, self.dve, self.pool, self.sp]
        for q in (self.sp, self.pool):
            for i in range(self.NDMA):
                q.dsems.append(self.newsem("d_%s%d" % (q.name, i)))
                q.dvals.append(0)
        self.final = []
        self.uid = 0

    def newsem(self, name):
        return self.stack.enter_context(self.nc.semaphore(name))

    def _deps(self, q, reads, writes):
        for b in reads:
            if b.w is not None and not (b.w[2] is q and q.is_pe):
                q.wait(b.w)
        for b in writes:
            if b.w is not None and not (b.w[2] is q and q.is_pe):
                q.wait(b.w)
            for owner, t in b.r.items():
                if owner is not q or not q.is_pe:
                    q.wait(t)

    def _mark(self, tok, reads, writes):
        for b in reads:
            b.r[tok[2]] = tok
        for b in writes:
            b.w = tok
            b.r = {}

    def op(self, q, fn, reads=(), writes=(), inc=True):
        self._deps(q, reads, writes)
        ins = fn()
        if inc:
            q.cnt += 1
            ins.then_inc(q.sem, 1)
            q.lazy = False
            tok = (q.sem, q.cnt, q)
        else:
            q.lazy = True
            tok = (q.sem, q.cnt + 1, q)
        self._mark(tok, reads, writes)
        return tok

    def dma(self, q, out, in_, reads=(), writes=(), final=False):
        slot = q.dma_i % self.NDMA
        q.dma_i += 1
        sem = q.dsems[slot]
        if q.dvals[slot] > 0:
            q.wait((sem, q.dvals[slot], sem))
        self._deps(q, reads, writes)
        ins = q.eng.dma_start(out=out, in_=in_)
        q.dvals[slot] += 16
        ins.then_inc(sem, 16)
        tok = (sem, q.dvals[slot], sem)
        self._mark(tok, reads, writes)
        if final:
            self.final.append(tok)
        return tok

    def barrier(self):
        toks = []
        for q in self.qs:
            assert not q.lazy, q.name
            if q.cnt > 0:
                toks.append((q.sem, q.cnt, q))
            for s, v in zip(q.dsems, q.dvals):
                if v > 0:
                    toks.append((s, v, s))
        for q in self.qs:
            for t in toks:
                if t[2] is not q:
                    q.wait(t)

    def name(self, base):
        self.uid += 1
        return "%s_%d" % (base, self.uid)

    def finish(self):
        self.barrier()


class Scope:
    def __init__(self, K):
        self.K = K
        self.stack = ExitStack()

    def sb(self, name, shape, dtype):
        return self.stack.enter_context(self.K.nc.sbuf_tensor(self.K.name(name), list(shape), dtype))

    def close(self):
        self.stack.close()


def mm(K, out, pairs, reads, writes, first=True, last=True):
    nc = K.nc
    n = len(pairs)
    tok = None
    for i, (l, r) in enumerate(pairs):
        st = first and i == 0
        sp = last and i == n - 1
        tok = K.op(K.pe, lambda: nc.tensor.matmul(out, l, r, start=st, stop=sp),
                   reads=reads, writes=writes, inc=(i == n - 1))
    return tok


def build(dbg=False, stop_after=99):
    nc = bass.Bass("TRN2", target_bir_lowering=False)
    K = Kern(nc)
    V, A, P, S = K.dve, K.act, K.pool, K.sp
    ins_used = []

    def din(name, shape, dt=F32):
        ins_used.append(name)
        return nc.dram_tensor(name, list(shape), dt, kind="ExternalInput").ap()

    def dout(name, shape, dt=F32):
        return nc.dram_tensor(name, list(shape), dt, kind="ExternalOutput").ap()

    def dscr(name, shape, dt):
        return nc.dram_tensor(name, list(shape), dt, kind=("ExternalOutput" if dbg else "Internal")).ap()

    top = Scope(K)
    banks = [K.stack.enter_context(nc.psum_tensor("bank%d" % i, [128, 512], F32)) for i in range(8)]
    bB = [Buf() for _ in range(8)]
    ident_f = top.sb("ident_f", [128, 128], F32)
    ident_b = top.sb("ident_b", [128, 128], BF16)
    ones_f = top.sb("ones_f", [128, 128], F32)
    mod = top.sb("mod", [128, 192, 2], F32)
    opsc = top.sb("opsc", [128, 2, 32, 2], F32)
    bconst = Buf()
    bmod = Buf()

    K.op(P, lambda: nc.gpsimd.memset(ident_f[:], 0.0), writes=[bconst])
    K.op(P, lambda: nc.gpsimd.affine_select(out=ident_f[:], in_=ident_f[:], pattern=[[-1, 128]],
                                            compare_op=ALU.not_equal, fill=1.0, base=0, channel_multiplier=1),
         reads=[bconst], writes=[bconst])
    K.op(P, lambda: nc.gpsimd.tensor_copy(out=ident_b[:], in_=ident_f[:]), reads=[bconst], writes=[bconst])
    K.op(P, lambda: nc.gpsimd.memset(ones_f[:], 1.0), writes=[bconst])
    epsc = top.sb("epsc", [128, 1], F32)
    K.op(P, lambda: nc.gpsimd.memset(epsc[:], EPS), writes=[bconst])

    def MOD(s, kc, j):
        return mod[:, s * 32 + kc, j:j + 1]

    def OPSC(w, kc, j):
        return opsc[:, w, kc, j:j + 1]

    condT = din("condT", [128, 32, 2])
    w_ada = din("w_ada", [D, 6 * D])
    b_adaT = din("b_adaT", [128, 192])
    sc0 = Scope(K)
    cnd = top.sb("cnd", [128, 32, 2], F32)
    sil = top.sb("sil", [128, 32, 2], BF16)
    bad = top.sb("bad", [128, 192], F32)
    bc = Buf()
    K.dma(S, cnd[:], condT, writes=[bc])
    K.dma(S, bad[:], b_adaT, writes=[bc])
    K.op(A, lambda: nc.scalar.activation(sil[:], cnd[:], AF.Silu), reads=[bc], writes=[bc])
    wav = w_ada.rearrange("(kc p) n -> p kc n", p=128)
    wr = [sc0.sb("wada%d" % i, [128, 32, 512], BF16) for i in range(3)]
    bwr = [Buf() for _ in range(3)]
    for ct in range(16):
        i = ct % 3
        K.dma(P, wr[i][:], wav[:, :, ct * 512:(ct + 1) * 512], writes=[bwr[i]])
        pb = ct % 2
        for ob in range(4):
            mm(K, banks[pb][:, ob * 2:ob * 2 + 2],
               [(wr[i][:, kc, ob * 128:(ob + 1) * 128], sil[:, kc, :]) for kc in range(32)],
               reads=[bwr[i], bc], writes=[bB[pb]])
        K.op(V, lambda: nc.vector.tensor_tensor(
            out=mod[:, ct * 4:(ct + 1) * 4, :],
            in0=banks[pb][:, 0:8].rearrange("p (a b) -> p a b", b=2),
            in1=bad[:, ct * 4:(ct + 1) * 4].unsqueeze(2).to_broadcast([128, 4, 2]), op=ALU.add),
            reads=[bB[pb], bc], writes=[bmod])
    K.op(V, lambda: nc.vector.tensor_scalar(out=opsc[:, 0, :, :], in0=mod[:, 32:64, :], scalar1=1.0, scalar2=None,
                                            op0=ALU.add), reads=[bmod], writes=[bmod])
    K.barrier()
    sc0.close()
    if stop_after <= 0:
        K.finish()
        return nc, ins_used

    xT = din("xT", [D, NALL])
    w_in = din("w_in", [D, 5440])
    g_qT = din("g_qT", [128, 6])
    g_kvT = din("g_kvT", [128, 4])
    w_dwT = din("w_dwT", [128, 16, 31])
    b_dwT = din("b_dwT", [128, 16])
    halo_mask = din("halo_mask", [128, 2])
    o_ckvT = dout("o_ckvT", [512, NP_])
    o_kpeT = dout("o_kpeT", [64, NP_])
    qcT_d = dscr("qcT_d", [768, NOWN], BF16)
    rstdq_d = dscr("rstdq_d", [128, NOWN], F32)
    ckvT_d = dscr("ckvT_d", [512, NALL], BF16)
    kpe_d = dscr("kpe_d", [64, NALL], F32)
    conv_d = dscr("conv_d", [2048, NOWN], F32)
    cstat_d = dscr("cstat_d", [2, 128, NOWN], F32)
    xv = xT.rearrange("(kc p) n -> p kc n", p=128)
    wiv = w_in.rearrange("(kc p) n -> p kc n", p=128)

    sc1 = Scope(K)
    gq = sc1.sb("gq", [128, 6], F32)
    gkv = sc1.sb("gkv", [128, 4], F32)
    wdw = sc1.sb("wdw", [128, 16, 31], F32)
    bdw = sc1.sb("bdw", [128, 16], F32)
    hmask = sc1.sb("hmask", [128, 2], F32)
    bsm = Buf()
    for t_, s_ in ((gq, g_qT), (gkv, g_kvT), (wdw, w_dwT), (bdw, b_dwT), (hmask, halo_mask)):
        K.dma(S, t_[:], s_, writes=[bsm])
    wt = [sc1.sb("wt%d" % i, [128, 32, 256], BF16) for i in range(2)]
    bwt = [Buf() for _ in range(2)]
    wti = [0]

    def load_w(c0, ncol, c1=None):
        i = wti[0] % 2
        wti[0] += 1
        if c1 is None:
            K.dma(P, wt[i][:, :, 0:ncol], wiv[:, :, c0:c0 + ncol], writes=[bwt[i]])
        else:
            K.dma(P, wt[i][:, :, 0:128], wiv[:, :, c0:c0 + 128], writes=[bwt[i]])
            K.dma(P, wt[i][:, :, 128:256], wiv[:, :, c1:c1 + 128], writes=[bwt[i]])
        return i

    def modulate(hT, bh, ncols, col0, ranges, sc):
        xs = [sc.sb("xs%d" % i, [128, ncols], F32) for i in range(2)]
        bxs = [Buf() for _ in range(2)]
        for kc in range(32):
            i = kc % 2
            K.dma(S, xs[i][:], xv[:, kc, col0:col0 + ncols], writes=[bxs[i]])
            for ri, (lo, hi, j) in enumerate(ranges):
                if (kc + ri) % 2 == 0:
                    K.op(V, lambda: nc.vector.tensor_scalar(out=hT[:, kc, lo:hi], in0=xs[i][:, lo:hi],
                                                            scalar1=OPSC(0, kc, j), scalar2=MOD(0, kc, j),
                                                            op0=ALU.mult, op1=ALU.add),
                         reads=[bxs[i], bmod], writes=[bh])
                else:
                    K.op(A, lambda: nc.scalar.activation(hT[:, kc, lo:hi], xs[i][:, lo:hi], AF.Identity,
                                                         bias=MOD(0, kc, j), scale=OPSC(0, kc, j)),
                         reads=[bxs[i], bmod], writes=[bh])

    pbi = [0]

    def next_bank(lo=0, hi=4):
        b = lo + pbi[0] % (hi - lo)
        pbi[0] += 1
        return b

    def rstd_from_sumsq(sq, bsq, ncols, nfeat, sc):
        for t in range(0, ncols, 512):
            w = min(512, ncols - t)
            mm(K, banks[7][:, 0:w], [(ones_f[:], sq[:, t:t + w])], reads=[bsq, bconst], writes=[bB[7]])
            K.op(A, lambda: nc.scalar.activation(sq[:, t:t + w], banks[7][:, 0:w], AF.Sqrt, bias=epsc[:, 0:1],
                                                 scale=1.0 / nfeat), reads=[bB[7], bconst], writes=[bsq])
        K.op(V, lambda: nc.vector.reciprocal(out=sq[:, 0:ncols], in_=sq[:, 0:ncols]), reads=[bsq], writes=[bsq])

    def proj_kv(hT, bh, ncols, col0, sc, is_own):
        ntile = ncols // 512
        raw = sc.sb("ckvraw", [128, 4, ncols], F32)
        sq = sc.sb("sqkv", [128, ncols], F32)
        sqt = [sc.sb("sqt%d" % i, [128, 512], F32) for i in range(2)]
        braw, bsq = Buf(), Buf()
        bsqt = [Buf(), Buf()]
        K.op(P, lambda: nc.gpsimd.memset(sq[:], 0.0), writes=[bsq])
        k = 0
        for tl in range(2):
            i = load_w(768 + tl * 256, 256)
            for ob in range(2):
                b4 = tl * 2 + ob
                for t in range(ntile):
                    pb = next_bank()
                    mm(K, banks[pb][:], [(wt[i][:, kc, ob * 128:(ob + 1) * 128], hT[:, kc, t * 512:(t + 1) * 512])
                                         for kc in range(32)], reads=[bwt[i], bh], writes=[bB[pb]])
                    K.op(A, lambda: nc.scalar.copy(raw[:, b4, t * 512:(t + 1) * 512], banks[pb][:]),
                         reads=[bB[pb]], writes=[braw])
                    j = k % 2
                    k += 1
                    K.op(A, lambda: nc.scalar.activation(sqt[j][:], banks[pb][:], AF.Square),
                         reads=[bB[pb]], writes=[bsqt[j]])
                    K.op(V, lambda: nc.vector.tensor_tensor(out=sq[:, t * 512:(t + 1) * 512],
                                                            in0=sq[:, t * 512:(t + 1) * 512], in1=sqt[j][:], op=ALU.add),
                         reads=[bsqt[j], bsq], writes=[bsq])
        rstd_from_sumsq(sq, bsq, ncols, 512.0, sc)
        nrm = [sc.sb("nrm%d" % i, [128, 512], F32) for i in range(2)]
        nrb = [sc.sb("nrb%d" % i, [128, 512], BF16) for i in range(2)]
        bn = [Buf(), Buf()]
        bnb = [Buf(), Buf()]
        k = 0
        for b4 in range(4):
            for t in range(ntile):
                j = k % 2
                k += 1
                cs = slice(t * 512, (t + 1) * 512)
                K.op(V, lambda: nc.vector.scalar_tensor_tensor(out=nrm[j][:], in0=raw[:, b4, cs], scalar=gkv[:, b4:b4 + 1],
                                                               in1=sq[:, cs], op0=ALU.mult, op1=ALU.mult),
                     reads=[braw, bsq, bsm], writes=[bn[j]])
                if is_own and t < 2:
                    K.dma(S, o_ckvT[b4 * 128:(b4 + 1) * 128, cs], nrm[j][:], reads=[bn[j]], final=True)
                K.op(A, lambda: nc.scalar.copy(nrb[j][:], nrm[j][:]), reads=[bn[j]], writes=[bnb[j]])
                K.dma(S, ckvT_d[b4 * 128:(b4 + 1) * 128, col0 + t * 512:col0 + (t + 1) * 512], nrb[j][:],
                      reads=[bnb[j]])
        i = load_w(1280, 64)
        kraw = sc.sb("kraw", [64, ncols], F32)
        bk = Buf()
        for t in range(ntile):
            pb = next_bank()
            mm(K, banks[pb][0:64, :], [(wt[i][:, kc, 0:64], hT[:, kc, t * 512:(t + 1) * 512]) for kc in range(32)],
               reads=[bwt[i], bh], writes=[bB[pb]])
            K.op(A, lambda: nc.scalar.copy(kraw[:, t * 512:(t + 1) * 512], banks[pb][0:64, :]),
                 reads=[bB[pb]], writes=[bk])
        K.dma(S, kpe_d[:, col0:col0 + ncols], kraw[:], reads=[bk])
        if is_own:
            K.dma(S, o_kpeT[:, :], kraw[:, 0:NP_], reads=[bk], final=True)

    scx = Scope(K)
    hTo = scx.sb("hTo", [128, 32, 512], BF16)
    bho = Buf()
    modulate(hTo, bho, 512, NOWN, [(0, 512, 1)], scx)
    proj_kv(hTo, bho, 512, NOWN, scx, False)
    K.barrier()
    scx.close()

    NH = NOWN + 16
    hT = sc1.sb("hT", [128, 32, NH], BF16)
    bh = Buf()
    sca = Scope(K)
    modulate(hT, bh, NH, 0, [(0, NP_, 0), (NP_, NH, 1)], sca)
    K.barrier()
    sca.close()
    sca = Scope(K)
    proj_kv(hT, bh, NOWN, 0, sca, True)
    sqq = sca.sb("sqq", [128, NOWN], F32)
    sqt2 = [sca.sb("sqt2%d" % i, [128, 512], F32) for i in range(2)]
    qst = [sca.sb("qst%d" % i, [128, NOWN], BF16) for i in range(2)]
    bsqq = Buf()
    bsqt2 = [Buf(), Buf()]
    bqst = [Buf(), Buf()]
    K.op(P, lambda: nc.gpsimd.memset(sqq[:], 0.0), writes=[bsqq])
    k = 0
    for tl in range(3):
        i = load_w(tl * 256, 256)
        for ob in range(2):
            qb = tl * 2 + ob
            for t in range(3):
                cs = slice(t * 512, (t + 1) * 512)
                pb = next_bank()
                mm(K, banks[pb][:], [(wt[i][:, kc, ob * 128:(ob + 1) * 128], hT[:, kc, cs]) for kc in range(32)],
                   reads=[bwt[i], bh], writes=[bB[pb]])
                K.op(A, lambda: nc.scalar.activation(qst[qb % 2][:, cs], banks[pb][:], AF.Identity,
                                                     scale=gq[:, qb:qb + 1]),
                     reads=[bB[pb], bsm], writes=[bqst[qb % 2]])
                j = k % 2
                k += 1
                K.op(A, lambda: nc.scalar.activation(sqt2[j][:], banks[pb][:], AF.Square),
                     reads=[bB[pb]], writes=[bsqt2[j]])
                K.op(V, lambda: nc.vector.tensor_tensor(out=sqq[:, cs], in0=sqq[:, cs], in1=sqt2[j][:], op=ALU.add),
                     reads=[bsqt2[j], bsqq], writes=[bsqq])
            K.dma(S, qcT_d[qb * 128:(qb + 1) * 128, :], qst[qb % 2][:], reads=[bqst[qb % 2]])
    rstd_from_sumsq(sqq, bsqq, NOWN, 768.0, sca)
    K.dma(S, rstdq_d, sqq[:], reads=[bsqq])
    K.barrier()
    sca.close()
    if stop_after <= 1:
        K.finish()
        return nc, ins_used

    scb = Scope(K)
    ypad = [scb.sb("ypad%d" % i, [128, YW], BF16) for i in range(2)]
    dg = [scb.sb("dg%d" % i, [128, 31, 128], BF16) for i in range(2)]
    sg = [scb.sb("sg%d" % i, [128, 512], F32) for i in range(2)]
    yh = scb.sb("yh", [128, 16], F32)
    cv = [scb.sb("cv%d" % i, [128, NOWN], F32) for i in range(2)]
    cq = scb.sb("cq", [128, NOWN], F32)
    s1c = scb.sb("s1c", [128, NOWN], F32)
    s2c = scb.sb("s2c", [128, NOWN], F32)
    byp = [Buf(), Buf()]
    bdg = [Buf(), Buf()]
    bsg = [Buf(), Buf()]
    byh, bcq, bs1, bs2 = Buf(), Buf(), Buf(), Buf()
    bcv = [Buf(), Buf()]
    for i in range(2):
        K.op(P, lambda: nc.gpsimd.memset(ypad[i][:], 0.0), writes=[byp[i]])
    K.op(P, lambda: nc.gpsimd.memset(s1c[:], 0.0), writes=[bs1])
    K.op(P, lambda: nc.gpsimd.memset(s2c[:], 0.0), writes=[bs2])

    def ywin(yp, t, k):
        if t < 2:
            return yp[:, t * 572:(t + 1) * 572].rearrange("p (s w) -> p s w", w=286)[:, :, k:k + 256]
        return yp[:, 1144 + k:1144 + k + 512]

    def conv_chunk(j):
        yp = ypad[j % 2]
        for t in range(3):
            if t < 2:
                o = banks[4 + t][:].rearrange("p (s w) -> p s w", w=256)
            else:
                o = banks[4 + t][:]
            mm(K, o, [(dg[j % 2][:, k, :], ywin(yp, t, k)) for k in range(31)],
               reads=[bdg[j % 2], byp[j % 2]], writes=[bB[4 + t]])
            cs = slice(t * 512, (t + 1) * 512)
            K.op(A, lambda: nc.scalar.activation(cv[j % 2][:, cs], banks[4 + t][:], AF.Identity,
                                                 bias=bdw[:, j:j + 1]),
                 reads=[bB[4 + t], bsm], writes=[bcv[j % 2]])
        K.dma(S, conv_d[j * 128:(j + 1) * 128, :], cv[j % 2][:], reads=[bcv[j % 2]])
        K.op(V, lambda: nc.vector.tensor_tensor(out=s1c[:], in0=s1c[:], in1=cv[j % 2][:], op=ALU.add),
             reads=[bcv[j % 2], bs1], writes=[bs1])
        K.op(A, lambda: nc.scalar.activation(cq[:], cv[j % 2][:], AF.Square), reads=[bcv[j % 2]], writes=[bcq])
        K.op(V, lambda: nc.vector.tensor_tensor(out=s2c[:], in0=s2c[:], in1=cq[:], op=ALU.add),
             reads=[bcq, bs2], writes=[bs2])

    for jp in range(8):
        ia = load_w(1344 + 256 * jp, 256)
        ig = load_w(3392 + 256 * jp, 256)
        for jj in range(2):
            j = jp * 2 + jj
            yp = ypad[j % 2]
            K.op(V, lambda: nc.vector.tensor_tensor(
                out=dg[j % 2][:], in0=ident_b[:].unsqueeze(1).to_broadcast([128, 31, 128]),
                in1=wdw[:, j, :].unsqueeze(2).to_broadcast([128, 31, 128]), op=ALU.mult),
                reads=[bconst, bsm], writes=[bdg[j % 2]])
            for t in range(4):
                if t < 3:
                    cs = slice(t * 512, (t + 1) * 512)
                    w_ = 512
                else:
                    cs = slice(NOWN, NOWN + 16)
                    w_ = 16
                pa = next_bank()
                pg = next_bank()
                mm(K, banks[pa][:, 0:w_], [(wt[ia][:, kc, jj * 128:(jj + 1) * 128], hT[:, kc, cs]) for kc in range(32)],
                   reads=[bwt[ia], bh], writes=[bB[pa]])
                mm(K, banks[pg][:, 0:w_], [(wt[ig][:, kc, jj * 128:(jj + 1) * 128], hT[:, kc, cs]) for kc in range(32)],
                   reads=[bwt[ig], bh], writes=[bB[pg]])
                s_ = sg[t % 2]
                K.op(A, lambda: nc.scalar.activation(s_[:, 0:w_], banks[pg][:, 0:w_], AF.Sigmoid),
                     reads=[bB[pg]], writes=[bsg[t % 2]])
                if t < 2:
                    o = yp[:, t * 572:(t + 1) * 572].rearrange("p (s w) -> p s w", w=286)[:, :, 15:271]
                    K.op(V, lambda: nc.vector.tensor_tensor(
                        out=o, in0=banks[pa][:].rearrange("p (s w) -> p s w", w=256),
                        in1=s_[:].rearrange("p (s w) -> p s w", w=256), op=ALU.mult),
                        reads=[bB[pa], bsg[t % 2]], writes=[byp[j % 2]])
                elif t == 2:
                    K.op(V, lambda: nc.vector.tensor_tensor(out=yp[:, 1159:1159 + 512], in0=banks[pa][:], in1=s_[:],
                                                            op=ALU.mult),
                         reads=[bB[pa], bsg[t % 2]], writes=[byp[j % 2]])
                else:
                    K.op(V, lambda: nc.vector.tensor_tensor(out=yh[:], in0=banks[pa][:, 0:16], in1=s_[:, 0:16],
                                                            op=ALU.mult),
                         reads=[bB[pa], bsg[t % 2]], writes=[byh])
                    K.op(V, lambda: nc.vector.tensor_scalar(out=yp[:, 1144:1159], in0=yh[:, 0:15],
                                                            scalar1=hmask[:, 0:1], scalar2=None, op0=ALU.mult),
                         reads=[byh, bsm], writes=[byp[j % 2]])
                    K.op(V, lambda: nc.vector.tensor_scalar(out=yp[:, 1671:1686], in0=yh[:, 0:15],
                                                            scalar1=hmask[:, 1:2], scalar2=None, op0=ALU.mult),
                         reads=[byh, bsm], writes=[byp[j % 2]])
            if j > 0:
                conv_chunk(j - 1)
    conv_chunk(15)
    for t in range(3):
        cs = slice(t * 512, (t + 1) * 512)
        mm(K, banks[0][:], [(ones_f[:], s1c[:, cs])], reads=[bs1, bconst], writes=[bB[0]])
        mm(K, banks[1][:], [(ones_f[:], s2c[:, cs])], reads=[bs2, bconst], writes=[bB[1]])
        K.op(V, lambda: nc.vector.tensor_scalar(out=s1c[:, cs], in0=banks[0][:], scalar1=1.0 / 2048, scalar2=None,
                                                op0=ALU.mult), reads=[bB[0]], writes=[bs1])
        K.op(V, lambda: nc.vector.tensor_tensor(out=cq[:, cs], in0=s1c[:, cs], in1=s1c[:, cs], op=ALU.mult),
             reads=[bs1], writes=[bcq])
        K.op(V, lambda: nc.vector.scalar_tensor_tensor(out=s2c[:, cs], in0=banks[1][:], scalar=1.0 / 2048,
                                                       in1=cq[:, cs], op0=ALU.mult, op1=ALU.subtract),
             reads=[bB[1], bcq], writes=[bs2])
    K.op(A, lambda: nc.scalar.activation(s2c[:], s2c[:], AF.Sqrt, bias=epsc[:, 0:1], scale=1.0),
         reads=[bs2, bconst], writes=[bs2])
    K.op(V, lambda: nc.vector.reciprocal(out=s2c[:], in_=s2c[:]), reads=[bs2], writes=[bs2])
    K.dma(S, cstat_d[0], s1c[:], reads=[bs1])
    K.dma(S, cstat_d[1], s2c[:], reads=[bs2])
    K.barrier()
    scb.close()
    sc1.close()
    if stop_after <= 2:
        K.finish()
        return nc, ins_used

    w_uq = din("w_uq", [768, 3072])
    w_ukv = din("w_ukv", [512, 4096])
    cache_ckvT = din("cache_ckvT", [128, 4, 256])
    cache_kpeT = din("cache_kpeT", [64, 256])
    cosT = din("cosT", [64, 1024])
    sinT = din("sinT", [64, 1024])
    rmatT = din("rmatT", [64, 64])
    attnT_d = dscr("attnT_d", [2048, NOWN], BF16)
    wuqv = w_uq.rearrange("(kc p) n -> p kc n", p=128)
    wukvv = w_ukv.rearrange("(kc p) n -> p kc n", p=128)
    sc2 = Scope(K)
    ckv = sc2.sb("ckv", [128, 4, NKEY], BF16)
    kpe = sc2.sb("kpeb", [64, NKEY], BF16)
    qc = sc2.sb("qc", [128, 6, NOWN], BF16)
    rq = sc2.sb("rq", [128, NOWN], F32)
    kraw = sc2.sb("kraw2", [64, NALL], F32)
    cos = sc2.sb("cos", [64, 1024], F32)
    sin = sc2.sb("sin", [64, 1024], F32)
    rm = sc2.sb("rm", [64, 64], F32)
    rt1 = sc2.sb("rt1", [64, 512], F32)
    rt2 = sc2.sb("rt2", [64, 512], F32)
    qpf = sc2.sb("qpf", [64, 512], F32)
    bckv, bkpe, bqc, brq, bkraw, btab, brt1, brt2, bqpf = [Buf() for _ in range(9)]
    K.dma(S, ckv[:, :, 0:NALL], ckvT_d.rearrange("(kc p) n -> p kc n", p=128), writes=[bckv])
    K.dma(P, ckv[:, :, NALL:NKEY], cache_ckvT, writes=[bckv])
    K.dma(S, kraw[:], kpe_d, writes=[bkraw])
    K.dma(P, kpe[:, NALL:NKEY], cache_kpeT, writes=[bkpe])
    K.dma(S, qc[:], qcT_d.rearrange("(kc p) n -> p kc n", p=128), writes=[bqc])
    K.dma(S, rq[:], rstdq_d, writes=[brq])
    K.dma(S, cos[:], cosT, writes=[btab])
    K.dma(S, sin[:], sinT, writes=[btab])
    K.dma(S, rm[:], rmatT, writes=[btab])
    K.op(A, lambda: nc.scalar.copy(kpe[:, 0:NP_], kraw[:, 0:NP_]), reads=[bkraw], writes=[bkpe])

    def rope(dst, bdst, src, bsrc, tab0):
        pb = next_bank(0, 6)
        mm(K, banks[pb][0:64, :], [(rm[:, :], src)], reads=[btab, bsrc], writes=[bB[pb]])
        K.op(V, lambda: nc.vector.tensor_tensor(out=rt1[:], in0=src, in1=cos[:, tab0:tab0 + 512], op=ALU.mult),
             reads=[bsrc, btab], writes=[brt1])
        K.op(V, lambda: nc.vector.tensor_tensor(out=rt2[:], in0=banks[pb][0:64, :], in1=sin[:, tab0:tab0 + 512],
                                                op=ALU.mult), reads=[bB[pb], btab], writes=[brt2])
        K.op(V, lambda: nc.vector.tensor_tensor(out=dst, in0=rt1[:], in1=rt2[:], op=ALU.add),
             reads=[brt1, brt2], writes=[bdst])

    for t in range(2):
        rope(kpe[:, NP_ + t * 512:NP_ + (t + 1) * 512], bkpe, kraw[:, NP_ + t * 512:NP_ + (t + 1) * 512], bkraw, t * 512)

    wq = [sc2.sb("wq%d" % i, [128, 6, 192], BF16) for i in range(2)]
    wkv = [sc2.sb("wkv%d" % i, [128, 4, 256], BF16) for i in range(2)]
    qn = [sc2.sb("qn%d" % i, [128, NOWN], BF16) for i in range(2)]
    qp = [sc2.sb("qp%d" % i, [64, NOWN], BF16) for i in range(2)]
    kn = [sc2.sb("kn%d" % i, [128, NKEY], BF16) for i in range(2)]
    vh = [sc2.sb("vh%d" % i, [128, 18, 128], BF16) for i in range(2)]
    ast = [sc2.sb("ast%d" % i, [128, NOWN], BF16) for i in range(2)]
    p32 = [sc2.sb("p32%d" % i, [128, 1280], F32) for i in range(2)]
    pn = [sc2.sb("pn%d" % i, [128, 1280], BF16) for i in range(2)]
    pt = [sc2.sb("pt%d" % i, [128, 10, 128], BF16) for i in range(2)]
    st = [sc2.sb("st%d" % i, [128, 8], F32) for i in range(2)]
    bwq, bwkv, bqn, bqp, bkn, bvh, bast, bp32, bpn, bpt, bst, bpv = [[Buf(), Buf()] for _ in range(12)]
    tb = [banks[6][:].bitcast(BF16), banks[7][:].bitcast(BF16)]

    qblocks = []
    for s_ in range(4):
        for hh in range(2):
            qblocks.append((s_ * 256 + hh * 128, s_ * 256, 256, 2 * s_))
    for i_ in range(4):
        qblocks.append((NP_ + i_ * 128, NP_, 1280, 8))

    _NH = int(os.environ.get('P2_HEADS', 16)); _NQ = int(os.environ.get('P2_QB', 12)); _STG = int(os.environ.get('P2_STAGE', 4))
    wr2 = [sc2.sb("wadb%d" % i, [128, 32, 512], BF16) for i in range(2)]
    bwr2 = [Buf(), Buf()]
    bmod2 = Buf()

    def emit_ada_tile(ct):
        i = ct % 2
        K.dma(P, wr2[i][:], wav[:, :, ct * 512:(ct + 1) * 512], writes=[bwr2[i]])
        pb = next_bank(0, 6)
        for ob in range(4):
            mm(K, banks[pb][:, ob * 2:ob * 2 + 2],
               [(wr2[i][:, kc, ob * 128:(ob + 1) * 128], sil[:, kc, :]) for kc in range(32)],
               reads=[bwr2[i], bc], writes=[bB[pb]])
        K.op(V, lambda: nc.vector.tensor_tensor(
            out=mod[:, ct * 4:(ct + 1) * 4, :],
            in0=banks[pb][:, 0:8].rearrange("p (a b) -> p a b", b=2),
            in1=bad[:, ct * 4:(ct + 1) * 4].unsqueeze(2).to_broadcast([128, 4, 2]), op=ALU.add),
            reads=[bB[pb], bc], writes=[bmod2])

    for h in range(_NH):
        hp = h % 2
        emit_ada_tile(16 + 2 * h)
        emit_ada_tile(17 + 2 * h)
        K.dma(P, wq[hp][:], wuqv[:, :, h * 192:(h + 1) * 192], writes=[bwq[hp]])
        K.dma(P, wkv[hp][:], wukvv[:, :, h * 256:(h + 1) * 256], writes=[bwkv[hp]])
        for t in range(3):
            cs = slice(t * 512, (t + 1) * 512)
            pb = next_bank(0, 6)
            mm(K, banks[pb][:], [(wq[hp][:, kc, 0:128], qc[:, kc, cs]) for kc in range(6)],
               reads=[bwq[hp], bqc], writes=[bB[pb]])
            K.op(V, lambda: nc.vector.tensor_tensor(out=qn[hp][:, cs], in0=banks[pb][:], in1=rq[:, cs], op=ALU.mult),
                 reads=[bB[pb], brq], writes=[bqn[hp]])
            pb = next_bank(0, 6)
            mm(K, banks[pb][0:64, :], [(wq[hp][:, kc, 128:192], qc[:, kc, cs]) for kc in range(6)],
               reads=[bwq[hp], bqc], writes=[bB[pb]])
            if t < 2:
                K.op(V, lambda: nc.vector.tensor_tensor(out=qp[hp][:, cs], in0=banks[pb][0:64, :], in1=rq[0:64, cs],
                                                        op=ALU.mult), reads=[bB[pb], brq], writes=[bqp[hp]])
            else:
                K.op(V, lambda: nc.vector.tensor_tensor(out=qpf[:], in0=banks[pb][0:64, :], in1=rq[0:64, cs],
                                                        op=ALU.mult), reads=[bB[pb], brq], writes=[bqpf])
                rope(qp[hp][:, cs], bqp[hp], qpf[:], bqpf, 0)
        for t in range(5):
            w_ = 512 if t < 4 else 256
            cs = slice(t * 512, t * 512 + w_)
            pb = next_bank(0, 6)
            mm(K, banks[pb][:, 0:w_], [(wkv[hp][:, kc, 0:128], ckv[:, kc, cs]) for kc in range(4)],
               reads=[bwkv[hp], bckv], writes=[bB[pb]])
            K.op(A, lambda: nc.scalar.copy(kn[hp][:, cs], banks[pb][:, 0:w_]), reads=[bB[pb]], writes=[bkn[hp]])
        for g in range(5):
            nb_ = 4 if g < 4 else 2
            pb = next_bank(0, 6)
            for kk in range(nb_):
                kb = g * 4 + kk
                mm(K, banks[pb][:, kk * 128:(kk + 1) * 128],
                   [(ckv[:, kc, kb * 128:(kb + 1) * 128], wkv[hp][:, kc, 128:256]) for kc in range(4)],
                   reads=[bwkv[hp], bckv], writes=[bB[pb]])
            K.op(A if g % 2 else V,
                 (lambda: nc.scalar.copy(vh[hp][:, g * 4:g * 4 + nb_, :],
                                         banks[pb][:, 0:nb_ * 128].rearrange("p (a b) -> p a b", b=128))) if g % 2 else
                 (lambda: nc.vector.tensor_copy(out=vh[hp][:, g * 4:g * 4 + nb_, :],
                                                in_=banks[pb][:, 0:nb_ * 128].rearrange("p (a b) -> p a b", b=128))),
                 reads=[bB[pb]], writes=[bvh[hp]])

        def emit_S(qi):
            q0, k0, nk, vb0 = qblocks[qi]
            base = 3 * (qi % 2)
            for kt in range((nk + 511) // 512):
                w_ = min(512, nk - kt * 512)
                ks = slice(k0 + kt * 512, k0 + kt * 512 + w_)
                mm(K, banks[base + kt][:, 0:w_],
                   [(qn[hp][:, q0:q0 + 128], kn[hp][:, ks]), (qp[hp][:, q0:q0 + 128], kpe[:, ks])],
                   reads=[bqn[hp], bqp[hp], bkn[hp], bkpe], writes=[bB[base + kt]])

        def emit_softmax(qi):
            q0, k0, nk, vb0 = qblocks[qi]
            base = 3 * (qi % 2)
            e = qi % 2
            nt = (nk + 511) // 512
            for kt in range(nt):
                w_ = min(512, nk - kt * 512)
                K.op(V, lambda: nc.vector.reduce_max(out=st[e][:, kt:kt + 1], in_=banks[base + kt][:, 0:w_], axis=AX.X),
                     reads=[bB[base + kt]], writes=[bst[e]])
            if nt > 1:
                K.op(V, lambda: nc.vector.reduce_max(out=st[e][:, 3:4], in_=st[e][:, 0:nt], axis=AX.X),
                     reads=[bst[e]], writes=[bst[e]])
                mcol = 3
            else:
                mcol = 0
            K.op(V, lambda: nc.vector.tensor_scalar(out=st[e][:, 4:5], in0=st[e][:, mcol:mcol + 1], scalar1=-SCALE,
                                                    scalar2=None, op0=ALU.mult), reads=[bst[e]], writes=[bst[e]])
            for kt in range(nt):
                w_ = min(512, nk - kt * 512)
                K.op(A, lambda: nc.scalar.activation(p32[e][:, kt * 512:kt * 512 + w_], banks[base + kt][:, 0:w_],
                                                     AF.Exp, bias=st[e][:, 4:5], scale=SCALE),
                     reads=[bB[base + kt], bst[e]], writes=[bp32[e]])
            K.op(V, lambda: nc.vector.reduce_sum(out=st[e][:, 5:6], in_=p32[e][:, 0:nk], axis=AX.X),
                 reads=[bp32[e]], writes=[bst[e]])
            K.op(V, lambda: nc.vector.reciprocal(out=st[e][:, 6:7], in_=st[e][:, 5:6]), reads=[bst[e]], writes=[bst[e]])
            K.op(A, lambda: nc.scalar.activation(pn[e][:, 0:nk], p32[e][:, 0:nk], AF.Identity, scale=st[e][:, 6:7]),
                 reads=[bp32[e], bst[e]], writes=[bpn[e]])

        def emit_PV(qi):
            q0, k0, nk, vb0 = qblocks[qi]
            base = 3 * (qi % 2)
            e = qi % 2
            nkb = nk // 128
            for kb in range(nkb):
                tbi = kb // 8
                K.op(K.pe, lambda: nc.tensor.transpose(tb[tbi][:, (kb % 8) * 128:(kb % 8 + 1) * 128],
                                                       pn[e][:, kb * 128:(kb + 1) * 128], ident_b[:]),
                     reads=[bpn[e], bconst], writes=[bB[6 + tbi]])
            n0 = min(nkb, 8)
            K.op(V, lambda: nc.vector.tensor_copy(out=pt[e][:, 0:n0, :],
                                                  in_=tb[0][:, 0:n0 * 128].rearrange("p (a b) -> p a b", b=128)),
                 reads=[bB[6]], writes=[bpt[e]])
            if nkb > 8:
                K.op(V, lambda: nc.vector.tensor_copy(out=pt[e][:, 8:nkb, :],
                                                      in_=tb[1][:, 0:(nkb - 8) * 128].rearrange("p (a b) -> p a b", b=128)),
                     reads=[bB[7]], writes=[bpt[e]])
            mm(K, banks[base + 2][:, 256:384], [(vh[hp][:, vb0 + kb, :], pt[e][:, kb, :]) for kb in range(nkb)],
               reads=[bvh[hp], bpt[e]], writes=[bB[base + 2]])
            K.op(A, lambda: nc.scalar.copy(ast[hp][:, q0:q0 + 128], banks[base + 2][:, 256:384]),
                 reads=[bB[base + 2]], writes=[bast[hp]])

        if _STG >= 2:
            emit_S(0)
        for qi in range(_NQ):
            if qi + 1 < _NQ and _STG >= 2:
                emit_S(qi + 1)
            if _STG >= 3:
                emit_softmax(qi)
            if _STG >= 4:
                emit_PV(qi)
        K.dma(S, attnT_d[h * 128:(h + 1) * 128, :], ast[hp][:], reads=[bast[hp]])
    K.op(V, lambda: nc.vector.tensor_scalar(out=opsc[:, 1, :, :], in0=mod[:, 128:160, :], scalar1=1.0, scalar2=None,
                                            op0=ALU.add), reads=[bmod2], writes=[bmod])
    if dbg:
        d_mod = dout("d_mod", [128, 192, 2])
        K.dma(S, d_mod, mod[:], reads=[bmod, bmod2])
    K.barrier()
    sc2.close()
    if stop_after <= 3:
        K.finish()
        return nc, ins_used

    def ln_stats(s1, bs1, s2, bs2, tmp, btmp, nfeat, ncols):
        for t in range(0, ncols, 512):
            cs = slice(t, t + 512)
            mm(K, banks[6][:], [(ones_f[:], s1[:, cs])], reads=[bs1, bconst], writes=[bB[6]])
            mm(K, banks[7][:], [(ones_f[:], s2[:, cs])], reads=[bs2, bconst], writes=[bB[7]])
            K.op(V, lambda: nc.vector.tensor_scalar(out=s1[:, cs], in0=banks[6][:], scalar1=1.0 / nfeat, scalar2=None,
                                                    op0=ALU.mult), reads=[bB[6]], writes=[bs1])
            K.op(V, lambda: nc.vector.tensor_tensor(out=tmp[:, cs], in0=s1[:, cs], in1=s1[:, cs], op=ALU.mult),
                 reads=[bs1], writes=[btmp])
            K.op(V, lambda: nc.vector.scalar_tensor_tensor(out=s2[:, cs], in0=banks[7][:], scalar=1.0 / nfeat,
                                                           in1=tmp[:, cs], op0=ALU.mult, op1=ALU.subtract),
                 reads=[bB[7], btmp], writes=[bs2])
        K.op(A, lambda: nc.scalar.activation(s2[:, 0:ncols], s2[:, 0:ncols], AF.Sqrt, bias=epsc[:, 0:1], scale=1.0),
             reads=[bs2, bconst], writes=[bs2])
        K.op(V, lambda: nc.vector.reciprocal(out=s2[:, 0:ncols], in_=s2[:, 0:ncols]), reads=[bs2], writes=[bs2])

    w_out = din("w_out", [D, D])
    g_cnT = din("g_cnT", [128, 16])
    b_cnT = din("b_cnT", [128, 16])
    ln1_gT = din("ln1_gT", [128, 32])
    ln1_bT = din("ln1_bT", [128, 32])
    zT_d = dscr("zT_d", [D, NOWN], F32)
    stat1_d = dscr("stat1_d", [2, 128, NOWN], F32)
    h2T_d = dscr("h2T_d", [D, NOWN], BF16)
    wov = w_out.rearrange("(kc p) n -> p kc n", p=128)
    sc3 = Scope(K)
    mix = sc3.sb("mix", [128, 32, NOWN], BF16)
    gcn = sc3.sb("gcn", [128, 16], F32)
    bcn = sc3.sb("bcn", [128, 16], F32)
    l1g = sc3.sb("l1g", [128, 32], F32)
    l1b = sc3.sb("l1b", [128, 32], F32)
    a1 = sc3.sb("a1", [128, 32, 2], F32)
    b1 = sc3.sb("b1", [128, 32, 2], F32)
    bmix, bsm3, bab = Buf(), Buf(), Buf()
    for t_, s_ in ((gcn, g_cnT), (bcn, b_cnT), (l1g, ln1_gT), (l1b, ln1_bT)):
        K.dma(S, t_[:], s_, writes=[bsm3])
    K.dma(S, mix[:, 0:16, :], attnT_d.rearrange("(kc p) n -> p kc n", p=128), writes=[bmix])
    K.op(V, lambda: nc.vector.tensor_tensor(out=a1[:], in0=opsc[:, 1, :, :],
                                            in1=l1g[:].unsqueeze(2).to_broadcast([128, 32, 2]), op=ALU.mult),
         reads=[bmod, bsm3], writes=[bab])
    K.op(V, lambda: nc.vector.tensor_tensor(out=b1[:], in0=opsc[:, 1, :, :],
                                            in1=l1b[:].unsqueeze(2).to_broadcast([128, 32, 2]), op=ALU.mult),
         reads=[bmod, bsm3], writes=[bab])
    K.op(V, lambda: nc.vector.tensor_tensor(out=b1[:], in0=b1[:], in1=mod[:, 96:128, :], op=ALU.add),
         reads=[bmod, bab], writes=[bab])
    s3a = Scope(K)
    cm = s3a.sb("cm", [128, NOWN], F32)
    cr = s3a.sb("cr", [128, NOWN], F32)
    cvl = [s3a.sb("cvl%d" % i, [128, NOWN], F32) for i in range(2)]
    cvt = [s3a.sb("cvt%d" % i, [128, NOWN], F32) for i in range(2)]
    bcs = Buf()
    bcvl = [Buf(), Buf()]
    bcvt = [Buf(), Buf()]
    K.dma(S, cm[:], cstat_d[0], writes=[bcs])
    K.dma(S, cr[:], cstat_d[1], writes=[bcs])
    for j in range(16):
        i = j % 2
        K.dma(S, cvl[i][:], conv_d[j * 128:(j + 1) * 128, :], writes=[bcvl[i]])
        K.op(V, lambda: nc.vector.tensor_tensor(out=cvt[i][:], in0=cvl[i][:], in1=cm[:], op=ALU.subtract),
             reads=[bcvl[i], bcs], writes=[bcvt[i]])
        K.op(P, lambda: nc.gpsimd.tensor_tensor(out=cvt[i][:], in0=cvt[i][:], in1=cr[:], op=ALU.mult),
             reads=[bcvt[i], bcs], writes=[bcvt[i]])
        K.op(A, lambda: nc.scalar.activation(mix[:, 16 + j, :], cvt[i][:], AF.Silu, bias=bcn[:, j:j + 1],
                                             scale=gcn[:, j:j + 1]), reads=[bcvt[i], bsm3], writes=[bmix])
    K.barrier()
    s3a.close()
    s3b = Scope(K)
    wo = [s3b.sb("wo%d" % i, [128, 32, 256], BF16) for i in range(2)]
    xs3 = [s3b.sb("xs3%d" % i, [128, NOWN], F32) for i in range(2)]
    zt = [s3b.sb("zt%d" % i, [128, NOWN], F32) for i in range(2)]
    zz = [s3b.sb("zz%d" % i, [128, NOWN], F32) for i in range(2)]
    z1 = s3b.sb("z1", [128, NOWN], F32)
    z2 = s3b.sb("z2", [128, NOWN], F32)
    zq = s3b.sb("zq", [128, NOWN], F32)
    bwo = [Buf() for _ in range(2)]
    bxs3, bzt, bzz = [[Buf(), Buf()] for _ in range(3)]
    bz1, bz2, bzq = Buf(), Buf(), Buf()
    K.op(P, lambda: nc.gpsimd.memset(z1[:], 0.0), writes=[bz1])
    K.op(P, lambda: nc.gpsimd.memset(z2[:], 0.0), writes=[bz2])
    for tl in range(16):
        i = tl % 2
        K.dma(P, wo[i][:], wov[:, :, tl * 256:(tl + 1) * 256], writes=[bwo[i]])
        for ob in range(2):
            db = tl * 2 + ob
            e = db % 2
            K.dma(S, xs3[e][:], xv[:, db, 0:NOWN], writes=[bxs3[e]])
            for t in range(3):
                cs = slice(t * 512, (t + 1) * 512)
                j = 0 if t < 2 else 1
                pb = next_bank(0, 6)
                mm(K, banks[pb][:], [(wo[i][:, kc, ob * 128:(ob + 1) * 128], mix[:, kc, cs]) for kc in range(32)],
                   reads=[bwo[i], bmix], writes=[bB[pb]])
                K.op(A, lambda: nc.scalar.activation(zt[e][:, cs], banks[pb][:], AF.Identity, scale=MOD(2, db, j)),
                     reads=[bB[pb], bmod], writes=[bzt[e]])
                K.op(V, lambda: nc.vector.scalar_tensor_tensor(out=zz[e][:, cs], in0=xs3[e][:, cs], scalar=ALPHA,
                                                               in1=zt[e][:, cs], op0=ALU.mult, op1=ALU.add),
                     reads=[bxs3[e], bzt[e]], writes=[bzz[e]])
            K.dma(S, zT_d[db * 128:(db + 1) * 128, :], zz[e][:], reads=[bzz[e]])
            K.op(P, lambda: nc.gpsimd.tensor_tensor(out=z1[:], in0=z1[:], in1=zz[e][:], op=ALU.add),
                 reads=[bzz[e], bz1], writes=[bz1])
            K.op(A, lambda: nc.scalar.activation(zq[:], zz[e][:], AF.Square), reads=[bzz[e]], writes=[bzq])
            K.op(P, lambda: nc.gpsimd.tensor_tensor(out=z2[:], in0=z2[:], in1=zq[:], op=ALU.add),
                 reads=[bzq, bz2], writes=[bz2])
    ln_stats(z1, bz1, z2, bz2, zq, bzq, 4096.0, NOWN)
    K.dma(S, stat1_d[0], z1[:], reads=[bz1])
    K.dma(S, stat1_d[1], z2[:], reads=[bz2])
    h2s = [s3b.sb("h2s%d" % i, [128, NOWN], BF16) for i in range(2)]
    bh2s = [Buf(), Buf()]
    for db in range(32):
        e = db % 2
        K.dma(S, xs3[e][:], zT_d[db * 128:(db + 1) * 128, :], writes=[bxs3[e]])
        K.op(V, lambda: nc.vector.tensor_tensor(out=zt[e][:], in0=xs3[e][:], in1=z1[:], op=ALU.subtract),
             reads=[bxs3[e], bz1], writes=[bzt[e]])
        K.op(P, lambda: nc.gpsimd.tensor_tensor(out=zt[e][:], in0=zt[e][:], in1=z2[:], op=ALU.mult),
             reads=[bzt[e], bz2], writes=[bzt[e]])
        for (lo, hi, j) in ((0, NP_, 0), (NP_, NOWN, 1)):
            K.op(A, lambda: nc.scalar.activation(h2s[e][:, lo:hi], zt[e][:, lo:hi], AF.Identity,
                                                 bias=b1[:, db, j:j + 1], scale=a1[:, db, j:j + 1]),
                 reads=[bzt[e], bab], writes=[bh2s[e]])
        K.dma(S, h2T_d[db * 128:(db + 1) * 128, :], h2s[e][:], reads=[bh2s[e]])
    K.barrier()
    s3b.close()
    sc3.close()
    if stop_after <= 4:
        K.finish()
        return nc, ins_used

    w_pq = din("w_pq", [D, D])
    skT = din("skT", [128, 32, 128])
    s_d = dscr("s_d", [8, NOWN, 256], F32)
    G_d = dscr("G_d", [128, 128, NOWN], BF16)
    wpv = w_pq.rearrange("(kc p) n -> p kc n", p=128)
    sc4 = Scope(K)
    h2 = sc4.sb("h2", [128, 32, NOWN], BF16)
    sk = sc4.sb("sk", [128, 32, 128], BF16)
    wp = [sc4.sb("wp%d" % i, [128, 32, 256], BF16) for i in range(3)]
    qhp = [sc4.sb("qhp%d" % i, [128, 2, NOWN], BF16) for i in range(2)]
    sst = [sc4.sb("sst%d" % i, [128, 12, 128], F32) for i in range(2)]
    bh2, bsk = Buf(), Buf()
    bwp = [Buf() for _ in range(3)]
    bqhp, bsst = [[Buf(), Buf()] for _ in range(2)]
    h2v = h2T_d.rearrange("(kc p) n -> p kc n", p=128)
    for q4 in range(4):
        K.dma(S, h2[:, q4 * 8:(q4 + 1) * 8, :], h2v[:, q4 * 8:(q4 + 1) * 8, :], writes=[bh2])
    K.dma(P, sk[:], skT, writes=[bsk])
    s_dv = s_d.rearrange("h (nb p) (t k) -> h t p nb k", p=128, k=128)
    cpy = [0]

    def evac(out, in_, reads, writes):
        cpy[0] += 1
        if cpy[0] % 2:
            return K.op(A, lambda: nc.scalar.copy(out, in_), reads=reads, writes=writes)
        return K.op(V, lambda: nc.vector.tensor_copy(out=out, in_=in_), reads=reads, writes=writes)

    for hp_ in range(16):
        i = hp_ % 3
        e = hp_ % 2
        K.dma(P, wp[i][:], wpv[:, :, hp_ * 256:(hp_ + 1) * 256], writes=[bwp[i]])
        for half in range(2):
            for t in range(3):
                cs = slice(t * 512, (t + 1) * 512)
                pb = next_bank(0, 4)
                mm(K, banks[pb][:], [(wp[i][:, kc, half * 128:(half + 1) * 128], h2[:, kc, cs]) for kc in range(32)],
                   reads=[bwp[i], bh2], writes=[bB[pb]])
                evac(qhp[e][:, half, cs], banks[pb][:], [bB[pb]], [bqhp[e]])
        for g in range(3):
            pb = 4 + (hp_ * 3 + g) % 4
            for kk in range(4):
                nb = g * 4 + kk
                mm(K, banks[pb][:, kk * 128:(kk + 1) * 128],
                   [(qhp[e][:, half, nb * 128:(nb + 1) * 128], sk[:, hp_ * 2 + half, :]) for half in range(2)],
                   reads=[bqhp[e], bsk], writes=[bB[pb]])
            evac(sst[e][:, g * 4:(g + 1) * 4, :], banks[pb][:].rearrange("p (a b) -> p a b", b=128), [bB[pb]], [bsst[e]])
        K.dma(S, s_dv[hp_ // 2, hp_ % 2], sst[e][:], reads=[bsst[e]])
    K.barrier()
    sc4.close()
    if stop_after <= 5:
        K.finish()
        return nc, ins_used

    scr = Scope(K)
    stm = [scr.sb("stm%d" % i, [128, 2048], F32) for i in range(2)]
    wrk = scr.sb("wrk", [128, 2048], F32)
    tp = scr.sb("tp", [128, 16, 16], F32)
    cand = scr.sb("cand", [128, 8, 256], F32)
    ctop = scr.sb("ctop", [128, 8, 16], F32)
    ce = scr.sb("ce", [128, 8, 16], F32)
    sm = scr.sb("sm", [128, 64], F32)
    At = scr.sb("At", [128, 3, 128], F32)
    AT = [scr.sb("AT%d" % i, [128, 3, 128], F32) for i in range(2)]
    srep = [scr.sb("srep%d" % i, [128, 16, 256], F32) for i in range(3)]
    zr = [scr.sb("zr%d" % i, [128, 16, 128], F32) for i in range(2)]
    er = [scr.sb("er%d" % i, [128, 16, 128], BF16) for i in range(2)]
    mk = [scr.sb("mk%d" % i, [128, 16, 128], BF16) for i in range(2)]
    Rb = [scr.sb("Rb%d" % i, [128, 16, 128], BF16) for i in range(2)]
    P1b = [scr.sb("P1b%d" % i, [128, 16, 128], BF16) for i in range(2)]
    gst = [scr.sb("gst%d" % i, [128, 128, 128], BF16) for i in range(2)]
    bstm, bAT, bsrep, bzr, bzc, ber, bmk, bRb, bP1b, bgst = [[Buf(), Buf(), Buf()] for _ in range(10)]
    bwrk, btp, bcand, bctop, bce, bsmm, bAt = [Buf() for _ in range(7)]
    tpv = tp[:].rearrange("p (h t) a -> p h t a", t=2)
    G_dv = G_d.rearrange("i j n -> j i n")

    bc816 = lambda ap: ap.unsqueeze(2).to_broadcast([128, 8, 16])
    btpg = [Buf() for _ in range(16)]
    bwkg = [Buf() for _ in range(16)]
    bctg = [Buf() for _ in range(8)]
    bcdg = [Buf() for _ in range(8)]

    def top16_batch(dsts, srcs, width, rds, wrs, wks, bwk):
        n = len(dsts)
        for i in range(n):
            K.op(V, lambda: nc.vector.max(out=dsts[i][:, 0:8], in_=srcs[i]), reads=rds[i], writes=[wrs[i]])
        for i in range(n):
            K.op(V, lambda: nc.vector.match_replace(out=wks[i], in_to_replace=dsts[i][:, 0:8], in_values=srcs[i],
                                                    imm_value=-1e30), reads=rds[i] + [wrs[i]], writes=[bwk[i]])
        for i in range(n):
            K.op(V, lambda: nc.vector.max(out=dsts[i][:, 8:16], in_=wks[i]), reads=[bwk[i]], writes=[wrs[i]])

    def topk_stage(nb):
        e = nb % 2
        K.dma(S, stm[e][:].rearrange("p (h c) -> p h c", c=256), s_d[:, nb * 128:(nb + 1) * 128, :].rearrange("h n c -> n h c"), writes=[bstm[e]])
        for q4 in range(4):
            hs = range(q4 * 4, q4 * 4 + 4)
            top16_batch([tp[:, i, :] for i in hs], [stm[e][:, i * 128:(i + 1) * 128] for i in hs], 128,
                        [[bstm[e]] for i in hs], [btpg[i] for i in hs],
                        [wrk[:, i * 128:(i + 1) * 128] for i in hs], [bwkg[i] for i in hs])
            yield
        for h in range(8):
            K.op(V, lambda: nc.vector.tensor_tensor(
                out=cand[:, h, :].rearrange("p (a b) -> p a b", b=16),
                in0=tpv[:, h, 0, :].unsqueeze(2).to_broadcast([128, 16, 16]),
                in1=tpv[:, h, 1, :].unsqueeze(1).to_broadcast([128, 16, 16]), op=ALU.add),
                reads=[btpg[2 * h], btpg[2 * h + 1]], writes=[bcdg[h]])
        yield
        for q2 in range(2):
            hs = range(q2 * 4, q2 * 4 + 4)
            top16_batch([ctop[:, i, :] for i in hs], [cand[:, i, :] for i in hs], 256,
                        [[bcdg[i]] for i in hs], [bctg[i] for i in hs],
                        [wrk[:, i * 256:(i + 1) * 256] for i in hs], [bwkg[2 * i] for i in hs])
            yield
        btp = btpg
        bctop = bctg
        K.op(V, lambda: nc.vector.tensor_reduce(out=sm[:, 0:8], in_=ctop[:], axis=AX.X, op=ALU.max),
             reads=bctop, writes=[bsmm])
        K.op(V, lambda: nc.vector.tensor_reduce(out=sm[:, 8:16], in_=ctop[:], axis=AX.X, op=ALU.min),
             reads=bctop, writes=[bsmm])
        K.op(V, lambda: nc.vector.tensor_tensor(out=ce[:], in0=ctop[:], in1=bc816(sm[:, 0:8]), op=ALU.subtract),
             reads=bctop + [bsmm], writes=[bce])
        K.op(A, lambda: nc.scalar.activation(ce[:], ce[:], AF.Exp), reads=[bce], writes=[bce])
        K.op(V, lambda: nc.vector.tensor_reduce(out=sm[:, 16:24], in_=ce[:], axis=AX.X, op=ALU.add),
             reads=[bce], writes=[bsmm])
        K.op(A, lambda: nc.scalar.activation(sm[:, 24:32], sm[:, 16:24], AF.Ln), reads=[bsmm], writes=[bsmm])
        K.op(V, lambda: nc.vector.tensor_tensor(out=sm[:, 32:40], in0=sm[:, 0:8], in1=sm[:, 24:32], op=ALU.add),
             reads=[bsmm], writes=[bsmm])
        K.op(V, lambda: nc.vector.tensor_copy(out=At[:, 0, :].rearrange("p (a h) -> p h a", h=8), in_=bc816(sm[:, 8:16])),
             reads=[bsmm], writes=[bAt])
        K.op(V, lambda: nc.vector.tensor_tensor(out=At[:, 1, :].rearrange("p (a h) -> p h a", h=8), in0=tpv[:, :, 0, :],
                                                in1=bc816(sm[:, 32:40]), op=ALU.subtract),
             reads=btp + [bsmm], writes=[bAt])
        K.op(V, lambda: nc.vector.tensor_copy(out=At[:, 2, :].rearrange("p (a h) -> p h a", h=8), in_=tpv[:, :, 0, :]),
             reads=btp, writes=[bAt])
        pbt = nb % 2
        for q3 in range(3):
            K.op(K.pe, lambda: nc.tensor.transpose(banks[pbt][:, q3 * 128:(q3 + 1) * 128], At[:, q3, :], ident_f[:]),
                 reads=[bAt, bconst], writes=[bB[pbt]])
        K.op(V, lambda: nc.vector.tensor_copy(out=AT[e][:], in_=banks[pbt][:, 0:384].rearrange("p (a b) -> p a b", b=128)),
             reads=[bB[pbt]], writes=[bAT[e]])
        yield

    def bcn(ap):
        return ap.unsqueeze(2).to_broadcast([128, 16, 128])

    def sub_block(nb, sb):
        e = nb % 2
        f = (nb * 8 + sb) % 2
        f3 = (nb * 8 + sb) % 3
        ns = slice(sb * 16, sb * 16 + 16)
        row0 = nb * 128 + sb * 16
        src = bass.AP(s_d.tensor, row0 * 256, [[0, 16], [NOWN * 256, 8], [1, 4096]])
        K.dma(S, srep[f3][:].rearrange("p a b -> p (a b)"), src, writes=[bsrep[f3]])
        K.op(V, lambda: nc.vector.tensor_tensor(out=zr[f][:], in0=srep[f3][:, :, 128:256], in1=bcn(AT[e][:, 2, ns]),
                                                op=ALU.add), reads=[bsrep[f3], bAT[e]], writes=[bzr[f]])
        for tk in range(16):
            n_ = sb * 16 + tk
            K.op(A, lambda: nc.scalar.activation(er[f][:, tk, :], srep[f3][:, tk, 128:256], AF.Exp,
                                                 bias=AT[e][:, 1, n_:n_ + 1]),
                 reads=[bsrep[f3], bAT[e]], writes=[ber[f]])
        K.op(V, lambda: nc.vector.tensor_tensor(out=mk[f][:], in0=zr[f][:], in1=bcn(AT[e][:, 0, ns]), op=ALU.is_ge),
             reads=[bzr[f], bAT[e]], writes=[bmk[f]])
        K.op(V, lambda: nc.vector.tensor_tensor(out=P1b[f][:], in0=srep[f3][:, :, 0:128], in1=bcn(AT[e][:, 2, ns]),
                                                op=ALU.is_equal), reads=[bsrep[f3], bAT[e]], writes=[bP1b[f]])
        K.op(V, lambda: nc.vector.tensor_tensor(out=Rb[f][:], in0=mk[f][:], in1=er[f][:], op=ALU.mult),
             reads=[bmk[f], ber[f]], writes=[bRb[f]])
        for q4 in range(4):
            pb = 2 + (sb * 4 + q4) % 6
            for tk in range(4):
                t16 = q4 * 4 + tk
                mm(K, banks[pb][:, tk * 128:(tk + 1) * 128], [(Rb[f][:, t16, :], P1b[f][:, t16, :])],
                   reads=[bRb[f], bP1b[f]], writes=[bB[pb]])
            n0 = sb * 16 + q4 * 4
            K.op(A, lambda: nc.scalar.copy(gst[e][:, :, n0:n0 + 4], banks[pb][:].rearrange("p (n i) -> p i n", n=4)),
                 reads=[bB[pb]], writes=[bgst[e]])

    for _ in topk_stage(0):
        pass
    for nb in range(12):
        nxt = topk_stage(nb + 1) if nb + 1 < 12 else iter(())
        for sb in range(8):
            sub_block(nb, sb)
            next(nxt, None)
        for _ in nxt:
            pass
        K.dma(S, G_dv[:, :, nb * 128:(nb + 1) * 128], gst[nb % 2][:], reads=[bgst[nb % 2]])
    K.barrier()
    scr.close()
    if stop_after <= 6:
        K.finish()
        return nc, ins_used

    peer_uT = din("peer_uT", [128, 128, 4096])
    peer_v = din("peer_v", [16384, D])
    ln2_gT = din("ln2_gT", [128, 32])
    ln2_bT = din("ln2_bT", [128, 32])
    o_yT = dout("o_yT", [D, NOWN])
    pvv = peer_v.rearrange("(g a p) d -> g p a d", a=4, p=128)
    sc5 = Scope(K)
    h2p = sc5.sb("h2p", [128, 32, 512], BF16)
    acc = sc5.sb("acc", [128, 32, 512], F32)
    l1g5 = sc5.sb("l1g5", [128, 32], F32)
    l1b5 = sc5.sb("l1b5", [128, 32], F32)
    l2g = sc5.sb("l2g", [128, 32], F32)
    l2b = sc5.sb("l2b", [128, 32], F32)
    bh2p, bsm5 = Buf(), Buf()
    bacc = [Buf() for _ in range(32)]
    for t_, s_ in ((l1g5, ln1_gT), (l1b5, ln1_bT), (l2g, ln2_gT), (l2b, ln2_bT)):
        K.dma(S, t_[:], s_, writes=[bsm5])
    _P5P = int(os.environ.get('P5_PASSES', 3)); _P5G = int(os.environ.get('P5_GROUPS', 32)); _P5E = int(os.environ.get('P5_EPI', 1))
    for pt_ in range(_P5P):
        c0 = pt_ * 512
        cj = 0 if pt_ < 2 else 1
        pcs = slice(c0, c0 + 512)
        K.dma(S, h2p[:], h2v[:, :, pcs], writes=[bh2p])
        for db in range(32):
            K.op(V, lambda: nc.vector.memset(acc[:, db, :], 0.0), writes=[bacc[db]])
        s5a = Scope(K)
        ut = [s5a.sb("ut%d" % i, [128, 32, 128], BF16) for i in range(3)]
        vt = [s5a.sb("vt%d" % i, [128, 4, D], BF16) for i in range(2)]
        gt = [s5a.sb("gt%d" % i, [128, 512], BF16) for i in range(3)]
        ga32 = [s5a.sb("ga32%d" % i, [128, 512], F32) for i in range(2)]
        gab = [s5a.sb("gab%d" % i, [128, 4, 512], BF16) for i in range(2)]
        but, bvt, bgt, bga32, bgab = [[Buf(), Buf(), Buf()] for _ in range(5)]

        def phaseB(g):
            gi = g % 2
            for db in range(32):
                pb = 2 + db % 6
                mm(K, banks[pb][:], [(vt[gi][:, a, db * 128:(db + 1) * 128], gab[gi][:, a, :]) for a in range(4)],
                   reads=[bvt[gi], bgab[gi]], writes=[bB[pb]])
                K.op(V, lambda: nc.vector.tensor_tensor(out=acc[:, db, :], in0=acc[:, db, :], in1=banks[pb][:], op=ALU.add),
                     reads=[bB[pb], bacc[db]], writes=[bacc[db]])

        for g in range(_P5G):
            gi = g % 2
            for a in range(4):
                eb = g * 4 + a
                ei = eb % 2
                u3 = eb % 3
                K.dma(P, ut[u3][:].rearrange("p a b -> p (a b)"), peer_uT[eb], writes=[but[u3]])
                K.dma(S, gt[u3][:], G_d[eb, :, pcs], writes=[bgt[u3]])
                mm(K, banks[ei][:], [(ut[u3][:, kc, :], h2p[:, kc, :]) for kc in range(32)],
                   reads=[but[u3], bh2p], writes=[bB[ei]])
                K.op(A, lambda: nc.scalar.activation(ga32[ei][:], banks[ei][:], AF.Gelu), reads=[bB[ei]], writes=[bga32[ei]])
                K.op(V, lambda: nc.vector.tensor_tensor(out=gab[gi][:, a, :], in0=ga32[ei][:], in1=gt[u3][:], op=ALU.mult),
                     reads=[bga32[ei], bgt[u3]], writes=[bgab[gi]])
            K.dma(P, vt[gi][:], pvv[g], writes=[bvt[gi]])
            if g > 0:
                phaseB(g - 1)
        phaseB(_P5G - 1)
        K.barrier()
        s5a.close()
        s5b = Scope(K)
        m1 = s5b.sb("m1", [128, 512], F32)
        r1 = s5b.sb("r1", [128, 512], F32)
        y1 = s5b.sb("y1", [128, 512], F32)
        y2 = s5b.sb("y2", [128, 512], F32)
        yq = s5b.sb("yq", [128, 512], F32)
        y1w = s5b.sb("y1w", [128, 4, 512], F32)
        y2w = s5b.sb("y2w", [128, 4, 512], F32)
        yq4 = s5b.sb("yq4", [128, 4, 512], F32)
        zl = [s5b.sb("zl%d" % i, [128, 4, 512], F32) for i in range(2)]
        yst = [s5b.sb("yst%d" % i, [128, 4, 512], F32) for i in range(2)]
        bst1, by1, by2, byq, by1w, by2w, byq4 = [Buf() for _ in range(7)]
        bzl, byst = [[Buf(), Buf()] for _ in range(2)]
        zTv = zT_d.rearrange("(kc p) n -> p kc n", p=128)
        oyv = o_yT.rearrange("(kc p) n -> p kc n", p=128)
        b4 = lambda ap: ap.unsqueeze(1).to_broadcast([128, 4, 512])
        K.dma(S, m1[:], stat1_d[0][:, pcs], writes=[bst1])
        K.dma(S, r1[:], stat1_d[1][:, pcs], writes=[bst1])
        K.op(P, lambda: nc.gpsimd.memset(y1w[:], 0.0), writes=[by1w])
        K.op(P, lambda: nc.gpsimd.memset(y2w[:], 0.0), writes=[by2w])
        for c4 in range(8 if _P5E else 0):
            e = c4 % 2
            dbs = range(c4 * 4, c4 * 4 + 4)
            ba4 = [bacc[db] for db in dbs]
            a4 = acc[:, c4 * 4:c4 * 4 + 4, :]
            K.dma(S, zl[e][:], zTv[:, c4 * 4:c4 * 4 + 4, pcs], writes=[bzl[e]])
            K.op(V, lambda: nc.vector.tensor_tensor(out=zl[e][:], in0=zl[e][:], in1=b4(m1[:]), op=ALU.subtract),
                 reads=[bzl[e], bst1], writes=[bzl[e]])
            K.op(P, lambda: nc.gpsimd.tensor_tensor(out=zl[e][:], in0=zl[e][:], in1=b4(r1[:]), op=ALU.mult),
                 reads=[bzl[e], bst1], writes=[bzl[e]])
            for q, db in enumerate(dbs):
                K.op(A, lambda: nc.scalar.activation(zl[e][:, q, :], zl[e][:, q, :], AF.Identity, bias=l1b5[:, db:db + 1],
                                                     scale=l1g5[:, db:db + 1]), reads=[bzl[e], bsm5], writes=[bzl[e]])
                K.op(A, lambda: nc.scalar.activation(acc[:, db, :], acc[:, db, :], AF.Identity, scale=MOD(5, db, cj)),
                     reads=[bacc[db], bmod], writes=[bacc[db]])
            K.op(V, lambda: nc.vector.scalar_tensor_tensor(out=a4, in0=zl[e][:], scalar=ALPHA, in1=a4,
                                                           op0=ALU.mult, op1=ALU.add),
                 reads=[bzl[e]] + ba4, writes=ba4)
            K.op(P, lambda: nc.gpsimd.tensor_tensor(out=y1w[:], in0=y1w[:], in1=a4, op=ALU.add),
                 reads=ba4 + [by1w], writes=[by1w])
            K.op(A, lambda: nc.scalar.activation(yq4[:], a4, AF.Square), reads=ba4, writes=[byq4])
            K.op(P, lambda: nc.gpsimd.tensor_tensor(out=y2w[:], in0=y2w[:], in1=yq4[:], op=ALU.add),
                 reads=[byq4, by2w], writes=[by2w])
        for (yw, byw, y_, by_) in ((y1w, by1w, y1, by1), (y2w, by2w, y2, by2)):
            K.op(V, lambda: nc.vector.tensor_tensor(out=y_[:], in0=yw[:, 0, :], in1=yw[:, 1, :], op=ALU.add),
                 reads=[byw], writes=[by_])
            K.op(V, lambda: nc.vector.tensor_tensor(out=y_[:], in0=y_[:], in1=yw[:, 2, :], op=ALU.add),
                 reads=[byw, by_], writes=[by_])
            K.op(V, lambda: nc.vector.tensor_tensor(out=y_[:], in0=y_[:], in1=yw[:, 3, :], op=ALU.add),
                 reads=[byw, by_], writes=[by_])
        ln_stats(y1, by1, y2, by2, yq, byq, 4096.0, 512)
        for c4 in range(8):
            e = c4 % 2
            dbs = range(c4 * 4, c4 * 4 + 4)
            ba4 = [bacc[db] for db in dbs]
            a4 = acc[:, c4 * 4:c4 * 4 + 4, :]
            K.op(V, lambda: nc.vector.tensor_tensor(out=yst[e][:], in0=a4, in1=b4(y1[:]), op=ALU.subtract),
                 reads=ba4 + [by1], writes=[byst[e]])
            K.op(P, lambda: nc.gpsimd.tensor_tensor(out=yst[e][:], in0=yst[e][:], in1=b4(y2[:]), op=ALU.mult),
                 reads=[byst[e], by2], writes=[byst[e]])
            for q, db in enumerate(dbs):
                K.op(A, lambda: nc.scalar.activation(yst[e][:, q, :], yst[e][:, q, :], AF.Identity, bias=l2b[:, db:db + 1],
                                                     scale=l2g[:, db:db + 1]), reads=[byst[e], bsm5], writes=[byst[e]])
            K.dma(S, oyv[:, c4 * 4:c4 * 4 + 4, pcs], yst[e][:], reads=[byst[e]], final=True)
        K.barrier()
        s5b.close()
    sc5.close()

    K.finish()
    return nc, ins_used


def _fm(v, nchunk):
    return np.ascontiguousarray(np.asarray(v, np.float32).reshape(nchunk, 128).T)


def _rope_tables(pos):
    row = (pos // 64).astype(np.float32)
    col = (pos % 64).astype(np.float32)
    inv = (10000.0 ** (-np.arange(16, dtype=np.float32) / 16)).astype(np.float32)
    ar = row[:, None] * inv
    ac = col[:, None] * inv
    ang = np.concatenate([ar, ar, ac, ac], -1).astype(np.float32)
    return np.ascontiguousarray(np.cos(ang).T.astype(np.float32)), np.ascontiguousarray(np.sin(ang).T.astype(np.float32))


def _rmat():
    R = np.zeros((64, 64), np.float32)
    for base in (0, 32):
        for i in range(16):
            R[base + i, base + 16 + i] = -1.0
            R[base + 16 + i, base + i] = 1.0
    return np.ascontiguousarray(R.T)


def sample_cols(hf):
    own = np.arange(hf * 512, hf * 512 + 512)
    if hf == 0:
        other = np.arange(512, 1024)
    else:
        other = np.concatenate([np.arange(497, 512), np.arange(0, 497)])
    return own, other


def prep_shared(inp):
    f = lambda k: np.asarray(inp[k], np.float32)
    sh = {}
    sh["w_ada"] = np.ascontiguousarray(f("w_ada")[0])
    sh["b_adaT"] = _fm(f("b_ada")[0], 192)
    sh["w_in"] = np.ascontiguousarray(f("w_in")[0])
    sh["g_qT"] = _fm(f("g_q")[0], 6)
    sh["g_kvT"] = _fm(f("g_kv")[0], 4)
    sh["w_uq"] = np.ascontiguousarray(f("w_uq")[0])
    sh["w_ukv"] = np.ascontiguousarray(f("w_ukv")[0])
    sh["w_dwT"] = np.ascontiguousarray(f("w_dw")[0].T.reshape(16, 128, 31).transpose(1, 0, 2))
    sh["b_dwT"] = _fm(f("b_dw")[0], 16)
    sh["g_cnT"] = _fm(f("g_cn")[0], 16)
    sh["b_cnT"] = _fm(f("b_cn")[0], 16)
    sh["w_out"] = np.ascontiguousarray(f("w_out")[0])
    sh["ln1_gT"] = _fm(f("ln1_g")[0], 32)
    sh["ln1_bT"] = _fm(f("ln1_b")[0], 32)
    sh["w_pq"] = np.ascontiguousarray(f("w_pq")[0])
    sk = f("sub_keys")[0]
    skT = sk.reshape(16, 128, 2, 128).transpose(3, 0, 2, 1)
    sh["skT"] = np.ascontiguousarray(skT.reshape(128, 32, 128))
    pu = f("peer_u")[0]
    sh["peer_uT"] = np.ascontiguousarray(pu.reshape(128, 128, 32, 128).transpose(0, 3, 2, 1)).reshape(128, 128, 4096)
    sh["peer_v"] = np.ascontiguousarray(f("peer_v")[0])
    sh["ln2_gT"] = _fm(f("ln2_g")[0], 32)
    sh["ln2_bT"] = _fm(f("ln2_b")[0], 32)
    sh["rmatT"] = _rmat()
    return sh


def prep_core(inp, c):
    f = lambda k: np.asarray(inp[k], np.float32)
    b, hf = c // 2, c % 2
    own, other = sample_cols(hf)
    xp = f("x_prompt")[4 * c:4 * c + 4].reshape(NP_, D)
    xs = f("x_sample")[b]
    xall = np.concatenate([xp, xs[own], xs[other]], 0)
    m = {}
    m["xT"] = np.ascontiguousarray(xall.T)
    cond = np.stack([f("c_ctx"), f("c")[b]], -1)
    m["condT"] = np.ascontiguousarray(cond.reshape(32, 128, 2).transpose(1, 0, 2))
    m["cache_ckvT"] = np.ascontiguousarray(f("cache_ckv")[b, 0].T.reshape(4, 128, 256).transpose(1, 0, 2))
    m["cache_kpeT"] = np.ascontiguousarray(f("cache_kpe")[b, 0].T)
    cosT, sinT = _rope_tables(np.concatenate([own, other]))
    m["cosT"] = cosT
    m["sinT"] = sinT
    hm = np.zeros((128, 2), np.float32)
    hm[:, 0] = 1.0 if hf == 1 else 0.0
    hm[:, 1] = 1.0 if hf == 0 else 0.0
    m["halo_mask"] = hm
    return m


_CACHE = {}


def kernel(**inputs):
    if "nc" not in _CACHE:
        _CACHE["nc"] = build()
    nc, used = _CACHE["nc"]
    sh = prep_shared(inputs)
    in_maps = []
    for c in range(8):
        m = prep_core(inputs, c)
        m.update(sh)
        in_maps.append({k: m[k] for k in used})
    res = run_bass_kernel_spmd(nc, in_maps, core_ids=list(range(8)))
    y_prompt = np.zeros((32, 256, D), np.float32)
    y_sample = np.zeros((4, 1024, D), np.float32)
    new_ckv = np.zeros((32, 1, 256, 512), np.float32)
    new_kpe = np.zeros((32, 1, 256, 64), np.float32)
    for c in range(8):
        r = res.results[c]
        b, hf = c // 2, c % 2
        yT = np.asarray(r["o_yT"], np.float32)
        y_prompt[4 * c:4 * c + 4] = yT[:, :NP_].T.reshape(4, 256, D)
        y_sample[b, hf * 512:(hf + 1) * 512] = yT[:, NP_:].T
        new_ckv[4 * c:4 * c + 4, 0] = np.asarray(r["o_ckvT"], np.float32).T.reshape(4, 256, 512)
        new_kpe[4 * c:4 * c + 4, 0] = np.asarray(r["o_kpeT"], np.float32).T.reshape(4, 256, 64)
    return (y_prompt, y_sample, new_ckv, new_kpe)
```

```python
import os
import numpy as np
import ml_dtypes
from contextlib import ExitStack
import concourse.bass as bass
import concourse.mybir as mybir
from concourse.bass_utils import run_bass_kernel_spmd

F32 = mybir.dt.float32
BF16 = mybir.dt.bfloat16
AF = mybir.ActivationFunctionType
ALU = mybir.AluOpType
AX = mybir.AxisListType

D = 4096
NP_ = 1024
NS = 512
NOWN = NP_ + NS
NALL = 2048
NKEY = 2304
ALPHA = 2.0 ** 0.25
EPS = 1e-6
SCALE = 192.0 ** -0.5
YW = 4 * 286 + 542


class Buf:
    __slots__ = ("w", "r")

    def __init__(self):
        self.w = None
        self.r = {}


class Q:
    def __init__(self, K, name, eng, is_pe=False):
        self.name = name
        self.eng = eng
        self.is_pe = is_pe
        self.sem = K.newsem("q_" + name)
        self.cnt = 0
        self.waited = {}
        self.dsems = []
        self.dvals = []
        self.dma_i = 0
        self.lazy = False

    def wait(self, tok):
        sem, val, owner = tok
        key = id(sem)
        if self.waited.get(key, 0) >= val:
            return
        self.eng.wait_ge(sem, val)
        self.waited[key] = val


class Kern:
    NDMA = 10

    def __init__(self, nc):
        self.nc = nc
        self.stack = ExitStack()
        self.pe = Q(self, "pe", nc.tensor, is_pe=True)
        self.act = Q(self, "act", nc.scalar)
        self.dve = Q(self, "dve", nc.vector)
        self.pool = Q(self, "pool", nc.gpsimd)
        self.sp = Q(self, "sp", nc.sync)
        self.qs = [self.pe, self.act, self.dve, self.pool, self.sp]
        for q in (self.sp, self.pool):
            for i in range(self.NDMA):
                q.dsems.append(self.newsem("d_%s%d" % (q.name, i)))
                q.dvals.append(0)
        self.final = []
        self.uid = 0

    def newsem(self, name):
        return self.stack.enter_context(self.nc.semaphore(name))

    def _deps(self, q, reads, writes):
        for b in reads:
            if b.w is not None and not (b.w[2] is q and q.is_pe):
                q.wait(b.w)
        for b in writes:
            if b.w is not None and not (b.w[2] is q and q.is_pe):
                q.wait(b.w)
            for owner, t in b.r.items():
                if owner is not q or not q.is_pe:
                    q.wait(t)

    def _mark(self, tok, reads, writes):
        for b in reads:
            b.r[tok[2]] = tok
        for b in writes:
            b.w = tok
            b.r = {}

    def op(self, q, fn, reads=(), writes=(), inc=True):
        self._deps(q, reads, writes)
        ins = fn()
        if inc:
            q.cnt += 1
            ins.then_inc(q.sem, 1)
            q.lazy = False
            tok = (q.sem, q.cnt, q)
        else:
            q.lazy = True
            tok = (q.sem, q.cnt + 1, q)
        self._mark(tok, reads, writes)
        return tok

    def dma(self, q, out, in_, reads=(), writes=(), final=False):
        slot = q.dma_i % self.NDMA
        q.dma_i += 1
        sem = q.dsems[slot]
        if q.dvals[slot] > 0:
            q.wait((sem, q.dvals[slot], sem))
        self._deps(q, reads, writes)
        ins = q.eng.dma_start(out=out, in_=in_)
        q.dvals[slot] += 16
        ins.then_inc(sem, 16)
        tok = (sem, q.dvals[slot], sem)
        self._mark(tok, reads, writes)
        if final:
            self.final.append(tok)
        return tok

    def barrier(self):
        toks = []
        for q in self.qs:
            assert not q.lazy, q.name
            if q.cnt > 0:
                toks.append((q.sem, q.cnt, q))
            for s, v in zip(q.dsems, q.dvals):
                if v > 0:
                    toks.append((s, v, s))
        for q in self.qs:
            for t in toks:
                if t[2] is not q:
                    q.wait(t)

    def name(self, base):
        self.uid += 1
        return "%s_%d" % (base, self.uid)

    def finish(self):
        self.barrier()


class Scope:
    def __init__(self, K):
        self.K = K
        self.stack = ExitStack()

    def sb(self, name, shape, dtype):
        return self.stack.enter_context(self.K.nc.sbuf_tensor(self.K.name(name), list(shape), dtype))

    def close(self):
        self.stack.close()


def mm(K, out, pairs, reads, writes, first=True, last=True):
    nc = K.nc
    n = len(pairs)
    tok = None
    for i, (l, r) in enumerate(pairs):
        st = first and i == 0
        sp = last and i == n - 1
        tok = K.op(K.pe, lambda: nc.tensor.matmul(out, l, r, start=st, stop=sp),
                   reads=reads, writes=writes, inc=(i == n - 1))
    return tok


def build(dbg=False, stop_after=99):
    nc = bass.Bass("TRN2", target_bir_lowering=False)
    K = Kern(nc)
    V, A, P, S = K.dve, K.act, K.pool, K.sp
    ins_used = []

    def din(name, shape, dt=F32):
        ins_used.append(name)
        return nc.dram_tensor(name, list(shape), dt, kind="ExternalInput").ap()

    def dout(name, shape, dt=F32):
        return nc.dram_tensor(name, list(shape), dt, kind="ExternalOutput").ap()

    def dscr(name, shape, dt):
        return nc.dram_tensor(name, list(shape), dt, kind=("ExternalOutput" if dbg else "Internal")).ap()

    top = Scope(K)
    banks = [K.stack.enter_context(nc.psum_tensor("bank%d" % i, [128, 512], F32)) for i in range(8)]
    bB = [Buf() for _ in range(8)]
    ident_f = top.sb("ident_f", [128, 128], F32)
    ident_b = top.sb("ident_b", [128, 128], BF16)
    ones_f = top.sb("ones_f", [128, 128], F32)
    mod = top.sb("mod", [128, 192, 2], F32)
    opsc = top.sb("opsc", [128, 2, 32, 2], F32)
    bconst = Buf()
    bmod = Buf()

    K.op(P, lambda: nc.gpsimd.memset(ident_f[:], 0.0), writes=[bconst])
    K.op(P, lambda: nc.gpsimd.affine_select(out=ident_f[:], in_=ident_f[:], pattern=[[-1, 128]],
                                            compare_op=ALU.not_equal, fill=1.0, base=0, channel_multiplier=1),
         reads=[bconst], writes=[bconst])
    K.op(P, lambda: nc.gpsimd.tensor_copy(out=ident_b[:], in_=ident_f[:]), reads=[bconst], writes=[bconst])
    K.op(P, lambda: nc.gpsimd.memset(ones_f[:], 1.0), writes=[bconst])
    epsc = top.sb("epsc", [128, 1], F32)
    K.op(P, lambda: nc.gpsimd.memset(epsc[:], EPS), writes=[bconst])

    def MOD(s, kc, j):
        return mod[:, s * 32 + kc, j:j + 1]

    def OPSC(w, kc, j):
        return opsc[:, w, kc, j:j + 1]

    condT = din("condT", [128, 32, 2])
    w_ada = din("w_ada", [D, 6 * D])
    b_adaT = din("b_adaT", [128, 192])
    sc0 = Scope(K)
    cnd = top.sb("cnd", [128, 32, 2], F32)
    sil = top.sb("sil", [128, 32, 2], BF16)
    bad = top.sb("bad", [128, 192], F32)
    bc = Buf()
    K.dma(S, cnd[:], condT, writes=[bc])
    K.dma(S, bad[:], b_adaT, writes=[bc])
    K.op(A, lambda: nc.scalar.activation(sil[:], cnd[:], AF.Silu), reads=[bc], writes=[bc])
    wav = w_ada.rearrange("(kc p) n -> p kc n", p=128)
    wr = [sc0.sb("wada%d" % i, [128, 32, 512], BF16) for i in range(3)]
    bwr = [Buf() for _ in range(3)]
    for ct in range(16):
        i = ct % 3
        K.dma(P, wr[i][:], wav[:, :, ct * 512:(ct + 1) * 512], writes=[bwr[i]])
        pb = ct % 2
        for ob in range(4):
            mm(K, banks[pb][:, ob * 2:ob * 2 + 2],
               [(wr[i][:, kc, ob * 128:(ob + 1) * 128], sil[:, kc, :]) for kc in range(32)],
               reads=[bwr[i], bc], writes=[bB[pb]])
        K.op(V, lambda: nc.vector.tensor_tensor(
            out=mod[:, ct * 4:(ct + 1) * 4, :],
            in0=banks[pb][:, 0:8].rearrange("p (a b) -> p a b", b=2),
            in1=bad[:, ct * 4:(ct + 1) * 4].unsqueeze(2).to_broadcast([128, 4, 2]), op=ALU.add),
            reads=[bB[pb], bc], writes=[bmod])
    K.op(V, lambda: nc.vector.tensor_scalar(out=opsc[:, 0, :, :], in0=mod[:, 32:64, :], scalar1=1.0, scalar2=None,
                                            op0=ALU.add), reads=[bmod], writes=[bmod])
    K.barrier()
    sc0.close()
    if stop_after <= 0:
        K.finish()
        return nc, ins_used

    xT = din("xT", [D, NALL])
    w_in = din("w_in", [D, 5440])
    g_qT = din("g_qT", [128, 6])
    g_kvT = din("g_kvT", [128, 4])
    w_dwT = din("w_dwT", [128, 16, 31])
    b_dwT = din("b_dwT", [128, 16])
    halo_mask = din("halo_mask", [128, 2])
    o_ckvT = dout("o_ckvT", [512, NP_])
    o_kpeT = dout("o_kpeT", [64, NP_])
    qcT_d = dscr("qcT_d", [768, NOWN], BF16)
    rstdq_d = dscr("rstdq_d", [128, NOWN], F32)
    ckvT_d = dscr("ckvT_d", [512, NALL], BF16)
    kpe_d = dscr("kpe_d", [64, NALL], F32)
    conv_d = dscr("conv_d", [2048, NOWN], F32)
    cstat_d = dscr("cstat_d", [2, 128, NOWN], F32)
    xv = xT.rearrange("(kc p) n -> p kc n", p=128)
    wiv = w_in.rearrange("(kc p) n -> p kc n", p=128)

    sc1 = Scope(K)
    gq = sc1.sb("gq", [128, 6], F32)
    gkv = sc1.sb("gkv", [128, 4], F32)
    wdw = sc1.sb("wdw", [128, 16, 31], F32)
    bdw = sc1.sb("bdw", [128, 16], F32)
    hmask = sc1.sb("hmask", [128, 2], F32)
    bsm = Buf()
    for t_, s_ in ((gq, g_qT), (gkv, g_kvT), (wdw, w_dwT), (bdw, b_dwT), (hmask, halo_mask)):
        K.dma(S, t_[:], s_, writes=[bsm])
    wt = [sc1.sb("wt%d" % i, [128, 32, 256], BF16) for i in range(2)]
    bwt = [Buf() for _ in range(2)]
    wti = [0]

    def load_w(c0, ncol, c1=None):
        i = wti[0] % 2
        wti[0] += 1
        if c1 is None:
            K.dma(P, wt[i][:, :, 0:ncol], wiv[:, :, c0:c0 + ncol], writes=[bwt[i]])
        else:
            K.dma(P, wt[i][:, :, 0:128], wiv[:, :, c0:c0 + 128], writes=[bwt[i]])
            K.dma(P, wt[i][:, :, 128:256], wiv[:, :, c1:c1 + 128], writes=[bwt[i]])
        return i

    def modulate(hT, bh, ncols, col0, ranges, sc):
        xs = [sc.sb("xs%d" % i, [128, ncols], F32) for i in range(2)]
        bxs = [Buf() for _ in range(2)]
        for kc in range(32):
            i = kc % 2
            K.dma(S, xs[i][:], xv[:, kc, col0:col0 + ncols], writes=[bxs[i]])
            for ri, (lo, hi, j) in enumerate(ranges):
                if (kc + ri) % 2 == 0:
                    K.op(V, lambda: nc.vector.tensor_scalar(out=hT[:, kc, lo:hi], in0=xs[i][:, lo:hi],
                                                            scalar1=OPSC(0, kc, j), scalar2=MOD(0, kc, j),
                                                            op0=ALU.mult, op1=ALU.add),
                         reads=[bxs[i], bmod], writes=[bh])
                else:
                    K.op(A, lambda: nc.scalar.activation(hT[:, kc, lo:hi], xs[i][:, lo:hi], AF.Identity,
                                                         bias=MOD(0, kc, j), scale=OPSC(0, kc, j)),
                         reads=[bxs[i], bmod], writes=[bh])

    pbi = [0]

    def next_bank(lo=0, hi=4):
        b = lo + pbi[0] % (hi - lo)
        pbi[0] += 1
        return b

    def rstd_from_sumsq(sq, bsq, ncols, nfeat, sc):
        for t in range(0, ncols, 512):
            w = min(512, ncols - t)
            mm(K, banks[7][:, 0:w], [(ones_f[:], sq[:, t:t + w])], reads=[bsq, bconst], writes=[bB[7]])
            K.op(A, lambda: nc.scalar.activation(sq[:, t:t + w], banks[7][:, 0:w], AF.Sqrt, bias=epsc[:, 0:1],
                                                 scale=1.0 / nfeat), reads=[bB[7], bconst], writes=[bsq])
        K.op(V, lambda: nc.vector.reciprocal(out=sq[:, 0:ncols], in_=sq[:, 0:ncols]), reads=[bsq], writes=[bsq])

    def proj_kv(hT, bh, ncols, col0, sc, is_own):
        ntile = ncols // 512
        raw = sc.sb("ckvraw", [128, 4, ncols], F32)
        sq = sc.sb("sqkv", [128, ncols], F32)
        sqt = [sc.sb("sqt%d" % i, [128, 512], F32) for i in range(2)]
        braw, bsq = Buf(), Buf()
        bsqt = [Buf(), Buf()]
        K.op(P, lambda: nc.gpsimd.memset(sq[:], 0.0), writes=[bsq])
        k = 0
        for tl in range(2):
            i = load_w(768 + tl * 256, 256)
            for ob in range(2):
                b4 = tl * 2 + ob
                for t in range(ntile):
                    pb = next_bank()
                    mm(K, banks[pb][:], [(wt[i][:, kc, ob * 128:(ob + 1) * 128], hT[:, kc, t * 512:(t + 1) * 512])
                                         for kc in range(32)], reads=[bwt[i], bh], writes=[bB[pb]])
                    K.op(A, lambda: nc.scalar.copy(raw[:, b4, t * 512:(t + 1) * 512], banks[pb][:]),
                         reads=[bB[pb]], writes=[braw])
                    j = k % 2
                    k += 1
                    K.op(A, lambda: nc.scalar.activation(sqt[j][:], banks[pb][:], AF.Square),
                         reads=[bB[pb]], writes=[bsqt[j]])
                    K.op(V, lambda: nc.vector.tensor_tensor(out=sq[:, t * 512:(t + 1) * 512],
                                                            in0=sq[:, t * 512:(t + 1) * 512], in1=sqt[j][:], op=ALU.add),
                         reads=[bsqt[j], bsq], writes=[bsq])
        rstd_from_sumsq(sq, bsq, ncols, 512.0, sc)
        nrm = [sc.sb("nrm%d" % i, [128, 512], F32) for i in range(2)]
        nrb = [sc.sb("nrb%d" % i, [128, 512], BF16) for i in range(2)]
        bn = [Buf(), Buf()]
        bnb = [Buf(), Buf()]
        k = 0
        for b4 in range(4):
            for t in range(ntile):
                j = k % 2
                k += 1
                cs = slice(t * 512, (t + 1) * 512)
                K.op(V, lambda: nc.vector.scalar_tensor_tensor(out=nrm[j][:], in0=raw[:, b4, cs], scalar=gkv[:, b4:b4 + 1],
                                                               in1=sq[:, cs], op0=ALU.mult, op1=ALU.mult),
                     reads=[braw, bsq, bsm], writes=[bn[j]])
                if is_own and t < 2:
                    K.dma(S, o_ckvT[b4 * 128:(b4 + 1) * 128, cs], nrm[j][:], reads=[bn[j]], final=True)
                K.op(A, lambda: nc.scalar.copy(nrb[j][:], nrm[j][:]), reads=[bn[j]], writes=[bnb[j]])
                K.dma(S, ckvT_d[b4 * 128:(b4 + 1) * 128, col0 + t * 512:col0 + (t + 1) * 512], nrb[j][:],
                      reads=[bnb[j]])
        i = load_w(1280, 64)
        kraw = sc.sb("kraw", [64, ncols], F32)
        bk = Buf()
        for t in range(ntile):
            pb = next_bank()
            mm(K, banks[pb][0:64, :], [(wt[i][:, kc, 0:64], hT[:, kc, t * 512:(t + 1) * 512]) for kc in range(32)],
               reads=[bwt[i], bh], writes=[bB[pb]])
            K.op(A, lambda: nc.scalar.copy(kraw[:, t * 512:(t + 1) * 512], banks[pb][0:64, :]),
                 reads=[bB[pb]], writes=[bk])
        K.dma(S, kpe_d[:, col0:col0 + ncols], kraw[:], reads=[bk])
        if is_own:
            K.dma(S, o_kpeT[:, :], kraw[:, 0:NP_], reads=[bk], final=True)

    scx = Scope(K)
    hTo = scx.sb("hTo", [128, 32, 512], BF16)
    bho = Buf()
    modulate(hTo, bho, 512, NOWN, [(0, 512, 1)], scx)
    proj_kv(hTo, bho, 512, NOWN, scx, False)
    K.barrier()
    scx.close()

    NH = NOWN + 16
    hT = sc1.sb("hT", [128, 32, NH], BF16)
    bh = Buf()
    sca = Scope(K)
    modulate(hT, bh, NH, 0, [(0, NP_, 0), (NP_, NH, 1)], sca)
    K.barrier()
    sca.close()
    sca = Scope(K)
    proj_kv(hT, bh, NOWN, 0, sca, True)
    sqq = sca.sb("sqq", [128, NOWN], F32)
    sqt2 = [sca.sb("sqt2%d" % i, [128, 512], F32) for i in range(2)]
    qst = [sca.sb("qst%d" % i, [128, NOWN], BF16) for i in range(2)]
    bsqq = Buf()
    bsqt2 = [Buf(), Buf()]
    bqst = [Buf(), Buf()]
    K.op(P, lambda: nc.gpsimd.memset(sqq[:], 0.0), writes=[bsqq])
    k = 0
    for tl in range(3):
        i = load_w(tl * 256, 256)
        for ob in range(2):
            qb = tl * 2 + ob
            for t in range(3):
                cs = slice(t * 512, (t + 1) * 512)
                pb = next_bank()
                mm(K, banks[pb][:], [(wt[i][:, kc, ob * 128:(ob + 1) * 128], hT[:, kc, cs]) for kc in range(32)],
                   reads=[bwt[i], bh], writes=[bB[pb]])
                K.op(A, lambda: nc.scalar.activation(qst[qb % 2][:, cs], banks[pb][:], AF.Identity,
                                                     scale=gq[:, qb:qb + 1]),
                     reads=[bB[pb], bsm], writes=[bqst[qb % 2]])
                j = k % 2
                k += 1
                K.op(A, lambda: nc.scalar.activation(sqt2[j][:], banks[pb][:], AF.Square),
                     reads=[bB[pb]], writes=[bsqt2[j]])
                K.op(V, lambda: nc.vector.tensor_tensor(out=sqq[:, cs], in0=sqq[:, cs], in1=sqt2[j][:], op=ALU.add),
                     reads=[bsqt2[j], bsqq], writes=[bsqq])
            K.dma(S, qcT_d[qb * 128:(qb + 1) * 128, :], qst[qb % 2][:], reads=[bqst[qb % 2]])
    rstd_from_sumsq(sqq, bsqq, NOWN, 768.0, sca)
    K.dma(S, rstdq_d, sqq[:], reads=[bsqq])
    K.barrier()
    sca.close()
    if stop_after <= 1:
        K.finish()
        return nc, ins_used

    scb = Scope(K)
    ypad = [scb.sb("ypad%d" % i, [128, YW], BF16) for i in range(2)]
    dg = [scb.sb("dg%d" % i, [128, 31, 128], BF16) for i in range(2)]
    sg = [scb.sb("sg%d" % i, [128, 512], F32) for i in range(2)]
    yh = scb.sb("yh", [128, 16], F32)
    cv = [scb.sb("cv%d" % i, [128, NOWN], F32) for i in range(2)]
    cq = scb.sb("cq", [128, NOWN], F32)
    s1c = scb.sb("s1c", [128, NOWN], F32)
    s2c = scb.sb("s2c", [128, NOWN], F32)
    byp = [Buf(), Buf()]
    bdg = [Buf(), Buf()]
    bsg = [Buf(), Buf()]
    byh, bcq, bs1, bs2 = Buf(), Buf(), Buf(), Buf()
    bcv = [Buf(), Buf()]
    for i in range(2):
        K.op(P, lambda: nc.gpsimd.memset(ypad[i][:], 0.0), writes=[byp[i]])
    K.op(P, lambda: nc.gpsimd.memset(s1c[:], 0.0), writes=[bs1])
    K.op(P, lambda: nc.gpsimd.memset(s2c[:], 0.0), writes=[bs2])

    def ywin(yp, t, k):
        if t < 2:
            return yp[:, t * 572:(t + 1) * 572].rearrange("p (s w) -> p s w", w=286)[:, :, k:k + 256]
        return yp[:, 1144 + k:1144 + k + 512]

    def conv_chunk(j):
        yp = ypad[j % 2]
        for t in range(3):
            if t < 2:
                o = banks[4 + t][:].rearrange("p (s w) -> p s w", w=256)
            else:
                o = banks[4 + t][:]
            mm(K, o, [(dg[j % 2][:, k, :], ywin(yp, t, k)) for k in range(31)],
               reads=[bdg[j % 2], byp[j % 2]], writes=[bB[4 + t]])
            cs = slice(t * 512, (t + 1) * 512)
            K.op(A, lambda: nc.scalar.activation(cv[j % 2][:, cs], banks[4 + t][:], AF.Identity,
                                                 bias=bdw[:, j:j + 1]),
                 reads=[bB[4 + t], bsm], writes=[bcv[j % 2]])
        K.dma(S, conv_d[j * 128:(j + 1) * 128, :], cv[j % 2][:], reads=[bcv[j % 2]])
        K.op(V, lambda: nc.vector.tensor_tensor(out=s1c[:], in0=s1c[:], in1=cv[j % 2][:], op=ALU.add),
             reads=[bcv[j % 2], bs1], writes=[bs1])
        K.op(A, lambda: nc.scalar.activation(cq[:], cv[j % 2][:], AF.Square), reads=[bcv[j % 2]], writes=[bcq])
        K.op(V, lambda: nc.vector.tensor_tensor(out=s2c[:], in0=s2c[:], in1=cq[:], op=ALU.add),
             reads=[bcq, bs2], writes=[bs2])

    for jp in range(8):
        ia = load_w(1344 + 256 * jp, 256)
        ig = load_w(3392 + 256 * jp, 256)
        for jj in range(2):
            j = jp * 2 + jj
            yp = ypad[j % 2]
            K.op(V, lambda: nc.vector.tensor_tensor(
                out=dg[j % 2][:], in0=ident_b[:].unsqueeze(1).to_broadcast([128, 31, 128]),
                in1=wdw[:, j, :].unsqueeze(2).to_broadcast([128, 31, 128]), op=ALU.mult),
                reads=[bconst, bsm], writes=[bdg[j % 2]])
            for t in range(4):
                if t < 3:
                    cs = slice(t * 512, (t + 1) * 512)
                    w_ = 512
                else:
                    cs = slice(NOWN, NOWN + 16)
                    w_ = 16
                pa = next_bank()
                pg = next_bank()
                mm(K, banks[pa][:, 0:w_], [(wt[ia][:, kc, jj * 128:(jj + 1) * 128], hT[:, kc, cs]) for kc in range(32)],
                   reads=[bwt[ia], bh], writes=[bB[pa]])
                mm(K, banks[pg][:, 0:w_], [(wt[ig][:, kc, jj * 128:(jj + 1) * 128], hT[:, kc, cs]) for kc in range(32)],
                   reads=[bwt[ig], bh], writes=[bB[pg]])
                s_ = sg[t % 2]
                K.op(A, lambda: nc.scalar.activation(s_[:, 0:w_], banks[pg][:, 0:w_], AF.Sigmoid),
                     reads=[bB[pg]], writes=[bsg[t % 2]])
                if t < 2:
                    o = yp[:, t * 572:(t + 1) * 572].rearrange("p (s w) -> p s w", w=286)[:, :, 15:271]
                    K.op(V, lambda: nc.vector.tensor_tensor(
                        out=o, in0=banks[pa][:].rearrange("p (s w) -> p s w", w=256),
                        in1=s_[:].rearrange("p (s w) -> p s w", w=256), op=ALU.mult),
                        reads=[bB[pa], bsg[t % 2]], writes=[byp[j % 2]])
                elif t == 2:
                    K.op(V, lambda: nc.vector.tensor_tensor(out=yp[:, 1159:1159 + 512], in0=banks[pa][:], in1=s_[:],
                                                            op=ALU.mult),
                         reads=[bB[pa], bsg[t % 2]], writes=[byp[j % 2]])
                else:
                    K.op(V, lambda: nc.vector.tensor_tensor(out=yh[:], in0=banks[pa][:, 0:16], in1=s_[:, 0:16],
                                                            op=ALU.mult),
                         reads=[bB[pa], bsg[t % 2]], writes=[byh])
                    K.op(V, lambda: nc.vector.tensor_scalar(out=yp[:, 1144:1159], in0=yh[:, 0:15],
                                                            scalar1=hmask[:, 0:1], scalar2=None, op0=ALU.mult),
                         reads=[byh, bsm], writes=[byp[j % 2]])
                    K.op(V, lambda: nc.vector.tensor_scalar(out=yp[:, 1671:1686], in0=yh[:, 0:15],
                                                            scalar1=hmask[:, 1:2], scalar2=None, op0=ALU.mult),
                         reads=[byh, bsm], writes=[byp[j % 2]])
            if j > 0:
                conv_chunk(j - 1)
    conv_chunk(15)
    for t in range(3):
        cs = slice(t * 512, (t + 1) * 512)
        mm(K, banks[0][:], [(ones_f[:], s1c[:, cs])], reads=[bs1, bconst], writes=[bB[0]])
        mm(K, banks[1][:], [(ones_f[:], s2c[:, cs])], reads=[bs2, bconst], writes=[bB[1]])
        K.op(V, lambda: nc.vector.tensor_scalar(out=s1c[:, cs], in0=banks[0][:], scalar1=1.0 / 2048, scalar2=None,
                                                op0=ALU.mult), reads=[bB[0]], writes=[bs1])
        K.op(V, lambda: nc.vector.tensor_tensor(out=cq[:, cs], in0=s1c[:, cs], in1=s1c[:, cs], op=ALU.mult),
             reads=[bs1], writes=[bcq])
        K.op(V, lambda: nc.vector.scalar_tensor_tensor(out=s2c[:, cs], in0=banks[1][:], scalar=1.0 / 2048,
                                                       in1=cq[:, cs], op0=ALU.mult, op1=ALU.subtract),
             reads=[bB[1], bcq], writes=[bs2])
    K.op(A, lambda: nc.scalar.activation(s2c[:], s2c[:], AF.Sqrt, bias=epsc[:, 0:1], scale=1.0),
         reads=[bs2, bconst], writes=[bs2])
    K.op(V, lambda: nc.vector.reciprocal(out=s2c[:], in_=s2c[:]), reads=[bs2], writes=[bs2])
    K.dma(S, cstat_d[0], s1c[:], reads=[bs1])
    K.dma(S, cstat_d[1], s2c[:], reads=[bs2])
    K.barrier()
    scb.close()
    sc1.close()
    if stop_after <= 2:
        K.finish()
        return nc, ins_used

    w_uq = din("w_uq", [768, 3072])
    w_ukv = din("w_ukv", [512, 4096])
    cache_ckvT = din("cache_ckvT", [128, 4, 256])
    cache_kpeT = din("cache_kpeT", [64, 256])
    cosT = din("cosT", [64, 1024])
    sinT = din("sinT", [64, 1024])
    rmatT = din("rmatT", [64, 64])
    attnT_d = dscr("attnT_d", [2048, NOWN], BF16)
    wuqv = w_uq.rearrange("(kc p) n -> p kc n", p=128)
    wukvv = w_ukv.rearrange("(kc p) n -> p kc n", p=128)
    sc2 = Scope(K)
    ckv = sc2.sb("ckv", [128, 4, NKEY], BF16)
    kpe = sc2.sb("kpeb", [64, NKEY], BF16)
    qc = sc2.sb("qc", [128, 6, NOWN], BF16)
    rq = sc2.sb("rq", [128, NOWN], F32)
    kraw = sc2.sb("kraw2", [64, NALL], F32)
    cos = sc2.sb("cos", [64, 1024], F32)
    sin = sc2.sb("sin", [64, 1024], F32)
    rm = sc2.sb("rm", [64, 64], F32)
    rt1 = sc2.sb("rt1", [64, 512], F32)
    rt2 = sc2.sb("rt2", [64, 512], F32)
    qpf = sc2.sb("qpf", [64, 512], F32)
    bckv, bkpe, bqc, brq, bkraw, btab, brt1, brt2, bqpf = [Buf() for _ in range(9)]
    K.dma(S, ckv[:, :, 0:NALL], ckvT_d.rearrange("(kc p) n -> p kc n", p=128), writes=[bckv])
    K.dma(P, ckv[:, :, NALL:NKEY], cache_ckvT, writes=[bckv])
    K.dma(S, kraw[:], kpe_d, writes=[bkraw])
    K.dma(P, kpe[:, NALL:NKEY], cache_kpeT, writes=[bkpe])
    K.dma(S, qc[:], qcT_d.rearrange("(kc p) n -> p kc n", p=128), writes=[bqc])
    K.dma(S, rq[:], rstdq_d, writes=[brq])
    K.dma(S, cos[:], cosT, writes=[btab])
    K.dma(S, sin[:], sinT, writes=[btab])
    K.dma(S, rm[:], rmatT, writes=[btab])
    K.op(A, lambda: nc.scalar.copy(kpe[:, 0:NP_], kraw[:, 0:NP_]), reads=[bkraw], writes=[bkpe])

    def rope(dst, bdst, src, bsrc, tab0):
        pb = next_bank(0, 6)
        mm(K, banks[pb][0:64, :], [(rm[:, :], src)], reads=[btab, bsrc], writes=[bB[pb]])
        K.op(V, lambda: nc.vector.tensor_tensor(out=rt1[:], in0=src, in1=cos[:, tab0:tab0 + 512], op=ALU.mult),
             reads=[bsrc, btab], writes=[brt1])
        K.op(V, lambda: nc.vector.tensor_tensor(out=rt2[:], in0=banks[pb][0:64, :], in1=sin[:, tab0:tab0 + 512],
                                                op=ALU.mult), reads=[bB[pb], btab], writes=[brt2])
        K.op(V, lambda: nc.vector.tensor_tensor(out=dst, in0=rt1[:], in1=rt2[:], op=ALU.add),
             reads=[brt1, brt2], writes=[bdst])

    for t in range(2):
        rope(kpe[:, NP_ + t * 512:NP_ + (t + 1) * 512], bkpe, kraw[:, NP_ + t * 512:NP_ + (t + 1) * 512], bkraw, t * 512)

    wq = [sc2.sb("wq%d" % i, [128, 6, 192], BF16) for i in range(2)]
    wkv = [sc2.sb("wkv%d" % i, [128, 4, 256], BF16) for i in range(2)]
    qn = [sc2.sb("qn%d" % i, [128, NOWN], BF16) for i in range(2)]
    qp = [sc2.sb("qp%d" % i, [64, NOWN], BF16) for i in range(2)]
    kn = [sc2.sb("kn%d" % i, [128, NKEY], BF16) for i in range(2)]
    vh = [sc2.sb("vh%d" % i, [128, 18, 128], BF16) for i in range(2)]
    ast = [sc2.sb("ast%d" % i, [128, NOWN], BF16) for i in range(2)]
    p32 = [sc2.sb("p32%d" % i, [128, 1280], F32) for i in range(2)]
    pn = [sc2.sb("pn%d" % i, [128, 1280], BF16) for i in range(2)]
    pt = [sc2.sb("pt%d" % i, [128, 10, 128], BF16) for i in range(2)]
    st = [sc2.sb("st%d" % i, [128, 8], F32) for i in range(2)]
    bwq, bwkv, bqn, bqp, bkn, bvh, bast, bp32, bpn, bpt, bst, bpv = [[Buf(), Buf()] for _ in range(12)]
    tb = [banks[6][:].bitcast(BF16), banks[7][:].bitcast(BF16)]

    qblocks = []
    for s_ in range(4):
        for hh in range(2):
            qblocks.append((s_ * 256 + hh * 128, s_ * 256, 256, 2 * s_))
    for i_ in range(4):
        qblocks.append((NP_ + i_ * 128, NP_, 1280, 8))

    _NH = int(os.environ.get('P2_HEADS', 16)); _NQ = int(os.environ.get('P2_QB', 12)); _STG = int(os.environ.get('P2_STAGE', 4))
    wr2 = [sc2.sb("wadb%d" % i, [128, 32, 512], BF16) for i in range(2)]
    bwr2 = [Buf(), Buf()]
    bmod2 = Buf()

    def emit_ada_tile(ct):
        i = ct % 2
        K.dma(P, wr2[i][:], wav[:, :, ct * 512:(ct + 1) * 512], writes=[bwr2[i]])
        pb = next_bank(0, 6)
        for ob in range(4):
            mm(K, banks[pb][:, ob * 2:ob * 2 + 2],
               [(wr2[i][:, kc, ob * 128:(ob + 1) * 128], sil[:, kc, :]) for kc in range(32)],
               reads=[bwr2[i], bc], writes=[bB[pb]])
        K.op(V, lambda: nc.vector.tensor_tensor(
            out=mod[:, ct * 4:(ct + 1) * 4, :],
            in0=banks[pb][:, 0:8].rearrange("p (a b) -> p a b", b=2),
            in1=bad[:, ct * 4:(ct + 1) * 4].unsqueeze(2).to_broadcast([128, 4, 2]), op=ALU.add),
            reads=[bB[pb], bc], writes=[bmod2])

    for h in range(_NH):
        hp = h % 2
        emit_ada_tile(16 + 2 * h)
        emit_ada_tile(17 + 2 * h)
        K.dma(P, wq[hp][:], wuqv[:, :, h * 192:(h + 1) * 192], writes=[bwq[hp]])
        K.dma(P, wkv[hp][:], wukvv[:, :, h * 256:(h + 1) * 256], writes=[bwkv[hp]])
        for t in range(3):
            cs = slice(t * 512, (t + 1) * 512)
            pb = next_bank(0, 6)
            mm(K, banks[pb][:], [(wq[hp][:, kc, 0:128], qc[:, kc, cs]) for kc in range(6)],
               reads=[bwq[hp], bqc], writes=[bB[pb]])
            K.op(V, lambda: nc.vector.tensor_tensor(out=qn[hp][:, cs], in0=banks[pb][:], in1=rq[:, cs], op=ALU.mult),
                 reads=[bB[pb], brq], writes=[bqn[hp]])
            pb = next_bank(0, 6)
            mm(K, banks[pb][0:64, :], [(wq[hp][:, kc, 128:192], qc[:, kc, cs]) for kc in range(6)],
               reads=[bwq[hp], bqc], writes=[bB[pb]])
            if t < 2:
                K.op(V, lambda: nc.vector.tensor_tensor(out=qp[hp][:, cs], in0=banks[pb][0:64, :], in1=rq[0:64, cs],
                                                        op=ALU.mult), reads=[bB[pb], brq], writes=[bqp[hp]])
            else:
                K.op(V, lambda: nc.vector.tensor_tensor(out=qpf[:], in0=banks[pb][0:64, :], in1=rq[0:64, cs],
                                                        op=ALU.mult), reads=[bB[pb], brq], writes=[bqpf])
                rope(qp[hp][:, cs], bqp[hp], qpf[:], bqpf, 0)
        for t in range(5):
            w_ = 512 if t < 4 else 256
            cs = slice(t * 512, t * 512 + w_)
            pb = next_bank(0, 6)
            mm(K, banks[pb][:, 0:w_], [(wkv[hp][:, kc, 0:128], ckv[:, kc, cs]) for kc in range(4)],
               reads=[bwkv[hp], bckv], writes=[bB[pb]])
            K.op(A, lambda: nc.scalar.copy(kn[hp][:, cs], banks[pb][:, 0:w_]), reads=[bB[pb]], writes=[bkn[hp]])
        for g in range(5):
            nb_ = 4 if g < 4 else 2
            pb = next_bank(0, 6)
            for kk in range(nb_):
                kb = g * 4 + kk
                mm(K, banks[pb][:, kk * 128:(kk + 1) * 128],
                   [(ckv[:, kc, kb * 128:(kb + 1) * 128], wkv[hp][:, kc, 128:256]) for kc in range(4)],
                   reads=[bwkv[hp], bckv], writes=[bB[pb]])
            K.op(A if g % 2 else V,
                 (lambda: nc.scalar.copy(vh[hp][:, g * 4:g * 4 + nb_, :],
                                         banks[pb][:, 0:nb_ * 128].rearrange("p (a b) -> p a b", b=128))) if g % 2 else
                 (lambda: nc.vector.tensor_copy(out=vh[hp][:, g * 4:g * 4 + nb_, :],
                                                in_=banks[pb][:, 0:nb_ * 128].rearrange("p (a b) -> p a b", b=128))),
                 reads=[bB[pb]], writes=[bvh[hp]])

        def emit_S(qi):
            q0, k0, nk, vb0 = qblocks[qi]
            base = 3 * (qi % 2)
            for kt in range((nk + 511) // 512):
                w_ = min(512, nk - kt * 512)
                ks = slice(k0 + kt * 512, k0 + kt * 512 + w_)
                mm(K, banks[base + kt][:, 0:w_],
                   [(qn[hp][:, q0:q0 + 128], kn[hp][:, ks]), (qp[hp][:, q0:q0 + 128], kpe[:, ks])],
                   reads=[bqn[hp], bqp[hp], bkn[hp], bkpe], writes=[bB[base + kt]])

        def emit_softmax(qi):
            q0, k0, nk, vb0 = qblocks[qi]
            base = 3 * (qi % 2)
            e = qi % 2
            nt = (nk + 511) // 512
            for kt in range(nt):
                w_ = min(512, nk - kt * 512)
                K.op(V, lambda: nc.vector.reduce_max(out=st[e][:, kt:kt + 1], in_=banks[base + kt][:, 0:w_], axis=AX.X),
                     reads=[bB[base + kt]], writes=[bst[e]])
            if nt > 1:
                K.op(V, lambda: nc.vector.reduce_max(out=st[e][:, 3:4], in_=st[e][:, 0:nt], axis=AX.X),
                     reads=[bst[e]], writes=[bst[e]])
                mcol = 3
            else:
                mcol = 0
            K.op(V, lambda: nc.vector.tensor_scalar(out=st[e][:, 4:5], in0=st[e][:, mcol:mcol + 1], scalar1=-SCALE,
                                                    scalar2=None, op0=ALU.mult), reads=[bst[e]], writes=[bst[e]])
            for kt in range(nt):
                w_ = min(512, nk - kt * 512)
                K.op(A, lambda: nc.scalar.activation(p32[e][:, kt * 512:kt * 512 + w_], banks[base + kt][:, 0:w_],
                                                     AF.Exp, bias=st[e][:, 4:5], scale=SCALE),
                     reads=[bB[base + kt], bst[e]], writes=[bp32[e]])
            K.op(V, lambda: nc.vector.reduce_sum(out=st[e][:, 5:6], in_=p32[e][:, 0:nk], axis=AX.X),
                 reads=[bp32[e]], writes=[bst[e]])
            K.op(V, lambda: nc.vector.reciprocal(out=st[e][:, 6:7], in_=st[e][:, 5:6]), reads=[bst[e]], writes=[bst[e]])
            K.op(A, lambda: nc.scalar.activation(pn[e][:, 0:nk], p32[e][:, 0:nk], AF.Identity, scale=st[e][:, 6:7]),
                 reads=[bp32[e], bst[e]], writes=[bpn[e]])

        def emit_PV(qi):
            q0, k0, nk, vb0 = qblocks[qi]
            base = 3 * (qi % 2)
            e = qi % 2
            nkb = nk // 128
            for kb in range(nkb):
                tbi = kb // 8
                K.op(K.pe, lambda: nc.tensor.transpose(tb[tbi][:, (kb % 8) * 128:(kb % 8 + 1) * 128],
                                                       pn[e][:, kb * 128:(kb + 1) * 128], ident_b[:]),
                     reads=[bpn[e], bconst], writes=[bB[6 + tbi]])
            n0 = min(nkb, 8)
            K.op(V, lambda: nc.vector.tensor_copy(out=pt[e][:, 0:n0, :],
                                                  in_=tb[0][:, 0:n0 * 128].rearrange("p (a b) -> p a b", b=128)),
                 reads=[bB[6]], writes=[bpt[e]])
            if nkb > 8:
                K.op(V, lambda: nc.vector.tensor_copy(out=pt[e][:, 8:nkb, :],
                                                      in_=tb[1][:, 0:(nkb - 8) * 128].rearrange("p (a b) -> p a b", b=128)),
                     reads=[bB[7]], writes=[bpt[e]])
            mm(K, banks[base + 2][:, 256:384], [(vh[hp][:, vb0 + kb, :], pt[e][:, kb, :]) for kb in range(nkb)],
               reads=[bvh[hp], bpt[e]], writes=[bB[base + 2]])
            K.op(A, lambda: nc.scalar.copy(ast[hp][:, q0:q0 + 128], banks[base + 2][:, 256:384]),
                 reads=[bB[base + 2]], writes=[bast[hp]])

        if _STG >= 2:
            emit_S(0)
        for qi in range(_NQ):
            if qi + 1 < _NQ and _STG >= 2:
                emit_S(qi + 1)
            if _STG >= 3:
                emit_softmax(qi)
            if _STG >= 4:
                emit_PV(qi)
        K.dma(S, attnT_d[h * 128:(h + 1) * 128, :], ast[hp][:], reads=[bast[hp]])
    K.op(V, lambda: nc.vector.tensor_scalar(out=opsc[:, 1, :, :], in0=mod[:, 128:160, :], scalar1=1.0, scalar2=None,
                                            op0=ALU.add), reads=[bmod2], writes=[bmod])
    if dbg:
        d_mod = dout("d_mod", [128, 192, 2])
        K.dma(S, d_mod, mod[:], reads=[bmod, bmod2])
    K.barrier()
    sc2.close()
    if stop_after <= 3:
        K.finish()
        return nc, ins_used

    def ln_stats(s1, bs1, s2, bs2, tmp, btmp, nfeat, ncols):
        for t in range(0, ncols, 512):
            cs = slice(t, t + 512)
            mm(K, banks[6][:], [(ones_f[:], s1[:, cs])], reads=[bs1, bconst], writes=[bB[6]])
            mm(K, banks[7][:], [(ones_f[:], s2[:, cs])], reads=[bs2, bconst], writes=[bB[7]])
            K.op(V, lambda: nc.vector.tensor_scalar(out=s1[:, cs], in0=banks[6][:], scalar1=1.0 / nfeat, scalar2=None,
                                                    op0=ALU.mult), reads=[bB[6]], writes=[bs1])
            K.op(V, lambda: nc.vector.tensor_tensor(out=tmp[:, cs], in0=s1[:, cs], in1=s1[:, cs], op=ALU.mult),
                 reads=[bs1], writes=[btmp])
            K.op(V, lambda: nc.vector.scalar_tensor_tensor(out=s2[:, cs], in0=banks[7][:], scalar=1.0 / nfeat,
                                                           in1=tmp[:, cs], op0=ALU.mult, op1=ALU.subtract),
                 reads=[bB[7], btmp], writes=[bs2])
        K.op(A, lambda: nc.scalar.activation(s2[:, 0:ncols], s2[:, 0:ncols], AF.Sqrt, bias=epsc[:, 0:1], scale=1.0),
             reads=[bs2, bconst], writes=[bs2])
        K.op(V, lambda: nc.vector.reciprocal(out=s2[:, 0:ncols], in_=s2[:, 0:ncols]), reads=[bs2], writes=[bs2])

    w_out = din("w_out", [D, D])
    g_cnT = din("g_cnT", [128, 16])
    b_cnT = din("b_cnT", [128, 16])
    ln1_gT = din("ln1_gT", [128, 32])
    ln1_bT = din("ln1_bT", [128, 32])
    zT_d = dscr("zT_d", [D, NOWN], F32)
    stat1_d = dscr("stat1_d", [2, 128, NOWN], F32)
    h2T_d = dscr("h2T_d", [D, NOWN], BF16)
    wov = w_out.rearrange("(kc p) n -> p kc n", p=128)
    sc3 = Scope(K)
    mix = sc3.sb("mix", [128, 32, NOWN], BF16)
    gcn = sc3.sb("gcn", [128, 16], F32)
    bcn = sc3.sb("bcn", [128, 16], F32)
    l1g = sc3.sb("l1g", [128, 32], F32)
    l1b = sc3.sb("l1b", [128, 32], F32)
    a1 = sc3.sb("a1", [128, 32, 2], F32)
    b1 = sc3.sb("b1", [128, 32, 2], F32)
    bmix, bsm3, bab = Buf(), Buf(), Buf()
    for t_, s_ in ((gcn, g_cnT), (bcn, b_cnT), (l1g, ln1_gT), (l1b, ln1_bT)):
        K.dma(S, t_[:], s_, writes=[bsm3])
    K.dma(S, mix[:, 0:16, :], attnT_d.rearrange("(kc p) n -> p kc n", p=128), writes=[bmix])
    K.op(V, lambda: nc.vector.tensor_tensor(out=a1[:], in0=opsc[:, 1, :, :],
                                            in1=l1g[:].unsqueeze(2).to_broadcast([128, 32, 2]), op=ALU.mult),
         reads=[bmod, bsm3], writes=[bab])
    K.op(V, lambda: nc.vector.tensor_tensor(out=b1[:], in0=opsc[:, 1, :, :],
                                            in1=l1b[:].unsqueeze(2).to_broadcast([128, 32, 2]), op=ALU.mult),
         reads=[bmod, bsm3], writes=[bab])
    K.op(V, lambda: nc.vector.tensor_tensor(out=b1[:], in0=b1[:], in1=mod[:, 96:128, :], op=ALU.add),
         reads=[bmod, bab], writes=[bab])
    s3a = Scope(K)
    cm = s3a.sb("cm", [128, NOWN], F32)
    cr = s3a.sb("cr", [128, NOWN], F32)
    cvl = [s3a.sb("cvl%d" % i, [128, NOWN], F32) for i in range(3)]
    cvt = [s3a.sb("cvt%d" % i, [128, NOWN], F32) for i in range(3)]
    bcs = Buf()
    bcvl = [Buf(), Buf(), Buf()]
    bcvt = [Buf(), Buf(), Buf()]
    K.dma(S, cm[:], cstat_d[0], writes=[bcs])
    K.dma(S, cr[:], cstat_d[1], writes=[bcs])
    for j in range(16):
        i = j % 3
        K.dma(S, cvl[i][:], conv_d[j * 128:(j + 1) * 128, :], writes=[bcvl[i]])
        K.op(V, lambda: nc.vector.tensor_tensor(out=cvt[i][:], in0=cvl[i][:], in1=cm[:], op=ALU.subtract),
             reads=[bcvl[i], bcs], writes=[bcvt[i]])
        K.op(P, lambda: nc.gpsimd.tensor_tensor(out=cvt[i][:], in0=cvt[i][:], in1=cr[:], op=ALU.mult),
             reads=[bcvt[i], bcs], writes=[bcvt[i]])
        K.op(A, lambda: nc.scalar.activation(mix[:, 16 + j, :], cvt[i][:], AF.Silu, bias=bcn[:, j:j + 1],
                                             scale=gcn[:, j:j + 1]), reads=[bcvt[i], bsm3], writes=[bmix])
    K.barrier()
    s3a.close()
    s3b = Scope(K)
    wo = [s3b.sb("wo%d" % i, [128, 32, 256], BF16) for i in range(2)]
    xs3 = [s3b.sb("xs3%d" % i, [128, NOWN], F32) for i in range(2)]
    zt = [s3b.sb("zt%d" % i, [128, NOWN], F32) for i in range(2)]
    zz = [s3b.sb("zz%d" % i, [128, NOWN], F32) for i in range(2)]
    z1 = s3b.sb("z1", [128, NOWN], F32)
    z2 = s3b.sb("z2", [128, NOWN], F32)
    zq = s3b.sb("zq", [128, NOWN], F32)
    bwo = [Buf() for _ in range(2)]
    bxs3, bzt, bzz = [[Buf(), Buf()] for _ in range(3)]
    bz1, bz2, bzq = Buf(), Buf(), Buf()
    K.op(P, lambda: nc.gpsimd.memset(z1[:], 0.0), writes=[bz1])
    K.op(P, lambda: nc.gpsimd.memset(z2[:], 0.0), writes=[bz2])
    for tl in range(16):
        i = tl % 2
        K.dma(P, wo[i][:], wov[:, :, tl * 256:(tl + 1) * 256], writes=[bwo[i]])
        for ob in range(2):
            db = tl * 2 + ob
            e = db % 2
            K.dma(S, xs3[e][:], xv[:, db, 0:NOWN], writes=[bxs3[e]])
            for t in range(3):
                cs = slice(t * 512, (t + 1) * 512)
                j = 0 if t < 2 else 1
                pb = next_bank(0, 6)
                mm(K, banks[pb][:], [(wo[i][:, kc, ob * 128:(ob + 1) * 128], mix[:, kc, cs]) for kc in range(32)],
                   reads=[bwo[i], bmix], writes=[bB[pb]])
                K.op(A, lambda: nc.scalar.activation(zt[e][:, cs], banks[pb][:], AF.Identity, scale=MOD(2, db, j)),
                     reads=[bB[pb], bmod], writes=[bzt[e]])
                K.op(V, lambda: nc.vector.scalar_tensor_tensor(out=zz[e][:, cs], in0=xs3[e][:, cs], scalar=ALPHA,
                                                               in1=zt[e][:, cs], op0=ALU.mult, op1=ALU.add),
                     reads=[bxs3[e], bzt[e]], writes=[bzz[e]])
            K.dma(S, zT_d[db * 128:(db + 1) * 128, :], zz[e][:], reads=[bzz[e]])
            K.op(P, lambda: nc.gpsimd.tensor_tensor(out=z1[:], in0=z1[:], in1=zz[e][:], op=ALU.add),
                 reads=[bzz[e], bz1], writes=[bz1])
            K.op(A, lambda: nc.scalar.activation(zq[:], zz[e][:], AF.Square), reads=[bzz[e]], writes=[bzq])
            K.op(P, lambda: nc.gpsimd.tensor_tensor(out=z2[:], in0=z2[:], in1=zq[:], op=ALU.add),
                 reads=[bzq, bz2], writes=[bz2])
    ln_stats(z1, bz1, z2, bz2, zq, bzq, 4096.0, NOWN)
    K.dma(S, stat1_d[0], z1[:], reads=[bz1])
    K.dma(S, stat1_d[1], z2[:], reads=[bz2])
    h2s = [s3b.sb("h2s%d" % i, [128, NOWN], BF16) for i in range(2)]
    bh2s = [Buf(), Buf()]
    zin = [xs3[0], xs3[1], zz[0]]
    bzin = [bxs3[0], bxs3[1], bzz[0]]
    znr = [zt[0], zt[1], zz[1]]
    bznr = [bzt[0], bzt[1], bzz[1]]
    for db in range(32):
        e = db % 2
        e3 = db % 3
        K.dma(S, zin[e3][:], zT_d[db * 128:(db + 1) * 128, :], writes=[bzin[e3]])
        K.op(V, lambda: nc.vector.tensor_tensor(out=znr[e3][:], in0=zin[e3][:], in1=z1[:], op=ALU.subtract),
             reads=[bzin[e3], bz1], writes=[bznr[e3]])
        K.op(P, lambda: nc.gpsimd.tensor_tensor(out=znr[e3][:], in0=znr[e3][:], in1=z2[:], op=ALU.mult),
             reads=[bznr[e3], bz2], writes=[bznr[e3]])
        for (lo, hi, j) in ((0, NP_, 0), (NP_, NOWN, 1)):
            K.op(A, lambda: nc.scalar.activation(h2s[e][:, lo:hi], znr[e3][:, lo:hi], AF.Identity,
                                                 bias=b1[:, db, j:j + 1], scale=a1[:, db, j:j + 1]),
                 reads=[bznr[e3], bab], writes=[bh2s[e]])
        K.dma(S, h2T_d[db * 128:(db + 1) * 128, :], h2s[e][:], reads=[bh2s[e]])
    K.barrier()
    s3b.close()
    sc3.close()
    if stop_after <= 4:
        K.finish()
        return nc, ins_used

    w_pq = din("w_pq", [D, D])
    skT = din("skT", [128, 32, 128])
    s_d = dscr("s_d", [8, NOWN, 256], F32)
    G_d = dscr("G_d", [128, 128, NOWN], BF16)
    wpv = w_pq.rearrange("(kc p) n -> p kc n", p=128)
    sc4 = Scope(K)
    h2 = sc4.sb("h2", [128, 32, NOWN], BF16)
    sk = sc4.sb("sk", [128, 32, 128], BF16)
    wp = [sc4.sb("wp%d" % i, [128, 32, 256], BF16) for i in range(3)]
    qhp = [sc4.sb("qhp%d" % i, [128, 2, NOWN], BF16) for i in range(2)]
    sst = [sc4.sb("sst%d" % i, [128, 12, 128], F32) for i in range(2)]
    bh2, bsk = Buf(), Buf()
    bwp = [Buf() for _ in range(3)]
    bqhp, bsst = [[Buf(), Buf()] for _ in range(2)]
    h2v = h2T_d.rearrange("(kc p) n -> p kc n", p=128)
    for q4 in range(4):
        K.dma(S, h2[:, q4 * 8:(q4 + 1) * 8, :], h2v[:, q4 * 8:(q4 + 1) * 8, :], writes=[bh2])
    K.dma(P, sk[:], skT, writes=[bsk])
    s_dv = s_d.rearrange("h (nb p) (t k) -> h t p nb k", p=128, k=128)
    cpy = [0]

    def evac(out, in_, reads, writes):
        cpy[0] += 1
        if cpy[0] % 2:
            return K.op(A, lambda: nc.scalar.copy(out, in_), reads=reads, writes=writes)
        return K.op(V, lambda: nc.vector.tensor_copy(out=out, in_=in_), reads=reads, writes=writes)

    for hp_ in range(16):
        i = hp_ % 3
        e = hp_ % 2
        K.dma(P, wp[i][:], wpv[:, :, hp_ * 256:(hp_ + 1) * 256], writes=[bwp[i]])
        for half in range(2):
            for t in range(3):
                cs = slice(t * 512, (t + 1) * 512)
                pb = next_bank(0, 4)
                mm(K, banks[pb][:], [(wp[i][:, kc, half * 128:(half + 1) * 128], h2[:, kc, cs]) for kc in range(32)],
                   reads=[bwp[i], bh2], writes=[bB[pb]])
                evac(qhp[e][:, half, cs], banks[pb][:], [bB[pb]], [bqhp[e]])
        for g in range(3):
            pb = 4 + (hp_ * 3 + g) % 4
            for kk in range(4):
                nb = g * 4 + kk
                mm(K, banks[pb][:, kk * 128:(kk + 1) * 128],
                   [(qhp[e][:, half, nb * 128:(nb + 1) * 128], sk[:, hp_ * 2 + half, :]) for half in range(2)],
                   reads=[bqhp[e], bsk], writes=[bB[pb]])
            evac(sst[e][:, g * 4:(g + 1) * 4, :], banks[pb][:].rearrange("p (a b) -> p a b", b=128), [bB[pb]], [bsst[e]])
        K.dma(S, s_dv[hp_ // 2, hp_ % 2], sst[e][:], reads=[bsst[e]])
    K.barrier()
    sc4.close()
    if stop_after <= 5:
        K.finish()
        return nc, ins_used

    scr = Scope(K)
    stm = [scr.sb("stm%d" % i, [128, 2048], F32) for i in range(2)]
    wrk = scr.sb("wrk", [128, 2048], F32)
    tp = scr.sb("tp", [128, 16, 16], F32)
    cand = scr.sb("cand", [128, 8, 256], F32)
    ctop = scr.sb("ctop", [128, 8, 16], F32)
    ce = scr.sb("ce", [128, 8, 16], F32)
    sm = scr.sb("sm", [128, 64], F32)
    At = scr.sb("At", [128, 3, 128], F32)
    AT = [scr.sb("AT%d" % i, [128, 3, 128], F32) for i in range(2)]
    srep = [scr.sb("srep%d" % i, [128, 16, 256], F32) for i in range(3)]
    zr = [scr.sb("zr%d" % i, [128, 16, 128], F32) for i in range(2)]
    er = [scr.sb("er%d" % i, [128, 16, 128], BF16) for i in range(2)]
    mk = [scr.sb("mk%d" % i, [128, 16, 128], BF16) for i in range(2)]
    Rb = [scr.sb("Rb%d" % i, [128, 16, 128], BF16) for i in range(2)]
    P1b = [scr.sb("P1b%d" % i, [128, 16, 128], BF16) for i in range(2)]
    gst = [scr.sb("gst%d" % i, [128, 128, 128], BF16) for i in range(2)]
    bstm, bAT, bsrep, bzr, bzc, ber, bmk, bRb, bP1b, bgst = [[Buf(), Buf(), Buf()] for _ in range(10)]
    bwrk, btp, bcand, bctop, bce, bsmm, bAt = [Buf() for _ in range(7)]
    tpv = tp[:].rearrange("p (h t) a -> p h t a", t=2)
    G_dv = G_d.rearrange("i j n -> j i n")

    bc816 = lambda ap: ap.unsqueeze(2).to_broadcast([128, 8, 16])
    btpg = [Buf() for _ in range(16)]
    bwkg = [Buf() for _ in range(16)]
    bctg = [Buf() for _ in range(8)]
    bcdg = [Buf() for _ in range(8)]

    def top16_batch(dsts, srcs, width, rds, wrs, wks, bwk):
        n = len(dsts)
        for i in range(n):
            K.op(V, lambda: nc.vector.max(out=dsts[i][:, 0:8], in_=srcs[i]), reads=rds[i], writes=[wrs[i]])
        for i in range(n):
            K.op(V, lambda: nc.vector.match_replace(out=wks[i], in_to_replace=dsts[i][:, 0:8], in_values=srcs[i],
                                                    imm_value=-1e30), reads=rds[i] + [wrs[i]], writes=[bwk[i]])
        for i in range(n):
            K.op(V, lambda: nc.vector.max(out=dsts[i][:, 8:16], in_=wks[i]), reads=[bwk[i]], writes=[wrs[i]])

    def topk_stage(nb):
        e = nb % 2
        K.dma(S, stm[e][:].rearrange("p (h c) -> p h c", c=256), s_d[:, nb * 128:(nb + 1) * 128, :].rearrange("h n c -> n h c"), writes=[bstm[e]])
        for q4 in range(4):
            hs = range(q4 * 4, q4 * 4 + 4)
            top16_batch([tp[:, i, :] for i in hs], [stm[e][:, i * 128:(i + 1) * 128] for i in hs], 128,
                        [[bstm[e]] for i in hs], [btpg[i] for i in hs],
                        [wrk[:, i * 128:(i + 1) * 128] for i in hs], [bwkg[i] for i in hs])
            yield
        for h in range(8):
            K.op(V, lambda: nc.vector.tensor_tensor(
                out=cand[:, h, :].rearrange("p (a b) -> p a b", b=16),
                in0=tpv[:, h, 0, :].unsqueeze(2).to_broadcast([128, 16, 16]),
                in1=tpv[:, h, 1, :].unsqueeze(1).to_broadcast([128, 16, 16]), op=ALU.add),
                reads=[btpg[2 * h], btpg[2 * h + 1]], writes=[bcdg[h]])
        yield
        for q2 in range(2):
            hs = range(q2 * 4, q2 * 4 + 4)
            top16_batch([ctop[:, i, :] for i in hs], [cand[:, i, :] for i in hs], 256,
                        [[bcdg[i]] for i in hs], [bctg[i] for i in hs],
                        [wrk[:, i * 256:(i + 1) * 256] for i in hs], [bwkg[2 * i] for i in hs])
            yield
        btp = btpg
        bctop = bctg
        K.op(V, lambda: nc.vector.tensor_reduce(out=sm[:, 0:8], in_=ctop[:], axis=AX.X, op=ALU.max),
             reads=bctop, writes=[bsmm])
        K.op(V, lambda: nc.vector.tensor_reduce(out=sm[:, 8:16], in_=ctop[:], axis=AX.X, op=ALU.min),
             reads=bctop, writes=[bsmm])
        K.op(V, lambda: nc.vector.tensor_tensor(out=ce[:], in0=ctop[:], in1=bc816(sm[:, 0:8]), op=ALU.subtract),
             reads=bctop + [bsmm], writes=[bce])
        K.op(A, lambda: nc.scalar.activation(ce[:], ce[:], AF.Exp), reads=[bce], writes=[bce])
        K.op(V, lambda: nc.vector.tensor_reduce(out=sm[:, 16:24], in_=ce[:], axis=AX.X, op=ALU.add),
             reads=[bce], writes=[bsmm])
        K.op(A, lambda: nc.scalar.activation(sm[:, 24:32], sm[:, 16:24], AF.Ln), reads=[bsmm], writes=[bsmm])
        K.op(V, lambda: nc.vector.tensor_tensor(out=sm[:, 32:40], in0=sm[:, 0:8], in1=sm[:, 24:32], op=ALU.add),
             reads=[bsmm], writes=[bsmm])
        K.op(V, lambda: nc.vector.tensor_copy(out=At[:, 0, :].rearrange("p (a h) -> p h a", h=8), in_=bc816(sm[:, 8:16])),
             reads=[bsmm], writes=[bAt])
        K.op(V, lambda: nc.vector.tensor_tensor(out=At[:, 1, :].rearrange("p (a h) -> p h a", h=8), in0=tpv[:, :, 0, :],
                                                in1=bc816(sm[:, 32:40]), op=ALU.subtract),
             reads=btp + [bsmm], writes=[bAt])
        K.op(V, lambda: nc.vector.tensor_copy(out=At[:, 2, :].rearrange("p (a h) -> p h a", h=8), in_=tpv[:, :, 0, :]),
             reads=btp, writes=[bAt])
        pbt = nb % 2
        for q3 in range(3):
            K.op(K.pe, lambda: nc.tensor.transpose(banks[pbt][:, q3 * 128:(q3 + 1) * 128], At[:, q3, :], ident_f[:]),
                 reads=[bAt, bconst], writes=[bB[pbt]])
        K.op(V, lambda: nc.vector.tensor_copy(out=AT[e][:], in_=banks[pbt][:, 0:384].rearrange("p (a b) -> p a b", b=128)),
             reads=[bB[pbt]], writes=[bAT[e]])
        yield

    def bcn(ap):
        return ap.unsqueeze(2).to_broadcast([128, 16, 128])

    def sub_block(nb, sb):
        e = nb % 2
        f = (nb * 8 + sb) % 2
        f3 = (nb * 8 + sb) % 3
        ns = slice(sb * 16, sb * 16 + 16)
        row0 = nb * 128 + sb * 16
        src = bass.AP(s_d.tensor, row0 * 256, [[0, 16], [NOWN * 256, 8], [1, 4096]])
        K.dma(S, srep[f3][:].rearrange("p a b -> p (a b)"), src, writes=[bsrep[f3]])
        K.op(V, lambda: nc.vector.tensor_tensor(out=zr[f][:], in0=srep[f3][:, :, 128:256], in1=bcn(AT[e][:, 2, ns]),
                                                op=ALU.add), reads=[bsrep[f3], bAT[e]], writes=[bzr[f]])
        for tk in range(16):
            n_ = sb * 16 + tk
            K.op(A, lambda: nc.scalar.activation(er[f][:, tk, :], srep[f3][:, tk, 128:256], AF.Exp,
                                                 bias=AT[e][:, 1, n_:n_ + 1]),
                 reads=[bsrep[f3], bAT[e]], writes=[ber[f]])
        K.op(V, lambda: nc.vector.tensor_tensor(out=mk[f][:], in0=zr[f][:], in1=bcn(AT[e][:, 0, ns]), op=ALU.is_ge),
             reads=[bzr[f], bAT[e]], writes=[bmk[f]])
        K.op(V, lambda: nc.vector.tensor_tensor(out=P1b[f][:], in0=srep[f3][:, :, 0:128], in1=bcn(AT[e][:, 2, ns]),
                                                op=ALU.is_equal), reads=[bsrep[f3], bAT[e]], writes=[bP1b[f]])
        K.op(V, lambda: nc.vector.tensor_tensor(out=Rb[f][:], in0=mk[f][:], in1=er[f][:], op=ALU.mult),
             reads=[bmk[f], ber[f]], writes=[bRb[f]])
        for q4 in range(4):
            pb = 2 + (sb * 4 + q4) % 6
            for tk in range(4):
                t16 = q4 * 4 + tk
                mm(K, banks[pb][:, tk * 128:(tk + 1) * 128], [(Rb[f][:, t16, :], P1b[f][:, t16, :])],
                   reads=[bRb[f], bP1b[f]], writes=[bB[pb]])
            n0 = sb * 16 + q4 * 4
            K.op(A, lambda: nc.scalar.copy(gst[e][:, :, n0:n0 + 4], banks[pb][:].rearrange("p (n i) -> p i n", n=4)),
                 reads=[bB[pb]], writes=[bgst[e]])

    for _ in topk_stage(0):
        pass
    for nb in range(12):
        nxt = topk_stage(nb + 1) if nb + 1 < 12 else iter(())
        for sb in range(8):
            sub_block(nb, sb)
            next(nxt, None)
        for _ in nxt:
            pass
        K.dma(S, G_dv[:, :, nb * 128:(nb + 1) * 128], gst[nb % 2][:], reads=[bgst[nb % 2]])
    K.barrier()
    scr.close()
    if stop_after <= 6:
        K.finish()
        return nc, ins_used

    peer_uT = din("peer_uT", [128, 128, 4096])
    peer_v = din("peer_v", [16384, D])
    ln2_gT = din("ln2_gT", [128, 32])
    ln2_bT = din("ln2_bT", [128, 32])
    o_yT = dout("o_yT", [D, NOWN])
    pvv = peer_v.rearrange("(g a p) d -> g p a d", a=4, p=128)
    sc5 = Scope(K)
    h2p = sc5.sb("h2p", [128, 32, 512], BF16)
    acc = sc5.sb("acc", [128, 32, 512], F32)
    l1g5 = sc5.sb("l1g5", [128, 32], F32)
    l1b5 = sc5.sb("l1b5", [128, 32], F32)
    l2g = sc5.sb("l2g", [128, 32], F32)
    l2b = sc5.sb("l2b", [128, 32], F32)
    bh2p, bsm5 = Buf(), Buf()
    bacc = [Buf() for _ in range(32)]
    for t_, s_ in ((l1g5, ln1_gT), (l1b5, ln1_bT), (l2g, ln2_gT), (l2b, ln2_bT)):
        K.dma(S, t_[:], s_, writes=[bsm5])
    _P5P = int(os.environ.get('P5_PASSES', 3)); _P5G = int(os.environ.get('P5_GROUPS', 32)); _P5E = int(os.environ.get('P5_EPI', 1))
    for pt_ in range(_P5P):
        c0 = pt_ * 512
        cj = 0 if pt_ < 2 else 1
        pcs = slice(c0, c0 + 512)
        K.dma(S, h2p[:], h2v[:, :, pcs], writes=[bh2p])
        for db in range(32):
            K.op(V, lambda: nc.vector.memset(acc[:, db, :], 0.0), writes=[bacc[db]])
        s5a = Scope(K)
        ut = [s5a.sb("ut%d" % i, [128, 32, 128], BF16) for i in range(3)]
        vt = [s5a.sb("vt%d" % i, [128, 4, D], BF16) for i in range(2)]
        gt = [s5a.sb("gt%d" % i, [128, 512], BF16) for i in range(3)]
        ga32 = [s5a.sb("ga32%d" % i, [128, 512], F32) for i in range(2)]
        gab = [s5a.sb("gab%d" % i, [128, 4, 512], BF16) for i in range(2)]
        but, bvt, bgt, bga32, bgab = [[Buf(), Buf(), Buf()] for _ in range(5)]

        def phaseB(g):
            gi = g % 2
            for db in range(32):
                pb = 2 + db % 6
                mm(K, banks[pb][:], [(vt[gi][:, a, db * 128:(db + 1) * 128], gab[gi][:, a, :]) for a in range(4)],
                   reads=[bvt[gi], bgab[gi]], writes=[bB[pb]])
                K.op(V, lambda: nc.vector.tensor_tensor(out=acc[:, db, :], in0=acc[:, db, :], in1=banks[pb][:], op=ALU.add),
                     reads=[bB[pb], bacc[db]], writes=[bacc[db]])

        for g in range(_P5G):
            gi = g % 2
            for a in range(4):
                eb = g * 4 + a
                ei = eb % 2
                u3 = eb % 3
                K.dma(P, ut[u3][:].rearrange("p a b -> p (a b)"), peer_uT[eb], writes=[but[u3]])
                K.dma(S, gt[u3][:], G_d[eb, :, pcs], writes=[bgt[u3]])
                mm(K, banks[ei][:], [(ut[u3][:, kc, :], h2p[:, kc, :]) for kc in range(32)],
                   reads=[but[u3], bh2p], writes=[bB[ei]])
                K.op(A, lambda: nc.scalar.activation(ga32[ei][:], banks[ei][:], AF.Gelu), reads=[bB[ei]], writes=[bga32[ei]])
                K.op(V, lambda: nc.vector.tensor_tensor(out=gab[gi][:, a, :], in0=ga32[ei][:], in1=gt[u3][:], op=ALU.mult),
                     reads=[bga32[ei], bgt[u3]], writes=[bgab[gi]])
            K.dma(P, vt[gi][:], pvv[g], writes=[bvt[gi]])
            if g > 0:
                phaseB(g - 1)
        phaseB(_P5G - 1)
        K.barrier()
        s5a.close()
        s5b = Scope(K)
        m1 = s5b.sb("m1", [128, 512], F32)
        r1 = s5b.sb("r1", [128, 512], F32)
        y1 = s5b.sb("y1", [128, 512], F32)
        y2 = s5b.sb("y2", [128, 512], F32)
        yq = s5b.sb("yq", [128, 512], F32)
        y1w = s5b.sb("y1w", [128, 4, 512], F32)
        y2w = s5b.sb("y2w", [128, 4, 512], F32)
        yq4 = s5b.sb("yq4", [128, 4, 512], F32)
        zl = [s5b.sb("zl%d" % i, [128, 4, 512], F32) for i in range(2)]
        yst = [s5b.sb("yst%d" % i, [128, 4, 512], F32) for i in range(2)]
        bst1, by1, by2, byq, by1w, by2w, byq4 = [Buf() for _ in range(7)]
        bzl, byst = [[Buf(), Buf()] for _ in range(2)]
        zTv = zT_d.rearrange("(kc p) n -> p kc n", p=128)
        oyv = o_yT.rearrange("(kc p) n -> p kc n", p=128)
        b4 = lambda ap: ap.unsqueeze(1).to_broadcast([128, 4, 512])
        K.dma(S, m1[:], stat1_d[0][:, pcs], writes=[bst1])
        K.dma(S, r1[:], stat1_d[1][:, pcs], writes=[bst1])
        K.op(P, lambda: nc.gpsimd.memset(y1w[:], 0.0), writes=[by1w])
        K.op(P, lambda: nc.gpsimd.memset(y2w[:], 0.0), writes=[by2w])
        for c4 in range(8 if _P5E else 0):
            e = c4 % 2
            dbs = range(c4 * 4, c4 * 4 + 4)
            ba4 = [bacc[db] for db in dbs]
            a4 = acc[:, c4 * 4:c4 * 4 + 4, :]
            K.dma(S, zl[e][:], zTv[:, c4 * 4:c4 * 4 + 4, pcs], writes=[bzl[e]])
            K.op(V, lambda: nc.vector.tensor_tensor(out=zl[e][:], in0=zl[e][:], in1=b4(m1[:]), op=ALU.subtract),
                 reads=[bzl[e], bst1], writes=[bzl[e]])
            K.op(P, lambda: nc.gpsimd.tensor_tensor(out=zl[e][:], in0=zl[e][:], in1=b4(r1[:]), op=ALU.mult),
                 reads=[bzl[e], bst1], writes=[bzl[e]])
            for q, db in enumerate(dbs):
                K.op(A, lambda: nc.scalar.activation(zl[e][:, q, :], zl[e][:, q, :], AF.Identity, bias=l1b5[:, db:db + 1],
                                                     scale=l1g5[:, db:db + 1]), reads=[bzl[e], bsm5], writes=[bzl[e]])
                K.op(A, lambda: nc.scalar.activation(acc[:, db, :], acc[:, db, :], AF.Identity, scale=MOD(5, db, cj)),
                     reads=[bacc[db], bmod], writes=[bacc[db]])
            K.op(V, lambda: nc.vector.scalar_tensor_tensor(out=a4, in0=zl[e][:], scalar=ALPHA, in1=a4,
                                                           op0=ALU.mult, op1=ALU.add),
                 reads=[bzl[e]] + ba4, writes=ba4)
            K.op(P, lambda: nc.gpsimd.tensor_tensor(out=y1w[:], in0=y1w[:], in1=a4, op=ALU.add),
                 reads=ba4 + [by1w], writes=[by1w])
            K.op(A, lambda: nc.scalar.activation(yq4[:], a4, AF.Square), reads=ba4, writes=[byq4])
            K.op(P, lambda: nc.gpsimd.tensor_tensor(out=y2w[:], in0=y2w[:], in1=yq4[:], op=ALU.add),
                 reads=[byq4, by2w], writes=[by2w])
        for (yw, byw, y_, by_) in ((y1w, by1w, y1, by1), (y2w, by2w, y2, by2)):
            K.op(V, lambda: nc.vector.tensor_tensor(out=y_[:], in0=yw[:, 0, :], in1=yw[:, 1, :], op=ALU.add),
                 reads=[byw], writes=[by_])
            K.op(V, lambda: nc.vector.tensor_tensor(out=y_[:], in0=y_[:], in1=yw[:, 2, :], op=ALU.add),
                 reads=[byw, by_], writes=[by_])
            K.op(V, lambda: nc.vector.tensor_tensor(out=y_[:], in0=y_[:], in1=yw[:, 3, :], op=ALU.add),
                 reads=[byw, by_], writes=[by_])
        ln_stats(y1, by1, y2, by2, yq, byq, 4096.0, 512)
        for c4 in range(8):
            e = c4 % 2
            dbs = range(c4 * 4, c4 * 4 + 4)
            ba4 = [bacc[db] for db in dbs]
            a4 = acc[:, c4 * 4:c4 * 4 + 4, :]
            K.op(V, lambda: nc.vector.tensor_tensor(out=yst[e][:], in0=a4, in1=b4(y1[:]), op=ALU.subtract),
                 reads=ba4 + [by1], writes=[byst[e]])
            K.op(P, lambda: nc.gpsimd.tensor_tensor(out=yst[e][:], in0=yst[e][:], in1=b4(y2[:]), op=ALU.mult),
                 reads=[byst[e], by2], writes=[byst[e]])
            for q, db in enumerate(dbs):
                K.op(A, lambda: nc.scalar.activation(yst[e][:, q, :], yst[e][:, q, :], AF.Identity, bias=l2b[:, db:db + 1],
                                                     scale=l2g[:, db:db + 1]), reads=[byst[e], bsm5], writes=[byst[e]])
            K.dma(S, oyv[:, c4 * 4:c4 * 4 + 4, pcs], yst[e][:], reads=[byst[e]], final=True)
        K.barrier()
        s5b.close()
    sc5.close()

    K.finish()
    return nc, ins_used


def _fm(v, nchunk):
    return np.ascontiguousarray(np.asarray(v, np.float32).reshape(nchunk, 128).T)


def _rope_tables(pos):
    row = (pos // 64).astype(np.float32)
    col = (pos % 64).astype(np.float32)
    inv = (10000.0 ** (-np.arange(16, dtype=np.float32) / 16)).astype(np.float32)
    ar = row[:, None] * inv
    ac = col[:, None] * inv
    ang = np.concatenate([ar, ar, ac, ac], -1).astype(np.float32)
    return np.ascontiguousarray(np.cos(ang).T.astype(np.float32)), np.ascontiguousarray(np.sin(ang).T.astype(np.float32))


def _rmat():
    R = np.zeros((64, 64), np.float32)
    for base in (0, 32):
        for i in range(16):
            R[base + i, base + 16 + i] = -1.0
            R[base + 16 + i, base + i] = 1.0
    return np.ascontiguousarray(R.T)


def sample_cols(hf):
    own = np.arange(hf * 512, hf * 512 + 512)
    if hf == 0:
        other = np.arange(512, 1024)
    else:
        other = np.concatenate([np.arange(497, 512), np.arange(0, 497)])
    return own, other


def prep_shared(inp):
    f = lambda k: np.asarray(inp[k], np.float32)
    sh = {}
    sh["w_ada"] = np.ascontiguousarray(f("w_ada")[0])
    sh["b_adaT"] = _fm(f("b_ada")[0], 192)
    sh["w_in"] = np.ascontiguousarray(f("w_in")[0])
    sh["g_qT"] = _fm(f("g_q")[0], 6)
    sh["g_kvT"] = _fm(f("g_kv")[0], 4)
    sh["w_uq"] = np.ascontiguousarray(f("w_uq")[0])
    sh["w_ukv"] = np.ascontiguousarray(f("w_ukv")[0])
    sh["w_dwT"] = np.ascontiguousarray(f("w_dw")[0].T.reshape(16, 128, 31).transpose(1, 0, 2))
    sh["b_dwT"] = _fm(f("b_dw")[0], 16)
    sh["g_cnT"] = _fm(f("g_cn")[0], 16)
    sh["b_cnT"] = _fm(f("b_cn")[0], 16)
    sh["w_out"] = np.ascontiguousarray(f("w_out")[0])
    sh["ln1_gT"] = _fm(f("ln1_g")[0], 32)
    sh["ln1_bT"] = _fm(f("ln1_b")[0], 32)
    sh["w_pq"] = np.ascontiguousarray(f("w_pq")[0])
    sk = f("sub_keys")[0]
    skT = sk.reshape(16, 128, 2, 128).transpose(3, 0, 2, 1)
    sh["skT"] = np.ascontiguousarray(skT.reshape(128, 32, 128))
    pu = f("peer_u")[0]
    sh["peer_uT"] = np.ascontiguousarray(pu.reshape(128, 128, 32, 128).transpose(0, 3, 2, 1)).reshape(128, 128, 4096)
    sh["peer_v"] = np.ascontiguousarray(f("peer_v")[0])
    sh["ln2_gT"] = _fm(f("ln2_g")[0], 32)
    sh["ln2_bT"] = _fm(f("ln2_b")[0], 32)
    sh["rmatT"] = _rmat()
    return sh


def prep_core(inp, c):
    f = lambda k: np.asarray(inp[k], np.float32)
    b, hf = c // 2, c % 2
    own, other = sample_cols(hf)
    xp = f("x_prompt")[4 * c:4 * c + 4].reshape(NP_, D)
    xs = f("x_sample")[b]
    xall = np.concatenate([xp, xs[own], xs[other]], 0)
    m = {}
    m["xT"] = np.ascontiguousarray(xall.T)
    cond = np.stack([f("c_ctx"), f("c")[b]], -1)
    m["condT"] = np.ascontiguousarray(cond.reshape(32, 128, 2).transpose(1, 0, 2))
    m["cache_ckvT"] = np.ascontiguousarray(f("cache_ckv")[b, 0].T.reshape(4, 128, 256).transpose(1, 0, 2))
    m["cache_kpeT"] = np.ascontiguousarray(f("cache_kpe")[b, 0].T)
    cosT, sinT = _rope_tables(np.concatenate([own, other]))
    m["cosT"] = cosT
    m["sinT"] = sinT
    hm = np.zeros((128, 2), np.float32)
    hm[:, 0] = 1.0 if hf == 1 else 0.0
    hm[:, 1] = 1.0 if hf == 0 else 0.0
    m["halo_mask"] = hm
    return m


_CACHE = {}


def kernel(**inputs):
    if "nc" not in _CACHE:
        _CACHE["nc"] = build()
    nc, used = _CACHE["nc"]
    sh = prep_shared(inputs)
    in_maps = []
    for c in range(8):
        m = prep_core(inputs, c)
        m.update(sh)
        in_maps.append({k: m[k] for k in used})
    res = run_bass_kernel_spmd(nc, in_maps, core_ids=list(range(8)))
    y_prompt = np.zeros((32, 256, D), np.float32)
    y_sample = np.zeros((4, 1024, D), np.float32)
    new_ckv = np.zeros((32, 1, 256, 512), np.float32)
    new_kpe = np.zeros((32, 1, 256, 64), np.float32)
    for c in range(8):
        r = res.results[c]
        b, hf = c // 2, c % 2
        yT = np.asarray(r["o_yT"], np.float32)
        y_prompt[4 * c:4 * c + 4] = yT[:, :NP_].T.reshape(4, 256, D)
        y_sample[b, hf * 512:(hf + 1) * 512] = yT[:, NP_:].T
        new_ckv[4 * c:4 * c + 4, 0] = np.asarray(r["o_ckvT"], np.float32).T.reshape(4, 256, 512)
        new_kpe[4 * c:4 * c + 4, 0] = np.asarray(r["o_kpeT"], np.float32).T.reshape(4, 256, 64)
    return (y_prompt, y_sample, new_ckv, new_kpe)
```
